# Optimizing a Trainium2 kernel written in Bass

```python
import math
import jax, jax.numpy as jnp
from jax import lax
import numpy as np

D_MODEL = 1024
BATCH = 16
SEQ = 4096
DEPTH = 4
DEC_BATCH = 4
DEC_SEQ = 4096
PAST_LEN = 128

N_EVEN = (DEPTH + 1) // 2
N_ODD = DEPTH // 2
ALPHA = (2.0 * DEPTH) ** 0.25
BETA = (8.0 * DEPTH) ** -0.25
LN_EPS = 1e-5
RMS_EPS = 1e-6

CONV_CH = D_MODEL // 2
CONV_W = 31
MLA_HEADS = 8
MLA_NOPE = 64
MLA_ROPE = 32
MLA_V = 64
MLA_Q_LORA = D_MODEL // 4
MLA_KV_LORA = D_MODEL // 8
ROPE_THETA = 10000.0
Q_BLOCK = 128
EV_SPLITS = (CONV_CH, CONV_CH, MLA_Q_LORA, MLA_KV_LORA, MLA_ROPE)
EV_IN = sum(EV_SPLITS)
EV_MIX = CONV_CH + MLA_HEADS * MLA_V

SSD_HEADS = 8
SSD_HEAD_DIM = 64
SSD_INNER = SSD_HEADS * SSD_HEAD_DIM
SSD_GROUPS = 2
SSD_STATE = 128
SSD_CONV_W = 5
SSD_CHUNK = 128
SSD_XBC = SSD_INNER + 2 * SSD_GROUPS * SSD_STATE
ML_HEADS = 8
ML_HEAD_DIM = 64
ML_INNER = ML_HEADS * ML_HEAD_DIM
ML_CHUNK = 128
OD_SPLITS = (SSD_INNER, SSD_XBC, 2 * SSD_HEADS, ML_INNER, ML_INNER, ML_INNER, ML_INNER, 2 * ML_HEADS, 2 * ML_HEADS)
OD_IN = sum(OD_SPLITS)
OD_MIX = SSD_INNER + ML_INNER

D_FF = 2816
N_EXPERTS = 8
TOP_K = 2
D_FF_EXPERT = 3584

kernel_name = 'hybrid_bidir_encoder_trunk'


def _split(x, sizes):
    return jnp.split(x, np.cumsum(sizes)[:-1].tolist(), axis=-1)


def _standardize(x):
    xf = x.astype(jnp.float32)
    mu = jnp.mean(xf, -1, keepdims=True)
    var = jnp.mean(jnp.square(xf - mu), -1, keepdims=True)
    return ((xf - mu) * lax.rsqrt(var + LN_EPS)).astype(x.dtype)


def _layernorm(x, g, b):
    return _standardize(x) * g + b


def _rmsnorm(x, g):
    xf = x.astype(jnp.float32)
    return (xf * lax.rsqrt(jnp.mean(xf * xf, -1, keepdims=True) + RMS_EPS)).astype(x.dtype) * g


def _dwconv(x, w, b):
    width = w.shape[0]
    y = lax.conv_general_dilated(x, w[:, None, :], (1,), [(width // 2, width // 2)],
                                 dimension_numbers=('NWC', 'WIO', 'NWC'),
                                 feature_group_count=x.shape[-1])
    return y + b


def _rope(x, pos):
    half = x.shape[-1] // 2
    inv_freq = ROPE_THETA ** (-jnp.arange(half, dtype=jnp.float32) / half)
    ang = pos.astype(jnp.float32)[:, None, None] * inv_freq
    cos, sin = jnp.cos(ang).astype(x.dtype), jnp.sin(ang).astype(x.dtype)
    x1, x2 = x[..., :half], x[..., half:]
    return jnp.concatenate([x1 * cos - x2 * sin, x2 * cos + x1 * sin], -1)


def _block_attention(q, k, v):
    B, S, H, Dq = q.shape
    nb = S // Q_BLOCK
    scale = Dq ** -0.5
    qb = q.reshape(B, nb, Q_BLOCK, H, Dq).transpose(1, 0, 2, 3, 4)

    def one(qblk):
        s = jnp.einsum('bqhd,bkhd->bhqk', qblk, k).astype(jnp.float32) * scale
        p = jax.nn.softmax(s, axis=-1).astype(v.dtype)
        return jnp.einsum('bhqk,bkhd->bqhd', p, v)

    o = lax.map(one, qb)
    return o.transpose(1, 0, 2, 3, 4).reshape(B, S, H, v.shape[-1])


def _segsum(a):
    T = a.shape[-1]
    cs = jnp.cumsum(a, axis=-1)
    seg = cs[..., :, None] - cs[..., None, :]
    return jnp.where(jnp.tril(jnp.ones((T, T), dtype=bool)), seg, -jnp.inf)


def _ssd_scan(xh, dt, a, bm, cm):
    B, S, H, P = xh.shape
    G, N = bm.shape[-2], bm.shape[-1]
    R = H // G
    L = SSD_CHUNK
    c = S // L
    f32 = jnp.float32
    dtf = dt.astype(f32)
    x = (xh.astype(f32) * dtf[..., None]).reshape(B, c, L, G, R, P)
    da = (dtf * a.astype(f32)).reshape(B, c, L, G, R).transpose(0, 3, 4, 1, 2)
    bmc = bm.astype(f32).reshape(B, c, L, G, N)
    cmc = cm.astype(f32).reshape(B, c, L, G, N)
    a_cs = jnp.cumsum(da, axis=-1)
    cb = jnp.einsum('bclgn,bcsgn->bcgls', cmc, bmc)
    y_diag = jnp.einsum('bcgls,bgrcls,bcsgrp->bclgrp', cb, jnp.exp(_segsum(da)), x)
    decay_states = jnp.exp(a_cs[..., -1:] - a_cs)
    states = jnp.einsum('bclgn,bgrcl,bclgrp->bcgrpn', bmc, decay_states, x)
    states = jnp.concatenate([jnp.zeros_like(states[:, :1]), states], axis=1)
    chunk_tot = jnp.pad(a_cs[..., -1], ((0, 0), (0, 0), (0, 0), (1, 0)))
    decay_chunk = jnp.exp(_segsum(chunk_tot))
    states = jnp.einsum('bgrzc,bcgrpn->bzgrpn', decay_chunk, states)[:, :-1]
    y_off = jnp.einsum('bclgn,bcgrpn,bgrcl->bclgrp', cmc, states, jnp.exp(a_cs))
    return (y_diag + y_off).reshape(B, S, H, P).astype(xh.dtype)


def _mlstm_chunkwise(q, k, v, i_pre, f_pre):
    B, S, H, Dk = q.shape
    Dv = v.shape[-1]
    L = ML_CHUNK
    c = S // L
    f32 = jnp.float32
    qc = q.astype(f32).reshape(B, c, L, H, Dk)
    kc = k.astype(f32).reshape(B, c, L, H, Dk)
    vc = v.astype(f32).reshape(B, c, L, H, Dv)
    lf = jax.nn.log_sigmoid(f_pre.astype(f32)).reshape(B, c, L, H).transpose(0, 1, 3, 2)
    li = i_pre.astype(f32).reshape(B, c, L, H).transpose(0, 1, 3, 2)
    bcum = jnp.cumsum(lf, axis=-1)
    g = bcum[..., -1]
    causal = jnp.tril(jnp.ones((L, L), dtype=bool))
    dmat = jnp.where(causal, bcum[..., :, None] - bcum[..., None, :] + li[..., None, :], -jnp.inf)
    w_end = g[..., None] - bcum + li
    m_loc = jnp.max(w_end, axis=-1)
    e_end = jnp.exp(w_end - m_loc[..., None])
    s_loc = jnp.einsum('bchl,bclhd,bclhe->bchde', e_end, kc, vc)
    n_loc = jnp.einsum('bchl,bclhd->bchd', e_end, kc)

    def step(carry, inp):
        c_st, n_st, m_st = carry
        s_i, n_i, g_i, ml_i = inp
        m_new = jnp.maximum(g_i + m_st, ml_i)
        a_old = jnp.exp(g_i + m_st - m_new)
        a_new = jnp.exp(ml_i - m_new)
        c_new = a_old[..., None, None] * c_st + a_new[..., None, None] * s_i
        n_new = a_old[..., None] * n_st + a_new[..., None] * n_i
        return (c_new, n_new, m_new), (c_st, n_st, m_st)

    init = (jnp.zeros((B, H, Dk, Dv), f32), jnp.zeros((B, H, Dk), f32), jnp.zeros((B, H), f32))
    seq_in = (jnp.moveaxis(s_loc, 1, 0), jnp.moveaxis(n_loc, 1, 0), jnp.moveaxis(g, 1, 0), jnp.moveaxis(m_loc, 1, 0))
    _, (c_prev, n_prev, m_prev) = lax.scan(step, init, seq_in)
    c_prev = jnp.moveaxis(c_prev, 0, 1)
    n_prev = jnp.moveaxis(n_prev, 0, 1)
    m_prev = jnp.moveaxis(m_prev, 0, 1)
    inter_log = bcum + m_prev[..., None]
    m_t = jnp.maximum(inter_log, jnp.max(dmat, axis=-1))
    w_intra = jnp.exp(dmat - m_t[..., None])
    w_inter = jnp.exp(inter_log - m_t)
    a_mat = jnp.einsum('bclhd,bcshd->bchls', qc, kc) * w_intra
    num = jnp.einsum('bchls,bcshe->bchle', a_mat, vc) + w_inter[..., None] * jnp.einsum('bclhd,bchde->bchle', qc, c_prev)
    den = jnp.sum(a_mat, axis=-1) + w_inter * jnp.einsum('bclhd,bchd->bchl', qc, n_prev)
    h = num / jnp.maximum(jnp.abs(den), jnp.exp(-m_t))[..., None]
    return h.transpose(0, 1, 3, 2, 4).reshape(B, S, H, Dv).astype(q.dtype)


def _even_mixer(x, w_in, dw_w, dw_b, cln_g, cln_b, qn_g, w_uq, kvn_g, w_ukv, w_out):
    B, S, _ = x.shape
    a_val, a_gate, q_lat, kv_lat, k_rot = _split(x @ w_in, EV_SPLITS)
    u = a_val * jax.nn.sigmoid(a_gate)
    u = _dwconv(u, dw_w, dw_b)
    u = jax.nn.silu(_layernorm(u, cln_g, cln_b))
    pos = jnp.arange(S)
    q = (_rmsnorm(q_lat, qn_g) @ w_uq).reshape(B, S, MLA_HEADS, MLA_NOPE + MLA_ROPE)
    kv = (_rmsnorm(kv_lat, kvn_g) @ w_ukv).reshape(B, S, MLA_HEADS, MLA_NOPE + MLA_V)
    k_pe = jnp.broadcast_to(_rope(k_rot[:, :, None, :], pos), (B, S, MLA_HEADS, MLA_ROPE))
    q = jnp.concatenate([q[..., :MLA_NOPE], _rope(q[..., MLA_NOPE:], pos)], -1)
    k = jnp.concatenate([kv[..., :MLA_NOPE], k_pe], -1)
    att = _block_attention(q, k, kv[..., MLA_NOPE:]).reshape(B, S, MLA_HEADS * MLA_V)
    return jnp.concatenate([u, att], -1) @ w_out


def _odd_mixer(x, w_in, cw, cb, dt_bias, a_log, d_skip, ssd_g, ig_b, fg_b, ml_g, w_out):
    B, S, _ = x.shape
    z, xbc, dt_raw, q, k, v, o, ig, fg = _split(x @ w_in, OD_SPLITS)
    xbc = jax.nn.silu(_dwconv(xbc, cw, cb))
    xs, bm, cm = _split(xbc, (SSD_INNER, SSD_GROUPS * SSD_STATE, SSD_GROUPS * SSD_STATE))
    xs = xs.reshape(B, S, SSD_HEADS, SSD_HEAD_DIM)
    bm = bm.reshape(B, S, SSD_GROUPS, SSD_STATE)
    cm = cm.reshape(B, S, SSD_GROUPS, SSD_STATE)
    dt = jax.nn.softplus(dt_raw.reshape(B, S, 2, SSD_HEADS) + dt_bias)
    a = -jnp.exp(a_log)
    y_f = _ssd_scan(xs, dt[:, :, 0], a[0], bm, cm)
    y_b = _ssd_scan(xs[:, ::-1], dt[:, ::-1, 1], a[1], bm[:, ::-1], cm[:, ::-1])[:, ::-1]
    y = (y_f + y_b + xs * d_skip[:, None]).reshape(B, S, SSD_INNER) * jax.nn.silu(z)
    y = _rmsnorm(y.reshape(B, S, SSD_GROUPS, SSD_INNER // SSD_GROUPS),
                 ssd_g.reshape(SSD_GROUPS, SSD_INNER // SSD_GROUPS)).reshape(B, S, SSD_INNER)
    sh = (B, S, ML_HEADS, ML_HEAD_DIM)
    qh, kh, vh = q.reshape(sh), k.reshape(sh) * (ML_HEAD_DIM ** -0.5), v.reshape(sh)
    ig = ig.reshape(B, S, 2, ML_HEADS) + ig_b
    fg = fg.reshape(B, S, 2, ML_HEADS) + fg_b
    h_f = _mlstm_chunkwise(qh, kh, vh, ig[:, :, 0], fg[:, :, 0])
    h_b = _mlstm_chunkwise(qh[:, ::-1], kh[:, ::-1], vh[:, ::-1], ig[:, ::-1, 1], fg[:, ::-1, 1])[:, ::-1]
    h = (_standardize(h_f + h_b) * ml_g.reshape(ML_HEADS, ML_HEAD_DIM)).reshape(B, S, ML_INNER)
    h = jax.nn.sigmoid(o) * h
    return jnp.concatenate([y, h], -1) @ w_out


def _swiglu(x, wg, wu, wd):
    return (jax.nn.silu(x @ wg) * (x @ wu)) @ wd


def _moe(x, router_w, router_b, wg, wu, wd):
    B, S, D = x.shape
    t = x.reshape(B * S, D)
    logits = (t @ router_w).astype(jnp.float32) + router_b
    top_v, top_i = lax.top_k(logits, TOP_K)
    gates = jax.nn.softmax(top_v, axis=-1)
    combine = jnp.sum(jax.nn.one_hot(top_i, N_EXPERTS, dtype=jnp.float32) * gates[..., None], axis=1).astype(x.dtype)
    out = jnp.zeros_like(t)
    for e in range(N_EXPERTS):
        out = out + combine[:, e:e + 1] * _swiglu(t, wg[e], wu[e], wd[e])
    return out.reshape(B, S, D)


def _trunk(x, p):
    for l in range(DEPTH):
        j = l // 2
        if l % 2 == 0:
            m = _even_mixer(x, p['ev_w_in'][j], p['conv_dw_w'][j], p['conv_dw_b'][j], p['conv_ln_g'][j],
                            p['conv_ln_b'][j], p['mla_q_norm_g'][j], p['mla_w_uq'][j], p['mla_kv_norm_g'][j],
                            p['mla_w_ukv'][j], p['ev_w_out'][j])
        else:
            m = _odd_mixer(x, p['od_w_in'][j], p['ssd_conv_w'][j], p['ssd_conv_b'][j], p['ssd_dt_bias'][j],
                           p['ssd_a_log'][j], p['ssd_d'][j], p['ssd_norm_g'][j], p['ml_igate_b'][j],
                           p['ml_fgate_b'][j], p['ml_norm_g'][j], p['od_w_out'][j])
        x = _layernorm(ALPHA * x + m, p['ln1_g'][l], p['ln1_b'][l])
        if l % 2 == 0:
            f = _swiglu(x, p['ffn_w_gate'][j], p['ffn_w_up'][j], p['ffn_w_down'][j])
        else:
            f = _moe(x, p['moe_router_w'][j], p['moe_router_b'][j], p['moe_w_gate'][j], p['moe_w_up'][j],
                     p['moe_w_down'][j])
        x = _layernorm(ALPHA * x + f, p['ln2_g'][l], p['ln2_b'][l])
    return x


def setup_inputs(seed: int = 0) -> dict:
    key = jax.random.key(seed)
    ks = iter(jax.random.split(key, 48))

    def nrm(shape, scale):
        return scale * jax.random.normal(next(ks), shape, jnp.float32)

    def gain(shape):
        return 1.0 + 0.02 * jax.random.normal(next(ks), shape, jnp.float32)

    def unif(shape, lo, hi):
        return jax.random.uniform(next(ks), shape, jnp.float32, lo, hi)

    NE, NO, D = N_EVEN, N_ODD, D_MODEL
    dt0 = jnp.exp(unif((NO, 2, SSD_HEADS), math.log(1e-3), math.log(1e-1)))
    return {
        'x_prompt': nrm((BATCH, SEQ, D), 1.0),
        'x_sample': nrm((DEC_BATCH, DEC_SEQ, D), 1.0),
        'ev_w_in': nrm((NE, D, EV_IN), D ** -0.5),
        'conv_dw_w': nrm((NE, CONV_W, CONV_CH), CONV_W ** -0.5),
        'conv_dw_b': nrm((NE, CONV_CH), 0.02),
        'conv_ln_g': gain((NE, CONV_CH)),
        'conv_ln_b': nrm((NE, CONV_CH), 0.02),
        'mla_q_norm_g': gain((NE, MLA_Q_LORA)),
        'mla_w_uq': nrm((NE, MLA_Q_LORA, MLA_HEADS * (MLA_NOPE + MLA_ROPE)), MLA_Q_LORA ** -0.5),
        'mla_kv_norm_g': gain((NE, MLA_KV_LORA)),
        'mla_w_ukv': nrm((NE, MLA_KV_LORA, MLA_HEADS * (MLA_NOPE + MLA_V)), MLA_KV_LORA ** -0.5),
        'ev_w_out': nrm((NE, EV_MIX, D), BETA * EV_MIX ** -0.5),
        'od_w_in': nrm((NO, D, OD_IN), D ** -0.5),
        'ssd_conv_w': nrm((NO, SSD_CONV_W, SSD_XBC), SSD_CONV_W ** -0.5),
        'ssd_conv_b': nrm((NO, SSD_XBC), 0.02),
        'ssd_dt_bias': dt0 + jnp.log(-jnp.expm1(-dt0)),
        'ssd_a_log': jnp.log(unif((NO, 2, SSD_HEADS), 1.0, 16.0)),
        'ssd_d': gain((NO, SSD_HEADS)),
        'ssd_norm_g': gain((NO, SSD_INNER)),
        'ml_igate_b': nrm((NO, 2, ML_HEADS), 0.1),
        'ml_fgate_b': unif((NO, 2, ML_HEADS), 3.0, 6.0),
        'ml_norm_g': gain((NO, ML_INNER)),
        'od_w_out': nrm((NO, OD_MIX, D), BETA * OD_MIX ** -0.5),
        'ffn_w_gate': nrm((NE, D, D_FF), D ** -0.5),
        'ffn_w_up': nrm((NE, D, D_FF), D ** -0.5),
        'ffn_w_down': nrm((NE, D_FF, D), BETA * D_FF ** -0.5),
        'moe_router_w': nrm((NO, D, N_EXPERTS), D ** -0.5),
        'moe_router_b': nrm((NO, N_EXPERTS), 0.01),
        'moe_w_gate': nrm((NO, N_EXPERTS, D, D_FF_EXPERT), D ** -0.5),
        'moe_w_up': nrm((NO, N_EXPERTS, D, D_FF_EXPERT), D ** -0.5),
        'moe_w_down': nrm((NO, N_EXPERTS, D_FF_EXPERT, D), BETA * D_FF_EXPERT ** -0.5),
        'ln1_g': gain((DEPTH, D)),
        'ln1_b': nrm((DEPTH, D), 0.02),
        'ln2_g': gain((DEPTH, D)),
        'ln2_b': nrm((DEPTH, D), 0.02),
    }


def reference(x_prompt, x_sample, ev_w_in, conv_dw_w, conv_dw_b, conv_ln_g, conv_ln_b, mla_q_norm_g, mla_w_uq,
              mla_kv_norm_g, mla_w_ukv, ev_w_out, od_w_in, ssd_conv_w, ssd_conv_b, ssd_dt_bias, ssd_a_log, ssd_d,
              ssd_norm_g, ml_igate_b, ml_fgate_b, ml_norm_g, od_w_out, ffn_w_gate, ffn_w_up, ffn_w_down,
              moe_router_w, moe_router_b, moe_w_gate, moe_w_up, moe_w_down, ln1_g, ln1_b, ln2_g, ln2_b):
    p = dict(ev_w_in=ev_w_in, conv_dw_w=conv_dw_w, conv_dw_b=conv_dw_b, conv_ln_g=conv_ln_g, conv_ln_b=conv_ln_b,
             mla_q_norm_g=mla_q_norm_g, mla_w_uq=mla_w_uq, mla_kv_norm_g=mla_kv_norm_g, mla_w_ukv=mla_w_ukv,
             ev_w_out=ev_w_out, od_w_in=od_w_in, ssd_conv_w=ssd_conv_w, ssd_conv_b=ssd_conv_b,
             ssd_dt_bias=ssd_dt_bias, ssd_a_log=ssd_a_log, ssd_d=ssd_d, ssd_norm_g=ssd_norm_g,
             ml_igate_b=ml_igate_b, ml_fgate_b=ml_fgate_b, ml_norm_g=ml_norm_g, od_w_out=od_w_out,
             ffn_w_gate=ffn_w_gate, ffn_w_up=ffn_w_up, ffn_w_down=ffn_w_down, moe_router_w=moe_router_w,
             moe_router_b=moe_router_b, moe_w_gate=moe_w_gate, moe_w_up=moe_w_up, moe_w_down=moe_w_down,
             ln1_g=ln1_g, ln1_b=ln1_b, ln2_g=ln2_g, ln2_b=ln2_b)
    y_prompt = _trunk(x_prompt, p)
    y_sample = _trunk(x_sample, p)
    return (y_prompt, y_sample)
```

```python
import math
from contextlib import ExitStack
import numpy as np
import concourse.bass as bass
import concourse.mybir as mybir
from concourse.bass_utils import run_bass_kernel_spmd

F32 = mybir.dt.float32
BF16 = mybir.dt.bfloat16
AF = mybir.ActivationFunctionType
ALU = mybir.AluOpType
AX = mybir.AxisListType

D = 1024
S = 4096
DEPTH = 4
ALPHA = (2.0 * DEPTH) ** 0.25
LN_EPS = 1e-5
RMS_EPS = 1e-6
NCORES = 8
NSLOT = 3
TT = 512
NT = S // TT
D_FF = 2816
NE = 8
D_FFE = 3584
EV_IN = 1440
OD_IN = 3632
NEG = -30000.0
SPARSE_MOE = True
CONV_SPLIT = 99
GCM = 512
NTL = 24
I32 = mybir.dt.int32


class Res:
    __slots__ = ("name", "w", "rs", "dsem", "dcount", "persist", "phase", "scope")

    def __init__(self, name, persist=False):
        self.name = name
        self.phase = False
        self.scope = None
        self.w = None
        self.rs = {}
        self.dsem = None
        self.dcount = 0
        self.persist = persist


class Eng:
    def __init__(self, name, h, sem):
        self.name, self.h, self.sem = name, h, sem
        self.count = 0
        self.seen = {}
        self.nins = 0
        self.nwait = 0


class Tile:
    def __init__(self, t, r):
        self.t, self.r = t, r

    def __getitem__(self, key):
        return self.t[key]


class K:
    def __init__(self, nc, stack):
        self.nc = nc
        self.stack = stack
        self.engs = {}
        for name, h in (("pe", nc.tensor), ("act", nc.scalar), ("dve", nc.vector),
                        ("pool", nc.gpsimd), ("sp", nc.sync)):
            sem = stack.enter_context(nc.semaphore("sem_" + name))
            self.engs[name] = Eng(name, h, sem)
        self.pe, self.act, self.dve, self.pool, self.sp = (
            self.engs[n] for n in ("pe", "act", "dve", "pool", "sp"))
        self.all_res = []
        self.nsem = 5
        self.free_dsems = []
        self.sem_count = {}

    def res(self, name, persist=False):
        r = Res(name, persist)
        self.all_res.append(r)
        return r

    def _waits(self, eng, reads, writes, partial_dst=None):
        need = {}

        def add(m, raw):
            sem, val, src = m
            if src is eng and not raw:
                return
            if src is None:
                val = max(val, self.sem_count.get(sem, 0))
            if eng.seen.get(sem, 0) >= val:
                return
            if need.get(sem, 0) < val:
                need[sem] = val

        for r in reads:
            if r.w is not None:
                add(r.w, True)
        for w in writes:
            if w.w is not None and not (w is partial_dst and w.w[0] is w.dsem):
                add(w.w, False)
            for sem, (val, src) in w.rs.items():
                add((sem, val, src), False)
        for sem, val in need.items():
            eng.h.wait_ge(sem, val)
            eng.seen[sem] = val
            eng.nwait += 1

    def _mark(self, m, reads, writes):
        sem, val, src = m
        for r in reads:
            o = r.rs.get(sem)
            if o is None or o[0] < val:
                r.rs[sem] = (val, src)
        for w in writes:
            w.w = m
            w.rs = {}

    def op(self, eng, fn, reads=(), writes=()):
        self._waits(eng, reads, writes)
        eng.count += 1
        eng.nins += 1
        ins = fn()
        ins.then_inc(eng.sem, 1)
        self._mark((eng.sem, eng.count, eng), reads, writes)
        return ins

    def dma(self, q, pairs, reads=(), writes=(), partial=False, **kw):
        r0 = writes[0]
        self._waits(q, reads, writes, partial_dst=r0 if partial else None)
        if r0.dsem is None:
            if self.free_dsems:
                r0.dsem, r0.dcount = self.free_dsems.pop()
            else:
                r0.dsem = self.stack.enter_context(self.nc.semaphore("dsem_%d" % self.nsem))
                r0.dcount = 0
                self.nsem += 1
        for (o, i) in pairs:
            q.h.dma_start(out=o, in_=i, **kw).then_inc(r0.dsem, 16)
            r0.dcount += 16
            q.nins += 1
        self.sem_count[r0.dsem] = r0.dcount
        self._mark((r0.dsem, r0.dcount, None), reads, writes)

    def idma(self, fn, reads, writes, partial=False):
        q = self.pool
        r0 = writes[0]
        self._waits(q, reads, writes, partial_dst=r0 if partial else None)
        if r0.dsem is None:
            if self.free_dsems:
                r0.dsem, r0.dcount = self.free_dsems.pop()
            else:
                r0.dsem = self.stack.enter_context(self.nc.semaphore("dsem_%d" % self.nsem))
                r0.dcount = 0
                self.nsem += 1
        fn().then_inc(r0.dsem, 16)
        r0.dcount += 16
        q.nins += 1
        self.sem_count[r0.dsem] = r0.dcount
        self._mark((r0.dsem, r0.dcount, None), reads, writes)

    def barrier(self, closing=None):
        sp = self.sp
        for r in self.all_res:
            if r.persist:
                continue
            if r.dsem is not None and sp.seen.get(r.dsem, 0) < r.dcount:
                sp.h.wait_ge(r.dsem, r.dcount)
                sp.seen[r.dsem] = r.dcount
        sp.count += 1
        sp.h.sem_inc(sp.sem, 1)
        for e in self.engs.values():
            for o in self.engs.values():
                if o is e or o.count == 0:
                    continue
                if e.seen.get(o.sem, 0) < o.count:
                    e.h.wait_ge(o.sem, o.count)
                    e.seen[o.sem] = o.count
            for r in self.all_res:
                if not r.persist and r.dsem is not None:
                    e.seen[r.dsem] = r.dcount
        for r in self.all_res:
            if not r.persist:
                r.w = None
                r.rs = {}
        keep = []
        for r in self.all_res:
            if r.phase:
                if r.dsem is not None:
                    self.free_dsems.append((r.dsem, r.dcount))
                    r.dsem = None
                if r.scope is closing:
                    continue
            keep.append(r)
        self.all_res = keep

    def finish(self):
        sp = self.sp
        for r in self.all_res:
            ms = [(s_, v, e) for s_, (v, e) in r.rs.items()]
            if r.w is not None:
                ms.append(r.w)
            for (sem, val, src) in ms:
                if sp.seen.get(sem, 0) >= val:
                    continue
                sp.h.wait_ge(sem, val)
                sp.seen[sem] = val


class Prog:
    def __init__(self, nslot=NSLOT, nlayers=DEPTH, debug=False):
        self.nslot = nslot
        self.nlayers = nlayers
        self.debug = debug
        self.nc = bass.Bass("TRN2", target_bir_lowering=False)
        self.in_names = []

    def din(self, name, shape, dt=F32):
        self.in_names.append(name)
        return self.nc.dram_tensor(name, list(shape), dt, kind="ExternalInput").ap()

    def dscr(self, name, shape, dt):
        return self.nc.dram_tensor(name, list(shape), dt).ap()

    def T(self, name, shape, dt=F32):
        t = self.ph.enter_context(self.nc.sbuf_tensor(name + "_%d" % self.uid(), list(shape), dt))
        r = self.k.res(name)
        r.phase = self.ph is not self.stack
        r.scope = self.ph
        return Tile(t, r)

    def TP(self, name, shape, dt=F32):
        t = self.stack.enter_context(self.nc.sbuf_tensor(name, list(shape), dt))
        return Tile(t, self.k.res(name, persist=True))

    def uid(self):
        self._uid += 1
        return self._uid

    def MM(self, out, oap, lt, lap, rt, rap, start, stop):
        nc = self.nc
        self.k.op(self.k.pe, lambda: nc.tensor.matmul(oap, lhsT=lap, rhs=rap, start=start, stop=stop),
                  [lt.r, rt.r], [out.r])

    def TR(self, out, oap, it, iap, ident):
        nc = self.nc
        self.k.op(self.k.pe, lambda: nc.tensor.transpose(oap, iap, ident[:]), [it.r, ident.r], [out.r])

    def ACT(self, out, oap, it, iap, func, bias=None, scale=None, accum=None, extra=()):
        nc = self.nc
        kw = {}
        if bias is not None:
            kw["bias"] = bias
        if scale is not None:
            kw["scale"] = scale
        if accum is not None:
            kw["accum_out"] = accum
        self.k.op(self.k.act, lambda: nc.scalar.activation(out=oap, in_=iap, func=func, **kw),
                  [it.r] + [e.r for e in extra], [out.r])

    def V(self, eng, fn, reads, writes):
        self.k.op(eng, fn, [t.r for t in reads], [t.r for t in writes])

    def TT_(self, out, oap, a, aap, b, bap, op, eng=None):
        nc = self.nc
        eng = eng or self.k.dve
        self.k.op(eng, lambda: eng.h.tensor_tensor(out=oap, in0=aap, in1=bap, op=op), [a.r, b.r], [out.r])

    def TS(self, out, oap, a, aap, s1, s2, op0, op1=None, extra=(), eng=None):
        eng = eng or self.k.dve
        if op1 is None:
            fn = lambda: eng.h.tensor_scalar(out=oap, in0=aap, scalar1=s1, scalar2=None, op0=op0)
        else:
            fn = lambda: eng.h.tensor_scalar(out=oap, in0=aap, scalar1=s1, scalar2=s2, op0=op0, op1=op1)
        self.k.op(eng, fn, [a.r] + [e.r for e in extra], [out.r])

    def STT(self, out, oap, a, aap, sc, b, bap, op0, op1, extra=(), eng=None):
        eng = eng or self.k.dve
        self.k.op(eng, lambda: eng.h.scalar_tensor_tensor(out=oap, in0=aap, scalar=sc, in1=bap, op0=op0, op1=op1),
                  [a.r, b.r] + [e.r for e in extra], [out.r])

    def CP(self, out, oap, it, iap, eng=None):
        eng = eng or self.k.dve
        if eng is self.k.act:
            self.k.op(eng, lambda: self.nc.scalar.copy(out=oap, in_=iap), [it.r], [out.r])
        else:
            self.k.op(eng, lambda: eng.h.tensor_copy(out=oap, in_=iap), [it.r], [out.r])

    def LD(self, tile, oap, src_ap, src_res=None, q=None, partial=False):
        self.k.dma(q or self.k.sp, [(oap, src_ap)], [src_res] if src_res is not None else [], [tile.r], partial=partial)

    def ST(self, dst_ap, dst_res, tile, iap, q=None):
        self.k.dma(q or self.k.sp, [(dst_ap, iap)], [tile.r], [dst_res], partial=True)

    def build(self):
        nc = self.nc
        self._uid = 0
        with ExitStack() as stack:
            self.stack = stack
            self.k = K(nc, stack)
            self.declare_io()
            self.setup_consts()
            self.convert_weights([l for l in range(self.nlayers) if l < CONV_SPLIT])
            for slot in range(self.nslot):
                self.run_slot(slot)
            self.k.barrier()
        return nc

    def declare_io(self):
        ns = self.nslot
        self.x_in = self.din("x", [ns, S, D])
        self.y_out = self.nc.dram_tensor("y", [ns, S, D], F32, kind="ExternalOutput").ap()
        self.r_y = self.k.res("y_out")
        w = {}
        w["ev_w_in"] = self.din("ev_w_in", [2, D, EV_IN])
        w["conv_dw_w"] = self.din("conv_dw_w", [2, 31, 512])
        w["conv_dw_b"] = self.din("conv_dw_b", [2, 512])
        w["conv_ln_g"] = self.din("conv_ln_g", [2, 512])
        w["conv_ln_b"] = self.din("conv_ln_b", [2, 512])
        w["mla_q_norm_g"] = self.din("mla_q_norm_g", [2, 256])
        w["mla_w_uq"] = self.din("mla_w_uq", [2, 256, 768])
        w["mla_kv_norm_g"] = self.din("mla_kv_norm_g", [2, 128])
        w["mla_w_ukv"] = self.din("mla_w_ukv", [2, 128, 1024])
        w["ev_w_out"] = self.din("ev_w_out", [2, 1024, 1024])
        w["od_w_in"] = self.din("od_w_in", [2, D, OD_IN])
        w["ssd_conv_w"] = self.din("ssd_conv_w", [2, 5, 1024])
        w["ssd_conv_b"] = self.din("ssd_conv_b", [2, 1024])
        w["ssd_dt_bias"] = self.din("ssd_dt_bias", [2, 16])
        w["ssd_a_log"] = self.din("ssd_a_log", [2, 16])
        w["ssd_d"] = self.din("ssd_d", [2, 8])
        w["ssd_norm_g"] = self.din("ssd_norm_g", [2, 512])
        w["ml_igate_b"] = self.din("ml_igate_b", [2, 16])
        w["ml_fgate_b"] = self.din("ml_fgate_b", [2, 16])
        w["ml_norm_g"] = self.din("ml_norm_g", [2, 512])
        w["od_w_out"] = self.din("od_w_out", [2, 1024, 1024])
        w["ffn_w_gate"] = self.din("ffn_w_gate", [2, D, D_FF])
        w["ffn_w_up"] = self.din("ffn_w_up", [2, D, D_FF])
        w["ffn_w_down"] = self.din("ffn_w_down", [2, D_FF, D])
        w["moe_router_w"] = self.din("moe_router_w", [2, D, NE])
        w["moe_router_b"] = self.din("moe_router_b", [2, NE])
        w["moe_w_gate"] = self.din("moe_w_gate", [2, NE, D, D_FFE])
        w["moe_w_up"] = self.din("moe_w_up", [2, NE, D, D_FFE])
        w["moe_w_down"] = self.din("moe_w_down", [2, NE, D_FFE, D])
        for n in ("ln1_g", "ln1_b", "ln2_g", "ln2_b"):
            w[n] = self.din(n, [4, D])
        self.w = w
        self.c_ident = self.din("c_ident", [128, 128])
        self.c_rope = self.din("c_rope", [2, 32, S])
        self.c_tri = self.din("c_tri", [4, 128, 128])
        self.c_pg = self.din("c_pg", [128, 9])
        self.xres = self.dscr("xres", [S, D], F32)
        self.r_xres = self.k.res("xres")
        self.xT = self.dscr("xT", [D, S + 4], BF16)
        self.r_xT = self.k.res("xT")
        if self.debug:
            self.dbg = self.nc.dram_tensor("dbg", [self.nlayers * 2, S, D], F32, kind="ExternalOutput").ap()
            self.r_dbg = self.k.res("dbg")

    def setup_consts(self):
        nc, k = self.nc, self.k
        self.ph = self.stack
        self.ident32 = self.TP("ident32", [128, 128], F32)
        self.LD(self.ident32, self.ident32[:], self.c_ident)
        self.ident16 = self.TP("ident16", [128, 128], BF16)
        self.CP(self.ident16, self.ident16[:], self.ident32, self.ident32[:])
        self.ones32 = self.TP("ones32", [128, 128], F32)
        self.V(k.dve, lambda: nc.vector.memset(self.ones32[:], 1.0), [], [self.ones32])
        self.zero16 = self.TP("zero16", [128, 64], BF16)
        self.V(k.dve, lambda: nc.vector.memset(self.zero16[:], 0.0), [], [self.zero16])
        self.zero32 = self.TP("zero32", [128, 64], F32)
        self.V(k.dve, lambda: nc.vector.memset(self.zero32[:], 0.0), [], [self.zero32])
        self.pb = []
        for i in range(7):
            t = self.stack.enter_context(nc.psum_tensor("pb%d" % i, [128, 512], F32))
            self.pb.append(Tile(t, k.res("pb%d" % i, persist=True)))
        t = self.stack.enter_context(nc.psum_tensor("ptr", [128, 1024], BF16))
        self.ptr = Tile(t, k.res("ptr", persist=True))
        xTv = self.xT.rearrange("(c p) s -> p c s", p=128)
        for lo in (0, S + 2):
            k.dma(k.sp, [(xTv[:, :, lo:lo + 2], self.zero16[:, 0:16].rearrange("p (c s) -> p c s", c=8))],
                  [self.zero16.r], [self.r_xT], partial=True)

    def convert_weights(self, layers):
        k = self.k
        GC = 256
        if not hasattr(self, "wb"):
            self.wb = {}
            self.wb_r = {}
            self._alloc_wb(GC)
        self._convert(layers, GC)

    def _alloc_wb(self, GC):
        plain = ["ev_w_in", "mla_w_uq", "mla_w_ukv", "ev_w_out", "od_w_in", "od_w_out"]
        for n in plain:
            self.wb[n] = self.dscr("wb_" + n, list(self.w[n].shape), BF16)
        self.wb["ffn_w_gate"] = self.dscr("wb_ffn_g", [2, 1, D_FF // GC, 128, 8, GC], BF16)
        self.wb["ffn_w_up"] = self.dscr("wb_ffn_u", [2, 1, D_FF // GC, 128, 8, GC], BF16)
        self.wb["ffn_w_down"] = self.dscr("wb_ffn_d", [2, 1, 2, 128, D_FF // 128, 512], BF16)
        self.wb["moe_w_gate"] = self.dscr("wb_moe_g", [2, NE, D_FFE // GCM, 128, 8, GCM], BF16)
        self.wb["moe_w_up"] = self.dscr("wb_moe_u", [2, NE, D_FFE // GCM, 128, 8, GCM], BF16)
        self.wb["moe_w_down"] = self.dscr("wb_moe_d", [2, NE, 2, 128, D_FFE // 128, 512], BF16)

    def _convert(self, layers, GC):
        k = self.k

        def conv_plain(n, j):
            src, dst = self.w[n][j], self.wb[n][j]
            r = self._grp
            self.wb_r[(n, j, 0)] = r
            rows = src.shape[0]
            prs = [(dst[r0:min(r0 + 256, rows), :], src[r0:min(r0 + 256, rows), :]) for r0 in range(0, rows, 256)]
            k.dma(k.pool, prs, [], [r], partial=True)

        def conv_ff(prefix, j, ne):
            for e in range(ne):
                for nm in ("gate", "up"):
                    n = "%s_w_%s" % (prefix, nm)
                    src = self.w[n][j] if ne == 1 else self.w[n][j, e]
                    dst = self.wb[n][j, e]
                    r = self._grp
                    self.wb_r[(n, j, e)] = r
                    sv = src.rearrange("(c p) n -> p c n", p=128)
                    gc = dst.shape[-1]
                    prs = [(dst[g], sv[:, :, g * gc:(g + 1) * gc]) for g in range(dst.shape[0])]
                    k.dma(k.pool, prs, [], [r], partial=True)
                n = "%s_w_down" % prefix
                src = self.w[n][j] if ne == 1 else self.w[n][j, e]
                dst = self.wb[n][j, e]
                r = self._grp
                self.wb_r[(n, j, e)] = r
                sv = src.rearrange("(f p) d -> p f d", p=128)
                nf = sv.shape[1]
                prs = []
                for half in range(2):
                    for f0 in range(0, nf, 7):
                        f1 = min(nf, f0 + 7)
                        prs.append((dst[half][:, f0:f1, :], sv[:, f0:f1, half * 512:(half + 1) * 512]))
                k.dma(k.pool, prs, [], [r], partial=True)

        for layer in layers:
            j = layer // 2
            self._grp = k.res("wbgrp_mix%d" % layer, persist=True)
            if layer % 2 == 0:
                for n in ("ev_w_in", "mla_w_uq", "mla_w_ukv", "ev_w_out"):
                    conv_plain(n, j)
                self._grp = k.res("wbgrp_ffn%d" % layer, persist=True)
                conv_ff("ffn", j, 1)
            else:
                for n in ("od_w_in", "od_w_out"):
                    conv_plain(n, j)
                self._grp = k.res("wbgrp_ffn%d" % layer, persist=True)
                conv_ff("moe", j, NE)

    def load_bcast(self, name, src_row_ap, n):
        t = self.T(name, [128, n], F32)
        self.LD(t, t[:], src_row_ap.partition_broadcast(128))
        return t

    def load_col(self, name, src_vec_ap, nchunk):
        t = self.T(name, [128, nchunk], F32)
        self.k.dma(self.k.sp, [(t[:], src_vec_ap.rearrange("(c p) -> p c", p=128))], [], [t.r],
                   allow_slow_non_contiguous=True)
        return t

    def run_slot(self, slot):
        k = self.k
        self.prologue(slot)
        for layer in range(self.nlayers):
            j = layer // 2
            last = (layer == self.nlayers - 1)
            if layer % 2 == 0:
                self.even_mixer(j, layer)
                self.ffn_like(layer, j, moe=False, dst=(self.y_out[slot], self.r_y) if last else None)
            else:
                self.odd_mixer(j, layer)
                if SPARSE_MOE:
                    self.moe_sparse(layer, j, dst=(self.y_out[slot], self.r_y) if last else None)
                else:
                    self.ffn_like(layer, j, moe=True, dst=(self.y_out[slot], self.r_y) if last else None)
                if slot == 0 and layer == 1 and self.nlayers > CONV_SPLIT:
                    self.convert_weights([l for l in range(self.nlayers) if l >= CONV_SPLIT])

    def phase(self):
        prog = self

        class _P:
            def __enter__(s):
                prog._phstack = ExitStack()
                prog._phstack.__enter__()
                prog.ph = prog._phstack
                return s

            def __exit__(s, *a):
                if a[0] is None:
                    prog.k.barrier(closing=prog._phstack)
                prog._phstack.__exit__(*a)
                prog.ph = prog.stack
                return False
        return _P()

    def prologue(self, slot):
        nc, k = self.nc, self.k
        with self.phase():
            xin = self.x_in[slot]
            for t in range(S // 128):
                xt = self.T("pro_x%d" % (t % 2), [128, D], F32) if t < 2 else None
                if t < 2:
                    if t == 0:
                        self._pro = []
                    self._pro.append(xt)
                xt = self._pro[t % 2]
                self.LD(xt, xt[:], xin[t * 128:(t + 1) * 128, :])
                self.ST(self.xres[t * 128:(t + 1) * 128, :], self.r_xres, xt, xt[:])
                self.emit_xT(xt, t * 128, "pro")

    def emit_xT(self, y32, tok0, tag):
        nc, k = self.nc, self.k
        key = "_xT_" + tag
        if not hasattr(self, key) or getattr(self, key)[0] is not self.ph:
            y16 = self.T(tag + "_y16", [128, D], BF16)
            xtt = self.T(tag + "_xtt", [128, 8, 128], BF16)
            setattr(self, key, (self.ph, y16, xtt))
        _, y16, xtt = getattr(self, key)
        self.CP(y16, y16[:], y32, y32[:], eng=k.act)
        for c in range(8):
            self.TR(self.ptr, self.ptr[:, c * 128:(c + 1) * 128], y16, y16[:, c * 128:(c + 1) * 128], self.ident16)
        self.CP(xtt, xtt[:].rearrange("p c s -> p (c s)"), self.ptr, self.ptr[:], eng=k.dve)
        xTv = self.xT.rearrange("(c p) s -> p c s", p=128)
        self.ST(xTv[:, :, 2 + tok0:2 + tok0 + 128], self.r_xT, xtt, xtt[:])

    def ln_consts(self, gname, bname, layer):
        g = self.load_bcast("lng", self.w[gname][layer:layer + 1, :], D)
        b = self.load_bcast("lnb", self.w[bname][layer:layer + 1, :], D)
        return g, b

    def epilogue(self, ps_halves, tok0, g, b, dst, tag, dbg_idx=None, add_tile=None):
        self.epilogue_multi([(ps_halves, tok0)], g, b, dst, tag, dbg_idx=dbg_idx, nch=1)

    def epilogue_multi(self, items, g, b, dst, tag, dbg_idx=None, nch=2):
        nc, k = self.nc, self.k
        key = "_ep_" + tag
        if not hasattr(self, key) or getattr(self, key)[0] is not self.ph:
            tiles = (self.ph,
                     [self.T(tag + "_ex%d" % i, [128, D], F32) for i in range(nch)],
                     [self.T(tag + "_et%d" % i, [128, D], F32) for i in range(nch)],
                     [self.T(tag + "_est%d" % i, [128, 2, 6], F32) for i in range(nch)],
                     [self.T(tag + "_emv%d" % i, [128, 2], F32) for i in range(nch)],
                     [self.T(tag + "_ers%d" % i, [128, 1], F32) for i in range(nch)],
                     [self.T(tag + "_y16%d" % i, [128, D], BF16) for i in range(nch)],
                     [self.T(tag + "_xtt%d" % i, [128, 8, 128], BF16) for i in range(nch)])
            setattr(self, key, tiles)
        _, xs_, ts_, stts, mvs, rss, y16s, xtts = getattr(self, key)
        n = len(items)
        assert n <= nch
        R = range(n)
        for c in R:
            tok0 = items[c][1]
            self.LD(xs_[c], xs_[c][:], self.xres[tok0:tok0 + 128, :], self.r_xres)
        for h in range(2):
            for c in R:
                pt, pap = items[c][0][h]
                self.STT(ts_[c], ts_[c][:, h * 512:(h + 1) * 512], xs_[c], xs_[c][:, h * 512:(h + 1) * 512], ALPHA, pt, pap,
                         ALU.mult, ALU.add)
        for h in range(2):
            for c in R:
                self.V(k.dve, (lambda c_, h_: (lambda: nc.vector.bn_stats(out=stts[c_][:, h_, :], in_=ts_[c_][:, h_ * 512:(h_ + 1) * 512])))(c, h),
                       [ts_[c]], [stts[c]])
        for c in R:
            self.V(k.dve, (lambda c_: (lambda: nc.vector.bn_aggr(out=mvs[c_][:], in_=stts[c_][:])))(c), [stts[c]], [mvs[c]])
        for c in R:
            self.ACT(rss[c], rss[c][:], mvs[c], mvs[c][:, 1:2], AF.Sqrt, bias=self.eps_ln[:, 0:1], scale=1.0, extra=[self.eps_ln])
        for c in R:
            self.V(k.dve, (lambda c_: (lambda: nc.vector.reciprocal(out=rss[c_][:], in_=rss[c_][:])))(c), [rss[c]], [rss[c]])
        for c in R:
            self.TS(ts_[c], ts_[c][:], ts_[c], ts_[c][:], mvs[c][:, 0:1], rss[c][:, 0:1], ALU.subtract, ALU.mult, extra=[mvs[c], rss[c]])
        for c in R:
            self.TT_(ts_[c], ts_[c][:], ts_[c], ts_[c][:], g, g[:], ALU.mult)
        for c in R:
            self.TT_(ts_[c], ts_[c][:], ts_[c], ts_[c][:], b, b[:], ALU.add, eng=k.pool)
        xTv = self.xT.rearrange("(c p) s -> p c s", p=128)
        for c in R:
            tok0 = items[c][1]
            if dst is not None:
                dap, dres = dst
                self.ST(dap[tok0:tok0 + 128, :], dres, ts_[c], ts_[c][:])
            else:
                self.ST(self.xres[tok0:tok0 + 128, :], self.r_xres, ts_[c], ts_[c][:])
            if self.debug and dbg_idx is not None:
                self.ST(self.dbg[dbg_idx, tok0:tok0 + 128, :], self.r_dbg, ts_[c], ts_[c][:])
        if dst is None:
            for c in R:
                self.CP(y16s[c], y16s[c][:], ts_[c], ts_[c][:], eng=k.act)
            for c in R:
                tok0 = items[c][1]
                for cc in range(8):
                    self.TR(self.ptr, self.ptr[:, cc * 128:(cc + 1) * 128], y16s[c], y16s[c][:, cc * 128:(cc + 1) * 128], self.ident16)
                self.CP(xtts[c], xtts[c][:].rearrange("p c s -> p (c s)"), self.ptr, self.ptr[:], eng=k.dve)
                self.ST(xTv[:, :, 2 + tok0:2 + tok0 + 128], self.r_xT, xtts[c], xtts[c][:])

    def eps_tiles(self):
        nc, k = self.nc, self.k
        if not hasattr(self, "eps_ln"):
            self.eps_ln = self.TP("eps_ln", [128, 1], F32)
            self.V(k.dve, lambda: nc.vector.memset(self.eps_ln[:], LN_EPS), [], [self.eps_ln])
            self.eps_rms = self.TP("eps_rms", [128, 1], F32)
            self.V(k.dve, lambda: nc.vector.memset(self.eps_rms[:], RMS_EPS), [], [self.eps_rms])

    def load_xT_full(self, name="xTs"):
        t = self.T(name, [128, 8, S + 4], BF16)
        xTv = self.xT.rearrange("(c p) s -> p c s", p=128)
        for c in range(8):
            self.k.dma(self.k.sp, [(t[:, c, :], xTv[:, c, :])], [self.r_xT], [t.r], partial=True)
        return t

    def load_w(self, name, src, kc, cols, res, c0=0):
        t = self.T(name, [128, kc, cols], BF16)
        v = src.rearrange("(c p) n -> p c n", p=128)
        self.k.dma(self.k.sp, [(t[:], v[:, :, c0:c0 + cols])], [res], [t.r])
        return t

    def ring(self, name, shape, dt, n, init=None):
        tiles = [self.T("%s%d" % (name, i), shape, dt) for i in range(n)]
        if init is not None:
            for t in tiles:
                self.V(self.k.dve, (lambda tt: (lambda: self.nc.vector.memset(tt[:], init)))(t), [], [t])
        st = [0]

        def nxt():
            t = tiles[st[0] % n]
            st[0] += 1
            return t
        return nxt

    def pA(self):
        self._pa = getattr(self, "_pa", 0) + 1
        return self.pb[self._pa % 4]

    def pB(self):
        self._pbi = getattr(self, "_pbi", 0) + 1
        return self.pb[4 + self._pbi % 3]

    def rsqrt_(self, out, oap, src, sap, scale, eps_tile):
        self.ACT(out, oap, src, sap, AF.Sqrt, bias=eps_tile[:, 0:1], scale=scale, extra=[eps_tile])
        self.V(self.k.dve, lambda: self.nc.vector.reciprocal(out=oap, in_=oap), [out], [out])

    def even_mixer(self, j, layer):
        nc, k = self.nc, self.k
        self.eps_tiles()
        W = self.w
        if not hasattr(self, "u_d"):
            self.u_d = self.dscr("u_d", [4, 128, S + 30], F32)
            self.r_u = k.res("u_d")
            self.q_d = self.dscr("q_d", [8, 96, S], BF16)
            self.r_q = k.res("q_d")
            self.kn_d = self.dscr("kn_d", [8, 64, S], BF16)
            self.r_kn = k.res("kn_d")
            self.kpe_d = self.dscr("kpe_d", [32, S], BF16)
            self.r_kpe = k.res("kpe_d")
            self.v_d = self.dscr("v_d", [S, 8 * 65], BF16)
            self.r_v = k.res("v_d")
            self.att_d = self.dscr("att_d", [S, 512], BF16)
            self.r_att = k.res("att_d")
            uv = self.u_d.rearrange("c p s -> p c s")
            for lo in (0, S + 15):
                k.dma(k.sp, [(uv[:, :, lo:lo + 15], self.zero32[:, 0:60].rearrange("p (c s) -> p c s", c=4))],
                      [self.zero32.r], [self.r_u], partial=True)
        sl = 96 ** -0.5
        with self.phase():
            xTs = self.load_xT_full()
            wr = self.wb_r[("ev_w_in", j, 0)]
            win = self.load_w("win", self.wb["ev_w_in"][j], 8, EV_IN, wr)
            wv_ = self.wb["ev_w_in"][j].rearrange("(c p) n -> p c n", p=128)
            wkrs = self.T("wkrs", [128, 8, 96], BF16)
            k.dma(k.sp, [(wkrs[:, :, 0:64], wv_[:, :, 1344:1408]), (wkrs[:, :, 64:80], wv_[:, :, 1424:1440]),
                         (wkrs[:, :, 80:96], wv_[:, :, 1408:1424])], [wr], [wkrs.r])
            wq_r = self.wb_r[("mla_w_uq", j, 0)]
            wuq = self.load_w("wuq", self.wb["mla_w_uq"][j], 2, 768, wq_r)
            wuqs = self.T("wuqs", [128, 2, 768], BF16)
            vq = self.wb["mla_w_uq"][j].rearrange("(c p) (h e) -> p c h e", p=128, e=96)
            wqv = wuqs[:].rearrange("p c (h e) -> p c h e", e=96)
            prs = []
            for c in range(2):
                prs += [(wqv[:, c, :, 0:64], vq[:, c, :, 0:64]), (wqv[:, c, :, 64:80], vq[:, c, :, 80:96]),
                        (wqv[:, c, :, 80:96], vq[:, c, :, 64:80])]
            k.dma(k.sp, prs, [wq_r], [wuqs.r])
            wkv_r = self.wb_r[("mla_w_ukv", j, 0)]
            wukv = self.load_w("wukv", self.wb["mla_w_ukv"][j], 1, 1024, wkv_r)
            wvv = self.T("wvv", [128, 8, 64], BF16)
            k.dma(k.sp, [(wvv[:], self.wb["mla_w_ukv"][j].rearrange("p (h e) -> p h e", e=128)[:, :, 64:128])],
                  [wkv_r], [wvv.r])
            gq = self.load_col("gq", W["mla_q_norm_g"][j], 2)
            gkv = self.load_col("gkv", W["mla_kv_norm_g"][j], 1)
            r_sig = self.ring("sig", [128, TT], F32, 2)
            r_u = self.ring("u", [128, TT], F32, 2)
            r_sq = self.ring("sq", [128, TT], F32, 3)
            r_rstd = self.ring("rstd", [128, TT], F32, 2)
            r_qn = self.ring("qn", [128, TT], BF16, 4)
            r_kvn = self.ring("kvn", [128, TT], BF16, 2)
            r_cs = self.ring("cs", [96, 2, TT], F32, 2)
            r_qt = self.ring("qt", [96, TT], BF16, 3)
            r_t1 = self.ring("t1", [96, TT], F32, 2)
            r_t2 = self.ring("t2", [96, TT], F32, 2)
            r_kn = self.ring("kn", [64, TT], BF16, 3)
            r_vt = self.ring("vt", [128, 8, 65], BF16, 3, init=1.0)
            for t in range(NT):
                c0 = 2 + t * TT
                cols = slice(t * TT, (t + 1) * TT)

                def proj(pt, pap, wt, wap_fn):
                    for c in range(8):
                        self.MM(pt, pap, wt, wap_fn(c), xTs, xTs[:, c, c0:c0 + TT], c == 0, c == 7)
                for jj in range(4):
                    pv, pg = self.pA(), self.pA()
                    proj(pv, pv[:], win, lambda c: win[:, c, jj * 128:(jj + 1) * 128])
                    proj(pg, pg[:], win, lambda c: win[:, c, 512 + jj * 128:512 + (jj + 1) * 128])
                    sig = r_sig()
                    self.ACT(sig, sig[:], pg, pg[:], AF.Sigmoid)
                    u = r_u()
                    self.TT_(u, u[:], pv, pv[:], sig, sig[:], ALU.mult)
                    self.ST(self.u_d[jj][:, 15 + t * TT:15 + (t + 1) * TT], self.r_u, u, u[:])
                pq = [self.pA(), self.pA()]
                for cq in range(2):
                    proj(pq[cq], pq[cq][:], win, lambda c: win[:, c, 1024 + cq * 128:1024 + (cq + 1) * 128])
                pkv = self.pB()
                proj(pkv, pkv[:], win, lambda c: win[:, c, 1280:1408])
                pkr = self.pB()
                proj(pkr, pkr[0:96, :], win, lambda c: win[:, c, 1344:1440])
                pkrs = self.pB()
                proj(pkrs, pkrs[0:96, :], wkrs, lambda c: wkrs[:, c, :])
                cs = r_cs()
                k.dma(k.sp, [(cs[64:96, 0, :], self.c_rope[0][:, cols]), (cs[64:96, 1, :], self.c_rope[1][:, cols])],
                      [], [cs.r])
                t1, t2, kpe = r_t1(), r_t2(), r_qt()
                self.TT_(t1, t1[64:96, :], pkr, pkr[64:96, :], cs, cs[64:96, 0, :], ALU.mult)
                self.TT_(t2, t2[64:96, :], pkrs, pkrs[64:96, :], cs, cs[64:96, 1, :], ALU.mult)
                self.TT_(kpe, kpe[64:96, :], t1, t1[64:96, :], t2, t2[64:96, :], ALU.add)
                self.ST(self.kpe_d[:, cols], self.r_kpe, kpe, kpe[64:96, :])
                sqs = []
                for cq in range(2):
                    sq = r_sq()
                    self.ACT(sq, sq[:], pq[cq], pq[cq][:], AF.Square)
                    sqs.append(sq)
                pss = self.pA()
                for cq in range(2):
                    self.MM(pss, pss[:], self.ones32, self.ones32[:], sqs[cq], sqs[cq][:], cq == 0, cq == 1)
                rstd = r_rstd()
                self.rsqrt_(rstd, rstd[:], pss, pss[:], 1.0 / 256.0, self.eps_rms)
                qn = []
                for cq in range(2):
                    q_ = r_qn()
                    self.STT(q_, q_[:], pq[cq], pq[cq][:], gq[:, cq:cq + 1], rstd, rstd[:], ALU.mult, ALU.mult, extra=[gq])
                    qn.append(q_)
                sq = r_sq()
                self.ACT(sq, sq[:], pkv, pkv[:], AF.Square)
                pss2 = self.pA()
                self.MM(pss2, pss2[:], self.ones32, self.ones32[:], sq, sq[:], True, True)
                rstdk = r_rstd()
                self.rsqrt_(rstdk, rstdk[:], pss2, pss2[:], 1.0 / 128.0, self.eps_rms)
                kvn = r_kvn()
                self.STT(kvn, kvn[:], pkv, pkv[:], gkv[:, 0:1], rstdk, rstdk[:], ALU.mult, ALU.mult, extra=[gkv])
                for h in range(8):
                    pqh, pqs = self.pA(), self.pA()
                    for c in range(2):
                        self.MM(pqh, pqh[0:96, :], wuq, wuq[:, c, 96 * h:96 * h + 96], qn[c], qn[c][:], c == 0, c == 1)
                    for c in range(2):
                        self.MM(pqs, pqs[0:96, :], wuqs, wuqs[:, c, 96 * h:96 * h + 96], qn[c], qn[c][:], c == 0, c == 1)
                    qt = r_qt()
                    self.CP(qt, qt[0:64, :], pqh, pqh[0:64, :], eng=k.act)
                    t1, t2 = r_t1(), r_t2()
                    self.TT_(t1, t1[64:96, :], pqh, pqh[64:96, :], cs, cs[64:96, 0, :], ALU.mult)
                    self.TT_(t2, t2[64:96, :], pqs, pqs[64:96, :], cs, cs[64:96, 1, :], ALU.mult)
                    self.TT_(qt, qt[64:96, :], t1, t1[64:96, :], t2, t2[64:96, :], ALU.add)
                    self.ST(self.q_d[h][:, cols], self.r_q, qt, qt[0:96, :])
                for h in range(8):
                    pk = self.pB()
                    self.MM(pk, pk[0:64, :], wukv, wukv[:, 0, 128 * h:128 * h + 64], kvn, kvn[:], True, True)
                    kn = r_kn()
                    self.CP(kn, kn[:], pk, pk[0:64, :], eng=(k.act if h % 2 else k.dve))
                    self.ST(self.kn_d[h][:, cols], self.r_kn, kn, kn[:])
                for s in range(4):
                    pv_ = self.pB()
                    self.MM(pv_, pv_[:], kvn, kvn[:, s * 128:(s + 1) * 128], wvv, wvv[:].rearrange("p h e -> p (h e)"),
                            True, True)
                    vt = r_vt()
                    self.CP(vt, vt[:, :, 0:64], pv_, pv_[:].rearrange("p (h e) -> p h e", e=64),
                            eng=(k.act if s % 2 else k.dve))
                    r0 = t * TT + s * 128
                    self.ST(self.v_d[r0:r0 + 128, :], self.r_v, vt, vt[:].rearrange("p h e -> p (h e)"))
        outer = self.phase()
        outer.__enter__()
        y_sb = self.T("y_sb", [128, 4, S], F32)
        clg = self.load_col("clg", W["conv_ln_g"][j], 4)
        clb = self.load_col("clb", W["conv_ln_b"][j], 4)
        with self.subphase():
            cw = self.T("cw", [128, 4, 31], F32)
            k.dma(k.sp, [(cw[:, c, :], W["conv_dw_w"][j].rearrange("k (c p) -> c p k", p=128)[c]) for c in range(4)],
                  [], [cw.r], allow_slow_non_contiguous=True)
            cb = self.load_col("cb", W["conv_dw_b"][j], 4)
            r_uu = self.ring("uu", [128, S + 30], F32, 2)
            conv_ops = []

            def mk_first(jj, u):
                return lambda: self.TS(y_sb, y_sb[:, jj, :], u, u[:, 0:S], cw[:, jj, 0:1], cb[:, jj:jj + 1], ALU.mult, ALU.add,
                                       extra=[cw, cb])

            def mk_tap(jj, u, tap):
                return lambda: self.STT(y_sb, y_sb[:, jj, :], u, u[:, tap:tap + S], cw[:, jj, tap:tap + 1], y_sb, y_sb[:, jj, :],
                                        ALU.mult, ALU.add, extra=[cw])

            def mk_load(jj, holder):
                def f():
                    u = r_uu()
                    self.LD(u, u[:], self.u_d[jj], self.r_u)
                    holder.append(u)
                return f
            holders = [[] for _ in range(4)]
            for jj in range(4):
                conv_ops.append(mk_load(jj, holders[jj]))
                conv_ops.append((lambda jj_: (lambda: mk_first(jj_, holders[jj_][0])()))(jj))
                for tap in range(1, 31):
                    conv_ops.append((lambda jj_, tap_: (lambda: mk_tap(jj_, holders[jj_][0], tap_)()))(jj, tap))
            conv_it = iter(conv_ops)

            def emit_conv(n):
                for _ in range(n):
                    f = next(conv_it, None)
                    if f is None:
                        return
                    f()
            emit_conv(2)
            r_K = self.ring("Kh", [96, S], BF16, 2)
            r_Q = self.ring("Qh", [96, S], BF16, 2)
            r_V = self.ring("Vh", [128, 32, 65], BF16, 2)
            r_pT = self.ring("pT", [128, TT], BF16, 4)
            r_rec = self.ring("rec", [128, 4, 1], F32, 2)
            r_at = self.ring("at", [128, 4, 64], BF16, 2)
            vdv = self.v_d.rearrange("(kc p) (h e) -> p kc h e", p=128, e=65)
            attv = self.att_d.rearrange("(b p) f -> p b f", p=128)
            heads = {}

            def get_head(h):
                if h not in heads:
                    Kh, Qh, Vh = r_K(), r_Q(), r_V()
                    k.dma(k.sp, [(Kh[0:64, :], self.kn_d[h]), (Kh[64:96, :], self.kpe_d)], [self.r_kn, self.r_kpe], [Kh.r])
                    self.LD(Qh, Qh[:], self.q_d[h], self.r_q)
                    self.LD(Vh, Vh[:], vdv[:, :, h, :], self.r_v)
                    heads[h] = (Kh, Qh, Vh)
                return heads[h]
            its = [(h, qt, kc) for h in range(8) for qt in range(NT) for kc in range(32)]
            LA = 2
            pss = {}

            def emit_qk(i):
                h, qt, kc = its[i]
                Kh, Qh, Vh = get_head(h)
                ps = self.pA()
                self.MM(ps, ps[:], Kh, Kh[:, kc * 128:(kc + 1) * 128], Qh, Qh[:, qt * TT:(qt + 1) * TT], True, True)
                pss[i] = ps
            for i in range(min(LA, len(its))):
                emit_qk(i)
            po = None
            for i, (h, qt, kc) in enumerate(its):
                if i + LA < len(its):
                    emit_qk(i + LA)
                Kh, Qh, Vh = heads[h]
                if kc == 0:
                    po = self.pB()
                pov = po[:, 0:260].rearrange("p (b e) -> p b e", e=65)
                ps = pss.pop(i)
                pT = r_pT()
                self.ACT(pT, pT[:], ps, ps[:], AF.Exp, scale=sl)
                for qb in range(4):
                    self.MM(po, pov[:, qb, :], pT, pT[:, qb * 128:(qb + 1) * 128], Vh, Vh[:, kc, :], kc == 0, kc == 31)
                if kc == 31:
                    rec = r_rec()
                    self.V(k.dve, (lambda rec_, pov_: (lambda: nc.vector.reciprocal(out=rec_[:], in_=pov_[:, :, 64:65])))(rec, pov),
                           [po], [rec])
                    at = r_at()
                    self.TT_(at, at[:], po, pov[:, :, 0:64], rec, rec[:].to_broadcast([128, 4, 64]), ALU.mult)
                    self.ST(attv[:, qt * 4:(qt + 1) * 4, 64 * h:64 * h + 64], self.r_att, at, at[:])
                    emit_conv(2)
            emit_conv(1000)
        if True:
            wout = self.load_w("wout", self.wb["ev_w_out"][j], 8, D, self.wb_r[("ev_w_out", j, 0)])
            g1, b1 = self.ln_consts("ln1_g", "ln1_b", layer)
            r_sq = self.ring("sq3", [128, TT], F32, 2)
            mean = self.T("mean", [128, TT], F32)
            m2 = self.T("m2", [128, TT], F32)
            rstd = self.T("rstd3", [128, TT], F32)
            r_z = self.ring("z3", [128, TT], F32, 2)
            r_mix = self.ring("mixT", [128, 8, TT], BF16, 2)
            r_att = self.ring("att_in", [128, 512], BF16, 2)
            for t in range(NT):
                cols = slice(t * TT, (t + 1) * TT)
                pss, psq = self.pB(), self.pB()
                for jj in range(4):
                    self.MM(pss, pss[:], self.ones32, self.ones32[:], y_sb, y_sb[:, jj, cols], jj == 0, jj == 3)
                for jj in range(4):
                    sq = r_sq()
                    self.ACT(sq, sq[:], y_sb, y_sb[:, jj, cols], AF.Square)
                    self.MM(psq, psq[:], self.ones32, self.ones32[:], sq, sq[:], jj == 0, jj == 3)
                self.V(k.act, lambda: nc.scalar.mul(out=mean[:], in_=pss[:], mul=1.0 / 512.0), [pss], [mean])
                self.TT_(m2, m2[:], mean, mean[:], mean, mean[:], ALU.mult)
                self.STT(m2, m2[:], psq, psq[:], 1.0 / 512.0, m2, m2[:], ALU.mult, ALU.subtract)
                self.rsqrt_(rstd, rstd[:], m2, m2[:], 1.0, self.eps_ln)
                mixT = r_mix()
                for jj in range(4):
                    z = r_z()
                    self.TT_(z, z[:], y_sb, y_sb[:, jj, cols], mean, mean[:], ALU.subtract)
                    self.TT_(z, z[:], z, z[:], rstd, rstd[:], ALU.mult)
                    self.ACT(mixT, mixT[:, jj, :], z, z[:], AF.Silu, bias=clb[:, jj:jj + 1], scale=clg[:, jj:jj + 1],
                             extra=[clb, clg])
                for s in range(4):
                    at = r_att()
                    r0 = t * TT + s * 128
                    self.LD(at, at[:], self.att_d[r0:r0 + 128, :], self.r_att)
                    for fc in range(4):
                        self.TR(self.ptr, self.ptr[:, fc * 128:(fc + 1) * 128], at, at[:, fc * 128:(fc + 1) * 128], self.ident16)
                    self.CP(mixT, mixT[:, 4:8, s * 128:(s + 1) * 128],
                            self.ptr, self.ptr[:, 0:512].rearrange("p (c s) -> p c s", c=4), eng=k.act)
                for sp in range(2):
                    items = []
                    for s in (2 * sp, 2 * sp + 1):
                        phs = [self.pA(), self.pA()]
                        for half in range(2):
                            for c in range(8):
                                self.MM(phs[half], phs[half][:], mixT, mixT[:, c, s * 128:(s + 1) * 128],
                                        wout, wout[:, c, half * 512:(half + 1) * 512], c == 0, c == 7)
                        items.append(([(phs[0], phs[0][:]), (phs[1], phs[1][:])], t * TT + s * 128))
                    self.epilogue_multi(items, g1, b1, None, "e3", dbg_idx=2 * layer, nch=2)
        outer.__exit__(None, None, None)

    def ffn_like(self, layer, j, moe, dst):
        nc, k = self.nc, self.k
        self.eps_tiles()
        W = self.w
        GC = 256
        pre = "moe" if moe else "ffn"
        nexp = NE if moe else 1
        dff = D_FFE if moe else D_FF
        nf = dff // 128
        ng = dff // GC
        with self.phase():
            g2, b2 = self.ln_consts("ln2_g", "ln2_b", layer)
            xTv = self.xT.rearrange("(c p) s -> p c s", p=128)
            r_xt = self.ring("xt", [128, 8, TT], BF16, 2)
            hT = self.T("hT", [128, nf, TT], BF16)
            r_wg = self.ring("wg", [128, 8, GC], BF16, 2)
            r_wu = self.ring("wu", [128, 8, GC], BF16, 2)
            r_wd = self.ring("wd", [128, nf, 512], BF16, 2)
            acc = self.T("acc", [128, 4, D], F32)
            r_sg = self.ring("sg", [128, TT], F32, 2)
            if moe:
                wr32 = self.T("wr32", [128, 8, NE], F32)
                k.dma(k.sp, [(wr32[:], W["moe_router_w"][j].rearrange("(c p) e -> p c e", p=128))], [], [wr32.r])
                rb = self.load_bcast("rb", W["moe_router_b"][j:j + 1, :], NE)
                r_x32 = self.ring("x32", [128, D], F32, 2)
                xT32 = self.T("xT32", [128, 8, 128], F32)
                comb = self.T("comb", [128, 4, NE], F32)
                lg = self.T("lg", [128, NE], F32)
                l2 = self.T("l2", [128, NE], F32)
                mk1 = self.T("mk1", [128, NE], F32)
                mk2 = self.T("mk2", [128, NE], F32)
                sm = self.T("sm", [128, 8], F32)
            for t in range(NT):
                xt = r_xt()
                self.LD(xt, xt[:], xTv[:, :, 2 + t * TT:2 + (t + 1) * TT], self.r_xT)
                if moe:
                    for s in range(4):
                        r0 = t * TT + s * 128
                        x32 = r_x32()
                        self.LD(x32, x32[:], self.xres[r0:r0 + 128, :], self.r_xres)
                        pts = [self.pB(), self.pB()]
                        for c in range(8):
                            pt = pts[c // 4]
                            self.TR(pt, pt[:, (c % 4) * 128:(c % 4 + 1) * 128], x32, x32[:, c * 128:(c + 1) * 128], self.ident32)
                        for hh in range(2):
                            self.CP(xT32, xT32[:, hh * 4:(hh + 1) * 4, :].rearrange("p c s -> p (c s)"), pts[hh], pts[hh][:],
                                    eng=(k.act if hh else k.dve))
                        pr = self.pB()
                        for c in range(8):
                            self.MM(pr, pr[:, 0:NE], xT32, xT32[:, c, :], wr32, wr32[:, c, :], c == 0, c == 7)
                        self.TT_(lg, lg[:], pr, pr[:, 0:NE], rb, rb[:], ALU.add)
                        self.V(k.dve, lambda: nc.vector.tensor_reduce(out=sm[:, 0:1], in_=lg[:], axis=AX.X, op=ALU.max), [lg], [sm])
                        self.TS(mk1, mk1[:], lg, lg[:], sm[:, 0:1], None, ALU.is_equal, extra=[sm])
                        self.STT(l2, l2[:], mk1, mk1[:], -1.0e30, lg, lg[:], ALU.mult, ALU.add)
                        self.V(k.dve, lambda: nc.vector.tensor_reduce(out=sm[:, 1:2], in_=l2[:], axis=AX.X, op=ALU.max), [l2], [sm])
                        self.TS(mk2, mk2[:], l2, l2[:], sm[:, 1:2], None, ALU.is_equal, extra=[sm])
                        self.TT_(sm, sm[:, 2:3], sm, sm[:, 1:2], sm, sm[:, 0:1], ALU.subtract)
                        self.ACT(sm, sm[:, 3:4], sm, sm[:, 2:3], AF.Exp)
                        self.TS(sm, sm[:, 4:5], sm, sm[:, 3:4], 1.0, None, ALU.add)
                        self.V(k.dve, lambda: nc.vector.reciprocal(out=sm[:, 5:6], in_=sm[:, 4:5]), [sm], [sm])
                        self.TT_(sm, sm[:, 6:7], sm, sm[:, 3:4], sm, sm[:, 5:6], ALU.mult)
                        self.TS(comb, comb[:, s, :], mk1, mk1[:], sm[:, 5:6], None, ALU.mult, extra=[sm])
                        self.STT(comb, comb[:, s, :], mk2, mk2[:], sm[:, 6:7], comb, comb[:, s, :], ALU.mult, ALU.add, extra=[sm])
                for e in range(nexp):
                    wg_src = self.wb["%s_w_gate" % pre][j, e]
                    wu_src = self.wb["%s_w_up" % pre][j, e]
                    wd_src = self.wb["%s_w_down" % pre][j, e]
                    rg = self.wb_r[("%s_w_gate" % pre, j, e)]
                    ru = self.wb_r[("%s_w_up" % pre, j, e)]
                    rd = self.wb_r[("%s_w_down" % pre, j, e)]
                    for g in range(ng):
                        wg, wu = r_wg(), r_wu()
                        self.LD(wg, wg[:], wg_src[g], rg)
                        self.LD(wu, wu[:], wu_src[g], ru)
                        for fi in range(GC // 128):
                            f = g * (GC // 128) + fi
                            pg, pu = self.pA(), self.pA()
                            for c in range(8):
                                self.MM(pg, pg[:], wg, wg[:, c, fi * 128:(fi + 1) * 128], xt, xt[:, c, :], c == 0, c == 7)
                            for c in range(8):
                                self.MM(pu, pu[:], wu, wu[:, c, fi * 128:(fi + 1) * 128], xt, xt[:, c, :], c == 0, c == 7)
                            sg = r_sg()
                            self.ACT(sg, sg[:], pg, pg[:], AF.Silu)
                            self.TT_(hT, hT[:, f, :], sg, sg[:], pu, pu[:], ALU.mult)
                    for half in range(2):
                        wd = r_wd()
                        self.LD(wd, wd[:], wd_src[half], rd)
                        for s in range(4):
                            po = self.pB()
                            for f in range(nf):
                                self.MM(po, po[:], hT, hT[:, f, s * 128:(s + 1) * 128], wd, wd[:, f, :], f == 0, f == nf - 1)
                            aap = acc[:, s, half * 512:(half + 1) * 512]
                            if not moe:
                                self.CP(acc, aap, po, po[:], eng=(k.act if s % 2 else k.dve))
                            elif e == 0:
                                self.TS(acc, aap, po, po[:], comb[:, s, e:e + 1], None, ALU.mult, extra=[comb])
                            else:
                                self.STT(acc, aap, po, po[:], comb[:, s, e:e + 1], acc, aap, ALU.mult, ALU.add, extra=[comb])
                for sp in range(2):
                    items = [([(acc, acc[:, s, 0:512]), (acc, acc[:, s, 512:1024])], t * TT + s * 128) for s in (2 * sp, 2 * sp + 1)]
                    self.epilogue_multi(items, g2, b2, dst, "ffn", dbg_idx=2 * layer + 1, nch=2)

    def moe_sparse(self, layer, j, dst):
        nc, k = self.nc, self.k
        self.eps_tiles()
        W = self.w
        GC = GCM
        nf = D_FFE // 128
        ng = D_FFE // GC
        if not hasattr(self, "xs_g"):
            self.xs_g = self.dscr("xs_g", [NTL * 512, D], BF16)
            self.r_xs_g = k.res("xs_g")
            self.ys_g = self.dscr("ys_g", [NTL * 512, D], F32)
            self.r_ys_g = k.res("ys_g")
            zt = self.TP("zrow16", [128, D], BF16)
            self.V(k.dve, lambda: nc.vector.memset(zt[:], 0.0), [], [zt])
            for i in range(NTL * 4):
                k.dma(k.sp, [(self.xs_g[i * 128:(i + 1) * 128, :], zt[:])], [zt.r], [self.r_xs_g], partial=True)
        rg = self.wb_r[("moe_w_gate", j, 0)]
        wbg2 = self.wb["moe_w_gate"].rearrange("j e g p c n -> (j e g p) (c n)")
        wbu2 = self.wb["moe_w_up"].rearrange("j e g p c n -> (j e g p) (c n)")
        wbd2 = self.wb["moe_w_down"].rearrange("j e h p f d -> (j e h p) (f d)")
        with self.phase():
            desti = self.T("desti", [128, 32, 2], I32)
            g12 = self.T("g12", [128, 32, 2], F32)
            widx = self.T("widx", [128, NTL, 9], I32)
            with self.subphase():
                wr32 = self.T("wr32", [128, 8, NE], F32)
                k.dma(k.sp, [(wr32[:], W["moe_router_w"][j].rearrange("(c p) e -> p c e", p=128))], [], [wr32.r])
                rb = self.load_bcast("rb", W["moe_router_b"][j:j + 1, :], NE)
                U = self.T("Uinc", [128, 128], F32)
                self.LD(U, U[:], self.c_tri[0])
                r_x32 = self.ring("x32", [128, D], F32, 3)
                xT32 = self.T("xT32", [128, 8, 128], F32)
                lg = self.T("lg", [128, NE], F32)
                l2 = self.T("l2", [128, NE], F32)
                sm = self.T("sm", [128, 8], F32)
                m1all = self.T("m1all", [128, 32, NE], F32)
                m2all = self.T("m2all", [128, 32, NE], F32)
                wdesc = self.T("wdesc", [128, NE], F32)
                for e in range(NE):
                    self.V(k.dve, (lambda e_: (lambda: nc.vector.memset(wdesc[:, e_:e_ + 1], float(NE - e_))))(e), [], [wdesc])
                tsc = self.T("tsc", [128, NE], F32)

                def onehot_first(mt, map_):
                    self.TT_(tsc, tsc[:], mt, map_, wdesc, wdesc[:], ALU.mult)
                    self.V(k.dve, lambda: nc.vector.tensor_reduce(out=sm[:, 7:8], in_=tsc[:], axis=AX.X, op=ALU.max), [tsc], [sm])
                    self.TS(mt, map_, tsc, tsc[:], sm[:, 7:8], None, ALU.is_equal, extra=[sm])
                for st in range(32):
                    r0 = st * 128
                    x32 = r_x32()
                    self.LD(x32, x32[:], self.xres[r0:r0 + 128, :], self.r_xres)
                    pts = [self.pB(), self.pB()]
                    for c in range(8):
                        pt = pts[c // 4]
                        self.TR(pt, pt[:, (c % 4) * 128:(c % 4 + 1) * 128], x32, x32[:, c * 128:(c + 1) * 128], self.ident32)
                    for hh in range(2):
                        self.CP(xT32, xT32[:, hh * 4:(hh + 1) * 4, :].rearrange("p c s -> p (c s)"), pts[hh], pts[hh][:],
                                eng=(k.act if hh else k.dve))
                    pr = self.pB()
                    for c in range(8):
                        self.MM(pr, pr[:, 0:NE], xT32, xT32[:, c, :], wr32, wr32[:, c, :], c == 0, c == 7)
                    self.TT_(lg, lg[:], pr, pr[:, 0:NE], rb, rb[:], ALU.add)
                    self.V(k.dve, lambda: nc.vector.tensor_reduce(out=sm[:, 0:1], in_=lg[:], axis=AX.X, op=ALU.max), [lg], [sm])
                    self.TS(m1all, m1all[:, st, :], lg, lg[:], sm[:, 0:1], None, ALU.is_equal, extra=[sm])
                    onehot_first(m1all, m1all[:, st, :])
                    self.STT(l2, l2[:], m1all, m1all[:, st, :], -1.0e30, lg, lg[:], ALU.mult, ALU.add)
                    self.V(k.dve, lambda: nc.vector.tensor_reduce(out=sm[:, 1:2], in_=l2[:], axis=AX.X, op=ALU.max), [l2], [sm])
                    self.TS(m2all, m2all[:, st, :], l2, l2[:], sm[:, 1:2], None, ALU.is_equal, extra=[sm])
                    onehot_first(m2all, m2all[:, st, :])
                    self.TT_(sm, sm[:, 2:3], sm, sm[:, 1:2], sm, sm[:, 0:1], ALU.subtract)
                    self.ACT(sm, sm[:, 3:4], sm, sm[:, 2:3], AF.Exp)
                    self.TS(sm, sm[:, 4:5], sm, sm[:, 3:4], 1.0, None, ALU.add)
                    self.V(k.dve, lambda: nc.vector.reciprocal(out=g12[:, st, 0:1], in_=sm[:, 4:5]), [sm], [g12])
                    self.TT_(g12, g12[:, st, 1:2], sm, sm[:, 3:4], g12, g12[:, st, 0:1], ALU.mult)
                sel = self.T("sel", [128, 32, NE], F32)
                excl = self.T("excl", [128, 32, NE], F32)
                tot = self.T("tot", [128, 32, NE], F32)
                pre = self.T("pre", [128, 32, NE], F32)
                flat = lambda t_: t_[:].rearrange("p s e -> p (s e)")
                self.TT_(sel, sel[:], m1all, m1all[:], m2all, m2all[:], ALU.add)
                pinc, ptot = self.pB(), self.pB()
                self.MM(pinc, pinc[:, 0:256], U, U[:], sel, flat(sel), True, True)
                self.MM(ptot, ptot[:, 0:256], self.ones32, self.ones32[:], sel, flat(sel), True, True)
                self.TT_(excl, flat(excl), pinc, pinc[:, 0:256], sel, flat(sel), ALU.subtract)
                self.CP(tot, flat(tot), ptot, ptot[:, 0:256])
                self.V(k.dve, lambda: nc.vector.memset(pre[:, 0, :], 0.0), [], [pre])
                for st in range(1, 32):
                    self.TT_(pre, pre[:, st, :], pre, pre[:, st - 1, :], tot, tot[:, st - 1, :], ALU.add)
                cnt = self.T("cnt", [128, NE], F32)
                self.TT_(cnt, cnt[:], pre, pre[:, 31, :], tot, tot[:, 31, :], ALU.add)
                cmpk = self.T("cmpk", [128, 8, NE], F32)
                for kk in range(8):
                    self.TS(cmpk, cmpk[:, kk, :], cnt, cnt[:], 512.0 * kk, None, ALU.is_gt)
                padded = self.T("padded", [128, NE], F32)
                self.V(k.dve, lambda: nc.vector.tensor_reduce(out=padded[:], in_=cmpk[:].rearrange("p k e -> p e k"),
                                                              axis=AX.X, op=ALU.add), [cmpk], [padded])
                self.TS(padded, padded[:], padded, padded[:], 512.0, None, ALU.mult)
                base = self.T("base", [128, NE], F32)
                self.V(k.dve, lambda: nc.vector.memset(base[:, 0:1], 0.0), [], [base])
                for e in range(1, NE):
                    self.TT_(base, base[:, e:e + 1], base, base[:, e - 1:e], padded, padded[:, e - 1:e], ALU.add)
                cumend = self.T("cumend", [128, NE], F32)
                self.TT_(cumend, cumend[:], base, base[:], padded, padded[:], ALU.add)
                self.TT_(excl, excl[:], excl, excl[:], pre, pre[:], ALU.add)
                self.TT_(excl, excl[:], excl, excl[:], base, base[:].unsqueeze(1).to_broadcast([128, 32, NE]), ALU.add)
                destf = self.T("destf", [128, 32, 2], F32)
                for r, mall in ((0, m1all), (1, m2all)):
                    self.TT_(tot, tot[:], mall, mall[:], excl, excl[:], ALU.mult)
                    self.V(k.dve, (lambda r_: (lambda: nc.vector.tensor_reduce(out=destf[:, :, r_], in_=tot[:], axis=AX.X, op=ALU.add)))(r),
                           [tot], [destf])
                self.CP(desti, desti[:], destf, destf[:])
                cmpt = self.T("cmpt", [128, NTL, NE], F32)
                for t in range(NTL):
                    self.TS(cmpt, cmpt[:, t, :], cumend, cumend[:], 512.0 * t, None, ALU.is_le)
                etf = self.T("etf", [128, NTL], F32)
                self.V(k.dve, lambda: nc.vector.tensor_reduce(out=etf[:], in_=cmpt[:], axis=AX.X, op=ALU.add), [cmpt], [etf])
                self.TS(etf, etf[:], etf, etf[:], float(NE - 1), 0.0, ALU.min, ALU.max)
                self.TS(etf, etf[:], etf, etf[:], float(j * NE), None, ALU.add)
                pg = self.T("pg", [128, 9], F32)
                self.LD(pg, pg[:], self.c_pg)
                widf = self.T("widf", [128, NTL, 9], F32)
                for g in range(9):
                    self.TS(widf, widf[:, :, g], etf, etf[:], float(ng * 128 if g < 7 else 2 * 128), pg[:, g:g + 1],
                            ALU.mult, ALU.add, extra=[pg])
                self.CP(widx, widx[:], widf, widf[:])
                for st in range(32):
                    r0 = st * 128
                    x32 = r_x32()
                    self.LD(x32, x32[:], self.xres[r0:r0 + 128, :], self.r_xres)
                    for r in range(2):
                        k.idma((lambda x_, st_, r_: (lambda: nc.gpsimd.indirect_dma_start(
                            out=self.xs_g[:, :], out_offset=bass.IndirectOffsetOnAxis(ap=desti[:, st_, r_:r_ + 1], axis=0),
                            in_=x_[:, :], in_offset=None)))(x32, st, r),
                            [x32.r, desti.r], [self.r_xs_g], partial=True)
            with self.subphase():
                r_xg = self.ring("xg", [128, 4, D], BF16, 2)
                r_xt = self.ring("xtg", [128, 8, TT], BF16, 2)
                hT = self.T("hTg", [128, nf, TT], BF16)
                r_wg = self.ring("wgg", [128, 8, GC], BF16, 2)
                r_wu = self.ring("wug", [128, 8, GC], BF16, 2)
                r_wd = self.ring("wdg", [128, nf, 512], BF16, 2)
                r_sg = self.ring("sgg", [128, TT], F32, 2)
                r_ysb = self.ring("ysb", [128, 4, D], F32, 1)
                for t in range(NTL):
                    def wgather(dst_t, src2d, col):
                        k.idma((lambda d_, c_: (lambda: nc.gpsimd.indirect_dma_start(
                            out=d_, out_offset=None, in_=src2d,
                            in_offset=bass.IndirectOffsetOnAxis(ap=widx[:, t, c_:c_ + 1], axis=0))))(dst_t[:].rearrange(
                                "p a b -> p (a b)"), col), [rg, widx.r], [dst_t.r])
                    xg = r_xg()
                    self.LD(xg, xg[:], self.xs_g[t * 512:(t + 1) * 512, :].rearrange("(s p) d -> p s d", p=128), self.r_xs_g)
                    xt = r_xt()
                    for s in range(4):
                        for c in range(8):
                            self.TR(self.ptr, self.ptr[:, c * 128:(c + 1) * 128], xg, xg[:, s, c * 128:(c + 1) * 128], self.ident16)
                        self.CP(xt, xt[:, :, s * 128:(s + 1) * 128], self.ptr, self.ptr[:].rearrange("p (c s) -> p c s", c=8),
                                eng=(k.act if s % 2 else k.dve))
                    for g in range(ng):
                        wg, wu = r_wg(), r_wu()
                        wgather(wg, wbg2, g)
                        wgather(wu, wbu2, g)
                        for fi in range(GC // 128):
                            f = g * (GC // 128) + fi
                            pg, pu = self.pA(), self.pA()
                            for c in range(8):
                                self.MM(pg, pg[:], wg, wg[:, c, fi * 128:(fi + 1) * 128], xt, xt[:, c, :], c == 0, c == 7)
                            for c in range(8):
                                self.MM(pu, pu[:], wu, wu[:, c, fi * 128:(fi + 1) * 128], xt, xt[:, c, :], c == 0, c == 7)
                            sg = r_sg()
                            self.ACT(sg, sg[:], pg, pg[:], AF.Silu)
                            self.TT_(hT, hT[:, f, :], sg, sg[:], pu, pu[:], ALU.mult)
                    ysb = r_ysb()
                    for half in range(2):
                        wd = r_wd()
                        wgather(wd, wbd2, 7 + half)
                        for s in range(4):
                            po = self.pB()
                            for f in range(nf):
                                self.MM(po, po[:], hT, hT[:, f, s * 128:(s + 1) * 128], wd, wd[:, f, :], f == 0, f == nf - 1)
                            self.CP(ysb, ysb[:, s, half * 512:(half + 1) * 512], po, po[:], eng=(k.act if s % 2 else k.dve))
                    self.ST(self.ys_g[t * 512:(t + 1) * 512, :].rearrange("(s p) d -> p s d", p=128), self.r_ys_g, ysb, ysb[:])
            with self.subphase():
                g2, b2 = self.ln_consts("ln2_g", "ln2_b", layer)
                r_ga = self.ring("ga", [128, D], F32, 12)
                r_f = self.ring("fmo", [128, D], F32, 8)
                NB = 4
                for sb in range(32 // NB):
                    items = []
                    for st in range(sb * NB, (sb + 1) * NB):
                        gas = []
                        for r in range(2):
                            ga = r_ga()
                            k.idma((lambda g_, st_, r_: (lambda: nc.gpsimd.indirect_dma_start(
                                out=g_[:, :], out_offset=None, in_=self.ys_g[:, :],
                                in_offset=bass.IndirectOffsetOnAxis(ap=desti[:, st_, r_:r_ + 1], axis=0))))(ga, st, r),
                                [self.r_ys_g, desti.r], [ga.r])
                            gas.append(ga)
                        f = r_f()
                        self.TS(f, f[:], gas[0], gas[0][:], g12[:, st, 0:1], None, ALU.mult, extra=[g12])
                        self.STT(f, f[:], gas[1], gas[1][:], g12[:, st, 1:2], f, f[:], ALU.mult, ALU.add, extra=[g12])
                        items.append(([(f, f[:, 0:512]), (f, f[:, 512:1024])], st * 128))
                    self.epilogue_multi(items, g2, b2, dst, "moe", dbg_idx=2 * layer + 1, nch=NB)

    def odd_mixer(self, j, layer):
        nc, k = self.nc, self.k
        self.eps_tiles()
        W = self.w
        if not hasattr(self, "z_d"):
            def mk(name, shape, dt):
                setattr(self, name, self.dscr(name, shape, dt))
                setattr(self, "r_" + name, k.res(name))
            mk("z_d", [S, 512], F32); mk("xs_d", [S, 512], F32); mk("bmt_d", [S, 256], BF16)
            mk("bcT_d", [4, 128, S], BF16); mk("dtda_d", [S, 32], F32); mk("qT_d", [8, 64, S], BF16)
            mk("kT_d", [8, 64, S], BF16); mk("kt_d", [S, 512], BF16); mk("v2_d", [S, 8 * 65], BF16)
            mk("og_d", [S, 512], F32); mk("gate_d", [S, 32], F32); mk("yf_d", [S, 512], F32)
            mk("hf_d", [S, 512], F32); mk("wcf_d", [5, D, 1024], BF16)
        wi_r = self.wb_r[("od_w_in", j, 0)]
        wi = self.wb["od_w_in"][j]
        with self.phase():
            cwb = [self.load_bcast("cwb%d" % tap, W["ssd_conv_w"][j, tap:tap + 1, :], 1024) for tap in range(5)]
            r_wr = self.ring("wrow", [128, 1024], F32, 2)
            r_wo = self.ring("wcfo", [128, 1024], BF16, 3)
            for rc in range(8):
                wr_ = r_wr()
                self.LD(wr_, wr_[:], W["od_w_in"][j, rc * 128:(rc + 1) * 128, 512:1536])
                for tap in range(5):
                    wo = r_wo()
                    self.TT_(wo, wo[:], wr_, wr_[:], cwb[tap], cwb[tap][:], ALU.mult)
                    self.ST(self.wcf_d[tap, rc * 128:(rc + 1) * 128, :], self.r_wcf_d, wo, wo[:])
        with self.phase():
            xTs = self.load_xT_full()
            cbias = self.load_bcast("cbias", W["ssd_conv_b"][j:j + 1, :], 1024)
            cbcol = self.load_col("cbcol", W["ssd_conv_b"][j], 8)
            dtb = self.load_bcast("dtb", W["ssd_dt_bias"][j:j + 1, :], 16)
            alog = self.load_bcast("alog", W["ssd_a_log"][j:j + 1, :], 16)
            igb = self.load_bcast("igb", W["ml_igate_b"][j:j + 1, :], 16)
            fgb = self.load_bcast("fgb", W["ml_fgate_b"][j:j + 1, :], 16)
            abc = self.T("abc", [128, 16], F32)
            self.ACT(abc, abc[:], alog, alog[:], AF.Exp)
            self.TS(abc, abc[:], abc, abc[:], -1.0, None, ALU.mult)
            wiv = wi.rearrange("(c p) n -> p c n", p=128)
            wcv = self.wcf_d.rearrange("t (c p) n -> p t c n", p=128)
            r_o32 = self.ring("o32", [128, 512], F32, 3)
            r_o16 = self.ring("o16", [128, 512], BF16, 3)
            r_vt = self.ring("vt2", [128, 8, 65], BF16, 3, init=1.0)
            r_sm = self.ring("smo", [128, 64], F32, 3)

            def tok_group(wt, ncols, conv, post):
                for st in range(S // 128):
                    t0 = st * 128
                    ps = self.pA()
                    if conv:
                        n = 0
                        for tap in range(5):
                            for c in range(8):
                                self.MM(ps, ps[:, 0:ncols], xTs, xTs[:, c, t0 + tap:t0 + tap + 128], wt, wt[:, tap, c, :],
                                        n == 0, n == 39)
                                n += 1
                    else:
                        for c in range(8):
                            self.MM(ps, ps[:, 0:ncols], xTs, xTs[:, c, 2 + t0:2 + t0 + 128], wt, wt[:, c, :], c == 0, c == 7)
                    post(ps, t0, st)

            def load_plain(c0, ncols, name):
                t = self.T(name, [128, 8, ncols], BF16)
                k.dma(k.sp, [(t[:], wiv[:, :, c0:c0 + ncols])], [wi_r], [t.r])
                return t

            def load_conv(c0, ncols, name):
                t = self.T(name, [128, 5, 8, ncols], BF16)
                k.dma(k.sp, [(t[:, tap, :, :], wcv[:, tap, :, c0:c0 + ncols]) for tap in range(5)], [self.r_wcf_d], [t.r])
                return t

            def post_z(ps, t0, st):
                o = r_o32()
                self.ACT(o, o[:], ps, ps[:], AF.Silu)
                self.ST(self.z_d[t0:t0 + 128, :], self.r_z_d, o, o[:])
            sub = self.subphase()
            sub.__enter__()
            tok_group(load_plain(0, 512, "w_z"), 512, False, post_z)

            def post_o(ps, t0, st):
                o = r_o32()
                self.ACT(o, o[:], ps, ps[:], AF.Sigmoid)
                self.ST(self.og_d[t0:t0 + 128, :], self.r_og_d, o, o[:])
            tok_group(load_plain(3088, 512, "w_o"), 512, False, post_o)

            def post_k(ps, t0, st):
                o = r_o16()
                self.V(k.act, lambda: nc.scalar.mul(out=o[:], in_=ps[:], mul=0.125), [ps], [o])
                self.ST(self.kt_d[t0:t0 + 128, :], self.r_kt_d, o, o[:])
            tok_group(load_plain(2064, 512, "w_k"), 512, False, post_k)

            def post_v(ps, t0, st):
                vt = r_vt()
                self.CP(vt, vt[:, :, 0:64], ps, ps[:].rearrange("p (h e) -> p h e", e=64), eng=(k.act if st % 2 else k.dve))
                self.ST(self.v2_d[t0:t0 + 128, :], self.r_v2_d, vt, vt[:].rearrange("p h e -> p (h e)"))
            tok_group(load_plain(2576, 512, "w_v"), 512, False, post_v)
            sub.__exit__(None, None, None)

            def post_xs(ps, t0, st):
                o = r_o32()
                self.TT_(o, o[:], ps, ps[:], cbias, cbias[:, 0:512], ALU.add)
                self.ACT(o, o[:], o, o[:], AF.Silu)
                self.ST(self.xs_d[t0:t0 + 128, :], self.r_xs_d, o, o[:])
            sub = self.subphase()
            sub.__enter__()
            tok_group(load_conv(0, 512, "w_xs"), 512, True, post_xs)

            def post_bm(ps, t0, st):
                o = r_o32()
                self.TT_(o, o[:, 0:256], ps, ps[:, 0:256], cbias, cbias[:, 512:768], ALU.add)
                o2 = r_o16()
                self.ACT(o2, o2[:, 0:256], o, o[:, 0:256], AF.Silu)
                self.ST(self.bmt_d[t0:t0 + 128, :], self.r_bmt_d, o2, o2[:, 0:256])
            tok_group(load_conv(512, 256, "w_bm"), 256, True, post_bm)

            def post_small(ps, t0, st):
                sm = r_sm()
                self.TT_(sm, sm[:, 0:16], ps, ps[:, 0:16], dtb, dtb[:], ALU.add)
                self.ACT(sm, sm[:, 0:16], sm, sm[:, 0:16], AF.Exp)
                self.ACT(sm, sm[:, 0:16], sm, sm[:, 0:16], AF.Ln, bias=1.0, scale=1.0)
                self.TT_(sm, sm[:, 16:32], sm, sm[:, 0:16], abc, abc[:], ALU.mult)
                self.ST(self.dtda_d[t0:t0 + 128, :], self.r_dtda_d, sm, sm[:, 0:32])
                sm2 = r_sm()
                self.TT_(sm2, sm2[:, 0:16], ps, ps[:, 16:32], igb, igb[:], ALU.add)
                self.TT_(sm2, sm2[:, 16:32], ps, ps[:, 32:48], fgb, fgb[:], ALU.add)
                self.ACT(sm2, sm2[:, 16:32], sm2, sm2[:, 16:32], AF.Exp, scale=-1.0)
                self.ACT(sm2, sm2[:, 16:32], sm2, sm2[:, 16:32], AF.Ln, bias=1.0, scale=1.0)
                self.TS(sm2, sm2[:, 16:32], sm2, sm2[:, 16:32], -1.0, None, ALU.mult)
                self.ST(self.gate_d[t0:t0 + 128, :], self.r_gate_d, sm2, sm2[:, 0:32])
            wsm = self.T("w_sm", [128, 8, 48], BF16)
            k.dma(k.sp, [(wsm[:, :, 0:16], wiv[:, :, 1536:1552]), (wsm[:, :, 16:48], wiv[:, :, 3600:3632])], [wi_r], [wsm.r])
            tok_group(wsm, 48, False, post_small)
            sub.__exit__(None, None, None)

            r_f16 = self.ring("f16", [128, TT], BF16, 3)
            with self.subphase():
                wbc = load_conv(512, 512, "w_bcT")
                for t in range(NT):
                    for i in range(4):
                        ps = self.pA()
                        n = 0
                        for tap in range(5):
                            for c in range(8):
                                self.MM(ps, ps[:], wbc, wbc[:, tap, c, i * 128:(i + 1) * 128],
                                        xTs, xTs[:, c, t * TT + tap:t * TT + tap + TT], n == 0, n == 39)
                                n += 1
                        o = r_f16()
                        self.ACT(o, o[:], ps, ps[:], AF.Silu, bias=cbcol[:, 4 + i:5 + i], scale=1.0, extra=[cbcol])
                        self.ST(self.bcT_d[i][:, t * TT:(t + 1) * TT], self.r_bcT_d, o, o[:])
            for (c0, dst, rdst, scl, nm) in ((1552, self.qT_d, self.r_qT_d, 1.0, "w_qT"), (2064, self.kT_d, self.r_kT_d, 0.125, "w_kT")):
                with self.subphase():
                    wq = load_plain(c0, 512, nm)
                    for t in range(NT):
                        for h in range(8):
                            ps = self.pA()
                            for c in range(8):
                                self.MM(ps, ps[0:64, :], wq, wq[:, c, h * 64:(h + 1) * 64], xTs, xTs[:, c, 2 + t * TT:2 + (t + 1) * TT],
                                        c == 0, c == 7)
                            o = r_f16()
                            self.V(k.act, (lambda o_, ps_: (lambda: nc.scalar.mul(out=o_[0:64, :], in_=ps_[0:64, :], mul=scl)))(o, ps), [ps], [o])
                            self.ST(dst[h][:, t * TT:(t + 1) * TT], rdst, o, o[0:64, :])
        for direction in (0, 1):
            with self.phase():
                self.scan_pass(j, layer, direction)

    def subphase(self):
        prog = self

        class _S:
            def __enter__(s):
                s.outer = prog.ph
                s.st = ExitStack()
                s.st.__enter__()
                prog.ph = s.st
                return s

            def __exit__(s, *a):
                if a[0] is None:
                    prog.k.barrier(closing=s.st)
                s.st.__exit__(*a)
                prog.ph = s.outer
                return False
        return _S()

    def scan_pass(self, j, layer, dr):
        nc, k = self.nc, self.k
        W = self.w
        last = (dr == 1)
        U = self.T("Udir", [128, 128], F32)
        self.LD(U, U[:], self.c_tri[dr])
        mask = self.T("mdir", [128, 128], F32)
        self.LD(mask, mask[:], self.c_tri[2 + dr])
        Sst = self.T("Sst", [128, 8, 64], F32)
        S16 = self.T("S16", [128, 8, 64], BF16)
        Cst = self.T("Cst", [64, 8, 65], F32)
        C16 = self.T("C16", [64, 8, 65], BF16)
        for t_ in (Sst, S16, Cst, C16):
            self.V(k.dve, (lambda tt: (lambda: nc.vector.memset(tt[:], 0.0)))(t_), [], [t_])
        R = self.ring
        r_xs = R("xs", [128, 512], F32, 2); r_dtda = R("dtda", [128, 32], F32, 2); r_bmt = R("bmt", [128, 256], BF16, 2)
        r_bcT = R("bcT", [128, 4, 128], BF16, 2); r_qT = R("qT", [64, 8, 128], BF16, 2); r_kT = R("kT", [64, 8, 128], BF16, 2)
        r_kt = R("kt", [128, 512], BF16, 2); r_v2 = R("v2", [128, 8, 65], BF16, 2); r_gate = R("gate", [128, 32], F32, 2)
        r_sc = R("sc", [128, 32], F32, 2); r_wall = R("wall", [128, 8, 128], F32, 2); r_tR = R("tR", [128, 8, 128], F32, 2)
        r_eD = R("eD", [128, 8, 128], F32, 2); r_cb = R("cb", [128, 2, 128], F32, 2); r_MT = R("MT", [128, 8, 128], BF16, 2)
        r_ex = R("ex", [128, 64], F32, 2); r_xdt = R("xdt", [128, 8, 64], BF16, 2); r_xdtd = R("xdtd", [128, 8, 64], BF16, 2)
        r_y = R("y", [128, 512], F32, 2); r_aT = R("aT", [128, 8, 128], BF16, 2); r_tot = R("tot", [128, 8, 65], F32, 2)
        r_hd = R("hd", [128, 8, 64], F32, 2); r_kd = R("kd", [128, 8, 64], BF16, 2)
        if last:
            r_yf = R("yf", [128, 512], F32, 2); r_hf = R("hf", [128, 512], F32, 2); r_zs = R("zs", [128, 512], F32, 2)
            r_og = R("og", [128, 512], F32, 2); r_mix = R("mix16", [128, D], BF16, 2); r_mixT = R("mixT2", [128, 8, 128], BF16, 2)
            r_tmp = R("tmp5", [128, 512], F32, 2)
            dsk = self.load_bcast("dsk", W["ssd_d"][j:j + 1, :], 8)
            ssdg = self.load_bcast("ssdg", W["ssd_norm_g"][j:j + 1, :], 512)
            mlg = self.load_bcast("mlg", W["ml_norm_g"][j:j + 1, :], 512)
            wout = self.load_w("wout2", self.wb["od_w_out"][j], 8, D, self.wb_r[("od_w_out", j, 0)])
            g1, b1 = self.ln_consts("ln1_g", "ln1_b", layer)
        bcTv = self.bcT_d.rearrange("i p s -> p i s")
        qTv = self.qT_d.rearrange("h p s -> p h s")
        kTv = self.kT_d.rearrange("h p s -> p h s")

        def bc3(ap2, n):
            return ap2.unsqueeze(2).to_broadcast([ap2.shape[0], ap2.shape[1], n])

        chunks = range(32) if dr == 0 else range(31, -1, -1)
        for c in chunks:
            r0 = c * 128
            rows = slice(r0, r0 + 128)
            xs, dtda, bmt, bcT, qT, kT, kt, v2, gate = r_xs(), r_dtda(), r_bmt(), r_bcT(), r_qT(), r_kT(), r_kt(), r_v2(), r_gate()
            self.LD(xs, xs[:], self.xs_d[rows, :], self.r_xs_d)
            self.LD(dtda, dtda[:], self.dtda_d[rows, :], self.r_dtda_d)
            self.LD(bmt, bmt[:], self.bmt_d[rows, :], self.r_bmt_d)
            self.LD(bcT, bcT[:], bcTv[:, :, rows], self.r_bcT_d)
            self.LD(qT, qT[:], qTv[:, :, rows], self.r_qT_d)
            self.LD(kT, kT[:], kTv[:, :, rows], self.r_kT_d)
            self.LD(kt, kt[:], self.kt_d[rows, :], self.r_kt_d)
            self.LD(v2, v2[:].rearrange("p h e -> p (h e)"), self.v2_d[rows, :], self.r_v2_d)
            self.LD(gate, gate[:], self.gate_d[rows, :], self.r_gate_d)
            dtd = dtda[:, dr * 8:dr * 8 + 8]
            dad = dtda[:, 16 + dr * 8:16 + dr * 8 + 8]
            lid = gate[:, dr * 8:dr * 8 + 8]
            lfd = gate[:, 16 + dr * 8:16 + dr * 8 + 8]
            pcol = self.pB()
            self.MM(pcol, pcol[:, 0:8], U, U[:], dtda, dad, True, True)
            self.MM(pcol, pcol[:, 8:16], self.ones32, self.ones32[:], dtda, dad, True, True)
            self.MM(pcol, pcol[:, 16:24], U, U[:], gate, lfd, True, True)
            self.MM(pcol, pcol[:, 24:32], self.ones32, self.ones32[:], gate, lfd, True, True)
            sc = r_sc()
            self.CP(sc, sc[:], pcol, pcol[:, 0:32])
            ex = r_ex()
            self.ACT(ex, ex[:, 0:16], sc, sc[:, 0:16], AF.Exp)
            self.TT_(ex, ex[:, 16:24], sc, sc[:, 8:16], sc, sc[:, 0:8], ALU.subtract)
            self.ACT(ex, ex[:, 16:24], ex, ex[:, 16:24], AF.Exp)
            self.TT_(ex, ex[:, 24:32], dtda, dtd, ex, ex[:, 16:24], ALU.mult)
            self.ACT(ex, ex[:, 32:48], sc, sc[:, 16:32], AF.Exp)
            self.TT_(ex, ex[:, 48:56], sc, sc[:, 24:32], sc, sc[:, 16:24], ALU.subtract)
            self.TT_(ex, ex[:, 48:56], ex, ex[:, 48:56], gate, lid, ALU.add)
            self.ACT(ex, ex[:, 48:56], ex, ex[:, 48:56], AF.Exp)
            self.TT_(ex, ex[:, 56:64], gate, lid, sc, sc[:, 16:24], ALU.subtract)

            def decay_mat(src_t, src_ap, shift_t, shift_ap, sign):
                wall = r_wall()
                self.TT_(wall, wall[:], U, U[:].unsqueeze(1).to_broadcast([128, 8, 128]), src_t, bc3(src_ap, 128), ALU.mult,
                         eng=k.pool)
                pR = [self.pA(), self.pA()]
                for hb in range(2):
                    self.MM(pR[hb], pR[hb][:], self.ones32, self.ones32[:], wall,
                            wall[:, hb * 4:(hb + 1) * 4, :].rearrange("p h l -> p (h l)"), True, True)
                tR = r_tR()
                for hb in range(2):
                    self.TT_(tR, tR[:, hb * 4:(hb + 1) * 4, :], pR[hb], pR[hb][:].rearrange("p (h l) -> p h l", h=4),
                             mask, mask[:].unsqueeze(1).to_broadcast([128, 4, 128]), ALU.add)
                self.TT_(tR, tR[:], tR, tR[:], shift_t, bc3(shift_ap, 128), ALU.subtract if sign < 0 else ALU.add)
                eD = r_eD()
                self.ACT(eD, eD[:], tR, tR[:], AF.Exp)
                return eD
            eD = decay_mat(dtda, dad, sc, sc[:, 0:8], -1)
            pcb = self.pB()
            for g in range(2):
                self.MM(pcb, pcb[:, g * 128:(g + 1) * 128], bcT, bcT[:, g, :], bcT, bcT[:, 2 + g, :], True, True)
            cb = r_cb()
            self.CP(cb, cb[:].rearrange("p g l -> p (g l)"), pcb, pcb[:, 0:256], eng=k.act)
            MT = r_MT()
            self.TT_(MT, MT[:].rearrange("p (g r) l -> p g r l", g=2), eD, eD[:].rearrange("p (g r) l -> p g r l", g=2),
                     cb, cb[:].unsqueeze(2).to_broadcast([128, 2, 4, 128]), ALU.mult)
            xv = xs[:].rearrange("p (h e) -> p h e", e=64)
            xdt, xdtd = r_xdt(), r_xdtd()
            self.TT_(xdt, xdt[:], xs, xv, dtda, bc3(dtd, 64), ALU.mult, eng=k.pool)
            self.TT_(xdtd, xdtd[:], xs, xv, ex, bc3(ex[:, 24:32], 64), ALU.mult, eng=k.pool)
            pyd, pyo = self.pA(), self.pA()
            for h in range(8):
                self.MM(pyd, pyd[:, h * 64:(h + 1) * 64], MT, MT[:, h, :], xdt, xdt[:, h, :], True, True)
            for h in range(8):
                self.MM(pyo, pyo[:, h * 64:(h + 1) * 64], bcT, bcT[:, 2 + h // 4, :], S16, S16[:, h, :], True, True)
            y = r_y()
            yv = y[:].rearrange("p (h e) -> p h e", e=64)
            self.TT_(y, yv, pyo, pyo[:].rearrange("p (h e) -> p h e", e=64), ex, bc3(ex[:, 0:8], 64), ALU.mult)
            self.TT_(y, y[:], y, y[:], pyd, pyd[:], ALU.add)
            pst = self.pB()
            for h in range(8):
                self.MM(pst, pst[:, h * 64:(h + 1) * 64], bmt, bmt[:, (h // 4) * 128:(h // 4 + 1) * 128], xdtd, xdtd[:, h, :], True, True)
            self.TT_(Sst, Sst[:], Sst, Sst[:], ex, bc3(ex[:, 8:16], 64), ALU.mult)
            self.TT_(Sst, Sst[:], Sst, Sst[:], pst, pst[:].rearrange("p (h e) -> p h e", e=64), ALU.add)
            self.CP(S16, S16[:], Sst, Sst[:], eng=k.act)
            wT = decay_mat(gate, lfd, ex, ex[:, 56:64], +1)
            pqk = [self.pA(), self.pA()]
            for h in range(8):
                self.MM(pqk[h // 4], pqk[h // 4][:, (h % 4) * 128:(h % 4 + 1) * 128], kT, kT[:, h, :], qT, qT[:, h, :], True, True)
            aT = r_aT()
            for hb in range(2):
                self.TT_(aT, aT[:, hb * 4:(hb + 1) * 4, :], pqk[hb], pqk[hb][:].rearrange("p (h l) -> p h l", h=4),
                         wT, wT[:, hb * 4:(hb + 1) * 4, :], ALU.mult)
            pn = [self.pA(), self.pA()]
            pi = [self.pA(), self.pA()]
            for h in range(8):
                self.MM(pn[h // 4], pn[h // 4][:, (h % 4) * 65:(h % 4 + 1) * 65], aT, aT[:, h, :], v2, v2[:, h, :], True, True)
            for h in range(8):
                self.MM(pi[h // 4], pi[h // 4][:, (h % 4) * 65:(h % 4 + 1) * 65], qT, qT[:, h, :], C16, C16[:, h, :], True, True)
            tot = r_tot()
            for hb in range(2):
                tv = tot[:, hb * 4:(hb + 1) * 4, :]
                self.TT_(tot, tv, pi[hb], pi[hb][:, 0:260].rearrange("p (h e) -> p h e", e=65),
                         ex, bc3(ex[:, 32 + hb * 4:32 + (hb + 1) * 4], 65), ALU.mult)
                self.TT_(tot, tv, tot, tv, pn[hb], pn[hb][:, 0:260].rearrange("p (h e) -> p h e", e=65), ALU.add)
            den = r_sc()
            self.ACT(den, den[:, 0:8], tot, tot[:, :, 64], AF.Abs)
            self.TS(den, den[:, 0:8], den, den[:, 0:8], 1.0, None, ALU.max)
            self.V(k.dve, lambda: nc.vector.reciprocal(out=den[:, 0:8], in_=den[:, 0:8]), [den], [den])
            hd = r_hd()
            self.TT_(hd, hd[:], tot, tot[:, :, 0:64], den, bc3(den[:, 0:8], 64), ALU.mult)
            kd = r_kd()
            self.TT_(kd, kd[:], kt, kt[:].rearrange("p (h e) -> p h e", e=64), ex, bc3(ex[:, 48:56], 64), ALU.mult, eng=k.pool)
            pC = [self.pB(), self.pB()]
            for h in range(8):
                self.MM(pC[h // 4], pC[h // 4][0:64, (h % 4) * 65:(h % 4 + 1) * 65], kd, kd[:, h, :], v2, v2[:, h, :], True, True)
            self.TT_(Cst, Cst[:], Cst, Cst[:], ex, bc3(ex[0:64, 40:48], 65), ALU.mult)
            for hb in range(2):
                cv = Cst[:, hb * 4:(hb + 1) * 4, :]
                self.TT_(Cst, cv, Cst, cv, pC[hb], pC[hb][0:64, 0:260].rearrange("p (h e) -> p h e", e=65), ALU.add)
            self.CP(C16, C16[:], Cst, Cst[:], eng=k.act)
            if not last:
                self.ST(self.yf_d[rows, :], self.r_yf_d, y, y[:])
                self.ST(self.hf_d[rows, :], self.r_hf_d, hd, hd[:].rearrange("p h e -> p (h e)"))
                continue
            yf, hf, zs, og = r_yf(), r_hf(), r_zs(), r_og()
            self.LD(yf, yf[:], self.yf_d[rows, :], self.r_yf_d)
            self.LD(hf, hf[:], self.hf_d[rows, :], self.r_hf_d)
            self.LD(zs, zs[:], self.z_d[rows, :], self.r_z_d)
            self.LD(og, og[:], self.og_d[rows, :], self.r_og_d)
            tmp = r_tmp()
            self.TT_(y, y[:], y, y[:], yf, yf[:], ALU.add)
            self.TT_(tmp, tmp[:].rearrange("p (h e) -> p h e", e=64), xs, xv, dsk, bc3(dsk[:, 0:8], 64), ALU.mult)
            self.TT_(y, y[:], y, y[:], tmp, tmp[:], ALU.add)
            self.TT_(y, y[:], y, y[:], zs, zs[:], ALU.mult)
            st = r_sc()
            for g in range(2):
                self.ACT(tmp, tmp[:, g * 256:(g + 1) * 256], y, y[:, g * 256:(g + 1) * 256], AF.Square, accum=st[:, g:g + 1],
                         extra=[])
            k.all_res
            st.r.w = tmp.r.w
            self.rsqrt_(st, st[:, 2:4], st, st[:, 0:2], 1.0 / 256.0, self.eps_rms)
            self.TT_(y, y[:].rearrange("p (g e) -> p g e", g=2), y, y[:].rearrange("p (g e) -> p g e", g=2),
                     st, bc3(st[:, 2:4], 256), ALU.mult)
            mix = r_mix()
            self.TT_(mix, mix[:, 0:512], y, y[:], ssdg, ssdg[:], ALU.mult)
            hv = hd[:]
            self.TT_(hd, hv, hd, hv, hf, hf[:].rearrange("p (h e) -> p h e", e=64), ALU.add)
            self.V(k.dve, lambda: nc.vector.tensor_reduce(out=st[:, 8:16], in_=hv, axis=AX.X, op=ALU.add), [hd], [st])
            self.TS(st, st[:, 8:16], st, st[:, 8:16], 1.0 / 64.0, None, ALU.mult)
            self.TT_(hd, hv, hd, hv, st, bc3(st[:, 8:16], 64), ALU.subtract)
            tv3 = tmp[:].rearrange("p (h e) -> p h e", e=64)
            self.TT_(tmp, tv3, hd, hv, hd, hv, ALU.mult)
            self.V(k.dve, lambda: nc.vector.tensor_reduce(out=st[:, 16:24], in_=tv3, axis=AX.X, op=ALU.add), [tmp], [st])
            self.rsqrt_(st, st[:, 24:32], st, st[:, 16:24], 1.0 / 64.0, self.eps_ln)
            self.TT_(hd, hv, hd, hv, st, bc3(st[:, 24:32], 64), ALU.mult)
            hflat = hd[:].rearrange("p h e -> p (h e)")
            self.TT_(hd, hflat, hd, hflat, mlg, mlg[:], ALU.mult)
            self.TT_(mix, mix[:, 512:1024], hd, hflat, og, og[:], ALU.mult)
            for cc in range(8):
                self.TR(self.ptr, self.ptr[:, cc * 128:(cc + 1) * 128], mix, mix[:, cc * 128:(cc + 1) * 128], self.ident16)
            mixT = r_mixT()
            self.CP(mixT, mixT[:].rearrange("p c s -> p (c s)"), self.ptr, self.ptr[:], eng=k.act)
            phs = [self.pB(), self.pB()]
            for half in range(2):
                for cc in range(8):
                    self.MM(phs[half], phs[half][:], mixT, mixT[:, cc, :], wout, wout[:, cc, half * 512:(half + 1) * 512],
                            cc == 0, cc == 7)
            self.epilogue([(phs[0], phs[0][:]), (phs[1], phs[1][:])], r0, g1, b1, None, "o3", dbg_idx=2 * layer)


def make_consts():
    half = 16
    inv = (10000.0 ** (-np.arange(half, dtype=np.float32) / half)).astype(np.float32)
    pos = np.arange(S, dtype=np.float32)
    ang = (pos[:, None] * inv[None, :]).astype(np.float32)
    cos = np.cos(ang).astype(np.float32).T
    sin = np.sin(ang).astype(np.float32).T
    rope = np.zeros((2, 32, S), np.float32)
    rope[0, :16] = cos
    rope[0, 16:] = cos
    rope[1, :16] = -sin
    rope[1, 16:] = sin
    kk = np.arange(128)
    U = (kk[:, None] <= kk[None, :]).astype(np.float32)
    UT = (kk[:, None] >= kk[None, :]).astype(np.float32)
    mF = np.where(kk[:, None] > kk[None, :], NEG, 0.0).astype(np.float32)
    mB = np.where(kk[:, None] < kk[None, :], NEG, 0.0).astype(np.float32)
    pg = np.zeros((128, 9), np.float32)
    for g in range(7):
        pg[:, g] = g * 128 + kk
    for h in range(2):
        pg[:, 7 + h] = h * 128 + kk
    return {"c_ident": np.eye(128, dtype=np.float32), "c_rope": rope, "c_tri": np.stack([U, UT, mF, mB]), "c_pg": pg}


_PROG_CACHE = {}


def get_prog(nslot, nlayers, debug):
    key = (nslot, nlayers, debug)
    if key not in _PROG_CACHE:
        p = Prog(nslot, nlayers, debug)
        p.build()
        _PROG_CACHE[key] = p
    return _PROG_CACHE[key]


def run(inputs, nslot=NSLOT, nlayers=DEPTH, debug=False, ncores=NCORES, seqs=None):
    p = get_prog(nslot, nlayers, debug)
    xall = np.concatenate([np.asarray(inputs["x_prompt"]), np.asarray(inputs["x_sample"])], axis=0)
    nseq = xall.shape[0]
    if seqs is None:
        seqs = [[min(c * nslot + s, nseq - 1) for s in range(nslot)] for c in range(ncores)]
        flat = list(range(nseq))
        seqs = []
        pos = 0
        for c in range(ncores):
            n = 3 if c < 4 else 2
            mine = flat[pos:pos + n]
            pos += n
            while len(mine) < nslot:
                mine.append(mine[0])
            seqs.append(mine[:nslot])
    consts = make_consts()
    shared = {}
    for n in p.in_names:
        if n == "x" or n in consts:
            continue
        a = np.ascontiguousarray(np.asarray(inputs[n], dtype=np.float32))
        if n in ("ssd_dt_bias", "ssd_a_log", "ml_igate_b", "ml_fgate_b"):
            a = a.reshape(2, 16)
        shared[n] = a
    shared.update(consts)
    in_maps = []
    for c in range(ncores):
        m = dict(shared)
        m["x"] = np.ascontiguousarray(xall[seqs[c]])
        in_maps.append(m)
    res = run_bass_kernel_spmd(p.nc, in_maps, core_ids=list(range(ncores)))
    return res, seqs, nseq


def kernel(**inputs):
    res, seqs, nseq = run(inputs)
    out = np.zeros((nseq, S, D), np.float32)
    done = set()
    for c in range(NCORES):
        y = np.asarray(res.results[c]["y"])
        for s, q in enumerate(seqs[c]):
            if q not in done:
                out[q] = y[s]
                done.add(q)
    nb = np.asarray(inputs["x_prompt"]).shape[0]
    return (out[:nb], out[nb:])
```

```python
import math
from contextlib import ExitStack
import numpy as np
import concourse.bass as bass
import concourse.mybir as mybir
from concourse.bass_utils import run_bass_kernel_spmd

F32 = mybir.dt.float32
BF16 = mybir.dt.bfloat16
AF = mybir.ActivationFunctionType
ALU = mybir.AluOpType
AX = mybir.AxisListType

D = 1024
S = 4096
DEPTH = 4
ALPHA = (2.0 * DEPTH) ** 0.25
LN_EPS = 1e-5
RMS_EPS = 1e-6
NCORES = 8
NSLOT = 3
TT = 512
NT = S // TT
D_FF = 2816
NE = 8
D_FFE = 3584
EV_IN = 1440
OD_IN = 3632
NEG = -30000.0
SPARSE_MOE = True
CONV_SPLIT = 99
GCM = 512
NTL = 24
I32 = mybir.dt.int32


class Res:
    __slots__ = ("name", "w", "rs", "dsem", "dcount", "persist", "phase", "scope", "multi", "msems", "mrr", "mw")

    def __init__(self, name, persist=False):
        self.name = name
        self.phase = False
        self.scope = None
        self.multi = 0
        self.msems = []
        self.mrr = 0
        self.mw = {}
        self.w = None
        self.rs = {}
        self.dsem = None
        self.dcount = 0
        self.persist = persist


class Eng:
    def __init__(self, name, h, sem):
        self.name, self.h, self.sem = name, h, sem
        self.count = 0
        self.seen = {}
        self.nins = 0
        self.nwait = 0


class Tile:
    def __init__(self, t, r):
        self.t, self.r = t, r

    def __getitem__(self, key):
        return self.t[key]


class K:
    def __init__(self, nc, stack):
        self.nc = nc
        self.stack = stack
        self.engs = {}
        for name, h in (("pe", nc.tensor), ("act", nc.scalar), ("dve", nc.vector),
                        ("pool", nc.gpsimd), ("sp", nc.sync)):
            sem = stack.enter_context(nc.semaphore("sem_" + name))
            self.engs[name] = Eng(name, h, sem)
        self.pe, self.act, self.dve, self.pool, self.sp = (
            self.engs[n] for n in ("pe", "act", "dve", "pool", "sp"))
        self.all_res = []
        self.nsem = 5
        self.free_dsems = []
        self.sem_count = {}
        self.exact_sems = set()
        self.gsems = []
        self.grr = 0

    def res(self, name, persist=False):
        r = Res(name, persist)
        self.all_res.append(r)
        return r

    def dres(self, name, n=2):
        r = self.res(name)
        r.multi = n
        return r

    def _waits(self, eng, reads, writes, partial_dst=None):
        need = {}

        def add(m, raw):
            sem, val, src = m
            if src is eng and not raw:
                return
            if src is None and sem not in self.exact_sems:
                val = max(val, self.sem_count.get(sem, 0))
            if eng.seen.get(sem, 0) >= val:
                return
            if need.get(sem, 0) < val:
                need[sem] = val

        for r in reads:
            if r.w is not None:
                add(r.w, True)
            for sem_, val_ in r.mw.items():
                add((sem_, val_, None), True)
        for w in writes:
            if w is not partial_dst:
                for sem_, val_ in w.mw.items():
                    add((sem_, val_, None), False)
            if w.w is not None and not (w is partial_dst and w.w[0] is w.dsem):
                add(w.w, False)
            for sem, (val, src) in w.rs.items():
                add((sem, val, src), False)
        for sem, val in need.items():
            eng.h.wait_ge(sem, val)
            eng.seen[sem] = val
            eng.nwait += 1

    def _mark(self, m, reads, writes):
        sem, val, src = m
        for r in reads:
            o = r.rs.get(sem)
            if o is None or o[0] < val:
                r.rs[sem] = (val, src)
        for w in writes:
            w.w = m
            w.rs = {}

    def op(self, eng, fn, reads=(), writes=()):
        self._waits(eng, reads, writes)
        eng.count += 1
        eng.nins += 1
        ins = fn()
        ins.then_inc(eng.sem, 1)
        self._mark((eng.sem, eng.count, eng), reads, writes)
        return ins

    def _multi_slot(self, q, r0):
        NG = 24
        if len(self.gsems) < NG:
            sem = self.stack.enter_context(self.nc.semaphore("gsem_%d" % len(self.gsems)))
            self.nsem += 1
            self.exact_sems.add(sem)
            self.gsems.append([sem, 0])
            slot = len(self.gsems) - 1
        else:
            slot = self.grr % NG
        self.grr += 1
        sem, cnt = self.gsems[slot]
        if q.seen.get(sem, 0) < cnt:
            q.h.wait_ge(sem, cnt)
            q.seen[sem] = cnt
        return slot, sem

    def _multi_done(self, r0, slot, sem, n, reads):
        self.gsems[slot][1] += 16 * n
        cnt = self.gsems[slot][1]
        for r in reads:
            o = r.rs.get(sem)
            if o is None or o[0] < cnt:
                r.rs[sem] = (cnt, None)
        r0.mw[sem] = cnt
        r0.rs = {}

    def dma(self, q, pairs, reads=(), writes=(), partial=False, **kw):
        r0 = writes[0]
        self._waits(q, reads, writes, partial_dst=r0 if partial else None)
        if r0.multi and partial:
            slot, sem = self._multi_slot(q, r0)
            for (o, i) in pairs:
                q.h.dma_start(out=o, in_=i, **kw).then_inc(sem, 16)
                q.nins += 1
            self._multi_done(r0, slot, sem, len(pairs), reads)
            return
        if r0.dsem is None:
            if self.free_dsems:
                r0.dsem, r0.dcount = self.free_dsems.pop()
            else:
                r0.dsem = self.stack.enter_context(self.nc.semaphore("dsem_%d" % self.nsem))
                r0.dcount = 0
                self.nsem += 1
        for (o, i) in pairs:
            q.h.dma_start(out=o, in_=i, **kw).then_inc(r0.dsem, 16)
            r0.dcount += 16
            q.nins += 1
        self.sem_count[r0.dsem] = r0.dcount
        self._mark((r0.dsem, r0.dcount, None), reads, writes)

    def idma(self, fn, reads, writes, partial=False):
        q = self.pool
        r0 = writes[0]
        self._waits(q, reads, writes, partial_dst=r0 if partial else None)
        if r0.multi and partial:
            slot, sem = self._multi_slot(q, r0)
            fn().then_inc(sem, 16)
            q.nins += 1
            self._multi_done(r0, slot, sem, 1, reads)
            return
        if r0.dsem is None:
            if self.free_dsems:
                r0.dsem, r0.dcount = self.free_dsems.pop()
            else:
                r0.dsem = self.stack.enter_context(self.nc.semaphore("dsem_%d" % self.nsem))
                r0.dcount = 0
                self.nsem += 1
        fn().then_inc(r0.dsem, 16)
        r0.dcount += 16
        q.nins += 1
        self.sem_count[r0.dsem] = r0.dcount
        self._mark((r0.dsem, r0.dcount, None), reads, writes)

    def barrier(self, closing=None):
        sp = self.sp
        for r in self.all_res:
            if r.persist:
                continue
            if r.dsem is not None and sp.seen.get(r.dsem, 0) < r.dcount:
                sp.h.wait_ge(r.dsem, r.dcount)
                sp.seen[r.dsem] = r.dcount
        for (sem_, cnt_) in self.gsems:
            if sp.seen.get(sem_, 0) < cnt_:
                sp.h.wait_ge(sem_, cnt_)
                sp.seen[sem_] = cnt_
        sp.count += 1
        sp.h.sem_inc(sp.sem, 1)
        for e in self.engs.values():
            for o in self.engs.values():
                if o is e or o.count == 0:
                    continue
                if e.seen.get(o.sem, 0) < o.count:
                    e.h.wait_ge(o.sem, o.count)
                    e.seen[o.sem] = o.count
            for r in self.all_res:
                if not r.persist and r.dsem is not None:
                    e.seen[r.dsem] = r.dcount
            for (sem_, cnt_) in self.gsems:
                e.seen[sem_] = cnt_
        for r in self.all_res:
            if not r.persist:
                r.w = None
                r.rs = {}
                r.mw = {}
        keep = []
        for r in self.all_res:
            if r.phase:
                if r.dsem is not None:
                    self.free_dsems.append((r.dsem, r.dcount))
                    r.dsem = None
                if r.scope is closing:
                    continue
            keep.append(r)
        self.all_res = keep

    def finish(self):
        sp = self.sp
        for r in self.all_res:
            ms = [(s_, v, e) for s_, (v, e) in r.rs.items()]
            if r.w is not None:
                ms.append(r.w)
            for (sem, val, src) in ms:
                if sp.seen.get(sem, 0) >= val:
                    continue
                sp.h.wait_ge(sem, val)
                sp.seen[sem] = val


class Prog:
    def __init__(self, nslot=NSLOT, nlayers=DEPTH, debug=False):
        self.nslot = nslot
        self.nlayers = nlayers
        self.debug = debug
        self.nc = bass.Bass("TRN2", target_bir_lowering=False)
        self.in_names = []

    def din(self, name, shape, dt=F32):
        self.in_names.append(name)
        return self.nc.dram_tensor(name, list(shape), dt, kind="ExternalInput").ap()

    def dscr(self, name, shape, dt):
        return self.nc.dram_tensor(name, list(shape), dt).ap()

    def T(self, name, shape, dt=F32):
        t = self.ph.enter_context(self.nc.sbuf_tensor(name + "_%d" % self.uid(), list(shape), dt))
        r = self.k.res(name)
        r.phase = self.ph is not self.stack
        r.scope = self.ph
        return Tile(t, r)

    def TP(self, name, shape, dt=F32):
        t = self.stack.enter_context(self.nc.sbuf_tensor(name, list(shape), dt))
        return Tile(t, self.k.res(name, persist=True))

    def uid(self):
        self._uid += 1
        return self._uid

    def MM(self, out, oap, lt, lap, rt, rap, start, stop):
        nc = self.nc
        self.k.op(self.k.pe, lambda: nc.tensor.matmul(oap, lhsT=lap, rhs=rap, start=start, stop=stop),
                  [lt.r, rt.r], [out.r])

    def TR(self, out, oap, it, iap, ident):
        nc = self.nc
        self.k.op(self.k.pe, lambda: nc.tensor.transpose(oap, iap, ident[:]), [it.r, ident.r], [out.r])

    def ACT(self, out, oap, it, iap, func, bias=None, scale=None, accum=None, extra=()):
        nc = self.nc
        kw = {}
        if bias is not None:
            kw["bias"] = bias
        if scale is not None:
            kw["scale"] = scale
        if accum is not None:
            kw["accum_out"] = accum
        self.k.op(self.k.act, lambda: nc.scalar.activation(out=oap, in_=iap, func=func, **kw),
                  [it.r] + [e.r for e in extra], [out.r])

    def V(self, eng, fn, reads, writes):
        self.k.op(eng, fn, [t.r for t in reads], [t.r for t in writes])

    def TT_(self, out, oap, a, aap, b, bap, op, eng=None):
        nc = self.nc
        eng = eng or self.k.dve
        self.k.op(eng, lambda: eng.h.tensor_tensor(out=oap, in0=aap, in1=bap, op=op), [a.r, b.r], [out.r])

    def TS(self, out, oap, a, aap, s1, s2, op0, op1=None, extra=(), eng=None):
        eng = eng or self.k.dve
        if op1 is None:
            fn = lambda: eng.h.tensor_scalar(out=oap, in0=aap, scalar1=s1, scalar2=None, op0=op0)
        else:
            fn = lambda: eng.h.tensor_scalar(out=oap, in0=aap, scalar1=s1, scalar2=s2, op0=op0, op1=op1)
        self.k.op(eng, fn, [a.r] + [e.r for e in extra], [out.r])

    def STT(self, out, oap, a, aap, sc, b, bap, op0, op1, extra=(), eng=None):
        eng = eng or self.k.dve
        self.k.op(eng, lambda: eng.h.scalar_tensor_tensor(out=oap, in0=aap, scalar=sc, in1=bap, op0=op0, op1=op1),
                  [a.r, b.r] + [e.r for e in extra], [out.r])

    def CP(self, out, oap, it, iap, eng=None):
        eng = eng or self.k.dve
        if eng is self.k.act:
            self.k.op(eng, lambda: self.nc.scalar.copy(out=oap, in_=iap), [it.r], [out.r])
        else:
            self.k.op(eng, lambda: eng.h.tensor_copy(out=oap, in_=iap), [it.r], [out.r])

    def LD(self, tile, oap, src_ap, src_res=None, q=None, partial=False):
        self.k.dma(q or self.k.sp, [(oap, src_ap)], [src_res] if src_res is not None else [], [tile.r], partial=partial)

    def ST(self, dst_ap, dst_res, tile, iap, q=None):
        self.k.dma(q or self.k.sp, [(dst_ap, iap)], [tile.r], [dst_res], partial=True)

    def build(self):
        nc = self.nc
        self._uid = 0
        with ExitStack() as stack:
            self.stack = stack
            self.k = K(nc, stack)
            self.declare_io()
            self.setup_consts()
            self.convert_weights([l for l in range(self.nlayers) if l < CONV_SPLIT])
            for slot in range(self.nslot):
                self.run_slot(slot)
            self.k.barrier()
        return nc

    def declare_io(self):
        ns = self.nslot
        self.x_in = self.din("x", [ns, S, D])
        self.y_out = self.nc.dram_tensor("y", [ns, S, D], F32, kind="ExternalOutput").ap()
        self.r_y = self.k.dres("y_out", 4)
        w = {}
        w["ev_w_in"] = self.din("ev_w_in", [2, D, EV_IN])
        w["conv_dw_w"] = self.din("conv_dw_w", [2, 31, 512])
        w["conv_dw_b"] = self.din("conv_dw_b", [2, 512])
        w["conv_ln_g"] = self.din("conv_ln_g", [2, 512])
        w["conv_ln_b"] = self.din("conv_ln_b", [2, 512])
        w["mla_q_norm_g"] = self.din("mla_q_norm_g", [2, 256])
        w["mla_w_uq"] = self.din("mla_w_uq", [2, 256, 768])
        w["mla_kv_norm_g"] = self.din("mla_kv_norm_g", [2, 128])
        w["mla_w_ukv"] = self.din("mla_w_ukv", [2, 128, 1024])
        w["ev_w_out"] = self.din("ev_w_out", [2, 1024, 1024])
        w["od_w_in"] = self.din("od_w_in", [2, D, OD_IN])
        w["ssd_conv_w"] = self.din("ssd_conv_w", [2, 5, 1024])
        w["ssd_conv_b"] = self.din("ssd_conv_b", [2, 1024])
        w["ssd_dt_bias"] = self.din("ssd_dt_bias", [2, 16])
        w["ssd_a_log"] = self.din("ssd_a_log", [2, 16])
        w["ssd_d"] = self.din("ssd_d", [2, 8])
        w["ssd_norm_g"] = self.din("ssd_norm_g", [2, 512])
        w["ml_igate_b"] = self.din("ml_igate_b", [2, 16])
        w["ml_fgate_b"] = self.din("ml_fgate_b", [2, 16])
        w["ml_norm_g"] = self.din("ml_norm_g", [2, 512])
        w["od_w_out"] = self.din("od_w_out", [2, 1024, 1024])
        w["ffn_w_gate"] = self.din("ffn_w_gate", [2, D, D_FF])
        w["ffn_w_up"] = self.din("ffn_w_up", [2, D, D_FF])
        w["ffn_w_down"] = self.din("ffn_w_down", [2, D_FF, D])
        w["moe_router_w"] = self.din("moe_router_w", [2, D, NE])
        w["moe_router_b"] = self.din("moe_router_b", [2, NE])
        w["moe_w_gate"] = self.din("moe_w_gate", [2, NE, D, D_FFE])
        w["moe_w_up"] = self.din("moe_w_up", [2, NE, D, D_FFE])
        w["moe_w_down"] = self.din("moe_w_down", [2, NE, D_FFE, D])
        for n in ("ln1_g", "ln1_b", "ln2_g", "ln2_b"):
            w[n] = self.din(n, [4, D])
        self.w = w
        self.c_ident = self.din("c_ident", [128, 128])
        self.c_rope = self.din("c_rope", [2, 32, S])
        self.c_tri = self.din("c_tri", [4, 128, 128])
        self.c_pg = self.din("c_pg", [128, 9])
        self.xres = self.dscr("xres", [S, D], F32)
        self.r_xres = self.k.dres("xres", 4)
        self.xT = self.dscr("xT", [D, S + 4], BF16)
        self.r_xT = self.k.dres("xT", 4)
        if self.debug:
            self.dbg = self.nc.dram_tensor("dbg", [self.nlayers * 2, S, D], F32, kind="ExternalOutput").ap()
            self.r_dbg = self.k.dres("dbg", 2)

    def setup_consts(self):
        nc, k = self.nc, self.k
        self.ph = self.stack
        self.ident32 = self.TP("ident32", [128, 128], F32)
        self.LD(self.ident32, self.ident32[:], self.c_ident)
        self.ident16 = self.TP("ident16", [128, 128], BF16)
        self.CP(self.ident16, self.ident16[:], self.ident32, self.ident32[:])
        self.ones32 = self.TP("ones32", [128, 128], F32)
        self.V(k.dve, lambda: nc.vector.memset(self.ones32[:], 1.0), [], [self.ones32])
        self.zero16 = self.TP("zero16", [128, 64], BF16)
        self.V(k.dve, lambda: nc.vector.memset(self.zero16[:], 0.0), [], [self.zero16])
        self.zero32 = self.TP("zero32", [128, 64], F32)
        self.V(k.dve, lambda: nc.vector.memset(self.zero32[:], 0.0), [], [self.zero32])
        self.pb = []
        for i in range(7):
            t = self.stack.enter_context(nc.psum_tensor("pb%d" % i, [128, 512], F32))
            self.pb.append(Tile(t, k.res("pb%d" % i, persist=True)))
        t = self.stack.enter_context(nc.psum_tensor("ptr", [128, 1024], BF16))
        self.ptr = Tile(t, k.res("ptr", persist=True))
        xTv = self.xT.rearrange("(c p) s -> p c s", p=128)
        for lo in (0, S + 2):
            k.dma(k.sp, [(xTv[:, :, lo:lo + 2], self.zero16[:, 0:16].rearrange("p (c s) -> p c s", c=8))],
                  [self.zero16.r], [self.r_xT], partial=True)

    def convert_weights(self, layers):
        k = self.k
        GC = 256
        if not hasattr(self, "wb"):
            self.wb = {}
            self.wb_r = {}
            self._alloc_wb(GC)
        self._convert(layers, GC)

    def _alloc_wb(self, GC):
        plain = ["ev_w_in", "mla_w_uq", "mla_w_ukv", "ev_w_out", "od_w_in", "od_w_out"]
        for n in plain:
            self.wb[n] = self.dscr("wb_" + n, list(self.w[n].shape), BF16)
        self.wb["ffn_w_gate"] = self.dscr("wb_ffn_g", [2, 1, D_FF // GC, 128, 8, GC], BF16)
        self.wb["ffn_w_up"] = self.dscr("wb_ffn_u", [2, 1, D_FF // GC, 128, 8, GC], BF16)
        self.wb["ffn_w_down"] = self.dscr("wb_ffn_d", [2, 1, 2, 128, D_FF // 128, 512], BF16)
        self.wb["moe_w_gate"] = self.dscr("wb_moe_g", [2, NE, D_FFE // GCM, 128, 8, GCM], BF16)
        self.wb["moe_w_up"] = self.dscr("wb_moe_u", [2, NE, D_FFE // GCM, 128, 8, GCM], BF16)
        self.wb["moe_w_down"] = self.dscr("wb_moe_d", [2, NE, 2, 128, D_FFE // 128, 512], BF16)

    def _convert(self, layers, GC):
        k = self.k

        def conv_plain(n, j):
            src, dst = self.w[n][j], self.wb[n][j]
            r = self._grp
            self.wb_r[(n, j, 0)] = r
            rows = src.shape[0]
            prs = [(dst[r0:min(r0 + 256, rows), :], src[r0:min(r0 + 256, rows), :]) for r0 in range(0, rows, 256)]
            k.dma(k.pool, prs, [], [r], partial=True)

        def conv_ff(prefix, j, ne):
            for e in range(ne):
                for nm in ("gate", "up"):
                    n = "%s_w_%s" % (prefix, nm)
                    src = self.w[n][j] if ne == 1 else self.w[n][j, e]
                    dst = self.wb[n][j, e]
                    r = self._grp
                    self.wb_r[(n, j, e)] = r
                    sv = src.rearrange("(c p) n -> p c n", p=128)
                    gc = dst.shape[-1]
                    prs = [(dst[g], sv[:, :, g * gc:(g + 1) * gc]) for g in range(dst.shape[0])]
                    k.dma(k.pool, prs, [], [r], partial=True)
                n = "%s_w_down" % prefix
                src = self.w[n][j] if ne == 1 else self.w[n][j, e]
                dst = self.wb[n][j, e]
                r = self._grp
                self.wb_r[(n, j, e)] = r
                sv = src.rearrange("(f p) d -> p f d", p=128)
                nf = sv.shape[1]
                prs = []
                for half in range(2):
                    for f0 in range(0, nf, 7):
                        f1 = min(nf, f0 + 7)
                        prs.append((dst[half][:, f0:f1, :], sv[:, f0:f1, half * 512:(half + 1) * 512]))
                k.dma(k.pool, prs, [], [r], partial=True)

        for layer in layers:
            j = layer // 2
            self._grp = k.res("wbgrp_mix%d" % layer, persist=True)
            if layer % 2 == 0:
                for n in ("ev_w_in", "mla_w_uq", "mla_w_ukv", "ev_w_out"):
                    conv_plain(n, j)
                self._grp = k.res("wbgrp_ffn%d" % layer, persist=True)
                conv_ff("ffn", j, 1)
            else:
                for n in ("od_w_in", "od_w_out"):
                    conv_plain(n, j)
                self._grp = k.res("wbgrp_ffn%d" % layer, persist=True)
                conv_ff("moe", j, NE)

    def load_bcast(self, name, src_row_ap, n):
        t = self.T(name, [128, n], F32)
        self.LD(t, t[:], src_row_ap.partition_broadcast(128))
        return t

    def load_col(self, name, src_vec_ap, nchunk):
        t = self.T(name, [128, nchunk], F32)
        self.k.dma(self.k.sp, [(t[:], src_vec_ap.rearrange("(c p) -> p c", p=128))], [], [t.r],
                   allow_slow_non_contiguous=True)
        return t

    def run_slot(self, slot):
        k = self.k
        self.prologue(slot)
        for layer in range(self.nlayers):
            j = layer // 2
            last = (layer == self.nlayers - 1)
            if layer % 2 == 0:
                self.even_mixer(j, layer)
                self.ffn_like(layer, j, moe=False, dst=(self.y_out[slot], self.r_y) if last else None)
            else:
                self.odd_mixer(j, layer)
                if SPARSE_MOE:
                    self.moe_sparse(layer, j, dst=(self.y_out[slot], self.r_y) if last else None)
                else:
                    self.ffn_like(layer, j, moe=True, dst=(self.y_out[slot], self.r_y) if last else None)
                if slot == 0 and layer == 1 and self.nlayers > CONV_SPLIT:
                    self.convert_weights([l for l in range(self.nlayers) if l >= CONV_SPLIT])

    def phase(self):
        prog = self

        class _P:
            def __enter__(s):
                prog._phstack = ExitStack()
                prog._phstack.__enter__()
                prog.ph = prog._phstack
                return s

            def __exit__(s, *a):
                if a[0] is None:
                    prog.k.barrier(closing=prog._phstack)
                prog._phstack.__exit__(*a)
                prog.ph = prog.stack
                return False
        return _P()

    def prologue(self, slot):
        nc, k = self.nc, self.k
        with self.phase():
            xin = self.x_in[slot]
            for t in range(S // 128):
                xt = self.T("pro_x%d" % (t % 2), [128, D], F32) if t < 2 else None
                if t < 2:
                    if t == 0:
                        self._pro = []
                    self._pro.append(xt)
                xt = self._pro[t % 2]
                self.LD(xt, xt[:], xin[t * 128:(t + 1) * 128, :])
                self.ST(self.xres[t * 128:(t + 1) * 128, :], self.r_xres, xt, xt[:])
                self.emit_xT(xt, t * 128, "pro")

    def emit_xT(self, y32, tok0, tag):
        nc, k = self.nc, self.k
        key = "_xT_" + tag
        if not hasattr(self, key) or getattr(self, key)[0] is not self.ph:
            y16 = self.T(tag + "_y16", [128, D], BF16)
            xtt = self.T(tag + "_xtt", [128, 8, 128], BF16)
            setattr(self, key, (self.ph, y16, xtt))
        _, y16, xtt = getattr(self, key)
        self.CP(y16, y16[:], y32, y32[:], eng=k.act)
        for c in range(8):
            self.TR(self.ptr, self.ptr[:, c * 128:(c + 1) * 128], y16, y16[:, c * 128:(c + 1) * 128], self.ident16)
        self.CP(xtt, xtt[:].rearrange("p c s -> p (c s)"), self.ptr, self.ptr[:], eng=k.dve)
        xTv = self.xT.rearrange("(c p) s -> p c s", p=128)
        self.ST(xTv[:, :, 2 + tok0:2 + tok0 + 128], self.r_xT, xtt, xtt[:])

    def ln_consts(self, gname, bname, layer):
        g = self.load_bcast("lng", self.w[gname][layer:layer + 1, :], D)
        b = self.load_bcast("lnb", self.w[bname][layer:layer + 1, :], D)
        return g, b

    def epilogue(self, ps_halves, tok0, g, b, dst, tag, dbg_idx=None, add_tile=None):
        self.epilogue_multi([(ps_halves, tok0)], g, b, dst, tag, dbg_idx=dbg_idx, nch=1)

    def epilogue_multi(self, items, g, b, dst, tag, dbg_idx=None, nch=2):
        nc, k = self.nc, self.k
        key = "_ep_" + tag
        if not hasattr(self, key) or getattr(self, key)[0] is not self.ph:
            tiles = (self.ph,
                     [self.T(tag + "_ex%d" % i, [128, D], F32) for i in range(nch)],
                     [self.T(tag + "_et%d" % i, [128, D], F32) for i in range(nch)],
                     [self.T(tag + "_est%d" % i, [128, 2, 6], F32) for i in range(nch)],
                     [self.T(tag + "_emv%d" % i, [128, 2], F32) for i in range(nch)],
                     [self.T(tag + "_ers%d" % i, [128, 1], F32) for i in range(nch)],
                     [self.T(tag + "_y16%d" % i, [128, D], BF16) for i in range(nch)],
                     [self.T(tag + "_xtt%d" % i, [128, 8, 128], BF16) for i in range(nch)])
            setattr(self, key, tiles)
        _, xs_, ts_, stts, mvs, rss, y16s, xtts = getattr(self, key)
        n = len(items)
        assert n <= nch
        R = range(n)
        for c in R:
            tok0 = items[c][1]
            self.LD(xs_[c], xs_[c][:], self.xres[tok0:tok0 + 128, :], self.r_xres)
        for h in range(2):
            for c in R:
                pt, pap = items[c][0][h]
                self.STT(ts_[c], ts_[c][:, h * 512:(h + 1) * 512], xs_[c], xs_[c][:, h * 512:(h + 1) * 512], ALPHA, pt, pap,
                         ALU.mult, ALU.add)
        for h in range(2):
            for c in R:
                self.V(k.dve, (lambda c_, h_: (lambda: nc.vector.bn_stats(out=stts[c_][:, h_, :], in_=ts_[c_][:, h_ * 512:(h_ + 1) * 512])))(c, h),
                       [ts_[c]], [stts[c]])
        for c in R:
            self.V(k.dve, (lambda c_: (lambda: nc.vector.bn_aggr(out=mvs[c_][:], in_=stts[c_][:])))(c), [stts[c]], [mvs[c]])
        for c in R:
            self.ACT(rss[c], rss[c][:], mvs[c], mvs[c][:, 1:2], AF.Sqrt, bias=self.eps_ln[:, 0:1], scale=1.0, extra=[self.eps_ln])
        for c in R:
            self.V(k.dve, (lambda c_: (lambda: nc.vector.reciprocal(out=rss[c_][:], in_=rss[c_][:])))(c), [rss[c]], [rss[c]])
        for c in R:
            self.TS(ts_[c], ts_[c][:], ts_[c], ts_[c][:], mvs[c][:, 0:1], rss[c][:, 0:1], ALU.subtract, ALU.mult, extra=[mvs[c], rss[c]])
        for c in R:
            self.TT_(ts_[c], ts_[c][:], ts_[c], ts_[c][:], g, g[:], ALU.mult)
        for c in R:
            self.TT_(ts_[c], ts_[c][:], ts_[c], ts_[c][:], b, b[:], ALU.add, eng=k.pool)
        xTv = self.xT.rearrange("(c p) s -> p c s", p=128)
        for c in R:
            tok0 = items[c][1]
            if dst is not None:
                dap, dres = dst
                self.ST(dap[tok0:tok0 + 128, :], dres, ts_[c], ts_[c][:])
            else:
                self.ST(self.xres[tok0:tok0 + 128, :], self.r_xres, ts_[c], ts_[c][:])
            if self.debug and dbg_idx is not None:
                self.ST(self.dbg[dbg_idx, tok0:tok0 + 128, :], self.r_dbg, ts_[c], ts_[c][:])
        if dst is None:
            for c in R:
                self.CP(y16s[c], y16s[c][:], ts_[c], ts_[c][:], eng=k.act)
            for c in R:
                tok0 = items[c][1]
                for cc in range(8):
                    self.TR(self.ptr, self.ptr[:, cc * 128:(cc + 1) * 128], y16s[c], y16s[c][:, cc * 128:(cc + 1) * 128], self.ident16)
                self.CP(xtts[c], xtts[c][:].rearrange("p c s -> p (c s)"), self.ptr, self.ptr[:], eng=k.dve)
                self.ST(xTv[:, :, 2 + tok0:2 + tok0 + 128], self.r_xT, xtts[c], xtts[c][:])

    def eps_tiles(self):
        nc, k = self.nc, self.k
        if not hasattr(self, "eps_ln"):
            self.eps_ln = self.TP("eps_ln", [128, 1], F32)
            self.V(k.dve, lambda: nc.vector.memset(self.eps_ln[:], LN_EPS), [], [self.eps_ln])
            self.eps_rms = self.TP("eps_rms", [128, 1], F32)
            self.V(k.dve, lambda: nc.vector.memset(self.eps_rms[:], RMS_EPS), [], [self.eps_rms])

    def load_xT_full(self, name="xTs"):
        t = self.T(name, [128, 8, S + 4], BF16)
        xTv = self.xT.rearrange("(c p) s -> p c s", p=128)
        for c in range(8):
            self.k.dma(self.k.sp, [(t[:, c, :], xTv[:, c, :])], [self.r_xT], [t.r], partial=True)
        return t

    def load_w(self, name, src, kc, cols, res, c0=0):
        t = self.T(name, [128, kc, cols], BF16)
        v = src.rearrange("(c p) n -> p c n", p=128)
        self.k.dma(self.k.sp, [(t[:], v[:, :, c0:c0 + cols])], [res], [t.r])
        return t

    def ring(self, name, shape, dt, n, init=None):
        tiles = [self.T("%s%d" % (name, i), shape, dt) for i in range(n)]
        if init is not None:
            for t in tiles:
                self.V(self.k.dve, (lambda tt: (lambda: self.nc.vector.memset(tt[:], init)))(t), [], [t])
        st = [0]

        def nxt():
            t = tiles[st[0] % n]
            st[0] += 1
            return t
        return nxt

    def pA(self):
        self._pa = getattr(self, "_pa", 0) + 1
        return self.pb[self._pa % 4]

    def pB(self):
        self._pbi = getattr(self, "_pbi", 0) + 1
        return self.pb[4 + self._pbi % 3]

    def rsqrt_(self, out, oap, src, sap, scale, eps_tile):
        self.ACT(out, oap, src, sap, AF.Sqrt, bias=eps_tile[:, 0:1], scale=scale, extra=[eps_tile])
        self.V(self.k.dve, lambda: self.nc.vector.reciprocal(out=oap, in_=oap), [out], [out])

    def even_mixer(self, j, layer):
        nc, k = self.nc, self.k
        self.eps_tiles()
        W = self.w
        if not hasattr(self, "u_d"):
            self.u_d = self.dscr("u_d", [4, 128, S + 30], F32)
            self.r_u = k.dres("u_d", 2)
            self.q_d = self.dscr("q_d", [8, 96, S], BF16)
            self.r_q = k.dres("q_d", 2)
            self.kn_d = self.dscr("kn_d", [8, 64, S], BF16)
            self.r_kn = k.dres("kn_d", 2)
            self.kpe_d = self.dscr("kpe_d", [32, S], BF16)
            self.r_kpe = k.dres("kpe_d", 2)
            self.v_d = self.dscr("v_d", [S, 8 * 65], BF16)
            self.r_v = k.dres("v_d", 2)
            self.att_d = self.dscr("att_d", [S, 512], BF16)
            self.r_att = k.dres("att_d", 2)
            uv = self.u_d.rearrange("c p s -> p c s")
            for lo in (0, S + 15):
                k.dma(k.sp, [(uv[:, :, lo:lo + 15], self.zero32[:, 0:60].rearrange("p (c s) -> p c s", c=4))],
                      [self.zero32.r], [self.r_u], partial=True)
        sl = 96 ** -0.5
        with self.phase():
            xTs = self.load_xT_full()
            wr = self.wb_r[("ev_w_in", j, 0)]
            win = self.load_w("win", self.wb["ev_w_in"][j], 8, EV_IN, wr)
            wv_ = self.wb["ev_w_in"][j].rearrange("(c p) n -> p c n", p=128)
            wkrs = self.T("wkrs", [128, 8, 96], BF16)
            k.dma(k.sp, [(wkrs[:, :, 0:64], wv_[:, :, 1344:1408]), (wkrs[:, :, 64:80], wv_[:, :, 1424:1440]),
                         (wkrs[:, :, 80:96], wv_[:, :, 1408:1424])], [wr], [wkrs.r])
            wq_r = self.wb_r[("mla_w_uq", j, 0)]
            wuq = self.load_w("wuq", self.wb["mla_w_uq"][j], 2, 768, wq_r)
            wuqs = self.T("wuqs", [128, 2, 768], BF16)
            vq = self.wb["mla_w_uq"][j].rearrange("(c p) (h e) -> p c h e", p=128, e=96)
            wqv = wuqs[:].rearrange("p c (h e) -> p c h e", e=96)
            prs = []
            for c in range(2):
                prs += [(wqv[:, c, :, 0:64], vq[:, c, :, 0:64]), (wqv[:, c, :, 64:80], vq[:, c, :, 80:96]),
                        (wqv[:, c, :, 80:96], vq[:, c, :, 64:80])]
            k.dma(k.sp, prs, [wq_r], [wuqs.r])
            wkv_r = self.wb_r[("mla_w_ukv", j, 0)]
            wukv = self.load_w("wukv", self.wb["mla_w_ukv"][j], 1, 1024, wkv_r)
            wvv = self.T("wvv", [128, 8, 64], BF16)
            k.dma(k.sp, [(wvv[:], self.wb["mla_w_ukv"][j].rearrange("p (h e) -> p h e", e=128)[:, :, 64:128])],
                  [wkv_r], [wvv.r])
            gq = self.load_col("gq", W["mla_q_norm_g"][j], 2)
            gkv = self.load_col("gkv", W["mla_kv_norm_g"][j], 1)
            r_sig = self.ring("sig", [128, TT], F32, 2)
            r_u = self.ring("u", [128, TT], F32, 2)
            r_sq = self.ring("sq", [128, TT], F32, 3)
            r_rstd = self.ring("rstd", [128, TT], F32, 2)
            r_qn = self.ring("qn", [128, TT], BF16, 4)
            r_kvn = self.ring("kvn", [128, TT], BF16, 2)
            r_cs = self.ring("cs", [96, 2, TT], F32, 2)
            r_qt = self.ring("qt", [96, TT], BF16, 3)
            r_t1 = self.ring("t1", [96, TT], F32, 2)
            r_t2 = self.ring("t2", [96, TT], F32, 2)
            r_kn = self.ring("kn", [64, TT], BF16, 3)
            r_vt = self.ring("vt", [128, 8, 65], BF16, 3, init=1.0)
            for t in range(NT):
                c0 = 2 + t * TT
                cols = slice(t * TT, (t + 1) * TT)

                def proj(pt, pap, wt, wap_fn):
                    for c in range(8):
                        self.MM(pt, pap, wt, wap_fn(c), xTs, xTs[:, c, c0:c0 + TT], c == 0, c == 7)
                for jj in range(4):
                    pv, pg = self.pA(), self.pA()
                    proj(pv, pv[:], win, lambda c: win[:, c, jj * 128:(jj + 1) * 128])
                    proj(pg, pg[:], win, lambda c: win[:, c, 512 + jj * 128:512 + (jj + 1) * 128])
                    sig = r_sig()
                    self.ACT(sig, sig[:], pg, pg[:], AF.Sigmoid)
                    u = r_u()
                    self.TT_(u, u[:], pv, pv[:], sig, sig[:], ALU.mult)
                    self.ST(self.u_d[jj][:, 15 + t * TT:15 + (t + 1) * TT], self.r_u, u, u[:])
                pq = [self.pA(), self.pA()]
                for cq in range(2):
                    proj(pq[cq], pq[cq][:], win, lambda c: win[:, c, 1024 + cq * 128:1024 + (cq + 1) * 128])
                pkv = self.pB()
                proj(pkv, pkv[:], win, lambda c: win[:, c, 1280:1408])
                pkr = self.pB()
                proj(pkr, pkr[0:96, :], win, lambda c: win[:, c, 1344:1440])
                pkrs = self.pB()
                proj(pkrs, pkrs[0:96, :], wkrs, lambda c: wkrs[:, c, :])
                cs = r_cs()
                k.dma(k.sp, [(cs[64:96, 0, :], self.c_rope[0][:, cols]), (cs[64:96, 1, :], self.c_rope[1][:, cols])],
                      [], [cs.r])
                t1, t2, kpe = r_t1(), r_t2(), r_qt()
                self.TT_(t1, t1[64:96, :], pkr, pkr[64:96, :], cs, cs[64:96, 0, :], ALU.mult)
                self.TT_(t2, t2[64:96, :], pkrs, pkrs[64:96, :], cs, cs[64:96, 1, :], ALU.mult)
                self.TT_(kpe, kpe[64:96, :], t1, t1[64:96, :], t2, t2[64:96, :], ALU.add)
                self.ST(self.kpe_d[:, cols], self.r_kpe, kpe, kpe[64:96, :])
                sqs = []
                for cq in range(2):
                    sq = r_sq()
                    self.ACT(sq, sq[:], pq[cq], pq[cq][:], AF.Square)
                    sqs.append(sq)
                pss = self.pA()
                for cq in range(2):
                    self.MM(pss, pss[:], self.ones32, self.ones32[:], sqs[cq], sqs[cq][:], cq == 0, cq == 1)
                rstd = r_rstd()
                self.rsqrt_(rstd, rstd[:], pss, pss[:], 1.0 / 256.0, self.eps_rms)
                qn = []
                for cq in range(2):
                    q_ = r_qn()
                    self.STT(q_, q_[:], pq[cq], pq[cq][:], gq[:, cq:cq + 1], rstd, rstd[:], ALU.mult, ALU.mult, extra=[gq])
                    qn.append(q_)
                sq = r_sq()
                self.ACT(sq, sq[:], pkv, pkv[:], AF.Square)
                pss2 = self.pA()
                self.MM(pss2, pss2[:], self.ones32, self.ones32[:], sq, sq[:], True, True)
                rstdk = r_rstd()
                self.rsqrt_(rstdk, rstdk[:], pss2, pss2[:], 1.0 / 128.0, self.eps_rms)
                kvn = r_kvn()
                self.STT(kvn, kvn[:], pkv, pkv[:], gkv[:, 0:1], rstdk, rstdk[:], ALU.mult, ALU.mult, extra=[gkv])
                for h in range(8):
                    pqh, pqs = self.pA(), self.pA()
                    for c in range(2):
                        self.MM(pqh, pqh[0:96, :], wuq, wuq[:, c, 96 * h:96 * h + 96], qn[c], qn[c][:], c == 0, c == 1)
                    for c in range(2):
                        self.MM(pqs, pqs[0:96, :], wuqs, wuqs[:, c, 96 * h:96 * h + 96], qn[c], qn[c][:], c == 0, c == 1)
                    qt = r_qt()
                    self.CP(qt, qt[0:64, :], pqh, pqh[0:64, :], eng=k.act)
                    t1, t2 = r_t1(), r_t2()
                    self.TT_(t1, t1[64:96, :], pqh, pqh[64:96, :], cs, cs[64:96, 0, :], ALU.mult)
                    self.TT_(t2, t2[64:96, :], pqs, pqs[64:96, :], cs, cs[64:96, 1, :], ALU.mult)
                    self.TT_(qt, qt[64:96, :], t1, t1[64:96, :], t2, t2[64:96, :], ALU.add)
                    self.ST(self.q_d[h][:, cols], self.r_q, qt, qt[0:96, :])
                for h in range(8):
                    pk = self.pB()
                    self.MM(pk, pk[0:64, :], wukv, wukv[:, 0, 128 * h:128 * h + 64], kvn, kvn[:], True, True)
                    kn = r_kn()
                    self.CP(kn, kn[:], pk, pk[0:64, :], eng=(k.act if h % 2 else k.dve))
                    self.ST(self.kn_d[h][:, cols], self.r_kn, kn, kn[:])
                for s in range(4):
                    pv_ = self.pB()
                    self.MM(pv_, pv_[:], kvn, kvn[:, s * 128:(s + 1) * 128], wvv, wvv[:].rearrange("p h e -> p (h e)"),
                            True, True)
                    vt = r_vt()
                    self.CP(vt, vt[:, :, 0:64], pv_, pv_[:].rearrange("p (h e) -> p h e", e=64),
                            eng=(k.act if s % 2 else k.dve))
                    r0 = t * TT + s * 128
                    self.ST(self.v_d[r0:r0 + 128, :], self.r_v, vt, vt[:].rearrange("p h e -> p (h e)"))
        outer = self.phase()
        outer.__enter__()
        y_sb = self.T("y_sb", [128, 4, S], F32)
        clg = self.load_col("clg", W["conv_ln_g"][j], 4)
        clb = self.load_col("clb", W["conv_ln_b"][j], 4)
        with self.subphase():
            cw = self.T("cw", [128, 4, 31], F32)
            k.dma(k.sp, [(cw[:, c, :], W["conv_dw_w"][j].rearrange("k (c p) -> c p k", p=128)[c]) for c in range(4)],
                  [], [cw.r], allow_slow_non_contiguous=True)
            cb = self.load_col("cb", W["conv_dw_b"][j], 4)
            r_uu = self.ring("uu", [128, S + 30], F32, 2)
            conv_ops = []

            def mk_first(jj, u):
                return lambda: self.TS(y_sb, y_sb[:, jj, :], u, u[:, 0:S], cw[:, jj, 0:1], cb[:, jj:jj + 1], ALU.mult, ALU.add,
                                       extra=[cw, cb])

            def mk_tap(jj, u, tap):
                return lambda: self.STT(y_sb, y_sb[:, jj, :], u, u[:, tap:tap + S], cw[:, jj, tap:tap + 1], y_sb, y_sb[:, jj, :],
                                        ALU.mult, ALU.add, extra=[cw])

            def mk_load(jj, holder):
                def f():
                    u = r_uu()
                    self.LD(u, u[:], self.u_d[jj], self.r_u)
                    holder.append(u)
                return f
            holders = [[] for _ in range(4)]
            for jj in range(4):
                conv_ops.append(mk_load(jj, holders[jj]))
                conv_ops.append((lambda jj_: (lambda: mk_first(jj_, holders[jj_][0])()))(jj))
                for tap in range(1, 31):
                    conv_ops.append((lambda jj_, tap_: (lambda: mk_tap(jj_, holders[jj_][0], tap_)()))(jj, tap))
            conv_it = iter(conv_ops)

            def emit_conv(n):
                for _ in range(n):
                    f = next(conv_it, None)
                    if f is None:
                        return
                    f()
            emit_conv(2)
            r_K = self.ring("Kh", [96, S], BF16, 2)
            r_Q = self.ring("Qh", [96, S], BF16, 2)
            r_V = self.ring("Vh", [128, 32, 65], BF16, 2)
            r_pT = self.ring("pT", [128, TT], BF16, 4)
            r_rec = self.ring("rec", [128, 4, 1], F32, 2)
            r_at = self.ring("at", [128, 4, 64], BF16, 2)
            vdv = self.v_d.rearrange("(kc p) (h e) -> p kc h e", p=128, e=65)
            attv = self.att_d.rearrange("(b p) f -> p b f", p=128)
            heads = {}

            def get_head(h):
                if h not in heads:
                    Kh, Qh, Vh = r_K(), r_Q(), r_V()
                    k.dma(k.sp, [(Kh[0:64, :], self.kn_d[h]), (Kh[64:96, :], self.kpe_d)], [self.r_kn, self.r_kpe], [Kh.r])
                    self.LD(Qh, Qh[:], self.q_d[h], self.r_q)
                    self.LD(Vh, Vh[:], vdv[:, :, h, :], self.r_v)
                    heads[h] = (Kh, Qh, Vh)
                return heads[h]
            its = [(h, qt, kc) for h in range(8) for qt in range(NT) for kc in range(32)]
            LA = 2
            pss = {}

            def emit_qk(i):
                h, qt, kc = its[i]
                Kh, Qh, Vh = get_head(h)
                ps = self.pA()
                self.MM(ps, ps[:], Kh, Kh[:, kc * 128:(kc + 1) * 128], Qh, Qh[:, qt * TT:(qt + 1) * TT], True, True)
                pss[i] = ps
            for i in range(min(LA, len(its))):
                emit_qk(i)
            po = None
            for i, (h, qt, kc) in enumerate(its):
                if i + LA < len(its):
                    emit_qk(i + LA)
                Kh, Qh, Vh = heads[h]
                if kc == 0:
                    po = self.pB()
                pov = po[:, 0:260].rearrange("p (b e) -> p b e", e=65)
                ps = pss.pop(i)
                pT = r_pT()
                self.ACT(pT, pT[:], ps, ps[:], AF.Exp, scale=sl)
                for qb in range(4):
                    self.MM(po, pov[:, qb, :], pT, pT[:, qb * 128:(qb + 1) * 128], Vh, Vh[:, kc, :], kc == 0, kc == 31)
                if kc == 31:
                    rec = r_rec()
                    self.V(k.dve, (lambda rec_, pov_: (lambda: nc.vector.reciprocal(out=rec_[:], in_=pov_[:, :, 64:65])))(rec, pov),
                           [po], [rec])
                    at = r_at()
                    self.TT_(at, at[:], po, pov[:, :, 0:64], rec, rec[:].to_broadcast([128, 4, 64]), ALU.mult)
                    self.ST(attv[:, qt * 4:(qt + 1) * 4, 64 * h:64 * h + 64], self.r_att, at, at[:])
                    emit_conv(2)
            emit_conv(1000)
        if True:
            wout = self.load_w("wout", self.wb["ev_w_out"][j], 8, D, self.wb_r[("ev_w_out", j, 0)])
            g1, b1 = self.ln_consts("ln1_g", "ln1_b", layer)
            r_sq = self.ring("sq3", [128, TT], F32, 2)
            mean = self.T("mean", [128, TT], F32)
            m2 = self.T("m2", [128, TT], F32)
            rstd = self.T("rstd3", [128, TT], F32)
            r_z = self.ring("z3", [128, TT], F32, 2)
            r_mix = self.ring("mixT", [128, 8, TT], BF16, 2)
            r_att = self.ring("att_in", [128, 512], BF16, 2)
            for t in range(NT):
                cols = slice(t * TT, (t + 1) * TT)
                pss, psq = self.pB(), self.pB()
                for jj in range(4):
                    self.MM(pss, pss[:], self.ones32, self.ones32[:], y_sb, y_sb[:, jj, cols], jj == 0, jj == 3)
                for jj in range(4):
                    sq = r_sq()
                    self.ACT(sq, sq[:], y_sb, y_sb[:, jj, cols], AF.Square)
                    self.MM(psq, psq[:], self.ones32, self.ones32[:], sq, sq[:], jj == 0, jj == 3)
                self.V(k.act, lambda: nc.scalar.mul(out=mean[:], in_=pss[:], mul=1.0 / 512.0), [pss], [mean])
                self.TT_(m2, m2[:], mean, mean[:], mean, mean[:], ALU.mult)
                self.STT(m2, m2[:], psq, psq[:], 1.0 / 512.0, m2, m2[:], ALU.mult, ALU.subtract)
                self.rsqrt_(rstd, rstd[:], m2, m2[:], 1.0, self.eps_ln)
                mixT = r_mix()
                for jj in range(4):
                    z = r_z()
                    self.TT_(z, z[:], y_sb, y_sb[:, jj, cols], mean, mean[:], ALU.subtract)
                    self.TT_(z, z[:], z, z[:], rstd, rstd[:], ALU.mult)
                    self.ACT(mixT, mixT[:, jj, :], z, z[:], AF.Silu, bias=clb[:, jj:jj + 1], scale=clg[:, jj:jj + 1],
                             extra=[clb, clg])
                for s in range(4):
                    at = r_att()
                    r0 = t * TT + s * 128
                    self.LD(at, at[:], self.att_d[r0:r0 + 128, :], self.r_att)
                    for fc in range(4):
                        self.TR(self.ptr, self.ptr[:, fc * 128:(fc + 1) * 128], at, at[:, fc * 128:(fc + 1) * 128], self.ident16)
                    self.CP(mixT, mixT[:, 4:8, s * 128:(s + 1) * 128],
                            self.ptr, self.ptr[:, 0:512].rearrange("p (c s) -> p c s", c=4), eng=k.act)
                for sp in range(2):
                    items = []
                    for s in (2 * sp, 2 * sp + 1):
                        phs = [self.pA(), self.pA()]
                        for half in range(2):
                            for c in range(8):
                                self.MM(phs[half], phs[half][:], mixT, mixT[:, c, s * 128:(s + 1) * 128],
                                        wout, wout[:, c, half * 512:(half + 1) * 512], c == 0, c == 7)
                        items.append(([(phs[0], phs[0][:]), (phs[1], phs[1][:])], t * TT + s * 128))
                    self.epilogue_multi(items, g1, b1, None, "e3", dbg_idx=2 * layer, nch=2)
        outer.__exit__(None, None, None)

    def ffn_like(self, layer, j, moe, dst):
        nc, k = self.nc, self.k
        self.eps_tiles()
        W = self.w
        GC = 256
        pre = "moe" if moe else "ffn"
        nexp = NE if moe else 1
        dff = D_FFE if moe else D_FF
        nf = dff // 128
        ng = dff // GC
        with self.phase():
            g2, b2 = self.ln_consts("ln2_g", "ln2_b", layer)
            xTv = self.xT.rearrange("(c p) s -> p c s", p=128)
            r_xt = self.ring("xt", [128, 8, TT], BF16, 2)
            hT = self.T("hT", [128, nf, TT], BF16)
            r_wg = self.ring("wg", [128, 8, GC], BF16, 2)
            r_wu = self.ring("wu", [128, 8, GC], BF16, 2)
            r_wd = self.ring("wd", [128, nf, 512], BF16, 2)
            acc = self.T("acc", [128, 4, D], F32)
            r_sg = self.ring("sg", [128, TT], F32, 2)
            if moe:
                wr32 = self.T("wr32", [128, 8, NE], F32)
                k.dma(k.sp, [(wr32[:], W["moe_router_w"][j].rearrange("(c p) e -> p c e", p=128))], [], [wr32.r])
                rb = self.load_bcast("rb", W["moe_router_b"][j:j + 1, :], NE)
                r_x32 = self.ring("x32", [128, D], F32, 2)
                xT32 = self.T("xT32", [128, 8, 128], F32)
                comb = self.T("comb", [128, 4, NE], F32)
                lg = self.T("lg", [128, NE], F32)
                l2 = self.T("l2", [128, NE], F32)
                mk1 = self.T("mk1", [128, NE], F32)
                mk2 = self.T("mk2", [128, NE], F32)
                sm = self.T("sm", [128, 8], F32)
            for t in range(NT):
                xt = r_xt()
                self.LD(xt, xt[:], xTv[:, :, 2 + t * TT:2 + (t + 1) * TT], self.r_xT)
                if moe:
                    for s in range(4):
                        r0 = t * TT + s * 128
                        x32 = r_x32()
                        self.LD(x32, x32[:], self.xres[r0:r0 + 128, :], self.r_xres)
                        pts = [self.pB(), self.pB()]
                        for c in range(8):
                            pt = pts[c // 4]
                            self.TR(pt, pt[:, (c % 4) * 128:(c % 4 + 1) * 128], x32, x32[:, c * 128:(c + 1) * 128], self.ident32)
                        for hh in range(2):
                            self.CP(xT32, xT32[:, hh * 4:(hh + 1) * 4, :].rearrange("p c s -> p (c s)"), pts[hh], pts[hh][:],
                                    eng=(k.act if hh else k.dve))
                        pr = self.pB()
                        for c in range(8):
                            self.MM(pr, pr[:, 0:NE], xT32, xT32[:, c, :], wr32, wr32[:, c, :], c == 0, c == 7)
                        self.TT_(lg, lg[:], pr, pr[:, 0:NE], rb, rb[:], ALU.add)
                        self.V(k.dve, lambda: nc.vector.tensor_reduce(out=sm[:, 0:1], in_=lg[:], axis=AX.X, op=ALU.max), [lg], [sm])
                        self.TS(mk1, mk1[:], lg, lg[:], sm[:, 0:1], None, ALU.is_equal, extra=[sm])
                        self.STT(l2, l2[:], mk1, mk1[:], -1.0e30, lg, lg[:], ALU.mult, ALU.add)
                        self.V(k.dve, lambda: nc.vector.tensor_reduce(out=sm[:, 1:2], in_=l2[:], axis=AX.X, op=ALU.max), [l2], [sm])
                        self.TS(mk2, mk2[:], l2, l2[:], sm[:, 1:2], None, ALU.is_equal, extra=[sm])
                        self.TT_(sm, sm[:, 2:3], sm, sm[:, 1:2], sm, sm[:, 0:1], ALU.subtract)
                        self.ACT(sm, sm[:, 3:4], sm, sm[:, 2:3], AF.Exp)
                        self.TS(sm, sm[:, 4:5], sm, sm[:, 3:4], 1.0, None, ALU.add)
                        self.V(k.dve, lambda: nc.vector.reciprocal(out=sm[:, 5:6], in_=sm[:, 4:5]), [sm], [sm])
                        self.TT_(sm, sm[:, 6:7], sm, sm[:, 3:4], sm, sm[:, 5:6], ALU.mult)
                        self.TS(comb, comb[:, s, :], mk1, mk1[:], sm[:, 5:6], None, ALU.mult, extra=[sm])
                        self.STT(comb, comb[:, s, :], mk2, mk2[:], sm[:, 6:7], comb, comb[:, s, :], ALU.mult, ALU.add, extra=[sm])
                for e in range(nexp):
                    wg_src = self.wb["%s_w_gate" % pre][j, e]
                    wu_src = self.wb["%s_w_up" % pre][j, e]
                    wd_src = self.wb["%s_w_down" % pre][j, e]
                    rg = self.wb_r[("%s_w_gate" % pre, j, e)]
                    ru = self.wb_r[("%s_w_up" % pre, j, e)]
                    rd = self.wb_r[("%s_w_down" % pre, j, e)]
                    for g in range(ng):
                        wg, wu = r_wg(), r_wu()
                        self.LD(wg, wg[:], wg_src[g], rg)
                        self.LD(wu, wu[:], wu_src[g], ru)
                        for fi in range(GC // 128):
                            f = g * (GC // 128) + fi
                            pg, pu = self.pA(), self.pA()
                            for c in range(8):
                                self.MM(pg, pg[:], wg, wg[:, c, fi * 128:(fi + 1) * 128], xt, xt[:, c, :], c == 0, c == 7)
                            for c in range(8):
                                self.MM(pu, pu[:], wu, wu[:, c, fi * 128:(fi + 1) * 128], xt, xt[:, c, :], c == 0, c == 7)
                            sg = r_sg()
                            self.ACT(sg, sg[:], pg, pg[:], AF.Silu)
                            self.TT_(hT, hT[:, f, :], sg, sg[:], pu, pu[:], ALU.mult)
                    for half in range(2):
                        wd = r_wd()
                        self.LD(wd, wd[:], wd_src[half], rd)
                        for s in range(4):
                            po = self.pB()
                            for f in range(nf):
                                self.MM(po, po[:], hT, hT[:, f, s * 128:(s + 1) * 128], wd, wd[:, f, :], f == 0, f == nf - 1)
                            aap = acc[:, s, half * 512:(half + 1) * 512]
                            if not moe:
                                self.CP(acc, aap, po, po[:], eng=(k.act if s % 2 else k.dve))
                            elif e == 0:
                                self.TS(acc, aap, po, po[:], comb[:, s, e:e + 1], None, ALU.mult, extra=[comb])
                            else:
                                self.STT(acc, aap, po, po[:], comb[:, s, e:e + 1], acc, aap, ALU.mult, ALU.add, extra=[comb])
                for sp in range(2):
                    items = [([(acc, acc[:, s, 0:512]), (acc, acc[:, s, 512:1024])], t * TT + s * 128) for s in (2 * sp, 2 * sp + 1)]
                    self.epilogue_multi(items, g2, b2, dst, "ffn", dbg_idx=2 * layer + 1, nch=2)

    def moe_sparse(self, layer, j, dst):
        nc, k = self.nc, self.k
        self.eps_tiles()
        W = self.w
        GC = GCM
        nf = D_FFE // 128
        ng = D_FFE // GC
        if not hasattr(self, "xs_g"):
            self.xs_g = self.dscr("xs_g", [NTL * 512, D], BF16)
            self.r_xs_g = k.dres("xs_g", 4)
            self.ys_g = self.dscr("ys_g", [NTL * 512, D], F32)
            self.r_ys_g = k.dres("ys_g", 2)
            zt = self.TP("zrow16", [128, D], BF16)
            self.V(k.dve, lambda: nc.vector.memset(zt[:], 0.0), [], [zt])
            for i in range(NTL * 4):
                k.dma(k.sp, [(self.xs_g[i * 128:(i + 1) * 128, :], zt[:])], [zt.r], [self.r_xs_g], partial=True)
        rg = self.wb_r[("moe_w_gate", j, 0)]
        wbg2 = self.wb["moe_w_gate"].rearrange("j e g p c n -> (j e g p) (c n)")
        wbu2 = self.wb["moe_w_up"].rearrange("j e g p c n -> (j e g p) (c n)")
        wbd2 = self.wb["moe_w_down"].rearrange("j e h p f d -> (j e h p) (f d)")
        with self.phase():
            desti = self.T("desti", [128, 32, 2], I32)
            g12 = self.T("g12", [128, 32, 2], F32)
            widx = self.T("widx", [128, NTL, 9], I32)
            with self.subphase():
                wr32 = self.T("wr32", [128, 8, NE], F32)
                k.dma(k.sp, [(wr32[:], W["moe_router_w"][j].rearrange("(c p) e -> p c e", p=128))], [], [wr32.r])
                rb = self.load_bcast("rb", W["moe_router_b"][j:j + 1, :], NE)
                U = self.T("Uinc", [128, 128], F32)
                self.LD(U, U[:], self.c_tri[0])
                r_x32 = self.ring("x32", [128, D], F32, 3)
                xT32 = self.T("xT32", [128, 8, 128], F32)
                lg = self.T("lg", [128, NE], F32)
                l2 = self.T("l2", [128, NE], F32)
                sm = self.T("sm", [128, 8], F32)
                m1all = self.T("m1all", [128, 32, NE], F32)
                m2all = self.T("m2all", [128, 32, NE], F32)
                wdesc = self.T("wdesc", [128, NE], F32)
                for e in range(NE):
                    self.V(k.dve, (lambda e_: (lambda: nc.vector.memset(wdesc[:, e_:e_ + 1], float(NE - e_))))(e), [], [wdesc])
                tsc = self.T("tsc", [128, NE], F32)

                def onehot_first(mt, map_):
                    self.TT_(tsc, tsc[:], mt, map_, wdesc, wdesc[:], ALU.mult)
                    self.V(k.dve, lambda: nc.vector.tensor_reduce(out=sm[:, 7:8], in_=tsc[:], axis=AX.X, op=ALU.max), [tsc], [sm])
                    self.TS(mt, map_, tsc, tsc[:], sm[:, 7:8], None, ALU.is_equal, extra=[sm])
                for st in range(32):
                    r0 = st * 128
                    x32 = r_x32()
                    self.LD(x32, x32[:], self.xres[r0:r0 + 128, :], self.r_xres)
                    pts = [self.pB(), self.pB()]
                    for c in range(8):
                        pt = pts[c // 4]
                        self.TR(pt, pt[:, (c % 4) * 128:(c % 4 + 1) * 128], x32, x32[:, c * 128:(c + 1) * 128], self.ident32)
                    for hh in range(2):
                        self.CP(xT32, xT32[:, hh * 4:(hh + 1) * 4, :].rearrange("p c s -> p (c s)"), pts[hh], pts[hh][:],
                                eng=(k.act if hh else k.dve))
                    pr = self.pB()
                    for c in range(8):
                        self.MM(pr, pr[:, 0:NE], xT32, xT32[:, c, :], wr32, wr32[:, c, :], c == 0, c == 7)
                    self.TT_(lg, lg[:], pr, pr[:, 0:NE], rb, rb[:], ALU.add)
                    self.V(k.dve, lambda: nc.vector.tensor_reduce(out=sm[:, 0:1], in_=lg[:], axis=AX.X, op=ALU.max), [lg], [sm])
                    self.TS(m1all, m1all[:, st, :], lg, lg[:], sm[:, 0:1], None, ALU.is_equal, extra=[sm])
                    onehot_first(m1all, m1all[:, st, :])
                    self.STT(l2, l2[:], m1all, m1all[:, st, :], -1.0e30, lg, lg[:], ALU.mult, ALU.add)
                    self.V(k.dve, lambda: nc.vector.tensor_reduce(out=sm[:, 1:2], in_=l2[:], axis=AX.X, op=ALU.max), [l2], [sm])
                    self.TS(m2all, m2all[:, st, :], l2, l2[:], sm[:, 1:2], None, ALU.is_equal, extra=[sm])
                    onehot_first(m2all, m2all[:, st, :])
                    self.TT_(sm, sm[:, 2:3], sm, sm[:, 1:2], sm, sm[:, 0:1], ALU.subtract)
                    self.ACT(sm, sm[:, 3:4], sm, sm[:, 2:3], AF.Exp)
                    self.TS(sm, sm[:, 4:5], sm, sm[:, 3:4], 1.0, None, ALU.add)
                    self.V(k.dve, lambda: nc.vector.reciprocal(out=g12[:, st, 0:1], in_=sm[:, 4:5]), [sm], [g12])
                    self.TT_(g12, g12[:, st, 1:2], sm, sm[:, 3:4], g12, g12[:, st, 0:1], ALU.mult)
                sel = self.T("sel", [128, 32, NE], F32)
                excl = self.T("excl", [128, 32, NE], F32)
                tot = self.T("tot", [128, 32, NE], F32)
                pre = self.T("pre", [128, 32, NE], F32)
                flat = lambda t_: t_[:].rearrange("p s e -> p (s e)")
                self.TT_(sel, sel[:], m1all, m1all[:], m2all, m2all[:], ALU.add)
                pinc, ptot = self.pB(), self.pB()
                self.MM(pinc, pinc[:, 0:256], U, U[:], sel, flat(sel), True, True)
                self.MM(ptot, ptot[:, 0:256], self.ones32, self.ones32[:], sel, flat(sel), True, True)
                self.TT_(excl, flat(excl), pinc, pinc[:, 0:256], sel, flat(sel), ALU.subtract)
                self.CP(tot, flat(tot), ptot, ptot[:, 0:256])
                self.V(k.dve, lambda: nc.vector.memset(pre[:, 0, :], 0.0), [], [pre])
                for st in range(1, 32):
                    self.TT_(pre, pre[:, st, :], pre, pre[:, st - 1, :], tot, tot[:, st - 1, :], ALU.add)
                cnt = self.T("cnt", [128, NE], F32)
                self.TT_(cnt, cnt[:], pre, pre[:, 31, :], tot, tot[:, 31, :], ALU.add)
                cmpk = self.T("cmpk", [128, 8, NE], F32)
                for kk in range(8):
                    self.TS(cmpk, cmpk[:, kk, :], cnt, cnt[:], 512.0 * kk, None, ALU.is_gt)
                padded = self.T("padded", [128, NE], F32)
                self.V(k.dve, lambda: nc.vector.tensor_reduce(out=padded[:], in_=cmpk[:].rearrange("p k e -> p e k"),
                                                              axis=AX.X, op=ALU.add), [cmpk], [padded])
                self.TS(padded, padded[:], padded, padded[:], 512.0, None, ALU.mult)
                base = self.T("base", [128, NE], F32)
                self.V(k.dve, lambda: nc.vector.memset(base[:, 0:1], 0.0), [], [base])
                for e in range(1, NE):
                    self.TT_(base, base[:, e:e + 1], base, base[:, e - 1:e], padded, padded[:, e - 1:e], ALU.add)
                cumend = self.T("cumend", [128, NE], F32)
                self.TT_(cumend, cumend[:], base, base[:], padded, padded[:], ALU.add)
                self.TT_(excl, excl[:], excl, excl[:], pre, pre[:], ALU.add)
                self.TT_(excl, excl[:], excl, excl[:], base, base[:].unsqueeze(1).to_broadcast([128, 32, NE]), ALU.add)
                destf = self.T("destf", [128, 32, 2], F32)
                for r, mall in ((0, m1all), (1, m2all)):
                    self.TT_(tot, tot[:], mall, mall[:], excl, excl[:], ALU.mult)
                    self.V(k.dve, (lambda r_: (lambda: nc.vector.tensor_reduce(out=destf[:, :, r_], in_=tot[:], axis=AX.X, op=ALU.add)))(r),
                           [tot], [destf])
                self.CP(desti, desti[:], destf, destf[:])
                cmpt = self.T("cmpt", [128, NTL, NE], F32)
                for t in range(NTL):
                    self.TS(cmpt, cmpt[:, t, :], cumend, cumend[:], 512.0 * t, None, ALU.is_le)
                etf = self.T("etf", [128, NTL], F32)
                self.V(k.dve, lambda: nc.vector.tensor_reduce(out=etf[:], in_=cmpt[:], axis=AX.X, op=ALU.add), [cmpt], [etf])
                self.TS(etf, etf[:], etf, etf[:], float(NE - 1), 0.0, ALU.min, ALU.max)
                self.TS(etf, etf[:], etf, etf[:], float(j * NE), None, ALU.add)
                pg = self.T("pg", [128, 9], F32)
                self.LD(pg, pg[:], self.c_pg)
                widf = self.T("widf", [128, NTL, 9], F32)
                for g in range(9):
                    self.TS(widf, widf[:, :, g], etf, etf[:], float(ng * 128 if g < 7 else 2 * 128), pg[:, g:g + 1],
                            ALU.mult, ALU.add, extra=[pg])
                self.CP(widx, widx[:], widf, widf[:])
                for st in range(32):
                    r0 = st * 128
                    x32 = r_x32()
                    self.LD(x32, x32[:], self.xres[r0:r0 + 128, :], self.r_xres)
                    for r in range(2):
                        k.idma((lambda x_, st_, r_: (lambda: nc.gpsimd.indirect_dma_start(
                            out=self.xs_g[:, :], out_offset=bass.IndirectOffsetOnAxis(ap=desti[:, st_, r_:r_ + 1], axis=0),
                            in_=x_[:, :], in_offset=None)))(x32, st, r),
                            [x32.r, desti.r], [self.r_xs_g], partial=True)
            with self.subphase():
                r_xg = self.ring("xg", [128, 4, D], BF16, 2)
                r_xt = self.ring("xtg", [128, 8, TT], BF16, 2)
                hT = self.T("hTg", [128, nf, TT], BF16)
                r_wg = self.ring("wgg", [128, 8, GC], BF16, 2)
                r_wu = self.ring("wug", [128, 8, GC], BF16, 2)
                r_wd = self.ring("wdg", [128, nf, 512], BF16, 2)
                r_sg = self.ring("sgg", [128, TT], F32, 2)
                r_ysb = self.ring("ysb", [128, 4, D], F32, 1)
                for t in range(NTL):
                    def wgather(dst_t, src2d, col):
                        k.idma((lambda d_, c_: (lambda: nc.gpsimd.indirect_dma_start(
                            out=d_, out_offset=None, in_=src2d,
                            in_offset=bass.IndirectOffsetOnAxis(ap=widx[:, t, c_:c_ + 1], axis=0))))(dst_t[:].rearrange(
                                "p a b -> p (a b)"), col), [rg, widx.r], [dst_t.r])
                    xg = r_xg()
                    self.LD(xg, xg[:], self.xs_g[t * 512:(t + 1) * 512, :].rearrange("(s p) d -> p s d", p=128), self.r_xs_g)
                    xt = r_xt()
                    for s in range(4):
                        for c in range(8):
                            self.TR(self.ptr, self.ptr[:, c * 128:(c + 1) * 128], xg, xg[:, s, c * 128:(c + 1) * 128], self.ident16)
                        self.CP(xt, xt[:, :, s * 128:(s + 1) * 128], self.ptr, self.ptr[:].rearrange("p (c s) -> p c s", c=8),
                                eng=(k.act if s % 2 else k.dve))
                    for g in range(ng):
                        wg, wu = r_wg(), r_wu()
                        wgather(wg, wbg2, g)
                        wgather(wu, wbu2, g)
                        for fi in range(GC // 128):
                            f = g * (GC // 128) + fi
                            pg, pu = self.pA(), self.pA()
                            for c in range(8):
                                self.MM(pg, pg[:], wg, wg[:, c, fi * 128:(fi + 1) * 128], xt, xt[:, c, :], c == 0, c == 7)
                            for c in range(8):
                                self.MM(pu, pu[:], wu, wu[:, c, fi * 128:(fi + 1) * 128], xt, xt[:, c, :], c == 0, c == 7)
                            sg = r_sg()
                            self.ACT(sg, sg[:], pg, pg[:], AF.Silu)
                            self.TT_(hT, hT[:, f, :], sg, sg[:], pu, pu[:], ALU.mult)
                    ysb = r_ysb()
                    for half in range(2):
                        wd = r_wd()
                        wgather(wd, wbd2, 7 + half)
                        for s in range(4):
                            po = self.pB()
                            for f in range(nf):
                                self.MM(po, po[:], hT, hT[:, f, s * 128:(s + 1) * 128], wd, wd[:, f, :], f == 0, f == nf - 1)
                            self.CP(ysb, ysb[:, s, half * 512:(half + 1) * 512], po, po[:], eng=(k.act if s % 2 else k.dve))
                    self.ST(self.ys_g[t * 512:(t + 1) * 512, :].rearrange("(s p) d -> p s d", p=128), self.r_ys_g, ysb, ysb[:])
            with self.subphase():
                g2, b2 = self.ln_consts("ln2_g", "ln2_b", layer)
                r_ga = self.ring("ga", [128, D], F32, 12)
                r_f = self.ring("fmo", [128, D], F32, 8)
                NB = 4
                for sb in range(32 // NB):
                    items = []
                    for st in range(sb * NB, (sb + 1) * NB):
                        gas = []
                        for r in range(2):
                            ga = r_ga()
                            k.idma((lambda g_, st_, r_: (lambda: nc.gpsimd.indirect_dma_start(
                                out=g_[:, :], out_offset=None, in_=self.ys_g[:, :],
                                in_offset=bass.IndirectOffsetOnAxis(ap=desti[:, st_, r_:r_ + 1], axis=0))))(ga, st, r),
                                [self.r_ys_g, desti.r], [ga.r])
                            gas.append(ga)
                        f = r_f()
                        self.TS(f, f[:], gas[0], gas[0][:], g12[:, st, 0:1], None, ALU.mult, extra=[g12])
                        self.STT(f, f[:], gas[1], gas[1][:], g12[:, st, 1:2], f, f[:], ALU.mult, ALU.add, extra=[g12])
                        items.append(([(f, f[:, 0:512]), (f, f[:, 512:1024])], st * 128))
                    self.epilogue_multi(items, g2, b2, dst, "moe", dbg_idx=2 * layer + 1, nch=NB)

    def odd_mixer(self, j, layer):
        nc, k = self.nc, self.k
        self.eps_tiles()
        W = self.w
        if not hasattr(self, "z_d"):
            def mk(name, shape, dt):
                setattr(self, name, self.dscr(name, shape, dt))
                setattr(self, "r_" + name, k.dres(name, 2))
            mk("z_d", [S, 512], F32); mk("xs_d", [S, 512], F32); mk("bmt_d", [S, 256], BF16)
            mk("bcT_d", [4, 128, S], BF16); mk("dtda_d", [S, 32], F32); mk("qT_d", [8, 64, S], BF16)
            mk("kT_d", [8, 64, S], BF16); mk("kt_d", [S, 512], BF16); mk("v2_d", [S, 8 * 65], BF16)
            mk("og_d", [S, 512], F32); mk("gate_d", [S, 32], F32); mk("yf_d", [S, 512], F32)
            mk("hf_d", [S, 512], F32); mk("wcf_d", [5, D, 1024], BF16)
        wi_r = self.wb_r[("od_w_in", j, 0)]
        wi = self.wb["od_w_in"][j]
        with self.phase():
            cwb = [self.load_bcast("cwb%d" % tap, W["ssd_conv_w"][j, tap:tap + 1, :], 1024) for tap in range(5)]
            r_wr = self.ring("wrow", [128, 1024], F32, 2)
            r_wo = self.ring("wcfo", [128, 1024], BF16, 3)
            for rc in range(8):
                wr_ = r_wr()
                self.LD(wr_, wr_[:], W["od_w_in"][j, rc * 128:(rc + 1) * 128, 512:1536])
                for tap in range(5):
                    wo = r_wo()
                    self.TT_(wo, wo[:], wr_, wr_[:], cwb[tap], cwb[tap][:], ALU.mult)
                    self.ST(self.wcf_d[tap, rc * 128:(rc + 1) * 128, :], self.r_wcf_d, wo, wo[:])
        with self.phase():
            xTs = self.load_xT_full()
            cbias = self.load_bcast("cbias", W["ssd_conv_b"][j:j + 1, :], 1024)
            cbcol = self.load_col("cbcol", W["ssd_conv_b"][j], 8)
            dtb = self.load_bcast("dtb", W["ssd_dt_bias"][j:j + 1, :], 16)
            alog = self.load_bcast("alog", W["ssd_a_log"][j:j + 1, :], 16)
            igb = self.load_bcast("igb", W["ml_igate_b"][j:j + 1, :], 16)
            fgb = self.load_bcast("fgb", W["ml_fgate_b"][j:j + 1, :], 16)
            abc = self.T("abc", [128, 16], F32)
            self.ACT(abc, abc[:], alog, alog[:], AF.Exp)
            self.TS(abc, abc[:], abc, abc[:], -1.0, None, ALU.mult)
            wiv = wi.rearrange("(c p) n -> p c n", p=128)
            wcv = self.wcf_d.rearrange("t (c p) n -> p t c n", p=128)
            r_o32 = self.ring("o32", [128, 512], F32, 3)
            r_o16 = self.ring("o16", [128, 512], BF16, 3)
            r_vt = self.ring("vt2", [128, 8, 65], BF16, 3, init=1.0)
            r_sm = self.ring("smo", [128, 64], F32, 3)

            def tok_group(wt, ncols, conv, post):
                for st in range(S // 128):
                    t0 = st * 128
                    ps = self.pA()
                    if conv:
                        n = 0
                        for tap in range(5):
                            for c in range(8):
                                self.MM(ps, ps[:, 0:ncols], xTs, xTs[:, c, t0 + tap:t0 + tap + 128], wt, wt[:, tap, c, :],
                                        n == 0, n == 39)
                                n += 1
                    else:
                        for c in range(8):
                            self.MM(ps, ps[:, 0:ncols], xTs, xTs[:, c, 2 + t0:2 + t0 + 128], wt, wt[:, c, :], c == 0, c == 7)
                    post(ps, t0, st)

            def load_plain(c0, ncols, name):
                t = self.T(name, [128, 8, ncols], BF16)
                k.dma(k.sp, [(t[:], wiv[:, :, c0:c0 + ncols])], [wi_r], [t.r])
                return t

            def load_conv(c0, ncols, name):
                t = self.T(name, [128, 5, 8, ncols], BF16)
                k.dma(k.sp, [(t[:, tap, :, :], wcv[:, tap, :, c0:c0 + ncols]) for tap in range(5)], [self.r_wcf_d], [t.r])
                return t

            def post_z(ps, t0, st):
                o = r_o32()
                self.ACT(o, o[:], ps, ps[:], AF.Silu)
                self.ST(self.z_d[t0:t0 + 128, :], self.r_z_d, o, o[:])
            sub = self.subphase()
            sub.__enter__()
            tok_group(load_plain(0, 512, "w_z"), 512, False, post_z)

            def post_o(ps, t0, st):
                o = r_o32()
                self.ACT(o, o[:], ps, ps[:], AF.Sigmoid)
                self.ST(self.og_d[t0:t0 + 128, :], self.r_og_d, o, o[:])
            tok_group(load_plain(3088, 512, "w_o"), 512, False, post_o)

            def post_k(ps, t0, st):
                o = r_o16()
                self.V(k.act, lambda: nc.scalar.mul(out=o[:], in_=ps[:], mul=0.125), [ps], [o])
                self.ST(self.kt_d[t0:t0 + 128, :], self.r_kt_d, o, o[:])
            tok_group(load_plain(2064, 512, "w_k"), 512, False, post_k)

            def post_v(ps, t0, st):
                vt = r_vt()
                self.CP(vt, vt[:, :, 0:64], ps, ps[:].rearrange("p (h e) -> p h e", e=64), eng=(k.act if st % 2 else k.dve))
                self.ST(self.v2_d[t0:t0 + 128, :], self.r_v2_d, vt, vt[:].rearrange("p h e -> p (h e)"))
            tok_group(load_plain(2576, 512, "w_v"), 512, False, post_v)
            sub.__exit__(None, None, None)

            def post_xs(ps, t0, st):
                o = r_o32()
                self.TT_(o, o[:], ps, ps[:], cbias, cbias[:, 0:512], ALU.add)
                self.ACT(o, o[:], o, o[:], AF.Silu)
                self.ST(self.xs_d[t0:t0 + 128, :], self.r_xs_d, o, o[:])
            sub = self.subphase()
            sub.__enter__()
            tok_group(load_conv(0, 512, "w_xs"), 512, True, post_xs)

            def post_bm(ps, t0, st):
                o = r_o32()
                self.TT_(o, o[:, 0:256], ps, ps[:, 0:256], cbias, cbias[:, 512:768], ALU.add)
                o2 = r_o16()
                self.ACT(o2, o2[:, 0:256], o, o[:, 0:256], AF.Silu)
                self.ST(self.bmt_d[t0:t0 + 128, :], self.r_bmt_d, o2, o2[:, 0:256])
            tok_group(load_conv(512, 256, "w_bm"), 256, True, post_bm)

            def post_small(ps, t0, st):
                sm = r_sm()
                self.TT_(sm, sm[:, 0:16], ps, ps[:, 0:16], dtb, dtb[:], ALU.add)
                self.ACT(sm, sm[:, 0:16], sm, sm[:, 0:16], AF.Exp)
                self.ACT(sm, sm[:, 0:16], sm, sm[:, 0:16], AF.Ln, bias=1.0, scale=1.0)
                self.TT_(sm, sm[:, 16:32], sm, sm[:, 0:16], abc, abc[:], ALU.mult)
                self.ST(self.dtda_d[t0:t0 + 128, :], self.r_dtda_d, sm, sm[:, 0:32])
                sm2 = r_sm()
                self.TT_(sm2, sm2[:, 0:16], ps, ps[:, 16:32], igb, igb[:], ALU.add)
                self.TT_(sm2, sm2[:, 16:32], ps, ps[:, 32:48], fgb, fgb[:], ALU.add)
                self.ACT(sm2, sm2[:, 16:32], sm2, sm2[:, 16:32], AF.Exp, scale=-1.0)
                self.ACT(sm2, sm2[:, 16:32], sm2, sm2[:, 16:32], AF.Ln, bias=1.0, scale=1.0)
                self.TS(sm2, sm2[:, 16:32], sm2, sm2[:, 16:32], -1.0, None, ALU.mult)
                self.ST(self.gate_d[t0:t0 + 128, :], self.r_gate_d, sm2, sm2[:, 0:32])
            wsm = self.T("w_sm", [128, 8, 48], BF16)
            k.dma(k.sp, [(wsm[:, :, 0:16], wiv[:, :, 1536:1552]), (wsm[:, :, 16:48], wiv[:, :, 3600:3632])], [wi_r], [wsm.r])
            tok_group(wsm, 48, False, post_small)
            sub.__exit__(None, None, None)

            r_f16 = self.ring("f16", [128, TT], BF16, 3)
            with self.subphase():
                wbc = load_conv(512, 512, "w_bcT")
                for t in range(NT):
                    for i in range(4):
                        ps = self.pA()
                        n = 0
                        for tap in range(5):
                            for c in range(8):
                                self.MM(ps, ps[:], wbc, wbc[:, tap, c, i * 128:(i + 1) * 128],
                                        xTs, xTs[:, c, t * TT + tap:t * TT + tap + TT], n == 0, n == 39)
                                n += 1
                        o = r_f16()
                        self.ACT(o, o[:], ps, ps[:], AF.Silu, bias=cbcol[:, 4 + i:5 + i], scale=1.0, extra=[cbcol])
                        self.ST(self.bcT_d[i][:, t * TT:(t + 1) * TT], self.r_bcT_d, o, o[:])
            for (c0, dst, rdst, scl, nm) in ((1552, self.qT_d, self.r_qT_d, 1.0, "w_qT"), (2064, self.kT_d, self.r_kT_d, 0.125, "w_kT")):
                with self.subphase():
                    wq = load_plain(c0, 512, nm)
                    for t in range(NT):
                        for h in range(8):
                            ps = self.pA()
                            for c in range(8):
                                self.MM(ps, ps[0:64, :], wq, wq[:, c, h * 64:(h + 1) * 64], xTs, xTs[:, c, 2 + t * TT:2 + (t + 1) * TT],
                                        c == 0, c == 7)
                            o = r_f16()
                            self.V(k.act, (lambda o_, ps_: (lambda: nc.scalar.mul(out=o_[0:64, :], in_=ps_[0:64, :], mul=scl)))(o, ps), [ps], [o])
                            self.ST(dst[h][:, t * TT:(t + 1) * TT], rdst, o, o[0:64, :])
        for direction in (0, 1):
            with self.phase():
                self.scan_pass(j, layer, direction)

    def subphase(self):
        prog = self

        class _S:
            def __enter__(s):
                s.outer = prog.ph
                s.st = ExitStack()
                s.st.__enter__()
                prog.ph = s.st
                return s

            def __exit__(s, *a):
                if a[0] is None:
                    prog.k.barrier(closing=s.st)
                s.st.__exit__(*a)
                prog.ph = s.outer
                return False
        return _S()

    def scan_pass(self, j, layer, dr):
        nc, k = self.nc, self.k
        W = self.w
        last = (dr == 1)
        U = self.T("Udir", [128, 128], F32)
        self.LD(U, U[:], self.c_tri[dr])
        mask = self.T("mdir", [128, 128], F32)
        self.LD(mask, mask[:], self.c_tri[2 + dr])
        Sst = self.T("Sst", [128, 8, 64], F32)
        S16 = self.T("S16", [128, 8, 64], BF16)
        Cst = self.T("Cst", [64, 8, 65], F32)
        C16 = self.T("C16", [64, 8, 65], BF16)
        for t_ in (Sst, S16, Cst, C16):
            self.V(k.dve, (lambda tt: (lambda: nc.vector.memset(tt[:], 0.0)))(t_), [], [t_])
        R = self.ring
        r_xs = R("xs", [128, 512], F32, 2); r_dtda = R("dtda", [128, 32], F32, 2); r_bmt = R("bmt", [128, 256], BF16, 2)
        r_bcT = R("bcT", [128, 4, 128], BF16, 2); r_qT = R("qT", [64, 8, 128], BF16, 2); r_kT = R("kT", [64, 8, 128], BF16, 2)
        r_kt = R("kt", [128, 512], BF16, 2); r_v2 = R("v2", [128, 8, 65], BF16, 2); r_gate = R("gate", [128, 32], F32, 2)
        r_sc = R("sc", [128, 32], F32, 2); r_wall = R("wall", [128, 8, 128], F32, 2); r_tR = R("tR", [128, 8, 128], F32, 2)
        r_eD = R("eD", [128, 8, 128], F32, 2); r_cb = R("cb", [128, 2, 128], F32, 2); r_MT = R("MT", [128, 8, 128], BF16, 2)
        r_ex = R("ex", [128, 64], F32, 2); r_xdt = R("xdt", [128, 8, 64], BF16, 2); r_xdtd = R("xdtd", [128, 8, 64], BF16, 2)
        r_y = R("y", [128, 512], F32, 2); r_aT = R("aT", [128, 8, 128], BF16, 2); r_tot = R("tot", [128, 8, 65], F32, 2)
        r_hd = R("hd", [128, 8, 64], F32, 2); r_kd = R("kd", [128, 8, 64], BF16, 2)
        if last:
            r_yf = R("yf", [128, 512], F32, 2); r_hf = R("hf", [128, 512], F32, 2); r_zs = R("zs", [128, 512], F32, 2)
            r_og = R("og", [128, 512], F32, 2); r_mix = R("mix16", [128, D], BF16, 2); r_mixT = R("mixT2", [128, 8, 128], BF16, 2)
            r_tmp = R("tmp5", [128, 512], F32, 2)
            dsk = self.load_bcast("dsk", W["ssd_d"][j:j + 1, :], 8)
            ssdg = self.load_bcast("ssdg", W["ssd_norm_g"][j:j + 1, :], 512)
            mlg = self.load_bcast("mlg", W["ml_norm_g"][j:j + 1, :], 512)
            wout = self.load_w("wout2", self.wb["od_w_out"][j], 8, D, self.wb_r[("od_w_out", j, 0)])
            g1, b1 = self.ln_consts("ln1_g", "ln1_b", layer)
        bcTv = self.bcT_d.rearrange("i p s -> p i s")
        qTv = self.qT_d.rearrange("h p s -> p h s")
        kTv = self.kT_d.rearrange("h p s -> p h s")

        def bc3(ap2, n):
            return ap2.unsqueeze(2).to_broadcast([ap2.shape[0], ap2.shape[1], n])

        chunks = range(32) if dr == 0 else range(31, -1, -1)
        for c in chunks:
            r0 = c * 128
            rows = slice(r0, r0 + 128)
            xs, dtda, bmt, bcT, qT, kT, kt, v2, gate = r_xs(), r_dtda(), r_bmt(), r_bcT(), r_qT(), r_kT(), r_kt(), r_v2(), r_gate()
            self.LD(xs, xs[:], self.xs_d[rows, :], self.r_xs_d)
            self.LD(dtda, dtda[:], self.dtda_d[rows, :], self.r_dtda_d)
            self.LD(bmt, bmt[:], self.bmt_d[rows, :], self.r_bmt_d)
            self.LD(bcT, bcT[:], bcTv[:, :, rows], self.r_bcT_d)
            self.LD(qT, qT[:], qTv[:, :, rows], self.r_qT_d)
            self.LD(kT, kT[:], kTv[:, :, rows], self.r_kT_d)
            self.LD(kt, kt[:], self.kt_d[rows, :], self.r_kt_d)
            self.LD(v2, v2[:].rearrange("p h e -> p (h e)"), self.v2_d[rows, :], self.r_v2_d)
            self.LD(gate, gate[:], self.gate_d[rows, :], self.r_gate_d)
            dtd = dtda[:, dr * 8:dr * 8 + 8]
            dad = dtda[:, 16 + dr * 8:16 + dr * 8 + 8]
            lid = gate[:, dr * 8:dr * 8 + 8]
            lfd = gate[:, 16 + dr * 8:16 + dr * 8 + 8]
            pcol = self.pB()
            self.MM(pcol, pcol[:, 0:8], U, U[:], dtda, dad, True, True)
            self.MM(pcol, pcol[:, 8:16], self.ones32, self.ones32[:], dtda, dad, True, True)
            self.MM(pcol, pcol[:, 16:24], U, U[:], gate, lfd, True, True)
            self.MM(pcol, pcol[:, 24:32], self.ones32, self.ones32[:], gate, lfd, True, True)
            sc = r_sc()
            self.CP(sc, sc[:], pcol, pcol[:, 0:32])
            ex = r_ex()
            self.ACT(ex, ex[:, 0:16], sc, sc[:, 0:16], AF.Exp)
            self.TT_(ex, ex[:, 16:24], sc, sc[:, 8:16], sc, sc[:, 0:8], ALU.subtract)
            self.ACT(ex, ex[:, 16:24], ex, ex[:, 16:24], AF.Exp)
            self.TT_(ex, ex[:, 24:32], dtda, dtd, ex, ex[:, 16:24], ALU.mult)
            self.ACT(ex, ex[:, 32:48], sc, sc[:, 16:32], AF.Exp)
            self.TT_(ex, ex[:, 48:56], sc, sc[:, 24:32], sc, sc[:, 16:24], ALU.subtract)
            self.TT_(ex, ex[:, 48:56], ex, ex[:, 48:56], gate, lid, ALU.add)
            self.ACT(ex, ex[:, 48:56], ex, ex[:, 48:56], AF.Exp)
            self.TT_(ex, ex[:, 56:64], gate, lid, sc, sc[:, 16:24], ALU.subtract)

            def decay_mat(src_t, src_ap, shift_t, shift_ap, sign):
                wall = r_wall()
                self.TT_(wall, wall[:], U, U[:].unsqueeze(1).to_broadcast([128, 8, 128]), src_t, bc3(src_ap, 128), ALU.mult,
                         eng=k.pool)
                pR = [self.pA(), self.pA()]
                for hb in range(2):
                    self.MM(pR[hb], pR[hb][:], self.ones32, self.ones32[:], wall,
                            wall[:, hb * 4:(hb + 1) * 4, :].rearrange("p h l -> p (h l)"), True, True)
                tR = r_tR()
                for hb in range(2):
                    self.TT_(tR, tR[:, hb * 4:(hb + 1) * 4, :], pR[hb], pR[hb][:].rearrange("p (h l) -> p h l", h=4),
                             mask, mask[:].unsqueeze(1).to_broadcast([128, 4, 128]), ALU.add)
                self.TT_(tR, tR[:], tR, tR[:], shift_t, bc3(shift_ap, 128), ALU.subtract if sign < 0 else ALU.add)
                eD = r_eD()
                self.ACT(eD, eD[:], tR, tR[:], AF.Exp)
                return eD
            eD = decay_mat(dtda, dad, sc, sc[:, 0:8], -1)
            pcb = self.pB()
            for g in range(2):
                self.MM(pcb, pcb[:, g * 128:(g + 1) * 128], bcT, bcT[:, g, :], bcT, bcT[:, 2 + g, :], True, True)
            cb = r_cb()
            self.CP(cb, cb[:].rearrange("p g l -> p (g l)"), pcb, pcb[:, 0:256], eng=k.act)
            MT = r_MT()
            self.TT_(MT, MT[:].rearrange("p (g r) l -> p g r l", g=2), eD, eD[:].rearrange("p (g r) l -> p g r l", g=2),
                     cb, cb[:].unsqueeze(2).to_broadcast([128, 2, 4, 128]), ALU.mult)
            xv = xs[:].rearrange("p (h e) -> p h e", e=64)
            xdt, xdtd = r_xdt(), r_xdtd()
            self.TT_(xdt, xdt[:], xs, xv, dtda, bc3(dtd, 64), ALU.mult, eng=k.pool)
            self.TT_(xdtd, xdtd[:], xs, xv, ex, bc3(ex[:, 24:32], 64), ALU.mult, eng=k.pool)
            pyd, pyo = self.pA(), self.pA()
            for h in range(8):
                self.MM(pyd, pyd[:, h * 64:(h + 1) * 64], MT, MT[:, h, :], xdt, xdt[:, h, :], True, True)
            for h in range(8):
                self.MM(pyo, pyo[:, h * 64:(h + 1) * 64], bcT, bcT[:, 2 + h // 4, :], S16, S16[:, h, :], True, True)
            y = r_y()
            yv = y[:].rearrange("p (h e) -> p h e", e=64)
            self.TT_(y, yv, pyo, pyo[:].rearrange("p (h e) -> p h e", e=64), ex, bc3(ex[:, 0:8], 64), ALU.mult)
            self.TT_(y, y[:], y, y[:], pyd, pyd[:], ALU.add)
            pst = self.pB()
            for h in range(8):
                self.MM(pst, pst[:, h * 64:(h + 1) * 64], bmt, bmt[:, (h // 4) * 128:(h // 4 + 1) * 128], xdtd, xdtd[:, h, :], True, True)
            self.TT_(Sst, Sst[:], Sst, Sst[:], ex, bc3(ex[:, 8:16], 64), ALU.mult)
            self.TT_(Sst, Sst[:], Sst, Sst[:], pst, pst[:].rearrange("p (h e) -> p h e", e=64), ALU.add)
            self.CP(S16, S16[:], Sst, Sst[:], eng=k.act)
            wT = decay_mat(gate, lfd, ex, ex[:, 56:64], +1)
            pqk = [self.pA(), self.pA()]
            for h in range(8):
                self.MM(pqk[h // 4], pqk[h // 4][:, (h % 4) * 128:(h % 4 + 1) * 128], kT, kT[:, h, :], qT, qT[:, h, :], True, True)
            aT = r_aT()
            for hb in range(2):
                self.TT_(aT, aT[:, hb * 4:(hb + 1) * 4, :], pqk[hb], pqk[hb][:].rearrange("p (h l) -> p h l", h=4),
                         wT, wT[:, hb * 4:(hb + 1) * 4, :], ALU.mult)
            pn = [self.pA(), self.pA()]
            pi = [self.pA(), self.pA()]
            for h in range(8):
                self.MM(pn[h // 4], pn[h // 4][:, (h % 4) * 65:(h % 4 + 1) * 65], aT, aT[:, h, :], v2, v2[:, h, :], True, True)
            for h in range(8):
                self.MM(pi[h // 4], pi[h // 4][:, (h % 4) * 65:(h % 4 + 1) * 65], qT, qT[:, h, :], C16, C16[:, h, :], True, True)
            tot = r_tot()
            for hb in range(2):
                tv = tot[:, hb * 4:(hb + 1) * 4, :]
                self.TT_(tot, tv, pi[hb], pi[hb][:, 0:260].rearrange("p (h e) -> p h e", e=65),
                         ex, bc3(ex[:, 32 + hb * 4:32 + (hb + 1) * 4], 65), ALU.mult)
                self.TT_(tot, tv, tot, tv, pn[hb], pn[hb][:, 0:260].rearrange("p (h e) -> p h e", e=65), ALU.add)
            den = r_sc()
            self.ACT(den, den[:, 0:8], tot, tot[:, :, 64], AF.Abs)
            self.TS(den, den[:, 0:8], den, den[:, 0:8], 1.0, None, ALU.max)
            self.V(k.dve, lambda: nc.vector.reciprocal(out=den[:, 0:8], in_=den[:, 0:8]), [den], [den])
            hd = r_hd()
            self.TT_(hd, hd[:], tot, tot[:, :, 0:64], den, bc3(den[:, 0:8], 64), ALU.mult)
            kd = r_kd()
            self.TT_(kd, kd[:], kt, kt[:].rearrange("p (h e) -> p h e", e=64), ex, bc3(ex[:, 48:56], 64), ALU.mult, eng=k.pool)
            pC = [self.pB(), self.pB()]
            for h in range(8):
                self.MM(pC[h // 4], pC[h // 4][0:64, (h % 4) * 65:(h % 4 + 1) * 65], kd, kd[:, h, :], v2, v2[:, h, :], True, True)
            self.TT_(Cst, Cst[:], Cst, Cst[:], ex, bc3(ex[0:64, 40:48], 65), ALU.mult)
            for hb in range(2):
                cv = Cst[:, hb * 4:(hb + 1) * 4, :]
                self.TT_(Cst, cv, Cst, cv, pC[hb], pC[hb][0:64, 0:260].rearrange("p (h e) -> p h e", e=65), ALU.add)
            self.CP(C16, C16[:], Cst, Cst[:], eng=k.act)
            if not last:
                self.ST(self.yf_d[rows, :], self.r_yf_d, y, y[:])
                self.ST(self.hf_d[rows, :], self.r_hf_d, hd, hd[:].rearrange("p h e -> p (h e)"))
                continue
            yf, hf, zs, og = r_yf(), r_hf(), r_zs(), r_og()
            self.LD(yf, yf[:], self.yf_d[rows, :], self.r_yf_d)
            self.LD(hf, hf[:], self.hf_d[rows, :], self.r_hf_d)
            self.LD(zs, zs[:], self.z_d[rows, :], self.r_z_d)
            self.LD(og, og[:], self.og_d[rows, :], self.r_og_d)
            tmp = r_tmp()
            self.TT_(y, y[:], y, y[:], yf, yf[:], ALU.add)
            self.TT_(tmp, tmp[:].rearrange("p (h e) -> p h e", e=64), xs, xv, dsk, bc3(dsk[:, 0:8], 64), ALU.mult)
            self.TT_(y, y[:], y, y[:], tmp, tmp[:], ALU.add)
            self.TT_(y, y[:], y, y[:], zs, zs[:], ALU.mult)
            st = r_sc()
            for g in range(2):
                self.ACT(tmp, tmp[:, g * 256:(g + 1) * 256], y, y[:, g * 256:(g + 1) * 256], AF.Square, accum=st[:, g:g + 1],
                         extra=[])
            k.all_res
            st.r.w = tmp.r.w
            self.rsqrt_(st, st[:, 2:4], st, st[:, 0:2], 1.0 / 256.0, self.eps_rms)
            self.TT_(y, y[:].rearrange("p (g e) -> p g e", g=2), y, y[:].rearrange("p (g e) -> p g e", g=2),
                     st, bc3(st[:, 2:4], 256), ALU.mult)
            mix = r_mix()
            self.TT_(mix, mix[:, 0:512], y, y[:], ssdg, ssdg[:], ALU.mult)
            hv = hd[:]
            self.TT_(hd, hv, hd, hv, hf, hf[:].rearrange("p (h e) -> p h e", e=64), ALU.add)
            self.V(k.dve, lambda: nc.vector.tensor_reduce(out=st[:, 8:16], in_=hv, axis=AX.X, op=ALU.add), [hd], [st])
            self.TS(st, st[:, 8:16], st, st[:, 8:16], 1.0 / 64.0, None, ALU.mult)
            self.TT_(hd, hv, hd, hv, st, bc3(st[:, 8:16], 64), ALU.subtract)
            tv3 = tmp[:].rearrange("p (h e) -> p h e", e=64)
            self.TT_(tmp, tv3, hd, hv, hd, hv, ALU.mult)
            self.V(k.dve, lambda: nc.vector.tensor_reduce(out=st[:, 16:24], in_=tv3, axis=AX.X, op=ALU.add), [tmp], [st])
            self.rsqrt_(st, st[:, 24:32], st, st[:, 16:24], 1.0 / 64.0, self.eps_ln)
            self.TT_(hd, hv, hd, hv, st, bc3(st[:, 24:32], 64), ALU.mult)
            hflat = hd[:].rearrange("p h e -> p (h e)")
            self.TT_(hd, hflat, hd, hflat, mlg, mlg[:], ALU.mult)
            self.TT_(mix, mix[:, 512:1024], hd, hflat, og, og[:], ALU.mult)
            for cc in range(8):
                self.TR(self.ptr, self.ptr[:, cc * 128:(cc + 1) * 128], mix, mix[:, cc * 128:(cc + 1) * 128], self.ident16)
            mixT = r_mixT()
            self.CP(mixT, mixT[:].rearrange("p c s -> p (c s)"), self.ptr, self.ptr[:], eng=k.act)
            phs = [self.pB(), self.pB()]
            for half in range(2):
                for cc in range(8):
                    self.MM(phs[half], phs[half][:], mixT, mixT[:, cc, :], wout, wout[:, cc, half * 512:(half + 1) * 512],
                            cc == 0, cc == 7)
            self.epilogue([(phs[0], phs[0][:]), (phs[1], phs[1][:])], r0, g1, b1, None, "o3", dbg_idx=2 * layer)


def make_consts():
    half = 16
    inv = (10000.0 ** (-np.arange(half, dtype=np.float32) / half)).astype(np.float32)
    pos = np.arange(S, dtype=np.float32)
    ang = (pos[:, None] * inv[None, :]).astype(np.float32)
    cos = np.cos(ang).astype(np.float32).T
    sin = np.sin(ang).astype(np.float32).T
    rope = np.zeros((2, 32, S), np.float32)
    rope[0, :16] = cos
    rope[0, 16:] = cos
    rope[1, :16] = -sin
    rope[1, 16:] = sin
    kk = np.arange(128)
    U = (kk[:, None] <= kk[None, :]).astype(np.float32)
    UT = (kk[:, None] >= kk[None, :]).astype(np.float32)
    mF = np.where(kk[:, None] > kk[None, :], NEG, 0.0).astype(np.float32)
    mB = np.where(kk[:, None] < kk[None, :], NEG, 0.0).astype(np.float32)
    pg = np.zeros((128, 9), np.float32)
    for g in range(7):
        pg[:, g] = g * 128 + kk
    for h in range(2):
        pg[:, 7 + h] = h * 128 + kk
    return {"c_ident": np.eye(128, dtype=np.float32), "c_rope": rope, "c_tri": np.stack([U, UT, mF, mB]), "c_pg": pg}


_PROG_CACHE = {}


def get_prog(nslot, nlayers, debug):
    key = (nslot, nlayers, debug)
    if key not in _PROG_CACHE:
        p = Prog(nslot, nlayers, debug)
        p.build()
        _PROG_CACHE[key] = p
    return _PROG_CACHE[key]


def run(inputs, nslot=NSLOT, nlayers=DEPTH, debug=False, ncores=NCORES, seqs=None):
    p = get_prog(nslot, nlayers, debug)
    xall = np.concatenate([np.asarray(inputs["x_prompt"]), np.asarray(inputs["x_sample"])], axis=0)
    nseq = xall.shape[0]
    if seqs is None:
        seqs = [[min(c * nslot + s, nseq - 1) for s in range(nslot)] for c in range(ncores)]
        flat = list(range(nseq))
        seqs = []
        pos = 0
        for c in range(ncores):
            n = 3 if c < 4 else 2
            mine = flat[pos:pos + n]
            pos += n
            while len(mine) < nslot:
                mine.append(mine[0])
            seqs.append(mine[:nslot])
    consts = make_consts()
    shared = {}
    for n in p.in_names:
        if n == "x" or n in consts:
            continue
        a = np.ascontiguousarray(np.asarray(inputs[n], dtype=np.float32))
        if n in ("ssd_dt_bias", "ssd_a_log", "ml_igate_b", "ml_fgate_b"):
            a = a.reshape(2, 16)
        shared[n] = a
    shared.update(consts)
    in_maps = []
    for c in range(ncores):
        m = dict(shared)
        m["x"] = np.ascontiguousarray(xall[seqs[c]])
        in_maps.append(m)
    res = run_bass_kernel_spmd(p.nc, in_maps, core_ids=list(range(ncores)))
    return res, seqs, nseq


def kernel(**inputs):
    res, seqs, nseq = run(inputs)
    out = np.zeros((nseq, S, D), np.float32)
    done = set()
    for c in range(NCORES):
        y = np.asarray(res.results[c]["y"])
        for s, q in enumerate(seqs[c]):
            if q not in done:
                out[q] = y[s]
                done.add(q)
    nb = np.asarray(inputs["x_prompt"]).shape[0]
    return (out[:nb], out[nb:])
```

```python
import math
from contextlib import ExitStack
import numpy as np
import concourse.bass as bass
import concourse.mybir as mybir
from concourse.bass_utils import run_bass_kernel_spmd

F32 = mybir.dt.float32
BF16 = mybir.dt.bfloat16
AF = mybir.ActivationFunctionType
ALU = mybir.AluOpType
AX = mybir.AxisListType

D = 1024
S = 4096
DEPTH = 4
ALPHA = (2.0 * DEPTH) ** 0.25
LN_EPS = 1e-5
RMS_EPS = 1e-6
NCORES = 8
NSLOT = 3
TT = 512
NT = S // TT
D_FF = 2816
NE = 8
D_FFE = 3584
EV_IN = 1440
OD_IN = 3632
NEG = -30000.0
SPARSE_MOE = True
CONV_SPLIT = 99
GCM = 512
NTL = 24
I32 = mybir.dt.int32


class Res:
    __slots__ = ("name", "w", "rs", "dsem", "dcount", "persist", "phase", "scope", "multi", "msems", "mrr", "mw")

    def __init__(self, name, persist=False):
        self.name = name
        self.phase = False
        self.scope = None
        self.multi = 0
        self.msems = []
        self.mrr = 0
        self.mw = {}
        self.w = None
        self.rs = {}
        self.dsem = None
        self.dcount = 0
        self.persist = persist


class Eng:
    def __init__(self, name, h, sem):
        self.name, self.h, self.sem = name, h, sem
        self.count = 0
        self.seen = {}
        self.nins = 0
        self.nwait = 0


class Tile:
    def __init__(self, t, r):
        self.t, self.r = t, r

    def __getitem__(self, key):
        return self.t[key]


class K:
    def __init__(self, nc, stack):
        self.nc = nc
        self.stack = stack
        self.engs = {}
        for name, h in (("pe", nc.tensor), ("act", nc.scalar), ("dve", nc.vector),
                        ("pool", nc.gpsimd), ("sp", nc.sync)):
            sem = stack.enter_context(nc.semaphore("sem_" + name))
            self.engs[name] = Eng(name, h, sem)
        self.pe, self.act, self.dve, self.pool, self.sp = (
            self.engs[n] for n in ("pe", "act", "dve", "pool", "sp"))
        self.all_res = []
        self.nsem = 5
        self.free_dsems = []
        self.sem_count = {}
        self.exact_sems = set()
        self.gsems = []
        self.grr = 0

    def res(self, name, persist=False):
        r = Res(name, persist)
        self.all_res.append(r)
        return r

    def dres(self, name, n=2):
        r = self.res(name)
        r.multi = n
        return r

    def _waits(self, eng, reads, writes, partial_dst=None):
        need = {}

        def add(m, raw):
            sem, val, src = m
            if src is eng and not raw:
                return
            if src is None and sem not in self.exact_sems:
                val = max(val, self.sem_count.get(sem, 0))
            if eng.seen.get(sem, 0) >= val:
                return
            if need.get(sem, 0) < val:
                need[sem] = val

        for r in reads:
            if r.w is not None:
                add(r.w, True)
            for sem_, val_ in r.mw.items():
                add((sem_, val_, None), True)
        for w in writes:
            if w is not partial_dst:
                for sem_, val_ in w.mw.items():
                    add((sem_, val_, None), False)
            if w.w is not None and not (w is partial_dst and w.w[0] is w.dsem):
                add(w.w, False)
            for sem, (val, src) in w.rs.items():
                add((sem, val, src), False)
        for sem, val in need.items():
            eng.h.wait_ge(sem, val)
            eng.seen[sem] = val
            eng.nwait += 1

    def _mark(self, m, reads, writes):
        sem, val, src = m
        for r in reads:
            o = r.rs.get(sem)
            if o is None or o[0] < val:
                r.rs[sem] = (val, src)
        for w in writes:
            w.w = m
            w.rs = {}

    def op(self, eng, fn, reads=(), writes=()):
        self._waits(eng, reads, writes)
        eng.count += 1
        eng.nins += 1
        ins = fn()
        ins.then_inc(eng.sem, 1)
        self._mark((eng.sem, eng.count, eng), reads, writes)
        return ins

    def _multi_slot(self, q, r0):
        NG = 24
        if len(self.gsems) < NG:
            sem = self.stack.enter_context(self.nc.semaphore("gsem_%d" % len(self.gsems)))
            self.nsem += 1
            self.exact_sems.add(sem)
            self.gsems.append([sem, 0])
            slot = len(self.gsems) - 1
        else:
            slot = self.grr % NG
        self.grr += 1
        sem, cnt = self.gsems[slot]
        if q.seen.get(sem, 0) < cnt:
            q.h.wait_ge(sem, cnt)
            q.seen[sem] = cnt
        return slot, sem

    def _multi_done(self, r0, slot, sem, n, reads):
        self.gsems[slot][1] += 16 * n
        cnt = self.gsems[slot][1]
        for r in reads:
            o = r.rs.get(sem)
            if o is None or o[0] < cnt:
                r.rs[sem] = (cnt, None)
        r0.mw[sem] = cnt
        r0.rs = {}

    def dma(self, q, pairs, reads=(), writes=(), partial=False, **kw):
        r0 = writes[0]
        self._waits(q, reads, writes, partial_dst=r0 if partial else None)
        if r0.multi and partial:
            slot, sem = self._multi_slot(q, r0)
            for (o, i) in pairs:
                q.h.dma_start(out=o, in_=i, **kw).then_inc(sem, 16)
                q.nins += 1
            self._multi_done(r0, slot, sem, len(pairs), reads)
            return
        if r0.dsem is None:
            if self.free_dsems:
                r0.dsem, r0.dcount = self.free_dsems.pop()
            else:
                r0.dsem = self.stack.enter_context(self.nc.semaphore("dsem_%d" % self.nsem))
                r0.dcount = 0
                self.nsem += 1
        for (o, i) in pairs:
            q.h.dma_start(out=o, in_=i, **kw).then_inc(r0.dsem, 16)
            r0.dcount += 16
            q.nins += 1
        self.sem_count[r0.dsem] = r0.dcount
        self._mark((r0.dsem, r0.dcount, None), reads, writes)

    def idma(self, fn, reads, writes, partial=False):
        q = self.pool
        r0 = writes[0]
        self._waits(q, reads, writes, partial_dst=r0 if partial else None)
        if r0.multi and partial:
            slot, sem = self._multi_slot(q, r0)
            fn().then_inc(sem, 16)
            q.nins += 1
            self._multi_done(r0, slot, sem, 1, reads)
            return
        if r0.dsem is None:
            if self.free_dsems:
                r0.dsem, r0.dcount = self.free_dsems.pop()
            else:
                r0.dsem = self.stack.enter_context(self.nc.semaphore("dsem_%d" % self.nsem))
                r0.dcount = 0
                self.nsem += 1
        fn().then_inc(r0.dsem, 16)
        r0.dcount += 16
        q.nins += 1
        self.sem_count[r0.dsem] = r0.dcount
        self._mark((r0.dsem, r0.dcount, None), reads, writes)

    def barrier(self, closing=None):
        sp = self.sp
        for r in self.all_res:
            if r.persist:
                continue
            if r.dsem is not None and sp.seen.get(r.dsem, 0) < r.dcount:
                sp.h.wait_ge(r.dsem, r.dcount)
                sp.seen[r.dsem] = r.dcount
        for (sem_, cnt_) in self.gsems:
            if sp.seen.get(sem_, 0) < cnt_:
                sp.h.wait_ge(sem_, cnt_)
                sp.seen[sem_] = cnt_
        sp.count += 1
        sp.h.sem_inc(sp.sem, 1)
        for e in self.engs.values():
            for o in self.engs.values():
                if o is e or o.count == 0:
                    continue
                if e.seen.get(o.sem, 0) < o.count:
                    e.h.wait_ge(o.sem, o.count)
                    e.seen[o.sem] = o.count
            for r in self.all_res:
                if not r.persist and r.dsem is not None:
                    e.seen[r.dsem] = r.dcount
            for (sem_, cnt_) in self.gsems:
                e.seen[sem_] = cnt_
        for r in self.all_res:
            if not r.persist:
                r.w = None
                r.rs = {}
                r.mw = {}
        keep = []
        for r in self.all_res:
            if r.phase:
                if r.dsem is not None:
                    self.free_dsems.append((r.dsem, r.dcount))
                    r.dsem = None
                if r.scope is closing:
                    continue
            keep.append(r)
        self.all_res = keep

    def finish(self):
        sp = self.sp
        for r in self.all_res:
            ms = [(s_, v, e) for s_, (v, e) in r.rs.items()]
            if r.w is not None:
                ms.append(r.w)
            for (sem, val, src) in ms:
                if sp.seen.get(sem, 0) >= val:
                    continue
                sp.h.wait_ge(sem, val)
                sp.seen[sem] = val


class Prog:
    def __init__(self, nslot=NSLOT, nlayers=DEPTH, debug=False):
        self.nslot = nslot
        self.nlayers = nlayers
        self.debug = debug
        self.nc = bass.Bass("TRN2", target_bir_lowering=False)
        self.in_names = []

    def din(self, name, shape, dt=F32):
        self.in_names.append(name)
        return self.nc.dram_tensor(name, list(shape), dt, kind="ExternalInput").ap()

    def dscr(self, name, shape, dt):
        return self.nc.dram_tensor(name, list(shape), dt).ap()

    def T(self, name, shape, dt=F32):
        t = self.ph.enter_context(self.nc.sbuf_tensor(name + "_%d" % self.uid(), list(shape), dt))
        r = self.k.res(name)
        r.phase = self.ph is not self.stack
        r.scope = self.ph
        return Tile(t, r)

    def TP(self, name, shape, dt=F32):
        t = self.stack.enter_context(self.nc.sbuf_tensor(name, list(shape), dt))
        return Tile(t, self.k.res(name, persist=True))

    def uid(self):
        self._uid += 1
        return self._uid

    def MM(self, out, oap, lt, lap, rt, rap, start, stop):
        nc = self.nc
        self.k.op(self.k.pe, lambda: nc.tensor.matmul(oap, lhsT=lap, rhs=rap, start=start, stop=stop),
                  [lt.r, rt.r], [out.r])

    def TR(self, out, oap, it, iap, ident):
        nc = self.nc
        self.k.op(self.k.pe, lambda: nc.tensor.transpose(oap, iap, ident[:]), [it.r, ident.r], [out.r])

    def ACT(self, out, oap, it, iap, func, bias=None, scale=None, accum=None, extra=()):
        nc = self.nc
        kw = {}
        if bias is not None:
            kw["bias"] = bias
        if scale is not None:
            kw["scale"] = scale
        if accum is not None:
            kw["accum_out"] = accum
        self.k.op(self.k.act, lambda: nc.scalar.activation(out=oap, in_=iap, func=func, **kw),
                  [it.r] + [e.r for e in extra], [out.r])

    def V(self, eng, fn, reads, writes):
        self.k.op(eng, fn, [t.r for t in reads], [t.r for t in writes])

    def TT_(self, out, oap, a, aap, b, bap, op, eng=None):
        nc = self.nc
        eng = eng or self.k.dve
        self.k.op(eng, lambda: eng.h.tensor_tensor(out=oap, in0=aap, in1=bap, op=op), [a.r, b.r], [out.r])

    def TS(self, out, oap, a, aap, s1, s2, op0, op1=None, extra=(), eng=None):
        eng = eng or self.k.dve
        if op1 is None:
            fn = lambda: eng.h.tensor_scalar(out=oap, in0=aap, scalar1=s1, scalar2=None, op0=op0)
        else:
            fn = lambda: eng.h.tensor_scalar(out=oap, in0=aap, scalar1=s1, scalar2=s2, op0=op0, op1=op1)
        self.k.op(eng, fn, [a.r] + [e.r for e in extra], [out.r])

    def STT(self, out, oap, a, aap, sc, b, bap, op0, op1, extra=(), eng=None):
        eng = eng or self.k.dve
        self.k.op(eng, lambda: eng.h.scalar_tensor_tensor(out=oap, in0=aap, scalar=sc, in1=bap, op0=op0, op1=op1),
                  [a.r, b.r] + [e.r for e in extra], [out.r])

    def CP(self, out, oap, it, iap, eng=None):
        eng = eng or self.k.dve
        if eng is self.k.act:
            self.k.op(eng, lambda: self.nc.scalar.copy(out=oap, in_=iap), [it.r], [out.r])
        else:
            self.k.op(eng, lambda: eng.h.tensor_copy(out=oap, in_=iap), [it.r], [out.r])

    def LD(self, tile, oap, src_ap, src_res=None, q=None, partial=False):
        self.k.dma(q or self.k.sp, [(oap, src_ap)], [src_res] if src_res is not None else [], [tile.r], partial=partial)

    def ST(self, dst_ap, dst_res, tile, iap, q=None):
        self.k.dma(q or self.k.sp, [(dst_ap, iap)], [tile.r], [dst_res], partial=True)

    def build(self):
        nc = self.nc
        self._uid = 0
        with ExitStack() as stack:
            self.stack = stack
            self.k = K(nc, stack)
            self.declare_io()
            self.setup_consts()
            self.convert_weights([l for l in range(self.nlayers) if l < CONV_SPLIT])
            for slot in range(self.nslot):
                self.run_slot(slot)
            self.k.barrier()
        return nc

    def declare_io(self):
        ns = self.nslot
        self.x_in = self.din("x", [ns, S, D])
        self.y_out = self.nc.dram_tensor("y", [ns, S, D], F32, kind="ExternalOutput").ap()
        self.r_y = self.k.dres("y_out", 4)
        w = {}
        w["ev_w_in"] = self.din("ev_w_in", [2, D, EV_IN])
        w["conv_dw_w"] = self.din("conv_dw_w", [2, 31, 512])
        w["conv_dw_b"] = self.din("conv_dw_b", [2, 512])
        w["conv_ln_g"] = self.din("conv_ln_g", [2, 512])
        w["conv_ln_b"] = self.din("conv_ln_b", [2, 512])
        w["mla_q_norm_g"] = self.din("mla_q_norm_g", [2, 256])
        w["mla_w_uq"] = self.din("mla_w_uq", [2, 256, 768])
        w["mla_kv_norm_g"] = self.din("mla_kv_norm_g", [2, 128])
        w["mla_w_ukv"] = self.din("mla_w_ukv", [2, 128, 1024])
        w["ev_w_out"] = self.din("ev_w_out", [2, 1024, 1024])
        w["od_w_in"] = self.din("od_w_in", [2, D, OD_IN])
        w["ssd_conv_w"] = self.din("ssd_conv_w", [2, 5, 1024])
        w["ssd_conv_b"] = self.din("ssd_conv_b", [2, 1024])
        w["ssd_dt_bias"] = self.din("ssd_dt_bias", [2, 16])
        w["ssd_a_log"] = self.din("ssd_a_log", [2, 16])
        w["ssd_d"] = self.din("ssd_d", [2, 8])
        w["ssd_norm_g"] = self.din("ssd_norm_g", [2, 512])
        w["ml_igate_b"] = self.din("ml_igate_b", [2, 16])
        w["ml_fgate_b"] = self.din("ml_fgate_b", [2, 16])
        w["ml_norm_g"] = self.din("ml_norm_g", [2, 512])
        w["od_w_out"] = self.din("od_w_out", [2, 1024, 1024])
        w["ffn_w_gate"] = self.din("ffn_w_gate", [2, D, D_FF])
        w["ffn_w_up"] = self.din("ffn_w_up", [2, D, D_FF])
        w["ffn_w_down"] = self.din("ffn_w_down", [2, D_FF, D])
        w["moe_router_w"] = self.din("moe_router_w", [2, D, NE])
        w["moe_router_b"] = self.din("moe_router_b", [2, NE])
        w["moe_w_gate"] = self.din("moe_w_gate", [2, NE, D, D_FFE])
        w["moe_w_up"] = self.din("moe_w_up", [2, NE, D, D_FFE])
        w["moe_w_down"] = self.din("moe_w_down", [2, NE, D_FFE, D])
        for n in ("ln1_g", "ln1_b", "ln2_g", "ln2_b"):
            w[n] = self.din(n, [4, D])
        self.w = w
        self.c_ident = self.din("c_ident", [128, 128])
        self.c_rope = self.din("c_rope", [2, 32, S])
        self.c_tri = self.din("c_tri", [4, 128, 128])
        self.c_pg = self.din("c_pg", [128, 9])
        self.xres = self.dscr("xres", [S, D], F32)
        self.r_xres = self.k.dres("xres", 4)
        self.xT = self.dscr("xT", [D, S + 4], BF16)
        self.r_xT = self.k.dres("xT", 4)
        if self.debug:
            self.dbg = self.nc.dram_tensor("dbg", [self.nlayers * 2, S, D], F32, kind="ExternalOutput").ap()
            self.r_dbg = self.k.dres("dbg", 2)

    def setup_consts(self):
        nc, k = self.nc, self.k
        self.ph = self.stack
        self.ident32 = self.TP("ident32", [128, 128], F32)
        self.LD(self.ident32, self.ident32[:], self.c_ident)
        self.ident16 = self.TP("ident16", [128, 128], BF16)
        self.CP(self.ident16, self.ident16[:], self.ident32, self.ident32[:])
        self.ones32 = self.TP("ones32", [128, 128], F32)
        self.V(k.dve, lambda: nc.vector.memset(self.ones32[:], 1.0), [], [self.ones32])
        self.zero16 = self.TP("zero16", [128, 64], BF16)
        self.V(k.dve, lambda: nc.vector.memset(self.zero16[:], 0.0), [], [self.zero16])
        self.zero32 = self.TP("zero32", [128, 64], F32)
        self.V(k.dve, lambda: nc.vector.memset(self.zero32[:], 0.0), [], [self.zero32])
        self.pb = []
        for i in range(7):
            t = self.stack.enter_context(nc.psum_tensor("pb%d" % i, [128, 512], F32))
            self.pb.append(Tile(t, k.res("pb%d" % i, persist=True)))
        t = self.stack.enter_context(nc.psum_tensor("ptr", [128, 1024], BF16))
        self.ptr = Tile(t, k.res("ptr", persist=True))
        xTv = self.xT.rearrange("(c p) s -> p c s", p=128)
        for lo in (0, S + 2):
            k.dma(k.sp, [(xTv[:, :, lo:lo + 2], self.zero16[:, 0:16].rearrange("p (c s) -> p c s", c=8))],
                  [self.zero16.r], [self.r_xT], partial=True)

    def convert_weights(self, layers):
        k = self.k
        GC = 256
        if not hasattr(self, "wb"):
            self.wb = {}
            self.wb_r = {}
            self._alloc_wb(GC)
        self._convert(layers, GC)

    def _alloc_wb(self, GC):
        plain = ["ev_w_in", "mla_w_uq", "mla_w_ukv", "ev_w_out", "od_w_in", "od_w_out"]
        for n in plain:
            self.wb[n] = self.dscr("wb_" + n, list(self.w[n].shape), BF16)
        self.wb["ffn_w_gate"] = self.dscr("wb_ffn_g", [2, 1, D_FF // GC, 128, 8, GC], BF16)
        self.wb["ffn_w_up"] = self.dscr("wb_ffn_u", [2, 1, D_FF // GC, 128, 8, GC], BF16)
        self.wb["ffn_w_down"] = self.dscr("wb_ffn_d", [2, 1, 2, 128, D_FF // 128, 512], BF16)
        self.wb["moe_w_gate"] = self.dscr("wb_moe_g", [2, NE, D_FFE // GCM, 128, 8, GCM], BF16)
        self.wb["moe_w_up"] = self.dscr("wb_moe_u", [2, NE, D_FFE // GCM, 128, 8, GCM], BF16)
        self.wb["moe_w_down"] = self.dscr("wb_moe_d", [2, NE, 2, 128, D_FFE // 128, 512], BF16)

    def _convert(self, layers, GC):
        k = self.k

        def conv_plain(n, j):
            src, dst = self.w[n][j], self.wb[n][j]
            r = self._grp
            self.wb_r[(n, j, 0)] = r
            rows = src.shape[0]
            prs = [(dst[r0:min(r0 + 256, rows), :], src[r0:min(r0 + 256, rows), :]) for r0 in range(0, rows, 256)]
            k.dma(k.pool, prs, [], [r], partial=True)

        def conv_ff(prefix, j, ne):
            for e in range(ne):
                for nm in ("gate", "up"):
                    n = "%s_w_%s" % (prefix, nm)
                    src = self.w[n][j] if ne == 1 else self.w[n][j, e]
                    dst = self.wb[n][j, e]
                    r = self._grp
                    self.wb_r[(n, j, e)] = r
                    sv = src.rearrange("(c p) n -> p c n", p=128)
                    gc = dst.shape[-1]
                    prs = [(dst[g], sv[:, :, g * gc:(g + 1) * gc]) for g in range(dst.shape[0])]
                    k.dma(k.pool, prs, [], [r], partial=True)
                n = "%s_w_down" % prefix
                src = self.w[n][j] if ne == 1 else self.w[n][j, e]
                dst = self.wb[n][j, e]
                r = self._grp
                self.wb_r[(n, j, e)] = r
                sv = src.rearrange("(f p) d -> p f d", p=128)
                nf = sv.shape[1]
                prs = []
                for half in range(2):
                    for f0 in range(0, nf, 7):
                        f1 = min(nf, f0 + 7)
                        prs.append((dst[half][:, f0:f1, :], sv[:, f0:f1, half * 512:(half + 1) * 512]))
                k.dma(k.pool, prs, [], [r], partial=True)

        for layer in layers:
            j = layer // 2
            self._grp = k.res("wbgrp_mix%d" % layer, persist=True)
            if layer % 2 == 0:
                for n in ("ev_w_in", "mla_w_uq", "mla_w_ukv", "ev_w_out"):
                    conv_plain(n, j)
                self._grp = k.res("wbgrp_ffn%d" % layer, persist=True)
                conv_ff("ffn", j, 1)
            else:
                for n in ("od_w_in", "od_w_out"):
                    conv_plain(n, j)
                self._grp = k.res("wbgrp_ffn%d" % layer, persist=True)
                conv_ff("moe", j, NE)

    def load_bcast(self, name, src_row_ap, n):
        t = self.T(name, [128, n], F32)
        self.LD(t, t[:], src_row_ap.partition_broadcast(128))
        return t

    def load_col(self, name, src_vec_ap, nchunk):
        t = self.T(name, [128, nchunk], F32)
        self.k.dma(self.k.sp, [(t[:], src_vec_ap.rearrange("(c p) -> p c", p=128))], [], [t.r],
                   allow_slow_non_contiguous=True)
        return t

    def run_slot(self, slot):
        k = self.k
        self.prologue(slot)
        for layer in range(self.nlayers):
            j = layer // 2
            last = (layer == self.nlayers - 1)
            if layer % 2 == 0:
                self.even_mixer(j, layer)
                self.ffn_like(layer, j, moe=False, dst=(self.y_out[slot], self.r_y) if last else None)
            else:
                self.odd_mixer(j, layer)
                if SPARSE_MOE:
                    self.moe_sparse(layer, j, dst=(self.y_out[slot], self.r_y) if last else None)
                else:
                    self.ffn_like(layer, j, moe=True, dst=(self.y_out[slot], self.r_y) if last else None)
                if slot == 0 and layer == 1 and self.nlayers > CONV_SPLIT:
                    self.convert_weights([l for l in range(self.nlayers) if l >= CONV_SPLIT])

    def phase(self):
        prog = self

        class _P:
            def __enter__(s):
                prog._phstack = ExitStack()
                prog._phstack.__enter__()
                prog.ph = prog._phstack
                return s

            def __exit__(s, *a):
                if a[0] is None:
                    prog.k.barrier(closing=prog._phstack)
                prog._phstack.__exit__(*a)
                prog.ph = prog.stack
                return False
        return _P()

    def prologue(self, slot):
        nc, k = self.nc, self.k
        with self.phase():
            xin = self.x_in[slot]
            for t in range(S // 128):
                xt = self.T("pro_x%d" % (t % 2), [128, D], F32) if t < 2 else None
                if t < 2:
                    if t == 0:
                        self._pro = []
                    self._pro.append(xt)
                xt = self._pro[t % 2]
                self.LD(xt, xt[:], xin[t * 128:(t + 1) * 128, :])
                self.ST(self.xres[t * 128:(t + 1) * 128, :], self.r_xres, xt, xt[:])
                self.emit_xT(xt, t * 128, "pro")

    def emit_xT(self, y32, tok0, tag):
        nc, k = self.nc, self.k
        key = "_xT_" + tag
        if not hasattr(self, key) or getattr(self, key)[0] is not self.ph:
            y16 = self.T(tag + "_y16", [128, D], BF16)
            xtt = self.T(tag + "_xtt", [128, 8, 128], BF16)
            setattr(self, key, (self.ph, y16, xtt))
        _, y16, xtt = getattr(self, key)
        self.CP(y16, y16[:], y32, y32[:], eng=k.act)
        for c in range(8):
            self.TR(self.ptr, self.ptr[:, c * 128:(c + 1) * 128], y16, y16[:, c * 128:(c + 1) * 128], self.ident16)
        self.CP(xtt, xtt[:].rearrange("p c s -> p (c s)"), self.ptr, self.ptr[:], eng=k.dve)
        xTv = self.xT.rearrange("(c p) s -> p c s", p=128)
        self.ST(xTv[:, :, 2 + tok0:2 + tok0 + 128], self.r_xT, xtt, xtt[:])

    def ln_consts(self, gname, bname, layer):
        g = self.load_bcast("lng", self.w[gname][layer:layer + 1, :], D)
        b = self.load_bcast("lnb", self.w[bname][layer:layer + 1, :], D)
        return g, b

    def epilogue(self, ps_halves, tok0, g, b, dst, tag, dbg_idx=None, add_tile=None):
        self.epilogue_multi([(ps_halves, tok0)], g, b, dst, tag, dbg_idx=dbg_idx, nch=1)

    def epilogue_multi(self, items, g, b, dst, tag, dbg_idx=None, nch=2):
        nc, k = self.nc, self.k
        key = "_ep_" + tag
        if not hasattr(self, key) or getattr(self, key)[0] is not self.ph:
            nset = 1 if tag == "ffn" else 2
            tiles = (self.ph,
                     [[self.T(tag + "_ex%d_%d" % (i, q), [128, D], F32) for i in range(nch)] for q in range(nset)],
                     [[self.T(tag + "_et%d_%d" % (i, q), [128, D], F32) for i in range(nch)] for q in range(nset)],
                     [self.T(tag + "_est%d" % i, [128, 2, 6], F32) for i in range(nch)],
                     [self.T(tag + "_emv%d" % i, [128, 2], F32) for i in range(nch)],
                     [self.T(tag + "_ers%d" % i, [128, 1], F32) for i in range(nch)],
                     [[self.T(tag + "_y16%d_%d" % (i, q), [128, D], BF16) for i in range(nch)] for q in range(nset)],
                     [[self.T(tag + "_xtt%d_%d" % (i, q), [128, 8, 128], BF16) for i in range(nch)] for q in range(nset)],
                     [0])
            setattr(self, key, tiles)
        _, xs_a, ts_a, stts, mvs, rss, y16s_a, xtts_a, cnt_ = getattr(self, key)
        par = cnt_[0] % len(xs_a)
        cnt_[0] += 1
        xs_, ts_, y16s, xtts = xs_a[par], ts_a[par], y16s_a[par], xtts_a[par]
        n = len(items)
        assert n <= nch
        R = range(n)
        for c in R:
            tok0 = items[c][1]
            self.LD(xs_[c], xs_[c][:], self.xres[tok0:tok0 + 128, :], self.r_xres)
        for h in range(2):
            for c in R:
                pt, pap = items[c][0][h]
                self.STT(ts_[c], ts_[c][:, h * 512:(h + 1) * 512], xs_[c], xs_[c][:, h * 512:(h + 1) * 512], ALPHA, pt, pap,
                         ALU.mult, ALU.add)
        for h in range(2):
            for c in R:
                self.V(k.dve, (lambda c_, h_: (lambda: nc.vector.bn_stats(out=stts[c_][:, h_, :], in_=ts_[c_][:, h_ * 512:(h_ + 1) * 512])))(c, h),
                       [ts_[c]], [stts[c]])
        for c in R:
            self.V(k.dve, (lambda c_: (lambda: nc.vector.bn_aggr(out=mvs[c_][:], in_=stts[c_][:])))(c), [stts[c]], [mvs[c]])
        for c in R:
            self.ACT(rss[c], rss[c][:], mvs[c], mvs[c][:, 1:2], AF.Sqrt, bias=self.eps_ln[:, 0:1], scale=1.0, extra=[self.eps_ln])
        for c in R:
            self.V(k.dve, (lambda c_: (lambda: nc.vector.reciprocal(out=rss[c_][:], in_=rss[c_][:])))(c), [rss[c]], [rss[c]])
        for c in R:
            self.TS(ts_[c], ts_[c][:], ts_[c], ts_[c][:], mvs[c][:, 0:1], rss[c][:, 0:1], ALU.subtract, ALU.mult, extra=[mvs[c], rss[c]])
        for c in R:
            self.TT_(ts_[c], ts_[c][:], ts_[c], ts_[c][:], g, g[:], ALU.mult)
        for c in R:
            self.TT_(ts_[c], ts_[c][:], ts_[c], ts_[c][:], b, b[:], ALU.add, eng=k.pool)
        xTv = self.xT.rearrange("(c p) s -> p c s", p=128)
        for c in R:
            tok0 = items[c][1]
            if dst is not None:
                dap, dres = dst
                self.ST(dap[tok0:tok0 + 128, :], dres, ts_[c], ts_[c][:])
            else:
                self.ST(self.xres[tok0:tok0 + 128, :], self.r_xres, ts_[c], ts_[c][:])
            if self.debug and dbg_idx is not None:
                self.ST(self.dbg[dbg_idx, tok0:tok0 + 128, :], self.r_dbg, ts_[c], ts_[c][:])
        if dst is None:
            for c in R:
                self.CP(y16s[c], y16s[c][:], ts_[c], ts_[c][:], eng=k.act)
            for c in R:
                tok0 = items[c][1]
                for cc in range(8):
                    self.TR(self.ptr, self.ptr[:, cc * 128:(cc + 1) * 128], y16s[c], y16s[c][:, cc * 128:(cc + 1) * 128], self.ident16)
                self.CP(xtts[c], xtts[c][:].rearrange("p c s -> p (c s)"), self.ptr, self.ptr[:], eng=k.dve)
                self.ST(xTv[:, :, 2 + tok0:2 + tok0 + 128], self.r_xT, xtts[c], xtts[c][:])

    def eps_tiles(self):
        nc, k = self.nc, self.k
        if not hasattr(self, "eps_ln"):
            self.eps_ln = self.TP("eps_ln", [128, 1], F32)
            self.V(k.dve, lambda: nc.vector.memset(self.eps_ln[:], LN_EPS), [], [self.eps_ln])
            self.eps_rms = self.TP("eps_rms", [128, 1], F32)
            self.V(k.dve, lambda: nc.vector.memset(self.eps_rms[:], RMS_EPS), [], [self.eps_rms])

    def load_xT_full(self, name="xTs"):
        t = self.T(name, [128, 8, S + 4], BF16)
        xTv = self.xT.rearrange("(c p) s -> p c s", p=128)
        for c in range(8):
            self.k.dma(self.k.sp, [(t[:, c, :], xTv[:, c, :])], [self.r_xT], [t.r], partial=True)
        return t

    def load_w(self, name, src, kc, cols, res, c0=0):
        t = self.T(name, [128, kc, cols], BF16)
        v = src.rearrange("(c p) n -> p c n", p=128)
        self.k.dma(self.k.sp, [(t[:], v[:, :, c0:c0 + cols])], [res], [t.r])
        return t

    def ring(self, name, shape, dt, n, init=None):
        tiles = [self.T("%s%d" % (name, i), shape, dt) for i in range(n)]
        if init is not None:
            for t in tiles:
                self.V(self.k.dve, (lambda tt: (lambda: self.nc.vector.memset(tt[:], init)))(t), [], [t])
        st = [0]

        def nxt():
            t = tiles[st[0] % n]
            st[0] += 1
            return t
        return nxt

    def pA(self):
        self._pa = getattr(self, "_pa", 0) + 1
        return self.pb[self._pa % 4]

    def pB(self):
        self._pbi = getattr(self, "_pbi", 0) + 1
        return self.pb[4 + self._pbi % 3]

    def rsqrt_(self, out, oap, src, sap, scale, eps_tile):
        self.ACT(out, oap, src, sap, AF.Sqrt, bias=eps_tile[:, 0:1], scale=scale, extra=[eps_tile])
        self.V(self.k.dve, lambda: self.nc.vector.reciprocal(out=oap, in_=oap), [out], [out])

    def even_mixer(self, j, layer):
        nc, k = self.nc, self.k
        self.eps_tiles()
        W = self.w
        if not hasattr(self, "u_d"):
            self.u_d = self.dscr("u_d", [4, 128, S + 30], F32)
            self.r_u = k.dres("u_d", 2)
            self.q_d = self.dscr("q_d", [8, 96, S], BF16)
            self.r_q = k.dres("q_d", 2)
            self.kn_d = self.dscr("kn_d", [8, 64, S], BF16)
            self.r_kn = k.dres("kn_d", 2)
            self.kpe_d = self.dscr("kpe_d", [32, S], BF16)
            self.r_kpe = k.dres("kpe_d", 2)
            self.v_d = self.dscr("v_d", [S, 8 * 65], BF16)
            self.r_v = k.dres("v_d", 2)
            self.att_d = self.dscr("att_d", [S, 512], BF16)
            self.r_att = k.dres("att_d", 2)
            uv = self.u_d.rearrange("c p s -> p c s")
            for lo in (0, S + 15):
                k.dma(k.sp, [(uv[:, :, lo:lo + 15], self.zero32[:, 0:60].rearrange("p (c s) -> p c s", c=4))],
                      [self.zero32.r], [self.r_u], partial=True)
        sl = 96 ** -0.5
        with self.phase():
            xTs = self.load_xT_full()
            wr = self.wb_r[("ev_w_in", j, 0)]
            win = self.load_w("win", self.wb["ev_w_in"][j], 8, EV_IN, wr)
            wv_ = self.wb["ev_w_in"][j].rearrange("(c p) n -> p c n", p=128)
            wkrs = self.T("wkrs", [128, 8, 96], BF16)
            k.dma(k.sp, [(wkrs[:, :, 0:64], wv_[:, :, 1344:1408]), (wkrs[:, :, 64:80], wv_[:, :, 1424:1440]),
                         (wkrs[:, :, 80:96], wv_[:, :, 1408:1424])], [wr], [wkrs.r])
            wq_r = self.wb_r[("mla_w_uq", j, 0)]
            wuq = self.load_w("wuq", self.wb["mla_w_uq"][j], 2, 768, wq_r)
            wuqs = self.T("wuqs", [128, 2, 768], BF16)
            vq = self.wb["mla_w_uq"][j].rearrange("(c p) (h e) -> p c h e", p=128, e=96)
            wqv = wuqs[:].rearrange("p c (h e) -> p c h e", e=96)
            prs = []
            for c in range(2):
                prs += [(wqv[:, c, :, 0:64], vq[:, c, :, 0:64]), (wqv[:, c, :, 64:80], vq[:, c, :, 80:96]),
                        (wqv[:, c, :, 80:96], vq[:, c, :, 64:80])]
            k.dma(k.sp, prs, [wq_r], [wuqs.r])
            wkv_r = self.wb_r[("mla_w_ukv", j, 0)]
            wukv = self.load_w("wukv", self.wb["mla_w_ukv"][j], 1, 1024, wkv_r)
            wvv = self.T("wvv", [128, 8, 64], BF16)
            k.dma(k.sp, [(wvv[:], self.wb["mla_w_ukv"][j].rearrange("p (h e) -> p h e", e=128)[:, :, 64:128])],
                  [wkv_r], [wvv.r])
            gq = self.load_col("gq", W["mla_q_norm_g"][j], 2)
            gkv = self.load_col("gkv", W["mla_kv_norm_g"][j], 1)
            r_sig = self.ring("sig", [128, TT], F32, 2)
            r_u = self.ring("u", [128, TT], F32, 2)
            r_sq = self.ring("sq", [128, TT], F32, 3)
            r_rstd = self.ring("rstd", [128, TT], F32, 2)
            r_qn = self.ring("qn", [128, TT], BF16, 4)
            r_kvn = self.ring("kvn", [128, TT], BF16, 2)
            r_cs = self.ring("cs", [96, 2, TT], F32, 2)
            r_qt = self.ring("qt", [96, TT], BF16, 3)
            r_t1 = self.ring("t1", [96, TT], F32, 2)
            r_t2 = self.ring("t2", [96, TT], F32, 2)
            r_kn = self.ring("kn", [64, TT], BF16, 3)
            r_vt = self.ring("vt", [128, 8, 65], BF16, 3, init=1.0)
            for t in range(NT):
                c0 = 2 + t * TT
                cols = slice(t * TT, (t + 1) * TT)

                def proj(pt, pap, wt, wap_fn):
                    for c in range(8):
                        self.MM(pt, pap, wt, wap_fn(c), xTs, xTs[:, c, c0:c0 + TT], c == 0, c == 7)
                for jj in range(4):
                    pv, pg = self.pA(), self.pA()
                    proj(pv, pv[:], win, lambda c: win[:, c, jj * 128:(jj + 1) * 128])
                    proj(pg, pg[:], win, lambda c: win[:, c, 512 + jj * 128:512 + (jj + 1) * 128])
                    sig = r_sig()
                    self.ACT(sig, sig[:], pg, pg[:], AF.Sigmoid)
                    u = r_u()
                    self.TT_(u, u[:], pv, pv[:], sig, sig[:], ALU.mult)
                    self.ST(self.u_d[jj][:, 15 + t * TT:15 + (t + 1) * TT], self.r_u, u, u[:])
                pq = [self.pA(), self.pA()]
                for cq in range(2):
                    proj(pq[cq], pq[cq][:], win, lambda c: win[:, c, 1024 + cq * 128:1024 + (cq + 1) * 128])
                pkv = self.pB()
                proj(pkv, pkv[:], win, lambda c: win[:, c, 1280:1408])
                pkr = self.pB()
                proj(pkr, pkr[0:96, :], win, lambda c: win[:, c, 1344:1440])
                pkrs = self.pB()
                proj(pkrs, pkrs[0:96, :], wkrs, lambda c: wkrs[:, c, :])
                cs = r_cs()
                k.dma(k.sp, [(cs[64:96, 0, :], self.c_rope[0][:, cols]), (cs[64:96, 1, :], self.c_rope[1][:, cols])],
                      [], [cs.r])
                t1, t2, kpe = r_t1(), r_t2(), r_qt()
                self.TT_(t1, t1[64:96, :], pkr, pkr[64:96, :], cs, cs[64:96, 0, :], ALU.mult)
                self.TT_(t2, t2[64:96, :], pkrs, pkrs[64:96, :], cs, cs[64:96, 1, :], ALU.mult)
                self.TT_(kpe, kpe[64:96, :], t1, t1[64:96, :], t2, t2[64:96, :], ALU.add)
                self.ST(self.kpe_d[:, cols], self.r_kpe, kpe, kpe[64:96, :])
                sqs = []
                for cq in range(2):
                    sq = r_sq()
                    self.ACT(sq, sq[:], pq[cq], pq[cq][:], AF.Square)
                    sqs.append(sq)
                pss = self.pA()
                for cq in range(2):
                    self.MM(pss, pss[:], self.ones32, self.ones32[:], sqs[cq], sqs[cq][:], cq == 0, cq == 1)
                rstd = r_rstd()
                self.rsqrt_(rstd, rstd[:], pss, pss[:], 1.0 / 256.0, self.eps_rms)
                qn = []
                for cq in range(2):
                    q_ = r_qn()
                    self.STT(q_, q_[:], pq[cq], pq[cq][:], gq[:, cq:cq + 1], rstd, rstd[:], ALU.mult, ALU.mult, extra=[gq])
                    qn.append(q_)
                sq = r_sq()
                self.ACT(sq, sq[:], pkv, pkv[:], AF.Square)
                pss2 = self.pA()
                self.MM(pss2, pss2[:], self.ones32, self.ones32[:], sq, sq[:], True, True)
                rstdk = r_rstd()
                self.rsqrt_(rstdk, rstdk[:], pss2, pss2[:], 1.0 / 128.0, self.eps_rms)
                kvn = r_kvn()
                self.STT(kvn, kvn[:], pkv, pkv[:], gkv[:, 0:1], rstdk, rstdk[:], ALU.mult, ALU.mult, extra=[gkv])
                for h in range(8):
                    pqh, pqs = self.pA(), self.pA()
                    for c in range(2):
                        self.MM(pqh, pqh[0:96, :], wuq, wuq[:, c, 96 * h:96 * h + 96], qn[c], qn[c][:], c == 0, c == 1)
                    for c in range(2):
                        self.MM(pqs, pqs[0:96, :], wuqs, wuqs[:, c, 96 * h:96 * h + 96], qn[c], qn[c][:], c == 0, c == 1)
                    qt = r_qt()
                    self.CP(qt, qt[0:64, :], pqh, pqh[0:64, :], eng=k.act)
                    t1, t2 = r_t1(), r_t2()
                    self.TT_(t1, t1[64:96, :], pqh, pqh[64:96, :], cs, cs[64:96, 0, :], ALU.mult)
                    self.TT_(t2, t2[64:96, :], pqs, pqs[64:96, :], cs, cs[64:96, 1, :], ALU.mult)
                    self.TT_(qt, qt[64:96, :], t1, t1[64:96, :], t2, t2[64:96, :], ALU.add)
                    self.ST(self.q_d[h][:, cols], self.r_q, qt, qt[0:96, :])
                for h in range(8):
                    pk = self.pB()
                    self.MM(pk, pk[0:64, :], wukv, wukv[:, 0, 128 * h:128 * h + 64], kvn, kvn[:], True, True)
                    kn = r_kn()
                    self.CP(kn, kn[:], pk, pk[0:64, :], eng=(k.act if h % 2 else k.dve))
                    self.ST(self.kn_d[h][:, cols], self.r_kn, kn, kn[:])
                for s in range(4):
                    pv_ = self.pB()
                    self.MM(pv_, pv_[:], kvn, kvn[:, s * 128:(s + 1) * 128], wvv, wvv[:].rearrange("p h e -> p (h e)"),
                            True, True)
                    vt = r_vt()
                    self.CP(vt, vt[:, :, 0:64], pv_, pv_[:].rearrange("p (h e) -> p h e", e=64),
                            eng=(k.act if s % 2 else k.dve))
                    r0 = t * TT + s * 128
                    self.ST(self.v_d[r0:r0 + 128, :], self.r_v, vt, vt[:].rearrange("p h e -> p (h e)"))
        outer = self.phase()
        outer.__enter__()
        y_sb = self.T("y_sb", [128, 4, S], F32)
        clg = self.load_col("clg", W["conv_ln_g"][j], 4)
        clb = self.load_col("clb", W["conv_ln_b"][j], 4)
        with self.subphase():
            cw = self.T("cw", [128, 4, 31], F32)
            k.dma(k.sp, [(cw[:, c, :], W["conv_dw_w"][j].rearrange("k (c p) -> c p k", p=128)[c]) for c in range(4)],
                  [], [cw.r], allow_slow_non_contiguous=True)
            cb = self.load_col("cb", W["conv_dw_b"][j], 4)
            r_uu = self.ring("uu", [128, S + 30], F32, 2)
            conv_ops = []

            def mk_first(jj, u):
                return lambda: self.TS(y_sb, y_sb[:, jj, :], u, u[:, 0:S], cw[:, jj, 0:1], cb[:, jj:jj + 1], ALU.mult, ALU.add,
                                       extra=[cw, cb])

            def mk_tap(jj, u, tap):
                return lambda: self.STT(y_sb, y_sb[:, jj, :], u, u[:, tap:tap + S], cw[:, jj, tap:tap + 1], y_sb, y_sb[:, jj, :],
                                        ALU.mult, ALU.add, extra=[cw])

            def mk_load(jj, holder):
                def f():
                    u = r_uu()
                    self.LD(u, u[:], self.u_d[jj], self.r_u)
                    holder.append(u)
                return f
            holders = [[] for _ in range(4)]
            for jj in range(4):
                conv_ops.append(mk_load(jj, holders[jj]))
                conv_ops.append((lambda jj_: (lambda: mk_first(jj_, holders[jj_][0])()))(jj))
                for tap in range(1, 31):
                    conv_ops.append((lambda jj_, tap_: (lambda: mk_tap(jj_, holders[jj_][0], tap_)()))(jj, tap))
            conv_it = iter(conv_ops)

            def emit_conv(n):
                for _ in range(n):
                    f = next(conv_it, None)
                    if f is None:
                        return
                    f()
            emit_conv(2)
            r_K = self.ring("Kh", [96, S], BF16, 2)
            r_Q = self.ring("Qh", [96, S], BF16, 2)
            r_V = self.ring("Vh", [128, 32, 65], BF16, 2)
            r_pT = self.ring("pT", [128, TT], BF16, 4)
            r_rec = self.ring("rec", [128, 4, 1], F32, 2)
            r_at = self.ring("at", [128, 4, 64], BF16, 2)
            vdv = self.v_d.rearrange("(kc p) (h e) -> p kc h e", p=128, e=65)
            attv = self.att_d.rearrange("(b p) f -> p b f", p=128)
            heads = {}

            def get_head(h):
                if h not in heads:
                    Kh, Qh, Vh = r_K(), r_Q(), r_V()
                    k.dma(k.sp, [(Kh[0:64, :], self.kn_d[h]), (Kh[64:96, :], self.kpe_d)], [self.r_kn, self.r_kpe], [Kh.r])
                    self.LD(Qh, Qh[:], self.q_d[h], self.r_q)
                    self.LD(Vh, Vh[:], vdv[:, :, h, :], self.r_v)
                    heads[h] = (Kh, Qh, Vh)
                return heads[h]
            its = [(h, qt, kc) for h in range(8) for qt in range(NT) for kc in range(32)]
            LA = 2
            pss = {}

            def emit_qk(i):
                h, qt, kc = its[i]
                Kh, Qh, Vh = get_head(h)
                ps = self.pA()
                self.MM(ps, ps[:], Kh, Kh[:, kc * 128:(kc + 1) * 128], Qh, Qh[:, qt * TT:(qt + 1) * TT], True, True)
                pss[i] = ps
            for i in range(min(LA, len(its))):
                emit_qk(i)
            po = None
            for i, (h, qt, kc) in enumerate(its):
                if i + LA < len(its):
                    emit_qk(i + LA)
                Kh, Qh, Vh = heads[h]
                if kc == 0:
                    po = self.pB()
                pov = po[:, 0:260].rearrange("p (b e) -> p b e", e=65)
                ps = pss.pop(i)
                pT = r_pT()
                self.ACT(pT, pT[:], ps, ps[:], AF.Exp, scale=sl)
                for qb in range(4):
                    self.MM(po, pov[:, qb, :], pT, pT[:, qb * 128:(qb + 1) * 128], Vh, Vh[:, kc, :], kc == 0, kc == 31)
                if kc == 31:
                    rec = r_rec()
                    self.V(k.dve, (lambda rec_, pov_: (lambda: nc.vector.reciprocal(out=rec_[:], in_=pov_[:, :, 64:65])))(rec, pov),
                           [po], [rec])
                    at = r_at()
                    self.TT_(at, at[:], po, pov[:, :, 0:64], rec, rec[:].to_broadcast([128, 4, 64]), ALU.mult)
                    self.ST(attv[:, qt * 4:(qt + 1) * 4, 64 * h:64 * h + 64], self.r_att, at, at[:])
                    emit_conv(2)
            emit_conv(1000)
        if True:
            wout = self.load_w("wout", self.wb["ev_w_out"][j], 8, D, self.wb_r[("ev_w_out", j, 0)])
            g1, b1 = self.ln_consts("ln1_g", "ln1_b", layer)
            r_sq = self.ring("sq3", [128, TT], F32, 2)
            mean = self.T("mean", [128, TT], F32)
            m2 = self.T("m2", [128, TT], F32)
            rstd = self.T("rstd3", [128, TT], F32)
            r_z = self.ring("z3", [128, TT], F32, 2)
            r_mix = self.ring("mixT", [128, 8, TT], BF16, 2)
            r_att = self.ring("att_in", [128, 512], BF16, 2)
            for t in range(NT):
                cols = slice(t * TT, (t + 1) * TT)
                pss, psq = self.pB(), self.pB()
                for jj in range(4):
                    self.MM(pss, pss[:], self.ones32, self.ones32[:], y_sb, y_sb[:, jj, cols], jj == 0, jj == 3)
                for jj in range(4):
                    sq = r_sq()
                    self.ACT(sq, sq[:], y_sb, y_sb[:, jj, cols], AF.Square)
                    self.MM(psq, psq[:], self.ones32, self.ones32[:], sq, sq[:], jj == 0, jj == 3)
                self.V(k.act, lambda: nc.scalar.mul(out=mean[:], in_=pss[:], mul=1.0 / 512.0), [pss], [mean])
                self.TT_(m2, m2[:], mean, mean[:], mean, mean[:], ALU.mult)
                self.STT(m2, m2[:], psq, psq[:], 1.0 / 512.0, m2, m2[:], ALU.mult, ALU.subtract)
                self.rsqrt_(rstd, rstd[:], m2, m2[:], 1.0, self.eps_ln)
                mixT = r_mix()
                for jj in range(4):
                    z = r_z()
                    self.TT_(z, z[:], y_sb, y_sb[:, jj, cols], mean, mean[:], ALU.subtract)
                    self.TT_(z, z[:], z, z[:], rstd, rstd[:], ALU.mult)
                    self.ACT(mixT, mixT[:, jj, :], z, z[:], AF.Silu, bias=clb[:, jj:jj + 1], scale=clg[:, jj:jj + 1],
                             extra=[clb, clg])
                for s in range(4):
                    at = r_att()
                    r0 = t * TT + s * 128
                    self.LD(at, at[:], self.att_d[r0:r0 + 128, :], self.r_att)
                    for fc in range(4):
                        self.TR(self.ptr, self.ptr[:, fc * 128:(fc + 1) * 128], at, at[:, fc * 128:(fc + 1) * 128], self.ident16)
                    self.CP(mixT, mixT[:, 4:8, s * 128:(s + 1) * 128],
                            self.ptr, self.ptr[:, 0:512].rearrange("p (c s) -> p c s", c=4), eng=k.act)
                for sp in range(2):
                    items = []
                    for s in (2 * sp, 2 * sp + 1):
                        phs = [self.pA(), self.pA()]
                        for half in range(2):
                            for c in range(8):
                                self.MM(phs[half], phs[half][:], mixT, mixT[:, c, s * 128:(s + 1) * 128],
                                        wout, wout[:, c, half * 512:(half + 1) * 512], c == 0, c == 7)
                        items.append(([(phs[0], phs[0][:]), (phs[1], phs[1][:])], t * TT + s * 128))
                    self.epilogue_multi(items, g1, b1, None, "e3", dbg_idx=2 * layer, nch=2)
        outer.__exit__(None, None, None)

    def ffn_like(self, layer, j, moe, dst):
        nc, k = self.nc, self.k
        self.eps_tiles()
        W = self.w
        GC = 256
        pre = "moe" if moe else "ffn"
        nexp = NE if moe else 1
        dff = D_FFE if moe else D_FF
        nf = dff // 128
        ng = dff // GC
        with self.phase():
            g2, b2 = self.ln_consts("ln2_g", "ln2_b", layer)
            xTv = self.xT.rearrange("(c p) s -> p c s", p=128)
            r_xt = self.ring("xt", [128, 8, TT], BF16, 2)
            hT = self.T("hT", [128, nf, TT], BF16)
            r_wg = self.ring("wg", [128, 8, GC], BF16, 2)
            r_wu = self.ring("wu", [128, 8, GC], BF16, 2)
            r_wd = self.ring("wd", [128, nf, 512], BF16, 2)
            acc = self.T("acc", [128, 4, D], F32)
            r_sg = self.ring("sg", [128, TT], F32, 2)
            if moe:
                wr32 = self.T("wr32", [128, 8, NE], F32)
                k.dma(k.sp, [(wr32[:], W["moe_router_w"][j].rearrange("(c p) e -> p c e", p=128))], [], [wr32.r])
                rb = self.load_bcast("rb", W["moe_router_b"][j:j + 1, :], NE)
                r_x32 = self.ring("x32", [128, D], F32, 2)
                xT32 = self.T("xT32", [128, 8, 128], F32)
                comb = self.T("comb", [128, 4, NE], F32)
                lg = self.T("lg", [128, NE], F32)
                l2 = self.T("l2", [128, NE], F32)
                mk1 = self.T("mk1", [128, NE], F32)
                mk2 = self.T("mk2", [128, NE], F32)
                sm = self.T("sm", [128, 8], F32)
            for t in range(NT):
                xt = r_xt()
                self.LD(xt, xt[:], xTv[:, :, 2 + t * TT:2 + (t + 1) * TT], self.r_xT)
                if moe:
                    for s in range(4):
                        r0 = t * TT + s * 128
                        x32 = r_x32()
                        self.LD(x32, x32[:], self.xres[r0:r0 + 128, :], self.r_xres)
                        pts = [self.pB(), self.pB()]
                        for c in range(8):
                            pt = pts[c // 4]
                            self.TR(pt, pt[:, (c % 4) * 128:(c % 4 + 1) * 128], x32, x32[:, c * 128:(c + 1) * 128], self.ident32)
                        for hh in range(2):
                            self.CP(xT32, xT32[:, hh * 4:(hh + 1) * 4, :].rearrange("p c s -> p (c s)"), pts[hh], pts[hh][:],
                                    eng=(k.act if hh else k.dve))
                        pr = self.pB()
                        for c in range(8):
                            self.MM(pr, pr[:, 0:NE], xT32, xT32[:, c, :], wr32, wr32[:, c, :], c == 0, c == 7)
                        self.TT_(lg, lg[:], pr, pr[:, 0:NE], rb, rb[:], ALU.add)
                        self.V(k.dve, lambda: nc.vector.tensor_reduce(out=sm[:, 0:1], in_=lg[:], axis=AX.X, op=ALU.max), [lg], [sm])
                        self.TS(mk1, mk1[:], lg, lg[:], sm[:, 0:1], None, ALU.is_equal, extra=[sm])
                        self.STT(l2, l2[:], mk1, mk1[:], -1.0e30, lg, lg[:], ALU.mult, ALU.add)
                        self.V(k.dve, lambda: nc.vector.tensor_reduce(out=sm[:, 1:2], in_=l2[:], axis=AX.X, op=ALU.max), [l2], [sm])
                        self.TS(mk2, mk2[:], l2, l2[:], sm[:, 1:2], None, ALU.is_equal, extra=[sm])
                        self.TT_(sm, sm[:, 2:3], sm, sm[:, 1:2], sm, sm[:, 0:1], ALU.subtract)
                        self.ACT(sm, sm[:, 3:4], sm, sm[:, 2:3], AF.Exp)
                        self.TS(sm, sm[:, 4:5], sm, sm[:, 3:4], 1.0, None, ALU.add)
                        self.V(k.dve, lambda: nc.vector.reciprocal(out=sm[:, 5:6], in_=sm[:, 4:5]), [sm], [sm])
                        self.TT_(sm, sm[:, 6:7], sm, sm[:, 3:4], sm, sm[:, 5:6], ALU.mult)
                        self.TS(comb, comb[:, s, :], mk1, mk1[:], sm[:, 5:6], None, ALU.mult, extra=[sm])
                        self.STT(comb, comb[:, s, :], mk2, mk2[:], sm[:, 6:7], comb, comb[:, s, :], ALU.mult, ALU.add, extra=[sm])
                for e in range(nexp):
                    wg_src = self.wb["%s_w_gate" % pre][j, e]
                    wu_src = self.wb["%s_w_up" % pre][j, e]
                    wd_src = self.wb["%s_w_down" % pre][j, e]
                    rg = self.wb_r[("%s_w_gate" % pre, j, e)]
                    ru = self.wb_r[("%s_w_up" % pre, j, e)]
                    rd = self.wb_r[("%s_w_down" % pre, j, e)]
                    for g in range(ng):
                        wg, wu = r_wg(), r_wu()
                        self.LD(wg, wg[:], wg_src[g], rg)
                        self.LD(wu, wu[:], wu_src[g], ru)
                        for fi in range(GC // 128):
                            f = g * (GC // 128) + fi
                            pg, pu = self.pA(), self.pA()
                            for c in range(8):
                                self.MM(pg, pg[:], wg, wg[:, c, fi * 128:(fi + 1) * 128], xt, xt[:, c, :], c == 0, c == 7)
                            for c in range(8):
                                self.MM(pu, pu[:], wu, wu[:, c, fi * 128:(fi + 1) * 128], xt, xt[:, c, :], c == 0, c == 7)
                            sg = r_sg()
                            self.ACT(sg, sg[:], pg, pg[:], AF.Silu)
                            self.TT_(hT, hT[:, f, :], sg, sg[:], pu, pu[:], ALU.mult)
                    for half in range(2):
                        wd = r_wd()
                        self.LD(wd, wd[:], wd_src[half], rd)
                        for s in range(4):
                            po = self.pB()
                            for f in range(nf):
                                self.MM(po, po[:], hT, hT[:, f, s * 128:(s + 1) * 128], wd, wd[:, f, :], f == 0, f == nf - 1)
                            aap = acc[:, s, half * 512:(half + 1) * 512]
                            if not moe:
                                self.CP(acc, aap, po, po[:], eng=(k.act if s % 2 else k.dve))
                            elif e == 0:
                                self.TS(acc, aap, po, po[:], comb[:, s, e:e + 1], None, ALU.mult, extra=[comb])
                            else:
                                self.STT(acc, aap, po, po[:], comb[:, s, e:e + 1], acc, aap, ALU.mult, ALU.add, extra=[comb])
                for sp in range(2):
                    items = [([(acc, acc[:, s, 0:512]), (acc, acc[:, s, 512:1024])], t * TT + s * 128) for s in (2 * sp, 2 * sp + 1)]
                    self.epilogue_multi(items, g2, b2, dst, "ffn", dbg_idx=2 * layer + 1, nch=2)

    def moe_sparse(self, layer, j, dst):
        nc, k = self.nc, self.k
        self.eps_tiles()
        W = self.w
        GC = GCM
        nf = D_FFE // 128
        ng = D_FFE // GC
        if not hasattr(self, "xs_g"):
            self.xs_g = self.dscr("xs_g", [NTL * 512, D], BF16)
            self.r_xs_g = k.dres("xs_g", 4)
            self.ys_g = self.dscr("ys_g", [NTL * 512, D], F32)
            self.r_ys_g = k.dres("ys_g", 2)
            zt = self.TP("zrow16", [128, D], BF16)
            self.V(k.dve, lambda: nc.vector.memset(zt[:], 0.0), [], [zt])
            for i in range(NTL * 4):
                k.dma(k.sp, [(self.xs_g[i * 128:(i + 1) * 128, :], zt[:])], [zt.r], [self.r_xs_g], partial=True)
        rg = self.wb_r[("moe_w_gate", j, 0)]
        wbg2 = self.wb["moe_w_gate"].rearrange("j e g p c n -> (j e g p) (c n)")
        wbu2 = self.wb["moe_w_up"].rearrange("j e g p c n -> (j e g p) (c n)")
        wbd2 = self.wb["moe_w_down"].rearrange("j e h p f d -> (j e h p) (f d)")
        with self.phase():
            desti = self.T("desti", [128, 32, 2], I32)
            g12 = self.T("g12", [128, 32, 2], F32)
            widx = self.T("widx", [128, NTL, 9], I32)
            with self.subphase():
                wr32 = self.T("wr32", [128, 8, NE], F32)
                k.dma(k.sp, [(wr32[:], W["moe_router_w"][j].rearrange("(c p) e -> p c e", p=128))], [], [wr32.r])
                rb = self.load_bcast("rb", W["moe_router_b"][j:j + 1, :], NE)
                U = self.T("Uinc", [128, 128], F32)
                self.LD(U, U[:], self.c_tri[0])
                r_x32 = self.ring("x32", [128, D], F32, 3)
                xT32 = self.T("xT32", [128, 8, 128], F32)
                lg = self.T("lg", [128, NE], F32)
                l2 = self.T("l2", [128, NE], F32)
                sm = self.T("sm", [128, 8], F32)
                m1all = self.T("m1all", [128, 32, NE], F32)
                m2all = self.T("m2all", [128, 32, NE], F32)
                wdesc = self.T("wdesc", [128, NE], F32)
                for e in range(NE):
                    self.V(k.dve, (lambda e_: (lambda: nc.vector.memset(wdesc[:, e_:e_ + 1], float(NE - e_))))(e), [], [wdesc])
                tsc = self.T("tsc", [128, NE], F32)

                def onehot_first(mt, map_):
                    self.TT_(tsc, tsc[:], mt, map_, wdesc, wdesc[:], ALU.mult)
                    self.V(k.dve, lambda: nc.vector.tensor_reduce(out=sm[:, 7:8], in_=tsc[:], axis=AX.X, op=ALU.max), [tsc], [sm])
                    self.TS(mt, map_, tsc, tsc[:], sm[:, 7:8], None, ALU.is_equal, extra=[sm])
                for st in range(32):
                    r0 = st * 128
                    x32 = r_x32()
                    self.LD(x32, x32[:], self.xres[r0:r0 + 128, :], self.r_xres)
                    pts = [self.pB(), self.pB()]
                    for c in range(8):
                        pt = pts[c // 4]
                        self.TR(pt, pt[:, (c % 4) * 128:(c % 4 + 1) * 128], x32, x32[:, c * 128:(c + 1) * 128], self.ident32)
                    for hh in range(2):
                        self.CP(xT32, xT32[:, hh * 4:(hh + 1) * 4, :].rearrange("p c s -> p (c s)"), pts[hh], pts[hh][:],
                                eng=(k.act if hh else k.dve))
                    pr = self.pB()
                    for c in range(8):
                        self.MM(pr, pr[:, 0:NE], xT32, xT32[:, c, :], wr32, wr32[:, c, :], c == 0, c == 7)
                    self.TT_(lg, lg[:], pr, pr[:, 0:NE], rb, rb[:], ALU.add)
                    self.V(k.dve, lambda: nc.vector.tensor_reduce(out=sm[:, 0:1], in_=lg[:], axis=AX.X, op=ALU.max), [lg], [sm])
                    self.TS(m1all, m1all[:, st, :], lg, lg[:], sm[:, 0:1], None, ALU.is_equal, extra=[sm])
                    onehot_first(m1all, m1all[:, st, :])
                    self.STT(l2, l2[:], m1all, m1all[:, st, :], -1.0e30, lg, lg[:], ALU.mult, ALU.add)
                    self.V(k.dve, lambda: nc.vector.tensor_reduce(out=sm[:, 1:2], in_=l2[:], axis=AX.X, op=ALU.max), [l2], [sm])
                    self.TS(m2all, m2all[:, st, :], l2, l2[:], sm[:, 1:2], None, ALU.is_equal, extra=[sm])
                    onehot_first(m2all, m2all[:, st, :])
                    self.TT_(sm, sm[:, 2:3], sm, sm[:, 1:2], sm, sm[:, 0:1], ALU.subtract)
                    self.ACT(sm, sm[:, 3:4], sm, sm[:, 2:3], AF.Exp)
                    self.TS(sm, sm[:, 4:5], sm, sm[:, 3:4], 1.0, None, ALU.add)
                    self.V(k.dve, lambda: nc.vector.reciprocal(out=g12[:, st, 0:1], in_=sm[:, 4:5]), [sm], [g12])
                    self.TT_(g12, g12[:, st, 1:2], sm, sm[:, 3:4], g12, g12[:, st, 0:1], ALU.mult)
                sel = self.T("sel", [128, 32, NE], F32)
                excl = self.T("excl", [128, 32, NE], F32)
                tot = self.T("tot", [128, 32, NE], F32)
                pre = self.T("pre", [128, 32, NE], F32)
                flat = lambda t_: t_[:].rearrange("p s e -> p (s e)")
                self.TT_(sel, sel[:], m1all, m1all[:], m2all, m2all[:], ALU.add)
                pinc, ptot = self.pB(), self.pB()
                self.MM(pinc, pinc[:, 0:256], U, U[:], sel, flat(sel), True, True)
                self.MM(ptot, ptot[:, 0:256], self.ones32, self.ones32[:], sel, flat(sel), True, True)
                self.TT_(excl, flat(excl), pinc, pinc[:, 0:256], sel, flat(sel), ALU.subtract)
                self.CP(tot, flat(tot), ptot, ptot[:, 0:256])
                self.V(k.dve, lambda: nc.vector.memset(pre[:, 0, :], 0.0), [], [pre])
                for st in range(1, 32):
                    self.TT_(pre, pre[:, st, :], pre, pre[:, st - 1, :], tot, tot[:, st - 1, :], ALU.add)
                cnt = self.T("cnt", [128, NE], F32)
                self.TT_(cnt, cnt[:], pre, pre[:, 31, :], tot, tot[:, 31, :], ALU.add)
                cmpk = self.T("cmpk", [128, 8, NE], F32)
                for kk in range(8):
                    self.TS(cmpk, cmpk[:, kk, :], cnt, cnt[:], 512.0 * kk, None, ALU.is_gt)
                padded = self.T("padded", [128, NE], F32)
                self.V(k.dve, lambda: nc.vector.tensor_reduce(out=padded[:], in_=cmpk[:].rearrange("p k e -> p e k"),
                                                              axis=AX.X, op=ALU.add), [cmpk], [padded])
                self.TS(padded, padded[:], padded, padded[:], 512.0, None, ALU.mult)
                base = self.T("base", [128, NE], F32)
                self.V(k.dve, lambda: nc.vector.memset(base[:, 0:1], 0.0), [], [base])
                for e in range(1, NE):
                    self.TT_(base, base[:, e:e + 1], base, base[:, e - 1:e], padded, padded[:, e - 1:e], ALU.add)
                cumend = self.T("cumend", [128, NE], F32)
                self.TT_(cumend, cumend[:], base, base[:], padded, padded[:], ALU.add)
                self.TT_(excl, excl[:], excl, excl[:], pre, pre[:], ALU.add)
                self.TT_(excl, excl[:], excl, excl[:], base, base[:].unsqueeze(1).to_broadcast([128, 32, NE]), ALU.add)
                destf = self.T("destf", [128, 32, 2], F32)
                for r, mall in ((0, m1all), (1, m2all)):
                    self.TT_(tot, tot[:], mall, mall[:], excl, excl[:], ALU.mult)
                    self.V(k.dve, (lambda r_: (lambda: nc.vector.tensor_reduce(out=destf[:, :, r_], in_=tot[:], axis=AX.X, op=ALU.add)))(r),
                           [tot], [destf])
                self.CP(desti, desti[:], destf, destf[:])
                cmpt = self.T("cmpt", [128, NTL, NE], F32)
                for t in range(NTL):
                    self.TS(cmpt, cmpt[:, t, :], cumend, cumend[:], 512.0 * t, None, ALU.is_le)
                etf = self.T("etf", [128, NTL], F32)
                self.V(k.dve, lambda: nc.vector.tensor_reduce(out=etf[:], in_=cmpt[:], axis=AX.X, op=ALU.add), [cmpt], [etf])
                self.TS(etf, etf[:], etf, etf[:], float(NE - 1), 0.0, ALU.min, ALU.max)
                self.TS(etf, etf[:], etf, etf[:], float(j * NE), None, ALU.add)
                pg = self.T("pg", [128, 9], F32)
                self.LD(pg, pg[:], self.c_pg)
                widf = self.T("widf", [128, NTL, 9], F32)
                for g in range(9):
                    self.TS(widf, widf[:, :, g], etf, etf[:], float(ng * 128 if g < 7 else 2 * 128), pg[:, g:g + 1],
                            ALU.mult, ALU.add, extra=[pg])
                self.CP(widx, widx[:], widf, widf[:])
                for st in range(32):
                    r0 = st * 128
                    x32 = r_x32()
                    self.LD(x32, x32[:], self.xres[r0:r0 + 128, :], self.r_xres)
                    for r in range(2):
                        k.idma((lambda x_, st_, r_: (lambda: nc.gpsimd.indirect_dma_start(
                            out=self.xs_g[:, :], out_offset=bass.IndirectOffsetOnAxis(ap=desti[:, st_, r_:r_ + 1], axis=0),
                            in_=x_[:, :], in_offset=None)))(x32, st, r),
                            [x32.r, desti.r], [self.r_xs_g], partial=True)
            with self.subphase():
                r_xg = self.ring("xg", [128, 4, D], BF16, 2)
                r_xt = self.ring("xtg", [128, 8, TT], BF16, 2)
                hT = self.T("hTg", [128, nf, TT], BF16)
                r_wg = self.ring("wgg", [128, 8, GC], BF16, 2)
                r_wu = self.ring("wug", [128, 8, GC], BF16, 2)
                r_wd = self.ring("wdg", [128, nf, 512], BF16, 2)
                r_sg = self.ring("sgg", [128, TT], F32, 2)
                r_ysb = self.ring("ysb", [128, 4, D], F32, 1)
                for t in range(NTL):
                    def wgather(dst_t, src2d, col):
                        k.idma((lambda d_, c_: (lambda: nc.gpsimd.indirect_dma_start(
                            out=d_, out_offset=None, in_=src2d,
                            in_offset=bass.IndirectOffsetOnAxis(ap=widx[:, t, c_:c_ + 1], axis=0))))(dst_t[:].rearrange(
                                "p a b -> p (a b)"), col), [rg, widx.r], [dst_t.r])
                    xg = r_xg()
                    self.LD(xg, xg[:], self.xs_g[t * 512:(t + 1) * 512, :].rearrange("(s p) d -> p s d", p=128), self.r_xs_g)
                    xt = r_xt()
                    for s in range(4):
                        for c in range(8):
                            self.TR(self.ptr, self.ptr[:, c * 128:(c + 1) * 128], xg, xg[:, s, c * 128:(c + 1) * 128], self.ident16)
                        self.CP(xt, xt[:, :, s * 128:(s + 1) * 128], self.ptr, self.ptr[:].rearrange("p (c s) -> p c s", c=8),
                                eng=(k.act if s % 2 else k.dve))
                    for g in range(ng):
                        wg, wu = r_wg(), r_wu()
                        wgather(wg, wbg2, g)
                        wgather(wu, wbu2, g)
                        for fi in range(GC // 128):
                            f = g * (GC // 128) + fi
                            pg, pu = self.pA(), self.pA()
                            for c in range(8):
                                self.MM(pg, pg[:], wg, wg[:, c, fi * 128:(fi + 1) * 128], xt, xt[:, c, :], c == 0, c == 7)
                            for c in range(8):
                                self.MM(pu, pu[:], wu, wu[:, c, fi * 128:(fi + 1) * 128], xt, xt[:, c, :], c == 0, c == 7)
                            sg = r_sg()
                            self.ACT(sg, sg[:], pg, pg[:], AF.Silu)
                            self.TT_(hT, hT[:, f, :], sg, sg[:], pu, pu[:], ALU.mult)
                    ysb = r_ysb()
                    for half in range(2):
                        wd = r_wd()
                        wgather(wd, wbd2, 7 + half)
                        for s in range(4):
                            po = self.pB()
                            for f in range(nf):
                                self.MM(po, po[:], hT, hT[:, f, s * 128:(s + 1) * 128], wd, wd[:, f, :], f == 0, f == nf - 1)
                            self.CP(ysb, ysb[:, s, half * 512:(half + 1) * 512], po, po[:], eng=(k.act if s % 2 else k.dve))
                    self.ST(self.ys_g[t * 512:(t + 1) * 512, :].rearrange("(s p) d -> p s d", p=128), self.r_ys_g, ysb, ysb[:])
            with self.subphase():
                g2, b2 = self.ln_consts("ln2_g", "ln2_b", layer)
                r_ga = self.ring("ga", [128, D], F32, 12)
                r_f = self.ring("fmo", [128, D], F32, 8)
                NB = 4
                for sb in range(32 // NB):
                    items = []
                    for st in range(sb * NB, (sb + 1) * NB):
                        gas = []
                        for r in range(2):
                            ga = r_ga()
                            k.idma((lambda g_, st_, r_: (lambda: nc.gpsimd.indirect_dma_start(
                                out=g_[:, :], out_offset=None, in_=self.ys_g[:, :],
                                in_offset=bass.IndirectOffsetOnAxis(ap=desti[:, st_, r_:r_ + 1], axis=0))))(ga, st, r),
                                [self.r_ys_g, desti.r], [ga.r])
                            gas.append(ga)
                        f = r_f()
                        self.TS(f, f[:], gas[0], gas[0][:], g12[:, st, 0:1], None, ALU.mult, extra=[g12])
                        self.STT(f, f[:], gas[1], gas[1][:], g12[:, st, 1:2], f, f[:], ALU.mult, ALU.add, extra=[g12])
                        items.append(([(f, f[:, 0:512]), (f, f[:, 512:1024])], st * 128))
                    self.epilogue_multi(items, g2, b2, dst, "moe", dbg_idx=2 * layer + 1, nch=NB)

    def odd_mixer(self, j, layer):
        nc, k = self.nc, self.k
        self.eps_tiles()
        W = self.w
        if not hasattr(self, "z_d"):
            def mk(name, shape, dt):
                setattr(self, name, self.dscr(name, shape, dt))
                setattr(self, "r_" + name, k.dres(name, 2))
            mk("z_d", [S, 512], F32); mk("xs_d", [S, 512], F32); mk("bmt_d", [S, 256], BF16)
            mk("bcT_d", [4, 128, S], BF16); mk("dtda_d", [S, 32], F32); mk("qT_d", [8, 64, S], BF16)
            mk("kT_d", [8, 64, S], BF16); mk("kt_d", [S, 512], BF16); mk("v2_d", [S, 8 * 65], BF16)
            mk("og_d", [S, 512], F32); mk("gate_d", [S, 32], F32); mk("yf_d", [S, 512], F32)
            mk("hf_d", [S, 512], F32); mk("wcf_d", [5, D, 1024], BF16)
        wi_r = self.wb_r[("od_w_in", j, 0)]
        wi = self.wb["od_w_in"][j]
        with self.phase():
            cwb = [self.load_bcast("cwb%d" % tap, W["ssd_conv_w"][j, tap:tap + 1, :], 1024) for tap in range(5)]
            r_wr = self.ring("wrow", [128, 1024], F32, 2)
            r_wo = self.ring("wcfo", [128, 1024], BF16, 3)
            for rc in range(8):
                wr_ = r_wr()
                self.LD(wr_, wr_[:], W["od_w_in"][j, rc * 128:(rc + 1) * 128, 512:1536])
                for tap in range(5):
                    wo = r_wo()
                    self.TT_(wo, wo[:], wr_, wr_[:], cwb[tap], cwb[tap][:], ALU.mult)
                    self.ST(self.wcf_d[tap, rc * 128:(rc + 1) * 128, :], self.r_wcf_d, wo, wo[:])
        with self.phase():
            xTs = self.load_xT_full()
            cbias = self.load_bcast("cbias", W["ssd_conv_b"][j:j + 1, :], 1024)
            cbcol = self.load_col("cbcol", W["ssd_conv_b"][j], 8)
            dtb = self.load_bcast("dtb", W["ssd_dt_bias"][j:j + 1, :], 16)
            alog = self.load_bcast("alog", W["ssd_a_log"][j:j + 1, :], 16)
            igb = self.load_bcast("igb", W["ml_igate_b"][j:j + 1, :], 16)
            fgb = self.load_bcast("fgb", W["ml_fgate_b"][j:j + 1, :], 16)
            abc = self.T("abc", [128, 16], F32)
            self.ACT(abc, abc[:], alog, alog[:], AF.Exp)
            self.TS(abc, abc[:], abc, abc[:], -1.0, None, ALU.mult)
            wiv = wi.rearrange("(c p) n -> p c n", p=128)
            wcv = self.wcf_d.rearrange("t (c p) n -> p t c n", p=128)
            r_o32 = self.ring("o32", [128, 512], F32, 3)
            r_o16 = self.ring("o16", [128, 512], BF16, 3)
            r_vt = self.ring("vt2", [128, 8, 65], BF16, 3, init=1.0)
            r_sm = self.ring("smo", [128, 64], F32, 3)

            def tok_group(wt, ncols, conv, post):
                for st in range(S // 128):
                    t0 = st * 128
                    ps = self.pA()
                    if conv:
                        n = 0
                        for tap in range(5):
                            for c in range(8):
                                self.MM(ps, ps[:, 0:ncols], xTs, xTs[:, c, t0 + tap:t0 + tap + 128], wt, wt[:, tap, c, :],
                                        n == 0, n == 39)
                                n += 1
                    else:
                        for c in range(8):
                            self.MM(ps, ps[:, 0:ncols], xTs, xTs[:, c, 2 + t0:2 + t0 + 128], wt, wt[:, c, :], c == 0, c == 7)
                    post(ps, t0, st)

            def load_plain(c0, ncols, name):
                t = self.T(name, [128, 8, ncols], BF16)
                k.dma(k.sp, [(t[:], wiv[:, :, c0:c0 + ncols])], [wi_r], [t.r])
                return t

            def load_conv(c0, ncols, name):
                t = self.T(name, [128, 5, 8, ncols], BF16)
                k.dma(k.sp, [(t[:, tap, :, :], wcv[:, tap, :, c0:c0 + ncols]) for tap in range(5)], [self.r_wcf_d], [t.r])
                return t

            def post_z(ps, t0, st):
                o = r_o32()
                self.ACT(o, o[:], ps, ps[:], AF.Silu)
                self.ST(self.z_d[t0:t0 + 128, :], self.r_z_d, o, o[:])
            sub = self.subphase()
            sub.__enter__()
            tok_group(load_plain(0, 512, "w_z"), 512, False, post_z)

            def post_o(ps, t0, st):
                o = r_o32()
                self.ACT(o, o[:], ps, ps[:], AF.Sigmoid)
                self.ST(self.og_d[t0:t0 + 128, :], self.r_og_d, o, o[:])
            tok_group(load_plain(3088, 512, "w_o"), 512, False, post_o)

            def post_k(ps, t0, st):
                o = r_o16()
                self.V(k.act, lambda: nc.scalar.mul(out=o[:], in_=ps[:], mul=0.125), [ps], [o])
                self.ST(self.kt_d[t0:t0 + 128, :], self.r_kt_d, o, o[:])
            tok_group(load_plain(2064, 512, "w_k"), 512, False, post_k)

            def post_v(ps, t0, st):
                vt = r_vt()
                self.CP(vt, vt[:, :, 0:64], ps, ps[:].rearrange("p (h e) -> p h e", e=64), eng=(k.act if st % 2 else k.dve))
                self.ST(self.v2_d[t0:t0 + 128, :], self.r_v2_d, vt, vt[:].rearrange("p h e -> p (h e)"))
            tok_group(load_plain(2576, 512, "w_v"), 512, False, post_v)
            sub.__exit__(None, None, None)

            def post_xs(ps, t0, st):
                o = r_o32()
                self.TT_(o, o[:], ps, ps[:], cbias, cbias[:, 0:512], ALU.add)
                self.ACT(o, o[:], o, o[:], AF.Silu)
                self.ST(self.xs_d[t0:t0 + 128, :], self.r_xs_d, o, o[:])
            sub = self.subphase()
            sub.__enter__()
            tok_group(load_conv(0, 512, "w_xs"), 512, True, post_xs)

            def post_bm(ps, t0, st):
                o = r_o32()
                self.TT_(o, o[:, 0:256], ps, ps[:, 0:256], cbias, cbias[:, 512:768], ALU.add)
                o2 = r_o16()
                self.ACT(o2, o2[:, 0:256], o, o[:, 0:256], AF.Silu)
                self.ST(self.bmt_d[t0:t0 + 128, :], self.r_bmt_d, o2, o2[:, 0:256])
            tok_group(load_conv(512, 256, "w_bm"), 256, True, post_bm)

            def post_small(ps, t0, st):
                sm = r_sm()
                self.TT_(sm, sm[:, 0:16], ps, ps[:, 0:16], dtb, dtb[:], ALU.add)
                self.ACT(sm, sm[:, 0:16], sm, sm[:, 0:16], AF.Exp)
                self.ACT(sm, sm[:, 0:16], sm, sm[:, 0:16], AF.Ln, bias=1.0, scale=1.0)
                self.TT_(sm, sm[:, 16:32], sm, sm[:, 0:16], abc, abc[:], ALU.mult)
                self.ST(self.dtda_d[t0:t0 + 128, :], self.r_dtda_d, sm, sm[:, 0:32])
                sm2 = r_sm()
                self.TT_(sm2, sm2[:, 0:16], ps, ps[:, 16:32], igb, igb[:], ALU.add)
                self.TT_(sm2, sm2[:, 16:32], ps, ps[:, 32:48], fgb, fgb[:], ALU.add)
                self.ACT(sm2, sm2[:, 16:32], sm2, sm2[:, 16:32], AF.Exp, scale=-1.0)
                self.ACT(sm2, sm2[:, 16:32], sm2, sm2[:, 16:32], AF.Ln, bias=1.0, scale=1.0)
                self.TS(sm2, sm2[:, 16:32], sm2, sm2[:, 16:32], -1.0, None, ALU.mult)
                self.ST(self.gate_d[t0:t0 + 128, :], self.r_gate_d, sm2, sm2[:, 0:32])
            wsm = self.T("w_sm", [128, 8, 48], BF16)
            k.dma(k.sp, [(wsm[:, :, 0:16], wiv[:, :, 1536:1552]), (wsm[:, :, 16:48], wiv[:, :, 3600:3632])], [wi_r], [wsm.r])
            tok_group(wsm, 48, False, post_small)
            sub.__exit__(None, None, None)

            r_f16 = self.ring("f16", [128, TT], BF16, 3)
            with self.subphase():
                wbc = load_conv(512, 512, "w_bcT")
                for t in range(NT):
                    for i in range(4):
                        ps = self.pA()
                        n = 0
                        for tap in range(5):
                            for c in range(8):
                                self.MM(ps, ps[:], wbc, wbc[:, tap, c, i * 128:(i + 1) * 128],
                                        xTs, xTs[:, c, t * TT + tap:t * TT + tap + TT], n == 0, n == 39)
                                n += 1
                        o = r_f16()
                        self.ACT(o, o[:], ps, ps[:], AF.Silu, bias=cbcol[:, 4 + i:5 + i], scale=1.0, extra=[cbcol])
                        self.ST(self.bcT_d[i][:, t * TT:(t + 1) * TT], self.r_bcT_d, o, o[:])
            for (c0, dst, rdst, scl, nm) in ((1552, self.qT_d, self.r_qT_d, 1.0, "w_qT"), (2064, self.kT_d, self.r_kT_d, 0.125, "w_kT")):
                with self.subphase():
                    wq = load_plain(c0, 512, nm)
                    for t in range(NT):
                        for h in range(8):
                            ps = self.pA()
                            for c in range(8):
                                self.MM(ps, ps[0:64, :], wq, wq[:, c, h * 64:(h + 1) * 64], xTs, xTs[:, c, 2 + t * TT:2 + (t + 1) * TT],
                                        c == 0, c == 7)
                            o = r_f16()
                            self.V(k.act, (lambda o_, ps_: (lambda: nc.scalar.mul(out=o_[0:64, :], in_=ps_[0:64, :], mul=scl)))(o, ps), [ps], [o])
                            self.ST(dst[h][:, t * TT:(t + 1) * TT], rdst, o, o[0:64, :])
        for direction in (0, 1):
            with self.phase():
                self.scan_pass(j, layer, direction)

    def subphase(self):
        prog = self

        class _S:
            def __enter__(s):
                s.outer = prog.ph
                s.st = ExitStack()
                s.st.__enter__()
                prog.ph = s.st
                return s

            def __exit__(s, *a):
                if a[0] is None:
                    prog.k.barrier(closing=s.st)
                s.st.__exit__(*a)
                prog.ph = s.outer
                return False
        return _S()

    def scan_pass(self, j, layer, dr):
        nc, k = self.nc, self.k
        W = self.w
        last = (dr == 1)
        U = self.T("Udir", [128, 128], F32)
        self.LD(U, U[:], self.c_tri[dr])
        mask = self.T("mdir", [128, 128], F32)
        self.LD(mask, mask[:], self.c_tri[2 + dr])
        Sst = self.T("Sst", [128, 8, 64], F32)
        S16 = self.T("S16", [128, 8, 64], BF16)
        Cst = self.T("Cst", [64, 8, 65], F32)
        C16 = self.T("C16", [64, 8, 65], BF16)
        for t_ in (Sst, S16, Cst, C16):
            self.V(k.dve, (lambda tt: (lambda: nc.vector.memset(tt[:], 0.0)))(t_), [], [t_])
        R = self.ring
        r_xs = R("xs", [128, 512], F32, 2); r_dtda = R("dtda", [128, 32], F32, 2); r_bmt = R("bmt", [128, 256], BF16, 2)
        r_bcT = R("bcT", [128, 4, 128], BF16, 2); r_qT = R("qT", [64, 8, 128], BF16, 2); r_kT = R("kT", [64, 8, 128], BF16, 2)
        r_kt = R("kt", [128, 512], BF16, 2); r_v2 = R("v2", [128, 8, 65], BF16, 2); r_gate = R("gate", [128, 32], F32, 2)
        r_sc = R("sc", [128, 32], F32, 2); r_wall = R("wall", [128, 8, 128], F32, 2); r_tR = R("tR", [128, 8, 128], F32, 2)
        r_eD = R("eD", [128, 8, 128], F32, 2); r_cb = R("cb", [128, 2, 128], F32, 2); r_MT = R("MT", [128, 8, 128], BF16, 2)
        r_ex = R("ex", [128, 64], F32, 2); r_xdt = R("xdt", [128, 8, 64], BF16, 2); r_xdtd = R("xdtd", [128, 8, 64], BF16, 2)
        r_y = R("y", [128, 512], F32, 2); r_aT = R("aT", [128, 8, 128], BF16, 2); r_tot = R("tot", [128, 8, 65], F32, 2)
        r_hd = R("hd", [128, 8, 64], F32, 2); r_kd = R("kd", [128, 8, 64], BF16, 2)
        if last:
            r_yf = R("yf", [128, 512], F32, 2); r_hf = R("hf", [128, 512], F32, 2); r_zs = R("zs", [128, 512], F32, 2)
            r_og = R("og", [128, 512], F32, 2); r_mix = R("mix16", [128, D], BF16, 2); r_mixT = R("mixT2", [128, 8, 128], BF16, 2)
            r_tmp = R("tmp5", [128, 512], F32, 2)
            dsk = self.load_bcast("dsk", W["ssd_d"][j:j + 1, :], 8)
            ssdg = self.load_bcast("ssdg", W["ssd_norm_g"][j:j + 1, :], 512)
            mlg = self.load_bcast("mlg", W["ml_norm_g"][j:j + 1, :], 512)
            wout = self.load_w("wout2", self.wb["od_w_out"][j], 8, D, self.wb_r[("od_w_out", j, 0)])
            g1, b1 = self.ln_consts("ln1_g", "ln1_b", layer)
        bcTv = self.bcT_d.rearrange("i p s -> p i s")
        qTv = self.qT_d.rearrange("h p s -> p h s")
        kTv = self.kT_d.rearrange("h p s -> p h s")

        def bc3(ap2, n):
            return ap2.unsqueeze(2).to_broadcast([ap2.shape[0], ap2.shape[1], n])

        chunks = range(32) if dr == 0 else range(31, -1, -1)
        for c in chunks:
            r0 = c * 128
            rows = slice(r0, r0 + 128)
            xs, dtda, bmt, bcT, qT, kT, kt, v2, gate = r_xs(), r_dtda(), r_bmt(), r_bcT(), r_qT(), r_kT(), r_kt(), r_v2(), r_gate()
            self.LD(xs, xs[:], self.xs_d[rows, :], self.r_xs_d)
            self.LD(dtda, dtda[:], self.dtda_d[rows, :], self.r_dtda_d)
            self.LD(bmt, bmt[:], self.bmt_d[rows, :], self.r_bmt_d)
            self.LD(bcT, bcT[:], bcTv[:, :, rows], self.r_bcT_d)
            self.LD(qT, qT[:], qTv[:, :, rows], self.r_qT_d)
            self.LD(kT, kT[:], kTv[:, :, rows], self.r_kT_d)
            self.LD(kt, kt[:], self.kt_d[rows, :], self.r_kt_d)
            self.LD(v2, v2[:].rearrange("p h e -> p (h e)"), self.v2_d[rows, :], self.r_v2_d)
            self.LD(gate, gate[:], self.gate_d[rows, :], self.r_gate_d)
            dtd = dtda[:, dr * 8:dr * 8 + 8]
            dad = dtda[:, 16 + dr * 8:16 + dr * 8 + 8]
            lid = gate[:, dr * 8:dr * 8 + 8]
            lfd = gate[:, 16 + dr * 8:16 + dr * 8 + 8]
            pcol = self.pB()
            self.MM(pcol, pcol[:, 0:8], U, U[:], dtda, dad, True, True)
            self.MM(pcol, pcol[:, 8:16], self.ones32, self.ones32[:], dtda, dad, True, True)
            self.MM(pcol, pcol[:, 16:24], U, U[:], gate, lfd, True, True)
            self.MM(pcol, pcol[:, 24:32], self.ones32, self.ones32[:], gate, lfd, True, True)
            sc = r_sc()
            self.CP(sc, sc[:], pcol, pcol[:, 0:32])
            ex = r_ex()
            self.ACT(ex, ex[:, 0:16], sc, sc[:, 0:16], AF.Exp)
            self.TT_(ex, ex[:, 16:24], sc, sc[:, 8:16], sc, sc[:, 0:8], ALU.subtract)
            self.ACT(ex, ex[:, 16:24], ex, ex[:, 16:24], AF.Exp)
            self.TT_(ex, ex[:, 24:32], dtda, dtd, ex, ex[:, 16:24], ALU.mult)
            self.ACT(ex, ex[:, 32:48], sc, sc[:, 16:32], AF.Exp)
            self.TT_(ex, ex[:, 48:56], sc, sc[:, 24:32], sc, sc[:, 16:24], ALU.subtract)
            self.TT_(ex, ex[:, 48:56], ex, ex[:, 48:56], gate, lid, ALU.add)
            self.ACT(ex, ex[:, 48:56], ex, ex[:, 48:56], AF.Exp)
            self.TT_(ex, ex[:, 56:64], gate, lid, sc, sc[:, 16:24], ALU.subtract)

            def decay_mat(src_t, src_ap, shift_t, shift_ap, sign):
                wall = r_wall()
                self.TT_(wall, wall[:], U, U[:].unsqueeze(1).to_broadcast([128, 8, 128]), src_t, bc3(src_ap, 128), ALU.mult,
                         eng=k.pool)
                pR = [self.pA(), self.pA()]
                for hb in range(2):
                    self.MM(pR[hb], pR[hb][:], self.ones32, self.ones32[:], wall,
                            wall[:, hb * 4:(hb + 1) * 4, :].rearrange("p h l -> p (h l)"), True, True)
                tR = r_tR()
                for hb in range(2):
                    self.TT_(tR, tR[:, hb * 4:(hb + 1) * 4, :], pR[hb], pR[hb][:].rearrange("p (h l) -> p h l", h=4),
                             mask, mask[:].unsqueeze(1).to_broadcast([128, 4, 128]), ALU.add)
                self.TT_(tR, tR[:], tR, tR[:], shift_t, bc3(shift_ap, 128), ALU.subtract if sign < 0 else ALU.add)
                eD = r_eD()
                self.ACT(eD, eD[:], tR, tR[:], AF.Exp)
                return eD
            eD = decay_mat(dtda, dad, sc, sc[:, 0:8], -1)
            pcb = self.pB()
            for g in range(2):
                self.MM(pcb, pcb[:, g * 128:(g + 1) * 128], bcT, bcT[:, g, :], bcT, bcT[:, 2 + g, :], True, True)
            cb = r_cb()
            self.CP(cb, cb[:].rearrange("p g l -> p (g l)"), pcb, pcb[:, 0:256], eng=k.act)
            MT = r_MT()
            self.TT_(MT, MT[:].rearrange("p (g r) l -> p g r l", g=2), eD, eD[:].rearrange("p (g r) l -> p g r l", g=2),
                     cb, cb[:].unsqueeze(2).to_broadcast([128, 2, 4, 128]), ALU.mult)
            xv = xs[:].rearrange("p (h e) -> p h e", e=64)
            xdt, xdtd = r_xdt(), r_xdtd()
            self.TT_(xdt, xdt[:], xs, xv, dtda, bc3(dtd, 64), ALU.mult, eng=k.pool)
            self.TT_(xdtd, xdtd[:], xs, xv, ex, bc3(ex[:, 24:32], 64), ALU.mult, eng=k.pool)
            pyd, pyo = self.pA(), self.pA()
            for h in range(8):
                self.MM(pyd, pyd[:, h * 64:(h + 1) * 64], MT, MT[:, h, :], xdt, xdt[:, h, :], True, True)
            for h in range(8):
                self.MM(pyo, pyo[:, h * 64:(h + 1) * 64], bcT, bcT[:, 2 + h // 4, :], S16, S16[:, h, :], True, True)
            y = r_y()
            yv = y[:].rearrange("p (h e) -> p h e", e=64)
            self.TT_(y, yv, pyo, pyo[:].rearrange("p (h e) -> p h e", e=64), ex, bc3(ex[:, 0:8], 64), ALU.mult)
            self.TT_(y, y[:], y, y[:], pyd, pyd[:], ALU.add)
            pst = self.pB()
            for h in range(8):
                self.MM(pst, pst[:, h * 64:(h + 1) * 64], bmt, bmt[:, (h // 4) * 128:(h // 4 + 1) * 128], xdtd, xdtd[:, h, :], True, True)
            self.TT_(Sst, Sst[:], Sst, Sst[:], ex, bc3(ex[:, 8:16], 64), ALU.mult)
            self.TT_(Sst, Sst[:], Sst, Sst[:], pst, pst[:].rearrange("p (h e) -> p h e", e=64), ALU.add)
            self.CP(S16, S16[:], Sst, Sst[:], eng=k.act)
            wT = decay_mat(gate, lfd, ex, ex[:, 56:64], +1)
            pqk = [self.pA(), self.pA()]
            for h in range(8):
                self.MM(pqk[h // 4], pqk[h // 4][:, (h % 4) * 128:(h % 4 + 1) * 128], kT, kT[:, h, :], qT, qT[:, h, :], True, True)
            aT = r_aT()
            for hb in range(2):
                self.TT_(aT, aT[:, hb * 4:(hb + 1) * 4, :], pqk[hb], pqk[hb][:].rearrange("p (h l) -> p h l", h=4),
                         wT, wT[:, hb * 4:(hb + 1) * 4, :], ALU.mult)
            pn = [self.pA(), self.pA()]
            pi = [self.pA(), self.pA()]
            for h in range(8):
                self.MM(pn[h // 4], pn[h // 4][:, (h % 4) * 65:(h % 4 + 1) * 65], aT, aT[:, h, :], v2, v2[:, h, :], True, True)
            for h in range(8):
                self.MM(pi[h // 4], pi[h // 4][:, (h % 4) * 65:(h % 4 + 1) * 65], qT, qT[:, h, :], C16, C16[:, h, :], True, True)
            tot = r_tot()
            for hb in range(2):
                tv = tot[:, hb * 4:(hb + 1) * 4, :]
                self.TT_(tot, tv, pi[hb], pi[hb][:, 0:260].rearrange("p (h e) -> p h e", e=65),
                         ex, bc3(ex[:, 32 + hb * 4:32 + (hb + 1) * 4], 65), ALU.mult)
                self.TT_(tot, tv, tot, tv, pn[hb], pn[hb][:, 0:260].rearrange("p (h e) -> p h e", e=65), ALU.add)
            den = r_sc()
            self.ACT(den, den[:, 0:8], tot, tot[:, :, 64], AF.Abs)
            self.TS(den, den[:, 0:8], den, den[:, 0:8], 1.0, None, ALU.max)
            self.V(k.dve, lambda: nc.vector.reciprocal(out=den[:, 0:8], in_=den[:, 0:8]), [den], [den])
            hd = r_hd()
            self.TT_(hd, hd[:], tot, tot[:, :, 0:64], den, bc3(den[:, 0:8], 64), ALU.mult)
            kd = r_kd()
            self.TT_(kd, kd[:], kt, kt[:].rearrange("p (h e) -> p h e", e=64), ex, bc3(ex[:, 48:56], 64), ALU.mult, eng=k.pool)
            pC = [self.pB(), self.pB()]
            for h in range(8):
                self.MM(pC[h // 4], pC[h // 4][0:64, (h % 4) * 65:(h % 4 + 1) * 65], kd, kd[:, h, :], v2, v2[:, h, :], True, True)
            self.TT_(Cst, Cst[:], Cst, Cst[:], ex, bc3(ex[0:64, 40:48], 65), ALU.mult)
            for hb in range(2):
                cv = Cst[:, hb * 4:(hb + 1) * 4, :]
                self.TT_(Cst, cv, Cst, cv, pC[hb], pC[hb][0:64, 0:260].rearrange("p (h e) -> p h e", e=65), ALU.add)
            self.CP(C16, C16[:], Cst, Cst[:], eng=k.act)
            if not last:
                self.ST(self.yf_d[rows, :], self.r_yf_d, y, y[:])
                self.ST(self.hf_d[rows, :], self.r_hf_d, hd, hd[:].rearrange("p h e -> p (h e)"))
                continue
            yf, hf, zs, og = r_yf(), r_hf(), r_zs(), r_og()
            self.LD(yf, yf[:], self.yf_d[rows, :], self.r_yf_d)
            self.LD(hf, hf[:], self.hf_d[rows, :], self.r_hf_d)
            self.LD(zs, zs[:], self.z_d[rows, :], self.r_z_d)
            self.LD(og, og[:], self.og_d[rows, :], self.r_og_d)
            tmp = r_tmp()
            self.TT_(y, y[:], y, y[:], yf, yf[:], ALU.add)
            self.TT_(tmp, tmp[:].rearrange("p (h e) -> p h e", e=64), xs, xv, dsk, bc3(dsk[:, 0:8], 64), ALU.mult)
            self.TT_(y, y[:], y, y[:], tmp, tmp[:], ALU.add)
            self.TT_(y, y[:], y, y[:], zs, zs[:], ALU.mult)
            st = r_sc()
            for g in range(2):
                self.ACT(tmp, tmp[:, g * 256:(g + 1) * 256], y, y[:, g * 256:(g + 1) * 256], AF.Square, accum=st[:, g:g + 1],
                         extra=[])
            k.all_res
            st.r.w = tmp.r.w
            self.rsqrt_(st, st[:, 2:4], st, st[:, 0:2], 1.0 / 256.0, self.eps_rms)
            self.TT_(y, y[:].rearrange("p (g e) -> p g e", g=2), y, y[:].rearrange("p (g e) -> p g e", g=2),
                     st, bc3(st[:, 2:4], 256), ALU.mult)
            mix = r_mix()
            self.TT_(mix, mix[:, 0:512], y, y[:], ssdg, ssdg[:], ALU.mult)
            hv = hd[:]
            self.TT_(hd, hv, hd, hv, hf, hf[:].rearrange("p (h e) -> p h e", e=64), ALU.add)
            self.V(k.dve, lambda: nc.vector.tensor_reduce(out=st[:, 8:16], in_=hv, axis=AX.X, op=ALU.add), [hd], [st])
            self.TS(st, st[:, 8:16], st, st[:, 8:16], 1.0 / 64.0, None, ALU.mult)
            self.TT_(hd, hv, hd, hv, st, bc3(st[:, 8:16], 64), ALU.subtract)
            tv3 = tmp[:].rearrange("p (h e) -> p h e", e=64)
            self.TT_(tmp, tv3, hd, hv, hd, hv, ALU.mult)
            self.V(k.dve, lambda: nc.vector.tensor_reduce(out=st[:, 16:24], in_=tv3, axis=AX.X, op=ALU.add), [tmp], [st])
            self.rsqrt_(st, st[:, 24:32], st, st[:, 16:24], 1.0 / 64.0, self.eps_ln)
            self.TT_(hd, hv, hd, hv, st, bc3(st[:, 24:32], 64), ALU.mult)
            hflat = hd[:].rearrange("p h e -> p (h e)")
            self.TT_(hd, hflat, hd, hflat, mlg, mlg[:], ALU.mult)
            self.TT_(mix, mix[:, 512:1024], hd, hflat, og, og[:], ALU.mult)
            for cc in range(8):
                self.TR(self.ptr, self.ptr[:, cc * 128:(cc + 1) * 128], mix, mix[:, cc * 128:(cc + 1) * 128], self.ident16)
            mixT = r_mixT()
            self.CP(mixT, mixT[:].rearrange("p c s -> p (c s)"), self.ptr, self.ptr[:], eng=k.act)
            phs = [self.pB(), self.pB()]
            for half in range(2):
                for cc in range(8):
                    self.MM(phs[half], phs[half][:], mixT, mixT[:, cc, :], wout, wout[:, cc, half * 512:(half + 1) * 512],
                            cc == 0, cc == 7)
            self.epilogue([(phs[0], phs[0][:]), (phs[1], phs[1][:])], r0, g1, b1, None, "o3", dbg_idx=2 * layer)


def make_consts():
    half = 16
    inv = (10000.0 ** (-np.arange(half, dtype=np.float32) / half)).astype(np.float32)
    pos = np.arange(S, dtype=np.float32)
    ang = (pos[:, None] * inv[None, :]).astype(np.float32)
    cos = np.cos(ang).astype(np.float32).T
    sin = np.sin(ang).astype(np.float32).T
    rope = np.zeros((2, 32, S), np.float32)
    rope[0, :16] = cos
    rope[0, 16:] = cos
    rope[1, :16] = -sin
    rope[1, 16:] = sin
    kk = np.arange(128)
    U = (kk[:, None] <= kk[None, :]).astype(np.float32)
    UT = (kk[:, None] >= kk[None, :]).astype(np.float32)
    mF = np.where(kk[:, None] > kk[None, :], NEG, 0.0).astype(np.float32)
    mB = np.where(kk[:, None] < kk[None, :], NEG, 0.0).astype(np.float32)
    pg = np.zeros((128, 9), np.float32)
    for g in range(7):
        pg[:, g] = g * 128 + kk
    for h in range(2):
        pg[:, 7 + h] = h * 128 + kk
    return {"c_ident": np.eye(128, dtype=np.float32), "c_rope": rope, "c_tri": np.stack([U, UT, mF, mB]), "c_pg": pg}


_PROG_CACHE = {}


def get_prog(nslot, nlayers, debug):
    key = (nslot, nlayers, debug)
    if key not in _PROG_CACHE:
        p = Prog(nslot, nlayers, debug)
        p.build()
        _PROG_CACHE[key] = p
    return _PROG_CACHE[key]


def run(inputs, nslot=NSLOT, nlayers=DEPTH, debug=False, ncores=NCORES, seqs=None):
    p = get_prog(nslot, nlayers, debug)
    xall = np.concatenate([np.asarray(inputs["x_prompt"]), np.asarray(inputs["x_sample"])], axis=0)
    nseq = xall.shape[0]
    if seqs is None:
        seqs = [[min(c * nslot + s, nseq - 1) for s in range(nslot)] for c in range(ncores)]
        flat = list(range(nseq))
        seqs = []
        pos = 0
        for c in range(ncores):
            n = 3 if c < 4 else 2
            mine = flat[pos:pos + n]
            pos += n
            while len(mine) < nslot:
                mine.append(mine[0])
            seqs.append(mine[:nslot])
    consts = make_consts()
    shared = {}
    for n in p.in_names:
        if n == "x" or n in consts:
            continue
        a = np.ascontiguousarray(np.asarray(inputs[n], dtype=np.float32))
        if n in ("ssd_dt_bias", "ssd_a_log", "ml_igate_b", "ml_fgate_b"):
            a = a.reshape(2, 16)
        shared[n] = a
    shared.update(consts)
    in_maps = []
    for c in range(ncores):
        m = dict(shared)
        m["x"] = np.ascontiguousarray(xall[seqs[c]])
        in_maps.append(m)
    res = run_bass_kernel_spmd(p.nc, in_maps, core_ids=list(range(ncores)))
    return res, seqs, nseq


def kernel(**inputs):
    res, seqs, nseq = run(inputs)
    out = np.zeros((nseq, S, D), np.float32)
    done = set()
    for c in range(NCORES):
        y = np.asarray(res.results[c]["y"])
        for s, q in enumerate(seqs[c]):
            if q not in done:
                out[q] = y[s]
                done.add(q)
    nb = np.asarray(inputs["x_prompt"]).shape[0]
    return (out[:nb], out[nb:])
```

```python
import math
from contextlib import ExitStack
import numpy as np
import concourse.bass as bass
import concourse.mybir as mybir
from concourse.bass_utils import run_bass_kernel_spmd

F32 = mybir.dt.float32
BF16 = mybir.dt.bfloat16
AF = mybir.ActivationFunctionType
ALU = mybir.AluOpType
AX = mybir.AxisListType

D = 1024
S = 4096
DEPTH = 4
ALPHA = (2.0 * DEPTH) ** 0.25
LN_EPS = 1e-5
RMS_EPS = 1e-6
NCORES = 8
NSLOT = 3
TT = 512
NT = S // TT
D_FF = 2816
NE = 8
D_FFE = 3584
EV_IN = 1440
OD_IN = 3632
NEG = -30000.0
SPARSE_MOE = True
CONV_SPLIT = 99
GCM = 512
NTL = 24
I32 = mybir.dt.int32


class Res:
    __slots__ = ("name", "w", "rs", "dsem", "dcount", "persist", "phase", "scope", "multi", "msems", "mrr", "mw")

    def __init__(self, name, persist=False):
        self.name = name
        self.phase = False
        self.scope = None
        self.multi = 0
        self.msems = []
        self.mrr = 0
        self.mw = {}
        self.w = None
        self.rs = {}
        self.dsem = None
        self.dcount = 0
        self.persist = persist


class Eng:
    def __init__(self, name, h, sem):
        self.name, self.h, self.sem = name, h, sem
        self.count = 0
        self.seen = {}
        self.nins = 0
        self.nwait = 0


class Tile:
    def __init__(self, t, r):
        self.t, self.r = t, r

    def __getitem__(self, key):
        return self.t[key]


class K:
    def __init__(self, nc, stack):
        self.nc = nc
        self.stack = stack
        self.engs = {}
        for name, h in (("pe", nc.tensor), ("act", nc.scalar), ("dve", nc.vector),
                        ("pool", nc.gpsimd), ("sp", nc.sync)):
            sem = stack.enter_context(nc.semaphore("sem_" + name))
            self.engs[name] = Eng(name, h, sem)
        self.pe, self.act, self.dve, self.pool, self.sp = (
            self.engs[n] for n in ("pe", "act", "dve", "pool", "sp"))
        self.all_res = []
        self.nsem = 5
        self.free_dsems = []
        self.sem_count = {}
        self.exact_sems = set()
        self.gsems = []
        self.grr = 0

    def res(self, name, persist=False):
        r = Res(name, persist)
        self.all_res.append(r)
        return r

    def dres(self, name, n=2):
        r = self.res(name)
        r.multi = n
        return r

    def _waits(self, eng, reads, writes, partial_dst=None):
        need = {}

        def add(m, raw):
            sem, val, src = m
            if src is eng and not raw:
                return
            if src is None and sem not in self.exact_sems:
                val = max(val, self.sem_count.get(sem, 0))
            if eng.seen.get(sem, 0) >= val:
                return
            if need.get(sem, 0) < val:
                need[sem] = val

        for r in reads:
            if r.w is not None:
                add(r.w, True)
            for sem_, val_ in r.mw.items():
                add((sem_, val_, None), True)
        for w in writes:
            if w is not partial_dst:
                for sem_, val_ in w.mw.items():
                    add((sem_, val_, None), False)
            if w.w is not None and not (w is partial_dst and w.w[0] is w.dsem):
                add(w.w, False)
            for sem, (val, src) in w.rs.items():
                add((sem, val, src), False)
        for sem, val in need.items():
            eng.h.wait_ge(sem, val)
            eng.seen[sem] = val
            eng.nwait += 1

    def _mark(self, m, reads, writes):
        sem, val, src = m
        for r in reads:
            o = r.rs.get(sem)
            if o is None or o[0] < val:
                r.rs[sem] = (val, src)
        for w in writes:
            w.w = m
            w.rs = {}

    def op(self, eng, fn, reads=(), writes=()):
        self._waits(eng, reads, writes)
        eng.count += 1
        eng.nins += 1
        ins = fn()
        ins.then_inc(eng.sem, 1)
        self._mark((eng.sem, eng.count, eng), reads, writes)
        return ins

    def _multi_slot(self, q, r0):
        NG = 24
        if len(self.gsems) < NG:
            sem = self.stack.enter_context(self.nc.semaphore("gsem_%d" % len(self.gsems)))
            self.nsem += 1
            self.exact_sems.add(sem)
            self.gsems.append([sem, 0])
            slot = len(self.gsems) - 1
        else:
            slot = self.grr % NG
        self.grr += 1
        sem, cnt = self.gsems[slot]
        if q.seen.get(sem, 0) < cnt:
            q.h.wait_ge(sem, cnt)
            q.seen[sem] = cnt
        return slot, sem

    def _multi_done(self, r0, slot, sem, n, reads):
        self.gsems[slot][1] += 16 * n
        cnt = self.gsems[slot][1]
        for r in reads:
            o = r.rs.get(sem)
            if o is None or o[0] < cnt:
                r.rs[sem] = (cnt, None)
        r0.mw[sem] = cnt
        r0.rs = {}

    def dma(self, q, pairs, reads=(), writes=(), partial=False, **kw):
        r0 = writes[0]
        self._waits(q, reads, writes, partial_dst=r0 if partial else None)
        if r0.multi and partial:
            slot, sem = self._multi_slot(q, r0)
            for (o, i) in pairs:
                q.h.dma_start(out=o, in_=i, **kw).then_inc(sem, 16)
                q.nins += 1
            self._multi_done(r0, slot, sem, len(pairs), reads)
            return
        if r0.dsem is None:
            if self.free_dsems:
                r0.dsem, r0.dcount = self.free_dsems.pop()
            else:
                r0.dsem = self.stack.enter_context(self.nc.semaphore("dsem_%d" % self.nsem))
                r0.dcount = 0
                self.nsem += 1
        for (o, i) in pairs:
            q.h.dma_start(out=o, in_=i, **kw).then_inc(r0.dsem, 16)
            r0.dcount += 16
            q.nins += 1
        self.sem_count[r0.dsem] = r0.dcount
        self._mark((r0.dsem, r0.dcount, None), reads, writes)

    def idma(self, fn, reads, writes, partial=False):
        q = self.pool
        r0 = writes[0]
        self._waits(q, reads, writes, partial_dst=r0 if partial else None)
        if r0.multi and partial:
            slot, sem = self._multi_slot(q, r0)
            fn().then_inc(sem, 16)
            q.nins += 1
            self._multi_done(r0, slot, sem, 1, reads)
            return
        if r0.dsem is None:
            if self.free_dsems:
                r0.dsem, r0.dcount = self.free_dsems.pop()
            else:
                r0.dsem = self.stack.enter_context(self.nc.semaphore("dsem_%d" % self.nsem))
                r0.dcount = 0
                self.nsem += 1
        fn().then_inc(r0.dsem, 16)
        r0.dcount += 16
        q.nins += 1
        self.sem_count[r0.dsem] = r0.dcount
        self._mark((r0.dsem, r0.dcount, None), reads, writes)

    def barrier(self, closing=None):
        sp = self.sp
        for r in self.all_res:
            if r.persist:
                continue
            if r.dsem is not None and sp.seen.get(r.dsem, 0) < r.dcount:
                sp.h.wait_ge(r.dsem, r.dcount)
                sp.seen[r.dsem] = r.dcount
        for (sem_, cnt_) in self.gsems:
            if sp.seen.get(sem_, 0) < cnt_:
                sp.h.wait_ge(sem_, cnt_)
                sp.seen[sem_] = cnt_
        sp.count += 1
        sp.h.sem_inc(sp.sem, 1)
        for e in self.engs.values():
            for o in self.engs.values():
                if o is e or o.count == 0:
                    continue
                if e.seen.get(o.sem, 0) < o.count:
                    e.h.wait_ge(o.sem, o.count)
                    e.seen[o.sem] = o.count
            for r in self.all_res:
                if not r.persist and r.dsem is not None:
                    e.seen[r.dsem] = r.dcount
            for (sem_, cnt_) in self.gsems:
                e.seen[sem_] = cnt_
        for r in self.all_res:
            if not r.persist:
                r.w = None
                r.rs = {}
                r.mw = {}
        keep = []
        for r in self.all_res:
            if r.phase:
                if r.dsem is not None:
                    self.free_dsems.append((r.dsem, r.dcount))
                    r.dsem = None
                if r.scope is closing:
                    continue
            keep.append(r)
        self.all_res = keep

    def finish(self):
        sp = self.sp
        for r in self.all_res:
            ms = [(s_, v, e) for s_, (v, e) in r.rs.items()]
            if r.w is not None:
                ms.append(r.w)
            for (sem, val, src) in ms:
                if sp.seen.get(sem, 0) >= val:
                    continue
                sp.h.wait_ge(sem, val)
                sp.seen[sem] = val


class Prog:
    def __init__(self, nslot=NSLOT, nlayers=DEPTH, debug=False):
        self.nslot = nslot
        self.nlayers = nlayers
        self.debug = debug
        self.nc = bass.Bass("TRN2", target_bir_lowering=False)
        self.in_names = []

    def din(self, name, shape, dt=F32):
        self.in_names.append(name)
        return self.nc.dram_tensor(name, list(shape), dt, kind="ExternalInput").ap()

    def dscr(self, name, shape, dt):
        return self.nc.dram_tensor(name, list(shape), dt).ap()

    def T(self, name, shape, dt=F32):
        t = self.ph.enter_context(self.nc.sbuf_tensor(name + "_%d" % self.uid(), list(shape), dt))
        r = self.k.res(name)
        r.phase = self.ph is not self.stack
        r.scope = self.ph
        return Tile(t, r)

    def TP(self, name, shape, dt=F32):
        t = self.stack.enter_context(self.nc.sbuf_tensor(name, list(shape), dt))
        return Tile(t, self.k.res(name, persist=True))

    def uid(self):
        self._uid += 1
        return self._uid

    def MM(self, out, oap, lt, lap, rt, rap, start, stop):
        nc = self.nc
        self.k.op(self.k.pe, lambda: nc.tensor.matmul(oap, lhsT=lap, rhs=rap, start=start, stop=stop),
                  [lt.r, rt.r], [out.r])

    def TR(self, out, oap, it, iap, ident):
        nc = self.nc
        self.k.op(self.k.pe, lambda: nc.tensor.transpose(oap, iap, ident[:]), [it.r, ident.r], [out.r])

    def ACT(self, out, oap, it, iap, func, bias=None, scale=None, accum=None, extra=()):
        nc = self.nc
        kw = {}
        if bias is not None:
            kw["bias"] = bias
        if scale is not None:
            kw["scale"] = scale
        if accum is not None:
            kw["accum_out"] = accum
        self.k.op(self.k.act, lambda: nc.scalar.activation(out=oap, in_=iap, func=func, **kw),
                  [it.r] + [e.r for e in extra], [out.r])

    def V(self, eng, fn, reads, writes):
        self.k.op(eng, fn, [t.r for t in reads], [t.r for t in writes])

    def TT_(self, out, oap, a, aap, b, bap, op, eng=None):
        nc = self.nc
        eng = eng or self.k.dve
        self.k.op(eng, lambda: eng.h.tensor_tensor(out=oap, in0=aap, in1=bap, op=op), [a.r, b.r], [out.r])

    def TS(self, out, oap, a, aap, s1, s2, op0, op1=None, extra=(), eng=None):
        eng = eng or self.k.dve
        if op1 is None:
            fn = lambda: eng.h.tensor_scalar(out=oap, in0=aap, scalar1=s1, scalar2=None, op0=op0)
        else:
            fn = lambda: eng.h.tensor_scalar(out=oap, in0=aap, scalar1=s1, scalar2=s2, op0=op0, op1=op1)
        self.k.op(eng, fn, [a.r] + [e.r for e in extra], [out.r])

    def STT(self, out, oap, a, aap, sc, b, bap, op0, op1, extra=(), eng=None):
        eng = eng or self.k.dve
        self.k.op(eng, lambda: eng.h.scalar_tensor_tensor(out=oap, in0=aap, scalar=sc, in1=bap, op0=op0, op1=op1),
                  [a.r, b.r] + [e.r for e in extra], [out.r])

    def CP(self, out, oap, it, iap, eng=None):
        eng = eng or self.k.dve
        if eng is self.k.act:
            self.k.op(eng, lambda: self.nc.scalar.copy(out=oap, in_=iap), [it.r], [out.r])
        else:
            self.k.op(eng, lambda: eng.h.tensor_copy(out=oap, in_=iap), [it.r], [out.r])

    def LD(self, tile, oap, src_ap, src_res=None, q=None, partial=False):
        self.k.dma(q or self.k.sp, [(oap, src_ap)], [src_res] if src_res is not None else [], [tile.r], partial=partial)

    def ST(self, dst_ap, dst_res, tile, iap, q=None):
        self.k.dma(q or self.k.sp, [(dst_ap, iap)], [tile.r], [dst_res], partial=True)

    def build(self):
        nc = self.nc
        self._uid = 0
        with ExitStack() as stack:
            self.stack = stack
            self.k = K(nc, stack)
            self.declare_io()
            self.setup_consts()
            self.convert_weights([l for l in range(self.nlayers) if l < CONV_SPLIT])
            for slot in range(self.nslot):
                self.run_slot(slot)
            self.k.barrier()
        return nc

    def declare_io(self):
        ns = self.nslot
        self.x_in = self.din("x", [ns, S, D])
        self.y_out = self.nc.dram_tensor("y", [ns, S, D], F32, kind="ExternalOutput").ap()
        self.r_y = self.k.dres("y_out", 4)
        w = {}
        w["ev_w_in"] = self.din("ev_w_in", [2, D, EV_IN])
        w["conv_dw_w"] = self.din("conv_dw_w", [2, 31, 512])
        w["conv_dw_b"] = self.din("conv_dw_b", [2, 512])
        w["conv_ln_g"] = self.din("conv_ln_g", [2, 512])
        w["conv_ln_b"] = self.din("conv_ln_b", [2, 512])
        w["mla_q_norm_g"] = self.din("mla_q_norm_g", [2, 256])
        w["mla_w_uq"] = self.din("mla_w_uq", [2, 256, 768])
        w["mla_kv_norm_g"] = self.din("mla_kv_norm_g", [2, 128])
        w["mla_w_ukv"] = self.din("mla_w_ukv", [2, 128, 1024])
        w["ev_w_out"] = self.din("ev_w_out", [2, 1024, 1024])
        w["od_w_in"] = self.din("od_w_in", [2, D, OD_IN])
        w["ssd_conv_w"] = self.din("ssd_conv_w", [2, 5, 1024])
        w["ssd_conv_b"] = self.din("ssd_conv_b", [2, 1024])
        w["ssd_dt_bias"] = self.din("ssd_dt_bias", [2, 16])
        w["ssd_a_log"] = self.din("ssd_a_log", [2, 16])
        w["ssd_d"] = self.din("ssd_d", [2, 8])
        w["ssd_norm_g"] = self.din("ssd_norm_g", [2, 512])
        w["ml_igate_b"] = self.din("ml_igate_b", [2, 16])
        w["ml_fgate_b"] = self.din("ml_fgate_b", [2, 16])
        w["ml_norm_g"] = self.din("ml_norm_g", [2, 512])
        w["od_w_out"] = self.din("od_w_out", [2, 1024, 1024])
        w["ffn_w_gate"] = self.din("ffn_w_gate", [2, D, D_FF])
        w["ffn_w_up"] = self.din("ffn_w_up", [2, D, D_FF])
        w["ffn_w_down"] = self.din("ffn_w_down", [2, D_FF, D])
        w["moe_router_w"] = self.din("moe_router_w", [2, D, NE])
        w["moe_router_b"] = self.din("moe_router_b", [2, NE])
        w["moe_w_gate"] = self.din("moe_w_gate", [2, NE, D, D_FFE])
        w["moe_w_up"] = self.din("moe_w_up", [2, NE, D, D_FFE])
        w["moe_w_down"] = self.din("moe_w_down", [2, NE, D_FFE, D])
        for n in ("ln1_g", "ln1_b", "ln2_g", "ln2_b"):
            w[n] = self.din(n, [4, D])
        self.w = w
        self.c_ident = self.din("c_ident", [128, 128])
        self.c_rope = self.din("c_rope", [2, 32, S])
        self.c_tri = self.din("c_tri", [4, 128, 128])
        self.c_pg = self.din("c_pg", [128, 9])
        self.xres = self.dscr("xres", [S, D], F32)
        self.r_xres = self.k.dres("xres", 4)
        self.xT = self.dscr("xT", [D, S + 4], BF16)
        self.r_xT = self.k.dres("xT", 4)
        if self.debug:
            self.dbg = self.nc.dram_tensor("dbg", [self.nlayers * 2, S, D], F32, kind="ExternalOutput").ap()
            self.r_dbg = self.k.dres("dbg", 2)

    def setup_consts(self):
        nc, k = self.nc, self.k
        self.ph = self.stack
        self.ident32 = self.TP("ident32", [128, 128], F32)
        self.LD(self.ident32, self.ident32[:], self.c_ident)
        self.ident16 = self.TP("ident16", [128, 128], BF16)
        self.CP(self.ident16, self.ident16[:], self.ident32, self.ident32[:])
        self.ones32 = self.TP("ones32", [128, 128], F32)
        self.V(k.dve, lambda: nc.vector.memset(self.ones32[:], 1.0), [], [self.ones32])
        self.zero16 = self.TP("zero16", [128, 64], BF16)
        self.V(k.dve, lambda: nc.vector.memset(self.zero16[:], 0.0), [], [self.zero16])
        self.zero32 = self.TP("zero32", [128, 64], F32)
        self.V(k.dve, lambda: nc.vector.memset(self.zero32[:], 0.0), [], [self.zero32])
        self.pb = []
        for i in range(7):
            t = self.stack.enter_context(nc.psum_tensor("pb%d" % i, [128, 512], F32))
            self.pb.append(Tile(t, k.res("pb%d" % i, persist=True)))
        t = self.stack.enter_context(nc.psum_tensor("ptr", [128, 1024], BF16))
        self.ptr = Tile(t, k.res("ptr", persist=True))
        xTv = self.xT.rearrange("(c p) s -> p c s", p=128)
        for lo in (0, S + 2):
            k.dma(k.sp, [(xTv[:, :, lo:lo + 2], self.zero16[:, 0:16].rearrange("p (c s) -> p c s", c=8))],
                  [self.zero16.r], [self.r_xT], partial=True)

    def convert_weights(self, layers):
        k = self.k
        GC = 256
        if not hasattr(self, "wb"):
            self.wb = {}
            self.wb_r = {}
            self._alloc_wb(GC)
        self._convert(layers, GC)

    def _alloc_wb(self, GC):
        plain = ["ev_w_in", "mla_w_uq", "mla_w_ukv", "ev_w_out", "od_w_in", "od_w_out"]
        for n in plain:
            self.wb[n] = self.dscr("wb_" + n, list(self.w[n].shape), BF16)
        self.wb["ffn_w_gate"] = self.dscr("wb_ffn_g", [2, 1, D_FF // GC, 128, 8, GC], BF16)
        self.wb["ffn_w_up"] = self.dscr("wb_ffn_u", [2, 1, D_FF // GC, 128, 8, GC], BF16)
        self.wb["ffn_w_down"] = self.dscr("wb_ffn_d", [2, 1, 2, 128, D_FF // 128, 512], BF16)
        self.wb["moe_w_gate"] = self.dscr("wb_moe_g", [2, NE, D_FFE // GCM, 128, 8, GCM], BF16)
        self.wb["moe_w_up"] = self.dscr("wb_moe_u", [2, NE, D_FFE // GCM, 128, 8, GCM], BF16)
        self.wb["moe_w_down"] = self.dscr("wb_moe_d", [2, NE, 2, 128, D_FFE // 128, 512], BF16)

    def _convert(self, layers, GC):
        k = self.k

        def conv_plain(n, j):
            src, dst = self.w[n][j], self.wb[n][j]
            r = self._grp
            self.wb_r[(n, j, 0)] = r
            rows = src.shape[0]
            prs = [(dst[r0:min(r0 + 256, rows), :], src[r0:min(r0 + 256, rows), :]) for r0 in range(0, rows, 256)]
            k.dma(k.pool, prs, [], [r], partial=True)

        def conv_ff(prefix, j, ne):
            for e in range(ne):
                for nm in ("gate", "up"):
                    n = "%s_w_%s" % (prefix, nm)
                    src = self.w[n][j] if ne == 1 else self.w[n][j, e]
                    dst = self.wb[n][j, e]
                    r = self._grp
                    self.wb_r[(n, j, e)] = r
                    sv = src.rearrange("(c p) n -> p c n", p=128)
                    gc = dst.shape[-1]
                    prs = [(dst[g], sv[:, :, g * gc:(g + 1) * gc]) for g in range(dst.shape[0])]
                    k.dma(k.pool, prs, [], [r], partial=True)
                n = "%s_w_down" % prefix
                src = self.w[n][j] if ne == 1 else self.w[n][j, e]
                dst = self.wb[n][j, e]
                r = self._grp
                self.wb_r[(n, j, e)] = r
                sv = src.rearrange("(f p) d -> p f d", p=128)
                nf = sv.shape[1]
                prs = []
                for half in range(2):
                    for f0 in range(0, nf, 7):
                        f1 = min(nf, f0 + 7)
                        prs.append((dst[half][:, f0:f1, :], sv[:, f0:f1, half * 512:(half + 1) * 512]))
                k.dma(k.pool, prs, [], [r], partial=True)

        for layer in layers:
            j = layer // 2
            self._grp = k.res("wbgrp_mix%d" % layer, persist=True)
            if layer % 2 == 0:
                for n in ("ev_w_in", "mla_w_uq", "mla_w_ukv", "ev_w_out"):
                    conv_plain(n, j)
                self._grp = k.res("wbgrp_ffn%d" % layer, persist=True)
                conv_ff("ffn", j, 1)
            else:
                for n in ("od_w_in", "od_w_out"):
                    conv_plain(n, j)
                self._grp = k.res("wbgrp_ffn%d" % layer, persist=True)
                conv_ff("moe", j, NE)

    def load_bcast(self, name, src_row_ap, n):
        t = self.T(name, [128, n], F32)
        self.LD(t, t[:], src_row_ap.partition_broadcast(128))
        return t

    def load_col(self, name, src_vec_ap, nchunk):
        t = self.T(name, [128, nchunk], F32)
        self.k.dma(self.k.sp, [(t[:], src_vec_ap.rearrange("(c p) -> p c", p=128))], [], [t.r],
                   allow_slow_non_contiguous=True)
        return t

    def run_slot(self, slot):
        k = self.k
        self.prologue(slot)
        for layer in range(self.nlayers):
            j = layer // 2
            last = (layer == self.nlayers - 1)
            if layer % 2 == 0:
                self.even_mixer(j, layer)
                self.ffn_like(layer, j, moe=False, dst=(self.y_out[slot], self.r_y) if last else None)
            else:
                self.odd_mixer(j, layer)
                if SPARSE_MOE:
                    self.moe_sparse(layer, j, dst=(self.y_out[slot], self.r_y) if last else None)
                else:
                    self.ffn_like(layer, j, moe=True, dst=(self.y_out[slot], self.r_y) if last else None)
                if slot == 0 and layer == 1 and self.nlayers > CONV_SPLIT:
                    self.convert_weights([l for l in range(self.nlayers) if l >= CONV_SPLIT])

    def phase(self):
        prog = self

        class _P:
            def __enter__(s):
                prog._phstack = ExitStack()
                prog._phstack.__enter__()
                prog.ph = prog._phstack
                return s

            def __exit__(s, *a):
                if a[0] is None:
                    prog.k.barrier(closing=prog._phstack)
                prog._phstack.__exit__(*a)
                prog.ph = prog.stack
                return False
        return _P()

    def prologue(self, slot):
        nc, k = self.nc, self.k
        with self.phase():
            xin = self.x_in[slot]
            for t in range(S // 128):
                xt = self.T("pro_x%d" % (t % 2), [128, D], F32) if t < 2 else None
                if t < 2:
                    if t == 0:
                        self._pro = []
                    self._pro.append(xt)
                xt = self._pro[t % 2]
                self.LD(xt, xt[:], xin[t * 128:(t + 1) * 128, :])
                self.ST(self.xres[t * 128:(t + 1) * 128, :], self.r_xres, xt, xt[:])
                self.emit_xT(xt, t * 128, "pro")

    def emit_xT(self, y32, tok0, tag):
        nc, k = self.nc, self.k
        key = "_xT_" + tag
        if not hasattr(self, key) or getattr(self, key)[0] is not self.ph:
            y16 = self.T(tag + "_y16", [128, D], BF16)
            xtt = self.T(tag + "_xtt", [128, 8, 128], BF16)
            setattr(self, key, (self.ph, y16, xtt))
        _, y16, xtt = getattr(self, key)
        self.CP(y16, y16[:], y32, y32[:], eng=k.act)
        for c in range(8):
            self.TR(self.ptr, self.ptr[:, c * 128:(c + 1) * 128], y16, y16[:, c * 128:(c + 1) * 128], self.ident16)
        self.CP(xtt, xtt[:].rearrange("p c s -> p (c s)"), self.ptr, self.ptr[:], eng=k.dve)
        xTv = self.xT.rearrange("(c p) s -> p c s", p=128)
        self.ST(xTv[:, :, 2 + tok0:2 + tok0 + 128], self.r_xT, xtt, xtt[:])

    def ln_consts(self, gname, bname, layer):
        g = self.load_bcast("lng", self.w[gname][layer:layer + 1, :], D)
        b = self.load_bcast("lnb", self.w[bname][layer:layer + 1, :], D)
        return g, b

    def epilogue(self, ps_halves, tok0, g, b, dst, tag, dbg_idx=None, add_tile=None):
        self.epilogue_multi([(ps_halves, tok0)], g, b, dst, tag, dbg_idx=dbg_idx, nch=1)

    def epilogue_multi(self, items, g, b, dst, tag, dbg_idx=None, nch=2):
        nc, k = self.nc, self.k
        key = "_ep_" + tag
        if not hasattr(self, key) or getattr(self, key)[0] is not self.ph:
            nset = 1 if tag == "ffn" else 2
            tiles = (self.ph,
                     [[self.T(tag + "_ex%d_%d" % (i, q), [128, D], F32) for i in range(nch)] for q in range(nset)],
                     [[self.T(tag + "_et%d_%d" % (i, q), [128, D], F32) for i in range(nch)] for q in range(nset)],
                     [self.T(tag + "_est%d" % i, [128, 2, 6], F32) for i in range(nch)],
                     [self.T(tag + "_emv%d" % i, [128, 2], F32) for i in range(nch)],
                     [self.T(tag + "_ers%d" % i, [128, 1], F32) for i in range(nch)],
                     [[self.T(tag + "_y16%d_%d" % (i, q), [128, D], BF16) for i in range(nch)] for q in range(nset)],
                     [[self.T(tag + "_xtt%d_%d" % (i, q), [128, 8, 128], BF16) for i in range(nch)] for q in range(nset)],
                     [0])
            setattr(self, key, tiles)
        _, xs_a, ts_a, stts, mvs, rss, y16s_a, xtts_a, cnt_ = getattr(self, key)
        par = cnt_[0] % len(xs_a)
        cnt_[0] += 1
        xs_, ts_, y16s, xtts = xs_a[par], ts_a[par], y16s_a[par], xtts_a[par]
        n = len(items)
        assert n <= nch
        R = range(n)
        for c in R:
            tok0 = items[c][1]
            self.LD(xs_[c], xs_[c][:], self.xres[tok0:tok0 + 128, :], None)
        for h in range(2):
            for c in R:
                pt, pap = items[c][0][h]
                self.STT(ts_[c], ts_[c][:, h * 512:(h + 1) * 512], xs_[c], xs_[c][:, h * 512:(h + 1) * 512], ALPHA, pt, pap,
                         ALU.mult, ALU.add)
        for h in range(2):
            for c in R:
                self.V(k.dve, (lambda c_, h_: (lambda: nc.vector.bn_stats(out=stts[c_][:, h_, :], in_=ts_[c_][:, h_ * 512:(h_ + 1) * 512])))(c, h),
                       [ts_[c]], [stts[c]])
        for c in R:
            self.V(k.dve, (lambda c_: (lambda: nc.vector.bn_aggr(out=mvs[c_][:], in_=stts[c_][:])))(c), [stts[c]], [mvs[c]])
        for c in R:
            self.ACT(rss[c], rss[c][:], mvs[c], mvs[c][:, 1:2], AF.Sqrt, bias=self.eps_ln[:, 0:1], scale=1.0, extra=[self.eps_ln])
        for c in R:
            self.V(k.dve, (lambda c_: (lambda: nc.vector.reciprocal(out=rss[c_][:], in_=rss[c_][:])))(c), [rss[c]], [rss[c]])
        for c in R:
            self.TS(ts_[c], ts_[c][:], ts_[c], ts_[c][:], mvs[c][:, 0:1], rss[c][:, 0:1], ALU.subtract, ALU.mult, extra=[mvs[c], rss[c]])
        for c in R:
            self.TT_(ts_[c], ts_[c][:], ts_[c], ts_[c][:], g, g[:], ALU.mult)
        for c in R:
            self.TT_(ts_[c], ts_[c][:], ts_[c], ts_[c][:], b, b[:], ALU.add, eng=k.pool)
        xTv = self.xT.rearrange("(c p) s -> p c s", p=128)
        for c in R:
            tok0 = items[c][1]
            if dst is not None:
                dap, dres = dst
                self.ST(dap[tok0:tok0 + 128, :], dres, ts_[c], ts_[c][:])
            else:
                self.ST(self.xres[tok0:tok0 + 128, :], self.r_xres, ts_[c], ts_[c][:])
            if self.debug and dbg_idx is not None:
                self.ST(self.dbg[dbg_idx, tok0:tok0 + 128, :], self.r_dbg, ts_[c], ts_[c][:])
        if dst is None:
            for c in R:
                self.CP(y16s[c], y16s[c][:], ts_[c], ts_[c][:], eng=k.act)
            for c in R:
                tok0 = items[c][1]
                for cc in range(8):
                    self.TR(self.ptr, self.ptr[:, cc * 128:(cc + 1) * 128], y16s[c], y16s[c][:, cc * 128:(cc + 1) * 128], self.ident16)
                self.CP(xtts[c], xtts[c][:].rearrange("p c s -> p (c s)"), self.ptr, self.ptr[:], eng=k.dve)
                self.ST(xTv[:, :, 2 + tok0:2 + tok0 + 128], self.r_xT, xtts[c], xtts[c][:])

    def eps_tiles(self):
        nc, k = self.nc, self.k
        if not hasattr(self, "eps_ln"):
            self.eps_ln = self.TP("eps_ln", [128, 1], F32)
            self.V(k.dve, lambda: nc.vector.memset(self.eps_ln[:], LN_EPS), [], [self.eps_ln])
            self.eps_rms = self.TP("eps_rms", [128, 1], F32)
            self.V(k.dve, lambda: nc.vector.memset(self.eps_rms[:], RMS_EPS), [], [self.eps_rms])

    def load_xT_full(self, name="xTs"):
        t = self.T(name, [128, 8, S + 4], BF16)
        xTv = self.xT.rearrange("(c p) s -> p c s", p=128)
        for c in range(8):
            self.k.dma(self.k.sp, [(t[:, c, :], xTv[:, c, :])], [self.r_xT], [t.r], partial=True)
        return t

    def load_w(self, name, src, kc, cols, res, c0=0):
        t = self.T(name, [128, kc, cols], BF16)
        v = src.rearrange("(c p) n -> p c n", p=128)
        self.k.dma(self.k.sp, [(t[:], v[:, :, c0:c0 + cols])], [res], [t.r])
        return t

    def ring(self, name, shape, dt, n, init=None):
        tiles = [self.T("%s%d" % (name, i), shape, dt) for i in range(n)]
        if init is not None:
            for t in tiles:
                self.V(self.k.dve, (lambda tt: (lambda: self.nc.vector.memset(tt[:], init)))(t), [], [t])
        st = [0]

        def nxt():
            t = tiles[st[0] % n]
            st[0] += 1
            return t
        return nxt

    def pA(self):
        self._pa = getattr(self, "_pa", 0) + 1
        return self.pb[self._pa % 4]

    def pB(self):
        self._pbi = getattr(self, "_pbi", 0) + 1
        return self.pb[4 + self._pbi % 3]

    def rsqrt_(self, out, oap, src, sap, scale, eps_tile):
        self.ACT(out, oap, src, sap, AF.Sqrt, bias=eps_tile[:, 0:1], scale=scale, extra=[eps_tile])
        self.V(self.k.dve, lambda: self.nc.vector.reciprocal(out=oap, in_=oap), [out], [out])

    def even_mixer(self, j, layer):
        nc, k = self.nc, self.k
        self.eps_tiles()
        W = self.w
        if not hasattr(self, "u_d"):
            self.u_d = self.dscr("u_d", [4, 128, S + 30], F32)
            self.r_u = k.dres("u_d", 2)
            self.q_d = self.dscr("q_d", [8, 96, S], BF16)
            self.r_q = k.dres("q_d", 2)
            self.kn_d = self.dscr("kn_d", [8, 64, S], BF16)
            self.r_kn = k.dres("kn_d", 2)
            self.kpe_d = self.dscr("kpe_d", [32, S], BF16)
            self.r_kpe = k.dres("kpe_d", 2)
            self.v_d = self.dscr("v_d", [S, 8 * 65], BF16)
            self.r_v = k.dres("v_d", 2)
            self.att_d = self.dscr("att_d", [S, 512], BF16)
            self.r_att = k.dres("att_d", 2)
            uv = self.u_d.rearrange("c p s -> p c s")
            for lo in (0, S + 15):
                k.dma(k.sp, [(uv[:, :, lo:lo + 15], self.zero32[:, 0:60].rearrange("p (c s) -> p c s", c=4))],
                      [self.zero32.r], [self.r_u], partial=True)
        sl = 96 ** -0.5
        with self.phase():
            xTs = self.load_xT_full()
            wr = self.wb_r[("ev_w_in", j, 0)]
            win = self.load_w("win", self.wb["ev_w_in"][j], 8, EV_IN, wr)
            wv_ = self.wb["ev_w_in"][j].rearrange("(c p) n -> p c n", p=128)
            wkrs = self.T("wkrs", [128, 8, 96], BF16)
            k.dma(k.sp, [(wkrs[:, :, 0:64], wv_[:, :, 1344:1408]), (wkrs[:, :, 64:80], wv_[:, :, 1424:1440]),
                         (wkrs[:, :, 80:96], wv_[:, :, 1408:1424])], [wr], [wkrs.r])
            wq_r = self.wb_r[("mla_w_uq", j, 0)]
            wuq = self.load_w("wuq", self.wb["mla_w_uq"][j], 2, 768, wq_r)
            wuqs = self.T("wuqs", [128, 2, 768], BF16)
            vq = self.wb["mla_w_uq"][j].rearrange("(c p) (h e) -> p c h e", p=128, e=96)
            wqv = wuqs[:].rearrange("p c (h e) -> p c h e", e=96)
            prs = []
            for c in range(2):
                prs += [(wqv[:, c, :, 0:64], vq[:, c, :, 0:64]), (wqv[:, c, :, 64:80], vq[:, c, :, 80:96]),
                        (wqv[:, c, :, 80:96], vq[:, c, :, 64:80])]
            k.dma(k.sp, prs, [wq_r], [wuqs.r])
            wkv_r = self.wb_r[("mla_w_ukv", j, 0)]
            wukv = self.load_w("wukv", self.wb["mla_w_ukv"][j], 1, 1024, wkv_r)
            wvv = self.T("wvv", [128, 8, 64], BF16)
            k.dma(k.sp, [(wvv[:], self.wb["mla_w_ukv"][j].rearrange("p (h e) -> p h e", e=128)[:, :, 64:128])],
                  [wkv_r], [wvv.r])
            gq = self.load_col("gq", W["mla_q_norm_g"][j], 2)
            gkv = self.load_col("gkv", W["mla_kv_norm_g"][j], 1)
            r_sig = self.ring("sig", [128, TT], F32, 3)
            r_u = self.ring("u", [128, TT], F32, 3)
            r_sq = self.ring("sq", [128, TT], F32, 3)
            r_rstd = self.ring("rstd", [128, TT], F32, 2)
            r_qn = self.ring("qn", [128, TT], BF16, 4)
            r_kvn = self.ring("kvn", [128, TT], BF16, 2)
            r_cs = self.ring("cs", [96, 2, TT], F32, 2)
            r_qt = self.ring("qt", [96, TT], BF16, 3)
            r_t1 = self.ring("t1", [96, TT], F32, 2)
            r_t2 = self.ring("t2", [96, TT], F32, 2)
            r_kn = self.ring("kn", [64, TT], BF16, 3)
            r_vt = self.ring("vt", [128, 8, 65], BF16, 3, init=1.0)
            for t in range(NT):
                c0 = 2 + t * TT
                cols = slice(t * TT, (t + 1) * TT)

                def proj(pt, pap, wt, wap_fn):
                    for c in range(8):
                        self.MM(pt, pap, wt, wap_fn(c), xTs, xTs[:, c, c0:c0 + TT], c == 0, c == 7)
                for jj in range(4):
                    pv, pg = self.pA(), self.pA()
                    proj(pv, pv[:], win, lambda c: win[:, c, jj * 128:(jj + 1) * 128])
                    proj(pg, pg[:], win, lambda c: win[:, c, 512 + jj * 128:512 + (jj + 1) * 128])
                    sig = r_sig()
                    self.ACT(sig, sig[:], pg, pg[:], AF.Sigmoid)
                    u = r_u()
                    self.TT_(u, u[:], pv, pv[:], sig, sig[:], ALU.mult)
                    self.ST(self.u_d[jj][:, 15 + t * TT:15 + (t + 1) * TT], self.r_u, u, u[:])
                pq = [self.pA(), self.pA()]
                for cq in range(2):
                    proj(pq[cq], pq[cq][:], win, lambda c: win[:, c, 1024 + cq * 128:1024 + (cq + 1) * 128])
                pkv = self.pB()
                proj(pkv, pkv[:], win, lambda c: win[:, c, 1280:1408])
                pkr = self.pB()
                proj(pkr, pkr[0:96, :], win, lambda c: win[:, c, 1344:1440])
                pkrs = self.pB()
                proj(pkrs, pkrs[0:96, :], wkrs, lambda c: wkrs[:, c, :])
                cs = r_cs()
                k.dma(k.sp, [(cs[64:96, 0, :], self.c_rope[0][:, cols]), (cs[64:96, 1, :], self.c_rope[1][:, cols])],
                      [], [cs.r])
                t1, t2, kpe = r_t1(), r_t2(), r_qt()
                self.TT_(t1, t1[64:96, :], pkr, pkr[64:96, :], cs, cs[64:96, 0, :], ALU.mult)
                self.TT_(t2, t2[64:96, :], pkrs, pkrs[64:96, :], cs, cs[64:96, 1, :], ALU.mult)
                self.TT_(kpe, kpe[64:96, :], t1, t1[64:96, :], t2, t2[64:96, :], ALU.add)
                self.ST(self.kpe_d[:, cols], self.r_kpe, kpe, kpe[64:96, :])
                sqs = []
                for cq in range(2):
                    sq = r_sq()
                    self.ACT(sq, sq[:], pq[cq], pq[cq][:], AF.Square)
                    sqs.append(sq)
                pss = self.pA()
                for cq in range(2):
                    self.MM(pss, pss[:], self.ones32, self.ones32[:], sqs[cq], sqs[cq][:], cq == 0, cq == 1)
                rstd = r_rstd()
                self.rsqrt_(rstd, rstd[:], pss, pss[:], 1.0 / 256.0, self.eps_rms)
                qn = []
                for cq in range(2):
                    q_ = r_qn()
                    self.STT(q_, q_[:], pq[cq], pq[cq][:], gq[:, cq:cq + 1], rstd, rstd[:], ALU.mult, ALU.mult, extra=[gq])
                    qn.append(q_)
                sq = r_sq()
                self.ACT(sq, sq[:], pkv, pkv[:], AF.Square)
                pss2 = self.pA()
                self.MM(pss2, pss2[:], self.ones32, self.ones32[:], sq, sq[:], True, True)
                rstdk = r_rstd()
                self.rsqrt_(rstdk, rstdk[:], pss2, pss2[:], 1.0 / 128.0, self.eps_rms)
                kvn = r_kvn()
                self.STT(kvn, kvn[:], pkv, pkv[:], gkv[:, 0:1], rstdk, rstdk[:], ALU.mult, ALU.mult, extra=[gkv])
                for h in range(8):
                    pqh, pqs = self.pA(), self.pA()
                    for c in range(2):
                        self.MM(pqh, pqh[0:96, :], wuq, wuq[:, c, 96 * h:96 * h + 96], qn[c], qn[c][:], c == 0, c == 1)
                    for c in range(2):
                        self.MM(pqs, pqs[0:96, :], wuqs, wuqs[:, c, 96 * h:96 * h + 96], qn[c], qn[c][:], c == 0, c == 1)
                    qt = r_qt()
                    self.CP(qt, qt[0:64, :], pqh, pqh[0:64, :], eng=k.act)
                    t1, t2 = r_t1(), r_t2()
                    self.TT_(t1, t1[64:96, :], pqh, pqh[64:96, :], cs, cs[64:96, 0, :], ALU.mult)
                    self.TT_(t2, t2[64:96, :], pqs, pqs[64:96, :], cs, cs[64:96, 1, :], ALU.mult)
                    self.TT_(qt, qt[64:96, :], t1, t1[64:96, :], t2, t2[64:96, :], ALU.add)
                    self.ST(self.q_d[h][:, cols], self.r_q, qt, qt[0:96, :])
                for h in range(8):
                    pk = self.pB()
                    self.MM(pk, pk[0:64, :], wukv, wukv[:, 0, 128 * h:128 * h + 64], kvn, kvn[:], True, True)
                    kn = r_kn()
                    self.CP(kn, kn[:], pk, pk[0:64, :], eng=(k.act if h % 2 else k.dve))
                    self.ST(self.kn_d[h][:, cols], self.r_kn, kn, kn[:])
                for s in range(4):
                    pv_ = self.pB()
                    self.MM(pv_, pv_[:], kvn, kvn[:, s * 128:(s + 1) * 128], wvv, wvv[:].rearrange("p h e -> p (h e)"),
                            True, True)
                    vt = r_vt()
                    self.CP(vt, vt[:, :, 0:64], pv_, pv_[:].rearrange("p (h e) -> p h e", e=64),
                            eng=(k.act if s % 2 else k.dve))
                    r0 = t * TT + s * 128
                    self.ST(self.v_d[r0:r0 + 128, :], self.r_v, vt, vt[:].rearrange("p h e -> p (h e)"))
        outer = self.phase()
        outer.__enter__()
        y_sb = self.T("y_sb", [128, 4, S], F32)
        clg = self.load_col("clg", W["conv_ln_g"][j], 4)
        clb = self.load_col("clb", W["conv_ln_b"][j], 4)
        with self.subphase():
            cw = self.T("cw", [128, 4, 31], F32)
            k.dma(k.sp, [(cw[:, c, :], W["conv_dw_w"][j].rearrange("k (c p) -> c p k", p=128)[c]) for c in range(4)],
                  [], [cw.r], allow_slow_non_contiguous=True)
            cb = self.load_col("cb", W["conv_dw_b"][j], 4)
            r_uu = self.ring("uu", [128, S + 30], F32, 2)
            conv_ops = []

            def mk_first(jj, u):
                return lambda: self.TS(y_sb, y_sb[:, jj, :], u, u[:, 0:S], cw[:, jj, 0:1], cb[:, jj:jj + 1], ALU.mult, ALU.add,
                                       extra=[cw, cb])

            def mk_tap(jj, u, tap):
                return lambda: self.STT(y_sb, y_sb[:, jj, :], u, u[:, tap:tap + S], cw[:, jj, tap:tap + 1], y_sb, y_sb[:, jj, :],
                                        ALU.mult, ALU.add, extra=[cw])

            def mk_load(jj, holder):
                def f():
                    u = r_uu()
                    self.LD(u, u[:], self.u_d[jj], self.r_u)
                    holder.append(u)
                return f
            holders = [[] for _ in range(4)]
            for jj in range(4):
                conv_ops.append(mk_load(jj, holders[jj]))
                conv_ops.append((lambda jj_: (lambda: mk_first(jj_, holders[jj_][0])()))(jj))
                for tap in range(1, 31):
                    conv_ops.append((lambda jj_, tap_: (lambda: mk_tap(jj_, holders[jj_][0], tap_)()))(jj, tap))
            conv_it = iter(conv_ops)

            def emit_conv(n):
                for _ in range(n):
                    f = next(conv_it, None)
                    if f is None:
                        return
                    f()
            emit_conv(2)
            r_K = self.ring("Kh", [96, S], BF16, 2)
            r_Q = self.ring("Qh", [96, S], BF16, 2)
            r_V = self.ring("Vh", [128, 32, 65], BF16, 2)
            r_pT = self.ring("pT", [128, TT], BF16, 4)
            r_rec = self.ring("rec", [128, 4, 1], F32, 2)
            r_at = self.ring("at", [128, 4, 64], BF16, 3)
            vdv = self.v_d.rearrange("(kc p) (h e) -> p kc h e", p=128, e=65)
            attv = self.att_d.rearrange("(b p) f -> p b f", p=128)
            heads = {}

            def get_head(h):
                if h not in heads:
                    Kh, Qh, Vh = r_K(), r_Q(), r_V()
                    k.dma(k.sp, [(Kh[0:64, :], self.kn_d[h]), (Kh[64:96, :], self.kpe_d)], [self.r_kn, self.r_kpe], [Kh.r])
                    self.LD(Qh, Qh[:], self.q_d[h], self.r_q)
                    self.LD(Vh, Vh[:], vdv[:, :, h, :], self.r_v)
                    heads[h] = (Kh, Qh, Vh)
                return heads[h]
            its = [(h, qt, kc) for h in range(8) for qt in range(NT) for kc in range(32)]
            LA = 2
            pss = {}

            def emit_qk(i):
                h, qt, kc = its[i]
                Kh, Qh, Vh = get_head(h)
                ps = self.pA()
                self.MM(ps, ps[:], Kh, Kh[:, kc * 128:(kc + 1) * 128], Qh, Qh[:, qt * TT:(qt + 1) * TT], True, True)
                pss[i] = ps
            for i in range(min(LA, len(its))):
                emit_qk(i)
            po = None
            for i, (h, qt, kc) in enumerate(its):
                if i + LA < len(its):
                    emit_qk(i + LA)
                Kh, Qh, Vh = heads[h]
                if kc == 0:
                    po = self.pB()
                pov = po[:, 0:260].rearrange("p (b e) -> p b e", e=65)
                ps = pss.pop(i)
                pT = r_pT()
                self.ACT(pT, pT[:], ps, ps[:], AF.Exp, scale=sl)
                for qb in range(4):
                    self.MM(po, pov[:, qb, :], pT, pT[:, qb * 128:(qb + 1) * 128], Vh, Vh[:, kc, :], kc == 0, kc == 31)
                if kc == 31:
                    rec = r_rec()
                    self.V(k.dve, (lambda rec_, pov_: (lambda: nc.vector.reciprocal(out=rec_[:], in_=pov_[:, :, 64:65])))(rec, pov),
                           [po], [rec])
                    at = r_at()
                    self.TT_(at, at[:], po, pov[:, :, 0:64], rec, rec[:].to_broadcast([128, 4, 64]), ALU.mult)
                    self.ST(attv[:, qt * 4:(qt + 1) * 4, 64 * h:64 * h + 64], self.r_att, at, at[:])
                    emit_conv(2)
            emit_conv(1000)
        if True:
            wout = self.load_w("wout", self.wb["ev_w_out"][j], 8, D, self.wb_r[("ev_w_out", j, 0)])
            g1, b1 = self.ln_consts("ln1_g", "ln1_b", layer)
            r_sq = self.ring("sq3", [128, TT], F32, 2)
            mean = self.T("mean", [128, TT], F32)
            m2 = self.T("m2", [128, TT], F32)
            rstd = self.T("rstd3", [128, TT], F32)
            r_z = self.ring("z3", [128, TT], F32, 2)
            r_mix = self.ring("mixT", [128, 8, TT], BF16, 2)
            r_att = self.ring("att_in", [128, 512], BF16, 2)
            for t in range(NT):
                cols = slice(t * TT, (t + 1) * TT)
                pss, psq = self.pB(), self.pB()
                for jj in range(4):
                    self.MM(pss, pss[:], self.ones32, self.ones32[:], y_sb, y_sb[:, jj, cols], jj == 0, jj == 3)
                for jj in range(4):
                    sq = r_sq()
                    self.ACT(sq, sq[:], y_sb, y_sb[:, jj, cols], AF.Square)
                    self.MM(psq, psq[:], self.ones32, self.ones32[:], sq, sq[:], jj == 0, jj == 3)
                self.V(k.act, lambda: nc.scalar.mul(out=mean[:], in_=pss[:], mul=1.0 / 512.0), [pss], [mean])
                self.TT_(m2, m2[:], mean, mean[:], mean, mean[:], ALU.mult)
                self.STT(m2, m2[:], psq, psq[:], 1.0 / 512.0, m2, m2[:], ALU.mult, ALU.subtract)
                self.rsqrt_(rstd, rstd[:], m2, m2[:], 1.0, self.eps_ln)
                mixT = r_mix()
                for jj in range(4):
                    z = r_z()
                    self.TT_(z, z[:], y_sb, y_sb[:, jj, cols], mean, mean[:], ALU.subtract)
                    self.TT_(z, z[:], z, z[:], rstd, rstd[:], ALU.mult)
                    self.ACT(mixT, mixT[:, jj, :], z, z[:], AF.Silu, bias=clb[:, jj:jj + 1], scale=clg[:, jj:jj + 1],
                             extra=[clb, clg])
                for s in range(4):
                    at = r_att()
                    r0 = t * TT + s * 128
                    self.LD(at, at[:], self.att_d[r0:r0 + 128, :], self.r_att)
                    for fc in range(4):
                        self.TR(self.ptr, self.ptr[:, fc * 128:(fc + 1) * 128], at, at[:, fc * 128:(fc + 1) * 128], self.ident16)
                    self.CP(mixT, mixT[:, 4:8, s * 128:(s + 1) * 128],
                            self.ptr, self.ptr[:, 0:512].rearrange("p (c s) -> p c s", c=4), eng=k.act)
                for sp in range(2):
                    items = []
                    for s in (2 * sp, 2 * sp + 1):
                        phs = [self.pA(), self.pA()]
                        for half in range(2):
                            for c in range(8):
                                self.MM(phs[half], phs[half][:], mixT, mixT[:, c, s * 128:(s + 1) * 128],
                                        wout, wout[:, c, half * 512:(half + 1) * 512], c == 0, c == 7)
                        items.append(([(phs[0], phs[0][:]), (phs[1], phs[1][:])], t * TT + s * 128))
                    self.epilogue_multi(items, g1, b1, None, "e3", dbg_idx=2 * layer, nch=2)
        outer.__exit__(None, None, None)

    def ffn_like(self, layer, j, moe, dst):
        nc, k = self.nc, self.k
        self.eps_tiles()
        W = self.w
        GC = 256
        pre = "moe" if moe else "ffn"
        nexp = NE if moe else 1
        dff = D_FFE if moe else D_FF
        nf = dff // 128
        ng = dff // GC
        with self.phase():
            g2, b2 = self.ln_consts("ln2_g", "ln2_b", layer)
            xTv = self.xT.rearrange("(c p) s -> p c s", p=128)
            r_xt = self.ring("xt", [128, 8, TT], BF16, 2)
            hT = self.T("hT", [128, nf, TT], BF16)
            r_wg = self.ring("wg", [128, 8, GC], BF16, 2)
            r_wu = self.ring("wu", [128, 8, GC], BF16, 2)
            r_wd = self.ring("wd", [128, nf, 512], BF16, 2)
            acc = self.T("acc", [128, 4, D], F32)
            r_sg = self.ring("sg", [128, TT], F32, 2)
            if moe:
                wr32 = self.T("wr32", [128, 8, NE], F32)
                k.dma(k.sp, [(wr32[:], W["moe_router_w"][j].rearrange("(c p) e -> p c e", p=128))], [], [wr32.r])
                rb = self.load_bcast("rb", W["moe_router_b"][j:j + 1, :], NE)
                r_x32 = self.ring("x32", [128, D], F32, 2)
                xT32 = self.T("xT32", [128, 8, 128], F32)
                comb = self.T("comb", [128, 4, NE], F32)
                lg = self.T("lg", [128, NE], F32)
                l2 = self.T("l2", [128, NE], F32)
                mk1 = self.T("mk1", [128, NE], F32)
                mk2 = self.T("mk2", [128, NE], F32)
                sm = self.T("sm", [128, 8], F32)
            for t in range(NT):
                xt = r_xt()
                self.LD(xt, xt[:], xTv[:, :, 2 + t * TT:2 + (t + 1) * TT], self.r_xT)
                if moe:
                    for s in range(4):
                        r0 = t * TT + s * 128
                        x32 = r_x32()
                        self.LD(x32, x32[:], self.xres[r0:r0 + 128, :], self.r_xres)
                        pts = [self.pB(), self.pB()]
                        for c in range(8):
                            pt = pts[c // 4]
                            self.TR(pt, pt[:, (c % 4) * 128:(c % 4 + 1) * 128], x32, x32[:, c * 128:(c + 1) * 128], self.ident32)
                        for hh in range(2):
                            self.CP(xT32, xT32[:, hh * 4:(hh + 1) * 4, :].rearrange("p c s -> p (c s)"), pts[hh], pts[hh][:],
                                    eng=(k.act if hh else k.dve))
                        pr = self.pB()
                        for c in range(8):
                            self.MM(pr, pr[:, 0:NE], xT32, xT32[:, c, :], wr32, wr32[:, c, :], c == 0, c == 7)
                        self.TT_(lg, lg[:], pr, pr[:, 0:NE], rb, rb[:], ALU.add)
                        self.V(k.dve, lambda: nc.vector.tensor_reduce(out=sm[:, 0:1], in_=lg[:], axis=AX.X, op=ALU.max), [lg], [sm])
                        self.TS(mk1, mk1[:], lg, lg[:], sm[:, 0:1], None, ALU.is_equal, extra=[sm])
                        self.STT(l2, l2[:], mk1, mk1[:], -1.0e30, lg, lg[:], ALU.mult, ALU.add)
                        self.V(k.dve, lambda: nc.vector.tensor_reduce(out=sm[:, 1:2], in_=l2[:], axis=AX.X, op=ALU.max), [l2], [sm])
                        self.TS(mk2, mk2[:], l2, l2[:], sm[:, 1:2], None, ALU.is_equal, extra=[sm])
                        self.TT_(sm, sm[:, 2:3], sm, sm[:, 1:2], sm, sm[:, 0:1], ALU.subtract)
                        self.ACT(sm, sm[:, 3:4], sm, sm[:, 2:3], AF.Exp)
                        self.TS(sm, sm[:, 4:5], sm, sm[:, 3:4], 1.0, None, ALU.add)
                        self.V(k.dve, lambda: nc.vector.reciprocal(out=sm[:, 5:6], in_=sm[:, 4:5]), [sm], [sm])
                        self.TT_(sm, sm[:, 6:7], sm, sm[:, 3:4], sm, sm[:, 5:6], ALU.mult)
                        self.TS(comb, comb[:, s, :], mk1, mk1[:], sm[:, 5:6], None, ALU.mult, extra=[sm])
                        self.STT(comb, comb[:, s, :], mk2, mk2[:], sm[:, 6:7], comb, comb[:, s, :], ALU.mult, ALU.add, extra=[sm])
                for e in range(nexp):
                    wg_src = self.wb["%s_w_gate" % pre][j, e]
                    wu_src = self.wb["%s_w_up" % pre][j, e]
                    wd_src = self.wb["%s_w_down" % pre][j, e]
                    rg = self.wb_r[("%s_w_gate" % pre, j, e)]
                    ru = self.wb_r[("%s_w_up" % pre, j, e)]
                    rd = self.wb_r[("%s_w_down" % pre, j, e)]
                    for g in range(ng):
                        wg, wu = r_wg(), r_wu()
                        self.LD(wg, wg[:], wg_src[g], rg)
                        self.LD(wu, wu[:], wu_src[g], ru)
                        for fi in range(GC // 128):
                            f = g * (GC // 128) + fi
                            pg, pu = self.pA(), self.pA()
                            for c in range(8):
                                self.MM(pg, pg[:], wg, wg[:, c, fi * 128:(fi + 1) * 128], xt, xt[:, c, :], c == 0, c == 7)
                            for c in range(8):
                                self.MM(pu, pu[:], wu, wu[:, c, fi * 128:(fi + 1) * 128], xt, xt[:, c, :], c == 0, c == 7)
                            sg = r_sg()
                            self.ACT(sg, sg[:], pg, pg[:], AF.Silu)
                            self.TT_(hT, hT[:, f, :], sg, sg[:], pu, pu[:], ALU.mult)
                    for half in range(2):
                        wd = r_wd()
                        self.LD(wd, wd[:], wd_src[half], rd)
                        for s in range(4):
                            po = self.pB()
                            for f in range(nf):
                                self.MM(po, po[:], hT, hT[:, f, s * 128:(s + 1) * 128], wd, wd[:, f, :], f == 0, f == nf - 1)
                            aap = acc[:, s, half * 512:(half + 1) * 512]
                            if not moe:
                                self.CP(acc, aap, po, po[:], eng=(k.act if s % 2 else k.dve))
                            elif e == 0:
                                self.TS(acc, aap, po, po[:], comb[:, s, e:e + 1], None, ALU.mult, extra=[comb])
                            else:
                                self.STT(acc, aap, po, po[:], comb[:, s, e:e + 1], acc, aap, ALU.mult, ALU.add, extra=[comb])
                for sp in range(2):
                    items = [([(acc, acc[:, s, 0:512]), (acc, acc[:, s, 512:1024])], t * TT + s * 128) for s in (2 * sp, 2 * sp + 1)]
                    self.epilogue_multi(items, g2, b2, dst, "ffn", dbg_idx=2 * layer + 1, nch=2)

    def moe_sparse(self, layer, j, dst):
        nc, k = self.nc, self.k
        self.eps_tiles()
        W = self.w
        GC = GCM
        nf = D_FFE // 128
        ng = D_FFE // GC
        if not hasattr(self, "xs_g"):
            self.xs_g = self.dscr("xs_g", [NTL * 512, D], BF16)
            self.r_xs_g = k.dres("xs_g", 4)
            self.ys_g = self.dscr("ys_g", [NTL * 512, D], F32)
            self.r_ys_g = k.dres("ys_g", 2)
            zt = self.TP("zrow16", [128, D], BF16)
            self.V(k.dve, lambda: nc.vector.memset(zt[:], 0.0), [], [zt])
            for i in range(NTL * 4):
                k.dma(k.sp, [(self.xs_g[i * 128:(i + 1) * 128, :], zt[:])], [zt.r], [self.r_xs_g], partial=True)
        rg = self.wb_r[("moe_w_gate", j, 0)]
        wbg2 = self.wb["moe_w_gate"].rearrange("j e g p c n -> (j e g p) (c n)")
        wbu2 = self.wb["moe_w_up"].rearrange("j e g p c n -> (j e g p) (c n)")
        wbd2 = self.wb["moe_w_down"].rearrange("j e h p f d -> (j e h p) (f d)")
        with self.phase():
            desti = self.T("desti", [128, 32, 2], I32)
            g12 = self.T("g12", [128, 32, 2], F32)
            widx = self.T("widx", [128, NTL, 9], I32)
            with self.subphase():
                wr32 = self.T("wr32", [128, 8, NE], F32)
                k.dma(k.sp, [(wr32[:], W["moe_router_w"][j].rearrange("(c p) e -> p c e", p=128))], [], [wr32.r])
                rb = self.load_bcast("rb", W["moe_router_b"][j:j + 1, :], NE)
                U = self.T("Uinc", [128, 128], F32)
                self.LD(U, U[:], self.c_tri[0])
                r_x32 = self.ring("x32", [128, D], F32, 3)
                xT32 = self.T("xT32", [128, 8, 128], F32)
                lg = self.T("lg", [128, NE], F32)
                l2 = self.T("l2", [128, NE], F32)
                sm = self.T("sm", [128, 8], F32)
                m1all = self.T("m1all", [128, 32, NE], F32)
                m2all = self.T("m2all", [128, 32, NE], F32)
                wdesc = self.T("wdesc", [128, NE], F32)
                for e in range(NE):
                    self.V(k.dve, (lambda e_: (lambda: nc.vector.memset(wdesc[:, e_:e_ + 1], float(NE - e_))))(e), [], [wdesc])
                tsc = self.T("tsc", [128, NE], F32)

                def onehot_first(mt, map_):
                    self.TT_(tsc, tsc[:], mt, map_, wdesc, wdesc[:], ALU.mult)
                    self.V(k.dve, lambda: nc.vector.tensor_reduce(out=sm[:, 7:8], in_=tsc[:], axis=AX.X, op=ALU.max), [tsc], [sm])
                    self.TS(mt, map_, tsc, tsc[:], sm[:, 7:8], None, ALU.is_equal, extra=[sm])
                for st in range(32):
                    r0 = st * 128
                    x32 = r_x32()
                    self.LD(x32, x32[:], self.xres[r0:r0 + 128, :], self.r_xres)
                    pts = [self.pB(), self.pB()]
                    for c in range(8):
                        pt = pts[c // 4]
                        self.TR(pt, pt[:, (c % 4) * 128:(c % 4 + 1) * 128], x32, x32[:, c * 128:(c + 1) * 128], self.ident32)
                    for hh in range(2):
                        self.CP(xT32, xT32[:, hh * 4:(hh + 1) * 4, :].rearrange("p c s -> p (c s)"), pts[hh], pts[hh][:],
                                eng=(k.act if hh else k.dve))
                    pr = self.pB()
                    for c in range(8):
                        self.MM(pr, pr[:, 0:NE], xT32, xT32[:, c, :], wr32, wr32[:, c, :], c == 0, c == 7)
                    self.TT_(lg, lg[:], pr, pr[:, 0:NE], rb, rb[:], ALU.add)
                    self.V(k.dve, lambda: nc.vector.tensor_reduce(out=sm[:, 0:1], in_=lg[:], axis=AX.X, op=ALU.max), [lg], [sm])
                    self.TS(m1all, m1all[:, st, :], lg, lg[:], sm[:, 0:1], None, ALU.is_equal, extra=[sm])
                    onehot_first(m1all, m1all[:, st, :])
                    self.STT(l2, l2[:], m1all, m1all[:, st, :], -1.0e30, lg, lg[:], ALU.mult, ALU.add)
                    self.V(k.dve, lambda: nc.vector.tensor_reduce(out=sm[:, 1:2], in_=l2[:], axis=AX.X, op=ALU.max), [l2], [sm])
                    self.TS(m2all, m2all[:, st, :], l2, l2[:], sm[:, 1:2], None, ALU.is_equal, extra=[sm])
                    onehot_first(m2all, m2all[:, st, :])
                    self.TT_(sm, sm[:, 2:3], sm, sm[:, 1:2], sm, sm[:, 0:1], ALU.subtract)
                    self.ACT(sm, sm[:, 3:4], sm, sm[:, 2:3], AF.Exp)
                    self.TS(sm, sm[:, 4:5], sm, sm[:, 3:4], 1.0, None, ALU.add)
                    self.V(k.dve, lambda: nc.vector.reciprocal(out=g12[:, st, 0:1], in_=sm[:, 4:5]), [sm], [g12])
                    self.TT_(g12, g12[:, st, 1:2], sm, sm[:, 3:4], g12, g12[:, st, 0:1], ALU.mult)
                sel = self.T("sel", [128, 32, NE], F32)
                excl = self.T("excl", [128, 32, NE], F32)
                tot = self.T("tot", [128, 32, NE], F32)
                pre = self.T("pre", [128, 32, NE], F32)
                flat = lambda t_: t_[:].rearrange("p s e -> p (s e)")
                self.TT_(sel, sel[:], m1all, m1all[:], m2all, m2all[:], ALU.add)
                pinc, ptot = self.pB(), self.pB()
                self.MM(pinc, pinc[:, 0:256], U, U[:], sel, flat(sel), True, True)
                self.MM(ptot, ptot[:, 0:256], self.ones32, self.ones32[:], sel, flat(sel), True, True)
                self.TT_(excl, flat(excl), pinc, pinc[:, 0:256], sel, flat(sel), ALU.subtract)
                self.CP(tot, flat(tot), ptot, ptot[:, 0:256])
                self.V(k.dve, lambda: nc.vector.memset(pre[:, 0, :], 0.0), [], [pre])
                for st in range(1, 32):
                    self.TT_(pre, pre[:, st, :], pre, pre[:, st - 1, :], tot, tot[:, st - 1, :], ALU.add)
                cnt = self.T("cnt", [128, NE], F32)
                self.TT_(cnt, cnt[:], pre, pre[:, 31, :], tot, tot[:, 31, :], ALU.add)
                cmpk = self.T("cmpk", [128, 8, NE], F32)
                for kk in range(8):
                    self.TS(cmpk, cmpk[:, kk, :], cnt, cnt[:], 512.0 * kk, None, ALU.is_gt)
                padded = self.T("padded", [128, NE], F32)
                self.V(k.dve, lambda: nc.vector.tensor_reduce(out=padded[:], in_=cmpk[:].rearrange("p k e -> p e k"),
                                                              axis=AX.X, op=ALU.add), [cmpk], [padded])
                self.TS(padded, padded[:], padded, padded[:], 512.0, None, ALU.mult)
                base = self.T("base", [128, NE], F32)
                self.V(k.dve, lambda: nc.vector.memset(base[:, 0:1], 0.0), [], [base])
                for e in range(1, NE):
                    self.TT_(base, base[:, e:e + 1], base, base[:, e - 1:e], padded, padded[:, e - 1:e], ALU.add)
                cumend = self.T("cumend", [128, NE], F32)
                self.TT_(cumend, cumend[:], base, base[:], padded, padded[:], ALU.add)
                self.TT_(excl, excl[:], excl, excl[:], pre, pre[:], ALU.add)
                self.TT_(excl, excl[:], excl, excl[:], base, base[:].unsqueeze(1).to_broadcast([128, 32, NE]), ALU.add)
                destf = self.T("destf", [128, 32, 2], F32)
                for r, mall in ((0, m1all), (1, m2all)):
                    self.TT_(tot, tot[:], mall, mall[:], excl, excl[:], ALU.mult)
                    self.V(k.dve, (lambda r_: (lambda: nc.vector.tensor_reduce(out=destf[:, :, r_], in_=tot[:], axis=AX.X, op=ALU.add)))(r),
                           [tot], [destf])
                self.CP(desti, desti[:], destf, destf[:])
                cmpt = self.T("cmpt", [128, NTL, NE], F32)
                for t in range(NTL):
                    self.TS(cmpt, cmpt[:, t, :], cumend, cumend[:], 512.0 * t, None, ALU.is_le)
                etf = self.T("etf", [128, NTL], F32)
                self.V(k.dve, lambda: nc.vector.tensor_reduce(out=etf[:], in_=cmpt[:], axis=AX.X, op=ALU.add), [cmpt], [etf])
                self.TS(etf, etf[:], etf, etf[:], float(NE - 1), 0.0, ALU.min, ALU.max)
                self.TS(etf, etf[:], etf, etf[:], float(j * NE), None, ALU.add)
                pg = self.T("pg", [128, 9], F32)
                self.LD(pg, pg[:], self.c_pg)
                widf = self.T("widf", [128, NTL, 9], F32)
                for g in range(9):
                    self.TS(widf, widf[:, :, g], etf, etf[:], float(ng * 128 if g < 7 else 2 * 128), pg[:, g:g + 1],
                            ALU.mult, ALU.add, extra=[pg])
                self.CP(widx, widx[:], widf, widf[:])
                for st in range(32):
                    r0 = st * 128
                    x32 = r_x32()
                    self.LD(x32, x32[:], self.xres[r0:r0 + 128, :], self.r_xres)
                    for r in range(2):
                        k.idma((lambda x_, st_, r_: (lambda: nc.gpsimd.indirect_dma_start(
                            out=self.xs_g[:, :], out_offset=bass.IndirectOffsetOnAxis(ap=desti[:, st_, r_:r_ + 1], axis=0),
                            in_=x_[:, :], in_offset=None)))(x32, st, r),
                            [x32.r, desti.r], [self.r_xs_g], partial=True)
            with self.subphase():
                r_xg = self.ring("xg", [128, 4, D], BF16, 2)
                r_xt = self.ring("xtg", [128, 8, TT], BF16, 2)
                hT = self.T("hTg", [128, nf, TT], BF16)
                r_wg = self.ring("wgg", [128, 8, GC], BF16, 2)
                r_wu = self.ring("wug", [128, 8, GC], BF16, 2)
                r_wd = self.ring("wdg", [128, nf, 512], BF16, 2)
                r_sg = self.ring("sgg", [128, TT], F32, 2)
                r_ysb = self.ring("ysb", [128, 4, D], F32, 1)
                for t in range(NTL):
                    def wgather(dst_t, src2d, col):
                        k.idma((lambda d_, c_: (lambda: nc.gpsimd.indirect_dma_start(
                            out=d_, out_offset=None, in_=src2d,
                            in_offset=bass.IndirectOffsetOnAxis(ap=widx[:, t, c_:c_ + 1], axis=0))))(dst_t[:].rearrange(
                                "p a b -> p (a b)"), col), [rg, widx.r], [dst_t.r])
                    xg = r_xg()
                    self.LD(xg, xg[:], self.xs_g[t * 512:(t + 1) * 512, :].rearrange("(s p) d -> p s d", p=128), self.r_xs_g)
                    xt = r_xt()
                    for s in range(4):
                        for c in range(8):
                            self.TR(self.ptr, self.ptr[:, c * 128:(c + 1) * 128], xg, xg[:, s, c * 128:(c + 1) * 128], self.ident16)
                        self.CP(xt, xt[:, :, s * 128:(s + 1) * 128], self.ptr, self.ptr[:].rearrange("p (c s) -> p c s", c=8),
                                eng=(k.act if s % 2 else k.dve))
                    for g in range(ng):
                        wg, wu = r_wg(), r_wu()
                        wgather(wg, wbg2, g)
                        wgather(wu, wbu2, g)
                        for fi in range(GC // 128):
                            f = g * (GC // 128) + fi
                            pg, pu = self.pA(), self.pA()
                            for c in range(8):
                                self.MM(pg, pg[:], wg, wg[:, c, fi * 128:(fi + 1) * 128], xt, xt[:, c, :], c == 0, c == 7)
                            for c in range(8):
                                self.MM(pu, pu[:], wu, wu[:, c, fi * 128:(fi + 1) * 128], xt, xt[:, c, :], c == 0, c == 7)
                            sg = r_sg()
                            self.ACT(sg, sg[:], pg, pg[:], AF.Silu)
                            self.TT_(hT, hT[:, f, :], sg, sg[:], pu, pu[:], ALU.mult)
                    ysb = r_ysb()
                    for half in range(2):
                        wd = r_wd()
                        wgather(wd, wbd2, 7 + half)
                        for s in range(4):
                            po = self.pB()
                            for f in range(nf):
                                self.MM(po, po[:], hT, hT[:, f, s * 128:(s + 1) * 128], wd, wd[:, f, :], f == 0, f == nf - 1)
                            self.CP(ysb, ysb[:, s, half * 512:(half + 1) * 512], po, po[:], eng=(k.act if s % 2 else k.dve))
                    self.ST(self.ys_g[t * 512:(t + 1) * 512, :].rearrange("(s p) d -> p s d", p=128), self.r_ys_g, ysb, ysb[:])
            with self.subphase():
                g2, b2 = self.ln_consts("ln2_g", "ln2_b", layer)
                r_ga = self.ring("ga", [128, D], F32, 12)
                r_f = self.ring("fmo", [128, D], F32, 8)
                NB = 4
                for sb in range(32 // NB):
                    items = []
                    for st in range(sb * NB, (sb + 1) * NB):
                        gas = []
                        for r in range(2):
                            ga = r_ga()
                            k.idma((lambda g_, st_, r_: (lambda: nc.gpsimd.indirect_dma_start(
                                out=g_[:, :], out_offset=None, in_=self.ys_g[:, :],
                                in_offset=bass.IndirectOffsetOnAxis(ap=desti[:, st_, r_:r_ + 1], axis=0))))(ga, st, r),
                                [self.r_ys_g, desti.r], [ga.r])
                            gas.append(ga)
                        f = r_f()
                        self.TS(f, f[:], gas[0], gas[0][:], g12[:, st, 0:1], None, ALU.mult, extra=[g12])
                        self.STT(f, f[:], gas[1], gas[1][:], g12[:, st, 1:2], f, f[:], ALU.mult, ALU.add, extra=[g12])
                        items.append(([(f, f[:, 0:512]), (f, f[:, 512:1024])], st * 128))
                    self.epilogue_multi(items, g2, b2, dst, "moe", dbg_idx=2 * layer + 1, nch=NB)

    def odd_mixer(self, j, layer):
        nc, k = self.nc, self.k
        self.eps_tiles()
        W = self.w
        if not hasattr(self, "z_d"):
            def mk(name, shape, dt):
                setattr(self, name, self.dscr(name, shape, dt))
                setattr(self, "r_" + name, k.dres(name, 2))
            mk("z_d", [S, 512], F32); mk("xs_d", [S, 512], F32); mk("bmt_d", [S, 256], BF16)
            mk("bcT_d", [4, 128, S], BF16); mk("dtda_d", [S, 32], F32); mk("qT_d", [8, 64, S], BF16)
            mk("kT_d", [8, 64, S], BF16); mk("kt_d", [S, 512], BF16); mk("v2_d", [S, 8 * 65], BF16)
            mk("og_d", [S, 512], F32); mk("gate_d", [S, 32], F32); mk("yf_d", [S, 512], F32)
            mk("hf_d", [S, 512], F32); mk("wcf_d", [5, D, 1024], BF16)
        wi_r = self.wb_r[("od_w_in", j, 0)]
        wi = self.wb["od_w_in"][j]
        with self.phase():
            cwb = [self.load_bcast("cwb%d" % tap, W["ssd_conv_w"][j, tap:tap + 1, :], 1024) for tap in range(5)]
            r_wr = self.ring("wrow", [128, 1024], F32, 2)
            r_wo = self.ring("wcfo", [128, 1024], BF16, 3)
            for rc in range(8):
                wr_ = r_wr()
                self.LD(wr_, wr_[:], W["od_w_in"][j, rc * 128:(rc + 1) * 128, 512:1536])
                for tap in range(5):
                    wo = r_wo()
                    self.TT_(wo, wo[:], wr_, wr_[:], cwb[tap], cwb[tap][:], ALU.mult)
                    self.ST(self.wcf_d[tap, rc * 128:(rc + 1) * 128, :], self.r_wcf_d, wo, wo[:])
        with self.phase():
            xTs = self.load_xT_full()
            cbias = self.load_bcast("cbias", W["ssd_conv_b"][j:j + 1, :], 1024)
            cbcol = self.load_col("cbcol", W["ssd_conv_b"][j], 8)
            dtb = self.load_bcast("dtb", W["ssd_dt_bias"][j:j + 1, :], 16)
            alog = self.load_bcast("alog", W["ssd_a_log"][j:j + 1, :], 16)
            igb = self.load_bcast("igb", W["ml_igate_b"][j:j + 1, :], 16)
            fgb = self.load_bcast("fgb", W["ml_fgate_b"][j:j + 1, :], 16)
            abc = self.T("abc", [128, 16], F32)
            self.ACT(abc, abc[:], alog, alog[:], AF.Exp)
            self.TS(abc, abc[:], abc, abc[:], -1.0, None, ALU.mult)
            wiv = wi.rearrange("(c p) n -> p c n", p=128)
            wcv = self.wcf_d.rearrange("t (c p) n -> p t c n", p=128)
            r_o32 = self.ring("o32", [128, 512], F32, 3)
            r_o16 = self.ring("o16", [128, 512], BF16, 3)
            r_vt = self.ring("vt2", [128, 8, 65], BF16, 3, init=1.0)
            r_sm = self.ring("smo", [128, 64], F32, 3)

            def tok_group(wt, ncols, conv, post):
                for st in range(S // 128):
                    t0 = st * 128
                    ps = self.pA()
                    if conv:
                        n = 0
                        for tap in range(5):
                            for c in range(8):
                                self.MM(ps, ps[:, 0:ncols], xTs, xTs[:, c, t0 + tap:t0 + tap + 128], wt, wt[:, tap, c, :],
                                        n == 0, n == 39)
                                n += 1
                    else:
                        for c in range(8):
                            self.MM(ps, ps[:, 0:ncols], xTs, xTs[:, c, 2 + t0:2 + t0 + 128], wt, wt[:, c, :], c == 0, c == 7)
                    post(ps, t0, st)

            def load_plain(c0, ncols, name):
                t = self.T(name, [128, 8, ncols], BF16)
                k.dma(k.sp, [(t[:], wiv[:, :, c0:c0 + ncols])], [wi_r], [t.r])
                return t

            def load_conv(c0, ncols, name):
                t = self.T(name, [128, 5, 8, ncols], BF16)
                k.dma(k.sp, [(t[:, tap, :, :], wcv[:, tap, :, c0:c0 + ncols]) for tap in range(5)], [self.r_wcf_d], [t.r])
                return t

            def post_z(ps, t0, st):
                o = r_o32()
                self.ACT(o, o[:], ps, ps[:], AF.Silu)
                self.ST(self.z_d[t0:t0 + 128, :], self.r_z_d, o, o[:])
            sub = self.subphase()
            sub.__enter__()
            tok_group(load_plain(0, 512, "w_z"), 512, False, post_z)

            def post_o(ps, t0, st):
                o = r_o32()
                self.ACT(o, o[:], ps, ps[:], AF.Sigmoid)
                self.ST(self.og_d[t0:t0 + 128, :], self.r_og_d, o, o[:])
            tok_group(load_plain(3088, 512, "w_o"), 512, False, post_o)

            def post_k(ps, t0, st):
                o = r_o16()
                self.V(k.act, lambda: nc.scalar.mul(out=o[:], in_=ps[:], mul=0.125), [ps], [o])
                self.ST(self.kt_d[t0:t0 + 128, :], self.r_kt_d, o, o[:])
            tok_group(load_plain(2064, 512, "w_k"), 512, False, post_k)

            def post_v(ps, t0, st):
                vt = r_vt()
                self.CP(vt, vt[:, :, 0:64], ps, ps[:].rearrange("p (h e) -> p h e", e=64), eng=(k.act if st % 2 else k.dve))
                self.ST(self.v2_d[t0:t0 + 128, :], self.r_v2_d, vt, vt[:].rearrange("p h e -> p (h e)"))
            tok_group(load_plain(2576, 512, "w_v"), 512, False, post_v)
            sub.__exit__(None, None, None)

            def post_xs(ps, t0, st):
                o = r_o32()
                self.TT_(o, o[:], ps, ps[:], cbias, cbias[:, 0:512], ALU.add)
                self.ACT(o, o[:], o, o[:], AF.Silu)
                self.ST(self.xs_d[t0:t0 + 128, :], self.r_xs_d, o, o[:])
            sub = self.subphase()
            sub.__enter__()
            tok_group(load_conv(0, 512, "w_xs"), 512, True, post_xs)

            def post_bm(ps, t0, st):
                o = r_o32()
                self.TT_(o, o[:, 0:256], ps, ps[:, 0:256], cbias, cbias[:, 512:768], ALU.add)
                o2 = r_o16()
                self.ACT(o2, o2[:, 0:256], o, o[:, 0:256], AF.Silu)
                self.ST(self.bmt_d[t0:t0 + 128, :], self.r_bmt_d, o2, o2[:, 0:256])
            tok_group(load_conv(512, 256, "w_bm"), 256, True, post_bm)

            def post_small(ps, t0, st):
                sm = r_sm()
                self.TT_(sm, sm[:, 0:16], ps, ps[:, 0:16], dtb, dtb[:], ALU.add)
                self.ACT(sm, sm[:, 0:16], sm, sm[:, 0:16], AF.Exp)
                self.ACT(sm, sm[:, 0:16], sm, sm[:, 0:16], AF.Ln, bias=1.0, scale=1.0)
                self.TT_(sm, sm[:, 16:32], sm, sm[:, 0:16], abc, abc[:], ALU.mult)
                self.ST(self.dtda_d[t0:t0 + 128, :], self.r_dtda_d, sm, sm[:, 0:32])
                sm2 = r_sm()
                self.TT_(sm2, sm2[:, 0:16], ps, ps[:, 16:32], igb, igb[:], ALU.add)
                self.TT_(sm2, sm2[:, 16:32], ps, ps[:, 32:48], fgb, fgb[:], ALU.add)
                self.ACT(sm2, sm2[:, 16:32], sm2, sm2[:, 16:32], AF.Exp, scale=-1.0)
                self.ACT(sm2, sm2[:, 16:32], sm2, sm2[:, 16:32], AF.Ln, bias=1.0, scale=1.0)
                self.TS(sm2, sm2[:, 16:32], sm2, sm2[:, 16:32], -1.0, None, ALU.mult)
                self.ST(self.gate_d[t0:t0 + 128, :], self.r_gate_d, sm2, sm2[:, 0:32])
            wsm = self.T("w_sm", [128, 8, 48], BF16)
            k.dma(k.sp, [(wsm[:, :, 0:16], wiv[:, :, 1536:1552]), (wsm[:, :, 16:48], wiv[:, :, 3600:3632])], [wi_r], [wsm.r])
            tok_group(wsm, 48, False, post_small)
            sub.__exit__(None, None, None)

            r_f16 = self.ring("f16", [128, TT], BF16, 3)
            with self.subphase():
                wbc = load_conv(512, 512, "w_bcT")
                for t in range(NT):
                    for i in range(4):
                        ps = self.pA()
                        n = 0
                        for tap in range(5):
                            for c in range(8):
                                self.MM(ps, ps[:], wbc, wbc[:, tap, c, i * 128:(i + 1) * 128],
                                        xTs, xTs[:, c, t * TT + tap:t * TT + tap + TT], n == 0, n == 39)
                                n += 1
                        o = r_f16()
                        self.ACT(o, o[:], ps, ps[:], AF.Silu, bias=cbcol[:, 4 + i:5 + i], scale=1.0, extra=[cbcol])
                        self.ST(self.bcT_d[i][:, t * TT:(t + 1) * TT], self.r_bcT_d, o, o[:])
            for (c0, dst, rdst, scl, nm) in ((1552, self.qT_d, self.r_qT_d, 1.0, "w_qT"), (2064, self.kT_d, self.r_kT_d, 0.125, "w_kT")):
                with self.subphase():
                    wq = load_plain(c0, 512, nm)
                    for t in range(NT):
                        for h in range(8):
                            ps = self.pA()
                            for c in range(8):
                                self.MM(ps, ps[0:64, :], wq, wq[:, c, h * 64:(h + 1) * 64], xTs, xTs[:, c, 2 + t * TT:2 + (t + 1) * TT],
                                        c == 0, c == 7)
                            o = r_f16()
                            self.V(k.act, (lambda o_, ps_: (lambda: nc.scalar.mul(out=o_[0:64, :], in_=ps_[0:64, :], mul=scl)))(o, ps), [ps], [o])
                            self.ST(dst[h][:, t * TT:(t + 1) * TT], rdst, o, o[0:64, :])
        for direction in (0, 1):
            with self.phase():
                self.scan_pass(j, layer, direction)

    def subphase(self):
        prog = self

        class _S:
            def __enter__(s):
                s.outer = prog.ph
                s.st = ExitStack()
                s.st.__enter__()
                prog.ph = s.st
                return s

            def __exit__(s, *a):
                if a[0] is None:
                    prog.k.barrier(closing=s.st)
                s.st.__exit__(*a)
                prog.ph = s.outer
                return False
        return _S()

    def scan_pass(self, j, layer, dr):
        nc, k = self.nc, self.k
        W = self.w
        last = (dr == 1)
        U = self.T("Udir", [128, 128], F32)
        self.LD(U, U[:], self.c_tri[dr])
        mask = self.T("mdir", [128, 128], F32)
        self.LD(mask, mask[:], self.c_tri[2 + dr])
        Sst = self.T("Sst", [128, 8, 64], F32)
        S16 = self.T("S16", [128, 8, 64], BF16)
        Cst = self.T("Cst", [64, 8, 65], F32)
        C16 = self.T("C16", [64, 8, 65], BF16)
        for t_ in (Sst, S16, Cst, C16):
            self.V(k.dve, (lambda tt: (lambda: nc.vector.memset(tt[:], 0.0)))(t_), [], [t_])
        R = self.ring
        r_xs = R("xs", [128, 512], F32, 2); r_dtda = R("dtda", [128, 32], F32, 2); r_bmt = R("bmt", [128, 256], BF16, 2)
        r_bcT = R("bcT", [128, 4, 128], BF16, 2); r_qT = R("qT", [64, 8, 128], BF16, 2); r_kT = R("kT", [64, 8, 128], BF16, 2)
        r_kt = R("kt", [128, 512], BF16, 2); r_v2 = R("v2", [128, 8, 65], BF16, 2); r_gate = R("gate", [128, 32], F32, 2)
        r_sc = R("sc", [128, 32], F32, 2); r_wall = R("wall", [128, 8, 128], F32, 2); r_tR = R("tR", [128, 8, 128], F32, 2)
        r_eD = R("eD", [128, 8, 128], F32, 2); r_cb = R("cb", [128, 2, 128], F32, 2); r_MT = R("MT", [128, 8, 128], BF16, 2)
        r_ex = R("ex", [128, 64], F32, 2); r_xdt = R("xdt", [128, 8, 64], BF16, 2); r_xdtd = R("xdtd", [128, 8, 64], BF16, 2)
        r_y = R("y", [128, 512], F32, 3); r_aT = R("aT", [128, 8, 128], BF16, 2); r_tot = R("tot", [128, 8, 65], F32, 2)
        r_hd = R("hd", [128, 8, 64], F32, 3); r_kd = R("kd", [128, 8, 64], BF16, 2)
        if last:
            r_yf = R("yf", [128, 512], F32, 2); r_hf = R("hf", [128, 512], F32, 2); r_zs = R("zs", [128, 512], F32, 2)
            r_og = R("og", [128, 512], F32, 2); r_mix = R("mix16", [128, D], BF16, 2); r_mixT = R("mixT2", [128, 8, 128], BF16, 2)
            r_tmp = R("tmp5", [128, 512], F32, 2)
            dsk = self.load_bcast("dsk", W["ssd_d"][j:j + 1, :], 8)
            ssdg = self.load_bcast("ssdg", W["ssd_norm_g"][j:j + 1, :], 512)
            mlg = self.load_bcast("mlg", W["ml_norm_g"][j:j + 1, :], 512)
            wout = self.load_w("wout2", self.wb["od_w_out"][j], 8, D, self.wb_r[("od_w_out", j, 0)])
            g1, b1 = self.ln_consts("ln1_g", "ln1_b", layer)
        bcTv = self.bcT_d.rearrange("i p s -> p i s")
        qTv = self.qT_d.rearrange("h p s -> p h s")
        kTv = self.kT_d.rearrange("h p s -> p h s")

        def bc3(ap2, n):
            return ap2.unsqueeze(2).to_broadcast([ap2.shape[0], ap2.shape[1], n])

        chunks = range(32) if dr == 0 else range(31, -1, -1)
        for c in chunks:
            r0 = c * 128
            rows = slice(r0, r0 + 128)
            xs, dtda, bmt, bcT, qT, kT, kt, v2, gate = r_xs(), r_dtda(), r_bmt(), r_bcT(), r_qT(), r_kT(), r_kt(), r_v2(), r_gate()
            self.LD(xs, xs[:], self.xs_d[rows, :], self.r_xs_d)
            self.LD(dtda, dtda[:], self.dtda_d[rows, :], self.r_dtda_d)
            self.LD(bmt, bmt[:], self.bmt_d[rows, :], self.r_bmt_d)
            self.LD(bcT, bcT[:], bcTv[:, :, rows], self.r_bcT_d)
            self.LD(qT, qT[:], qTv[:, :, rows], self.r_qT_d)
            self.LD(kT, kT[:], kTv[:, :, rows], self.r_kT_d)
            self.LD(kt, kt[:], self.kt_d[rows, :], self.r_kt_d)
            self.LD(v2, v2[:].rearrange("p h e -> p (h e)"), self.v2_d[rows, :], self.r_v2_d)
            self.LD(gate, gate[:], self.gate_d[rows, :], self.r_gate_d)
            dtd = dtda[:, dr * 8:dr * 8 + 8]
            dad = dtda[:, 16 + dr * 8:16 + dr * 8 + 8]
            lid = gate[:, dr * 8:dr * 8 + 8]
            lfd = gate[:, 16 + dr * 8:16 + dr * 8 + 8]
            pcol = self.pB()
            self.MM(pcol, pcol[:, 0:8], U, U[:], dtda, dad, True, True)
            self.MM(pcol, pcol[:, 8:16], self.ones32, self.ones32[:], dtda, dad, True, True)
            self.MM(pcol, pcol[:, 16:24], U, U[:], gate, lfd, True, True)
            self.MM(pcol, pcol[:, 24:32], self.ones32, self.ones32[:], gate, lfd, True, True)
            sc = r_sc()
            self.CP(sc, sc[:], pcol, pcol[:, 0:32])
            ex = r_ex()
            self.ACT(ex, ex[:, 0:16], sc, sc[:, 0:16], AF.Exp)
            self.TT_(ex, ex[:, 16:24], sc, sc[:, 8:16], sc, sc[:, 0:8], ALU.subtract)
            self.ACT(ex, ex[:, 16:24], ex, ex[:, 16:24], AF.Exp)
            self.TT_(ex, ex[:, 24:32], dtda, dtd, ex, ex[:, 16:24], ALU.mult)
            self.ACT(ex, ex[:, 32:48], sc, sc[:, 16:32], AF.Exp)
            self.TT_(ex, ex[:, 48:56], sc, sc[:, 24:32], sc, sc[:, 16:24], ALU.subtract)
            self.TT_(ex, ex[:, 48:56], ex, ex[:, 48:56], gate, lid, ALU.add)
            self.ACT(ex, ex[:, 48:56], ex, ex[:, 48:56], AF.Exp)
            self.TT_(ex, ex[:, 56:64], gate, lid, sc, sc[:, 16:24], ALU.subtract)

            def decay_mat(src_t, src_ap, shift_t, shift_ap, sign):
                wall = r_wall()
                self.TT_(wall, wall[:], U, U[:].unsqueeze(1).to_broadcast([128, 8, 128]), src_t, bc3(src_ap, 128), ALU.mult,
                         eng=k.pool)
                pR = [self.pA(), self.pA()]
                for hb in range(2):
                    self.MM(pR[hb], pR[hb][:], self.ones32, self.ones32[:], wall,
                            wall[:, hb * 4:(hb + 1) * 4, :].rearrange("p h l -> p (h l)"), True, True)
                tR = r_tR()
                for hb in range(2):
                    self.TT_(tR, tR[:, hb * 4:(hb + 1) * 4, :], pR[hb], pR[hb][:].rearrange("p (h l) -> p h l", h=4),
                             mask, mask[:].unsqueeze(1).to_broadcast([128, 4, 128]), ALU.add)
                self.TT_(tR, tR[:], tR, tR[:], shift_t, bc3(shift_ap, 128), ALU.subtract if sign < 0 else ALU.add)
                eD = r_eD()
                self.ACT(eD, eD[:], tR, tR[:], AF.Exp)
                return eD
            eD = decay_mat(dtda, dad, sc, sc[:, 0:8], -1)
            pcb = self.pB()
            for g in range(2):
                self.MM(pcb, pcb[:, g * 128:(g + 1) * 128], bcT, bcT[:, g, :], bcT, bcT[:, 2 + g, :], True, True)
            cb = r_cb()
            self.CP(cb, cb[:].rearrange("p g l -> p (g l)"), pcb, pcb[:, 0:256], eng=k.act)
            MT = r_MT()
            self.TT_(MT, MT[:].rearrange("p (g r) l -> p g r l", g=2), eD, eD[:].rearrange("p (g r) l -> p g r l", g=2),
                     cb, cb[:].unsqueeze(2).to_broadcast([128, 2, 4, 128]), ALU.mult)
            xv = xs[:].rearrange("p (h e) -> p h e", e=64)
            xdt, xdtd = r_xdt(), r_xdtd()
            self.TT_(xdt, xdt[:], xs, xv, dtda, bc3(dtd, 64), ALU.mult, eng=k.pool)
            self.TT_(xdtd, xdtd[:], xs, xv, ex, bc3(ex[:, 24:32], 64), ALU.mult, eng=k.pool)
            pyd, pyo = self.pA(), self.pA()
            for h in range(8):
                self.MM(pyd, pyd[:, h * 64:(h + 1) * 64], MT, MT[:, h, :], xdt, xdt[:, h, :], True, True)
            for h in range(8):
                self.MM(pyo, pyo[:, h * 64:(h + 1) * 64], bcT, bcT[:, 2 + h // 4, :], S16, S16[:, h, :], True, True)
            y = r_y()
            yv = y[:].rearrange("p (h e) -> p h e", e=64)
            self.TT_(y, yv, pyo, pyo[:].rearrange("p (h e) -> p h e", e=64), ex, bc3(ex[:, 0:8], 64), ALU.mult)
            self.TT_(y, y[:], y, y[:], pyd, pyd[:], ALU.add)
            pst = self.pB()
            for h in range(8):
                self.MM(pst, pst[:, h * 64:(h + 1) * 64], bmt, bmt[:, (h // 4) * 128:(h // 4 + 1) * 128], xdtd, xdtd[:, h, :], True, True)
            self.TT_(Sst, Sst[:], Sst, Sst[:], ex, bc3(ex[:, 8:16], 64), ALU.mult)
            self.TT_(Sst, Sst[:], Sst, Sst[:], pst, pst[:].rearrange("p (h e) -> p h e", e=64), ALU.add)
            self.CP(S16, S16[:], Sst, Sst[:], eng=k.act)
            wT = decay_mat(gate, lfd, ex, ex[:, 56:64], +1)
            pqk = [self.pA(), self.pA()]
            for h in range(8):
                self.MM(pqk[h // 4], pqk[h // 4][:, (h % 4) * 128:(h % 4 + 1) * 128], kT, kT[:, h, :], qT, qT[:, h, :], True, True)
            aT = r_aT()
            for hb in range(2):
                self.TT_(aT, aT[:, hb * 4:(hb + 1) * 4, :], pqk[hb], pqk[hb][:].rearrange("p (h l) -> p h l", h=4),
                         wT, wT[:, hb * 4:(hb + 1) * 4, :], ALU.mult)
            pn = [self.pA(), self.pA()]
            pi = [self.pA(), self.pA()]
            for h in range(8):
                self.MM(pn[h // 4], pn[h // 4][:, (h % 4) * 65:(h % 4 + 1) * 65], aT, aT[:, h, :], v2, v2[:, h, :], True, True)
            for h in range(8):
                self.MM(pi[h // 4], pi[h // 4][:, (h % 4) * 65:(h % 4 + 1) * 65], qT, qT[:, h, :], C16, C16[:, h, :], True, True)
            tot = r_tot()
            for hb in range(2):
                tv = tot[:, hb * 4:(hb + 1) * 4, :]
                self.TT_(tot, tv, pi[hb], pi[hb][:, 0:260].rearrange("p (h e) -> p h e", e=65),
                         ex, bc3(ex[:, 32 + hb * 4:32 + (hb + 1) * 4], 65), ALU.mult)
                self.TT_(tot, tv, tot, tv, pn[hb], pn[hb][:, 0:260].rearrange("p (h e) -> p h e", e=65), ALU.add)
            den = r_sc()
            self.ACT(den, den[:, 0:8], tot, tot[:, :, 64], AF.Abs)
            self.TS(den, den[:, 0:8], den, den[:, 0:8], 1.0, None, ALU.max)
            self.V(k.dve, lambda: nc.vector.reciprocal(out=den[:, 0:8], in_=den[:, 0:8]), [den], [den])
            hd = r_hd()
            self.TT_(hd, hd[:], tot, tot[:, :, 0:64], den, bc3(den[:, 0:8], 64), ALU.mult)
            kd = r_kd()
            self.TT_(kd, kd[:], kt, kt[:].rearrange("p (h e) -> p h e", e=64), ex, bc3(ex[:, 48:56], 64), ALU.mult, eng=k.pool)
            pC = [self.pB(), self.pB()]
            for h in range(8):
                self.MM(pC[h // 4], pC[h // 4][0:64, (h % 4) * 65:(h % 4 + 1) * 65], kd, kd[:, h, :], v2, v2[:, h, :], True, True)
            self.TT_(Cst, Cst[:], Cst, Cst[:], ex, bc3(ex[0:64, 40:48], 65), ALU.mult)
            for hb in range(2):
                cv = Cst[:, hb * 4:(hb + 1) * 4, :]
                self.TT_(Cst, cv, Cst, cv, pC[hb], pC[hb][0:64, 0:260].rearrange("p (h e) -> p h e", e=65), ALU.add)
            self.CP(C16, C16[:], Cst, Cst[:], eng=k.act)
            if not last:
                self.ST(self.yf_d[rows, :], self.r_yf_d, y, y[:])
                self.ST(self.hf_d[rows, :], self.r_hf_d, hd, hd[:].rearrange("p h e -> p (h e)"))
                continue
            yf, hf, zs, og = r_yf(), r_hf(), r_zs(), r_og()
            self.LD(yf, yf[:], self.yf_d[rows, :], self.r_yf_d)
            self.LD(hf, hf[:], self.hf_d[rows, :], self.r_hf_d)
            self.LD(zs, zs[:], self.z_d[rows, :], self.r_z_d)
            self.LD(og, og[:], self.og_d[rows, :], self.r_og_d)
            tmp = r_tmp()
            self.TT_(y, y[:], y, y[:], yf, yf[:], ALU.add)
            self.TT_(tmp, tmp[:].rearrange("p (h e) -> p h e", e=64), xs, xv, dsk, bc3(dsk[:, 0:8], 64), ALU.mult)
            self.TT_(y, y[:], y, y[:], tmp, tmp[:], ALU.add)
            self.TT_(y, y[:], y, y[:], zs, zs[:], ALU.mult)
            st = r_sc()
            for g in range(2):
                self.ACT(tmp, tmp[:, g * 256:(g + 1) * 256], y, y[:, g * 256:(g + 1) * 256], AF.Square, accum=st[:, g:g + 1],
                         extra=[])
            k.all_res
            st.r.w = tmp.r.w
            self.rsqrt_(st, st[:, 2:4], st, st[:, 0:2], 1.0 / 256.0, self.eps_rms)
            self.TT_(y, y[:].rearrange("p (g e) -> p g e", g=2), y, y[:].rearrange("p (g e) -> p g e", g=2),
                     st, bc3(st[:, 2:4], 256), ALU.mult)
            mix = r_mix()
            self.TT_(mix, mix[:, 0:512], y, y[:], ssdg, ssdg[:], ALU.mult)
            hv = hd[:]
            self.TT_(hd, hv, hd, hv, hf, hf[:].rearrange("p (h e) -> p h e", e=64), ALU.add)
            self.V(k.dve, lambda: nc.vector.tensor_reduce(out=st[:, 8:16], in_=hv, axis=AX.X, op=ALU.add), [hd], [st])
            self.TS(st, st[:, 8:16], st, st[:, 8:16], 1.0 / 64.0, None, ALU.mult)
            self.TT_(hd, hv, hd, hv, st, bc3(st[:, 8:16], 64), ALU.subtract)
            tv3 = tmp[:].rearrange("p (h e) -> p h e", e=64)
            self.TT_(tmp, tv3, hd, hv, hd, hv, ALU.mult)
            self.V(k.dve, lambda: nc.vector.tensor_reduce(out=st[:, 16:24], in_=tv3, axis=AX.X, op=ALU.add), [tmp], [st])
            self.rsqrt_(st, st[:, 24:32], st, st[:, 16:24], 1.0 / 64.0, self.eps_ln)
            self.TT_(hd, hv, hd, hv, st, bc3(st[:, 24:32], 64), ALU.mult)
            hflat = hd[:].rearrange("p h e -> p (h e)")
            self.TT_(hd, hflat, hd, hflat, mlg, mlg[:], ALU.mult)
            self.TT_(mix, mix[:, 512:1024], hd, hflat, og, og[:], ALU.mult)
            for cc in range(8):
                self.TR(self.ptr, self.ptr[:, cc * 128:(cc + 1) * 128], mix, mix[:, cc * 128:(cc + 1) * 128], self.ident16)
            mixT = r_mixT()
            self.CP(mixT, mixT[:].rearrange("p c s -> p (c s)"), self.ptr, self.ptr[:], eng=k.act)
            phs = [self.pB(), self.pB()]
            for half in range(2):
                for cc in range(8):
                    self.MM(phs[half], phs[half][:], mixT, mixT[:, cc, :], wout, wout[:, cc, half * 512:(half + 1) * 512],
                            cc == 0, cc == 7)
            self.epilogue([(phs[0], phs[0][:]), (phs[1], phs[1][:])], r0, g1, b1, None, "o3", dbg_idx=2 * layer)


def make_consts():
    half = 16
    inv = (10000.0 ** (-np.arange(half, dtype=np.float32) / half)).astype(np.float32)
    pos = np.arange(S, dtype=np.float32)
    ang = (pos[:, None] * inv[None, :]).astype(np.float32)
    cos = np.cos(ang).astype(np.float32).T
    sin = np.sin(ang).astype(np.float32).T
    rope = np.zeros((2, 32, S), np.float32)
    rope[0, :16] = cos
    rope[0, 16:] = cos
    rope[1, :16] = -sin
    rope[1, 16:] = sin
    kk = np.arange(128)
    U = (kk[:, None] <= kk[None, :]).astype(np.float32)
    UT = (kk[:, None] >= kk[None, :]).astype(np.float32)
    mF = np.where(kk[:, None] > kk[None, :], NEG, 0.0).astype(np.float32)
    mB = np.where(kk[:, None] < kk[None, :], NEG, 0.0).astype(np.float32)
    pg = np.zeros((128, 9), np.float32)
    for g in range(7):
        pg[:, g] = g * 128 + kk
    for h in range(2):
        pg[:, 7 + h] = h * 128 + kk
    return {"c_ident": np.eye(128, dtype=np.float32), "c_rope": rope, "c_tri": np.stack([U, UT, mF, mB]), "c_pg": pg}


_PROG_CACHE = {}


def get_prog(nslot, nlayers, debug):
    key = (nslot, nlayers, debug)
    if key not in _PROG_CACHE:
        p = Prog(nslot, nlayers, debug)
        p.build()
        _PROG_CACHE[key] = p
    return _PROG_CACHE[key]


def run(inputs, nslot=NSLOT, nlayers=DEPTH, debug=False, ncores=NCORES, seqs=None):
    p = get_prog(nslot, nlayers, debug)
    xall = np.concatenate([np.asarray(inputs["x_prompt"]), np.asarray(inputs["x_sample"])], axis=0)
    nseq = xall.shape[0]
    if seqs is None:
        seqs = [[min(c * nslot + s, nseq - 1) for s in range(nslot)] for c in range(ncores)]
        flat = list(range(nseq))
        seqs = []
        pos = 0
        for c in range(ncores):
            n = 3 if c < 4 else 2
            mine = flat[pos:pos + n]
            pos += n
            while len(mine) < nslot:
                mine.append(mine[0])
            seqs.append(mine[:nslot])
    consts = make_consts()
    shared = {}
    for n in p.in_names:
        if n == "x" or n in consts:
            continue
        a = np.ascontiguousarray(np.asarray(inputs[n], dtype=np.float32))
        if n in ("ssd_dt_bias", "ssd_a_log", "ml_igate_b", "ml_fgate_b"):
            a = a.reshape(2, 16)
        shared[n] = a
    shared.update(consts)
    in_maps = []
    for c in range(ncores):
        m = dict(shared)
        m["x"] = np.ascontiguousarray(xall[seqs[c]])
        in_maps.append(m)
    res = run_bass_kernel_spmd(p.nc, in_maps, core_ids=list(range(ncores)))
    return res, seqs, nseq


def kernel(**inputs):
    res, seqs, nseq = run(inputs)
    out = np.zeros((nseq, S, D), np.float32)
    done = set()
    for c in range(NCORES):
        y = np.asarray(res.results[c]["y"])
        for s, q in enumerate(seqs[c]):
            if q not in done:
                out[q] = y[s]
                done.add(q)
    nb = np.asarray(inputs["x_prompt"]).shape[0]
    return (out[:nb], out[nb:])
```
